# Optimizing a Trainium2 kernel written in Bass

```python
import math
import jax, jax.numpy as jnp
from jax import lax
import numpy as np

D_MODEL = 1024
BATCH = 16
SEQ = 2048
DEPTH = 2

CHUNK = 64
EPS = 1e-6
GM_WIDTH = D_MODEL // 2
GM_GROUPS = 4
GM_GROUP_CH = GM_WIDTH // GM_GROUPS
GM_BLOCK = 128
DA_HEADS = 4
DA_HEAD_DIM = 64
DA_QK_WIDTH = DA_HEADS * 2 * DA_HEAD_DIM
DA_V_DIM = 2 * DA_HEAD_DIM
DA_V_WIDTH = DA_HEADS * DA_V_DIM
DA_QBLOCK = 128
ROPE_THETA = 10000.0
DN_HEADS = 4
DN_HEAD_DIM = 128
DN_WIDTH = DN_HEADS * DN_HEAD_DIM
DN_CONV = 4
N_BRANCH = 3
IN_SPLITS = (GM_WIDTH, GM_WIDTH, DA_QK_WIDTH, DA_QK_WIDTH, DA_V_WIDTH,
             DN_WIDTH, DN_WIDTH, DN_WIDTH, DN_WIDTH, DN_HEADS, DN_HEADS,
             N_BRANCH * D_MODEL)
IN_WIDTH = (2 * GM_WIDTH + 2 * DA_QK_WIDTH + DA_V_WIDTH + 4 * DN_WIDTH
            + 2 * DN_HEADS + N_BRANCH * D_MODEL)
FF_DENSE = 256 * ((8 * D_MODEL // 3 + 255) // 256)
N_EXPERTS = 8
TOP_K = 2
FF_EXPERT = 7 * D_MODEL // 2
MOE_BLOCK = 256
N_DENSE = (DEPTH + 1) // 2
N_MOE = DEPTH // 2

kernel_name = 'hybrid_gmlp_diffattn_gdn_moe_adaln'


def lambda_init_fn(layer):
    return 0.8 - 0.6 * math.exp(-0.3 * layer)


def rmsnorm(x, g):
    xf = x.astype(jnp.float32)
    y = xf * lax.rsqrt(jnp.mean(xf * xf, axis=-1, keepdims=True) + EPS)
    return (y * g.astype(jnp.float32)).astype(x.dtype)


def layernorm(x, g, b):
    xf = x.astype(jnp.float32)
    mu = jnp.mean(xf, axis=-1, keepdims=True)
    xc = xf - mu
    var = jnp.mean(xc * xc, axis=-1, keepdims=True)
    y = xc * lax.rsqrt(var + EPS) * g.astype(jnp.float32) + b.astype(jnp.float32)
    return y.astype(x.dtype)


def rope(x, cos, sin):
    x1, x2 = jnp.split(x, 2, axis=-1)
    return jnp.concatenate([x1 * cos - x2 * sin, x2 * cos + x1 * sin], axis=-1)


def causal_depthwise_conv(x, w):
    C = x.shape[-1]
    return lax.conv_general_dilated(
        x, w[:, None, :].astype(x.dtype), window_strides=(1,),
        padding=[(DN_CONV - 1, 0)], dimension_numbers=('NWC', 'WIO', 'NWC'),
        feature_group_count=C)


def gmlp_branch(u, v, ln_g, ln_b, w_s, b_s):
    B, S, _ = u.shape
    u = jax.nn.gelu(u, approximate=False)
    v = layernorm(jax.nn.gelu(v, approximate=False), ln_g, ln_b)
    vb = v.reshape(B, S // GM_BLOCK, GM_BLOCK, GM_GROUPS, GM_GROUP_CH)
    pos_chunk = jnp.arange(GM_BLOCK) // CHUNK
    allowed = pos_chunk[None, :] <= pos_chunk[:, None]
    w = jnp.where(allowed, w_s, 0.0).astype(v.dtype)
    sv = jnp.einsum('gij,bnjgc->bnigc', w, vb) + b_s.T[:, :, None].astype(v.dtype)
    return u * sv.reshape(B, S, GM_WIDTH)


def diff_attention(q, k, v, cos, sin, lam_p, subln_g, lambda_init):
    B, S, _ = q.shape
    q = q.reshape(B, S, DA_HEADS, 2, DA_HEAD_DIM)
    k = k.reshape(B, S, DA_HEADS, 2, DA_HEAD_DIM)
    v = v.reshape(B, S, DA_HEADS, DA_V_DIM)
    cs = cos[:, :, None, None, :]
    sn = sin[:, :, None, None, :]
    q = rope(q, cs, sn) * (DA_HEAD_DIM ** -0.5)
    k = rope(k, cs, sn)
    lp = lam_p.astype(jnp.float32)
    lam = jnp.exp(jnp.sum(lp[0] * lp[1])) - jnp.exp(jnp.sum(lp[2] * lp[3])) + lambda_init
    outs = []
    for qb in range(S // DA_QBLOCK):
        q0 = qb * DA_QBLOCK
        kend = q0 + DA_QBLOCK
        s = jnp.einsum('bqhpd,bkhpd->bhpqk', q[:, q0:kend], k[:, :kend]).astype(jnp.float32)
        q_chunk = (q0 + jnp.arange(DA_QBLOCK)) // CHUNK
        k_chunk = jnp.arange(kend) // CHUNK
        s = jnp.where(k_chunk[None, :] <= q_chunk[:, None], s, -jnp.inf)
        a = jax.nn.softmax(s, axis=-1)
        a = a[:, :, 0] - lam * a[:, :, 1]
        outs.append(jnp.einsum('bhqk,bkhe->bqhe', a.astype(v.dtype), v[:, :kend]))
    o = jnp.concatenate(outs, axis=1)
    o = rmsnorm(o, subln_g) * (1.0 - lambda_init)
    return o.reshape(B, S, DA_V_WIDTH)


def gated_delta_rule(q, k, v, g, beta):
    B, S, H, dk = q.shape
    dv = v.shape[-1]
    N = S // CHUNK

    def blocks(t):
        return jnp.moveaxis(t.reshape((B, N, CHUNK) + t.shape[2:]), 3, 1)

    q, k, v, g, beta = blocks(q), blocks(k), blocks(v), blocks(g), blocks(beta)
    g = jnp.cumsum(g, axis=-1)
    idx = jnp.arange(CHUNK)
    tril = idx[:, None] >= idx[None, :]
    strict = idx[:, None] > idx[None, :]
    decay = jnp.exp(jnp.where(tril, g[..., :, None] - g[..., None, :], -jnp.inf))
    kb = k * beta[..., None]
    A = jnp.where(strict, jnp.einsum('bhnid,bhnjd->bhnij', kb, k) * decay, 0.0)
    L = A + jnp.eye(CHUNK, dtype=jnp.float32)
    rhs = jnp.concatenate([v * beta[..., None], kb * jnp.exp(g)[..., None]], axis=-1)
    sol = lax.linalg.triangular_solve(L, rhs, left_side=True, lower=True, unit_diagonal=True)
    u, w = sol[..., :dv], sol[..., dv:]
    qk = jnp.einsum('bhnid,bhnjd->bhnij', q, k) * decay
    qg = q * jnp.exp(g)[..., None]
    kd = k * jnp.exp(g[..., -1:] - g)[..., None]
    gl = jnp.exp(g[..., -1])

    def step(state, inp):
        qg_c, kd_c, u_c, w_c, qk_c, gl_c = inp
        v_new = u_c - jnp.einsum('bhck,bhkv->bhcv', w_c, state)
        o = (jnp.einsum('bhck,bhkv->bhcv', qg_c, state)
             + jnp.einsum('bhij,bhjv->bhiv', qk_c, v_new))
        state = state * gl_c[..., None, None] + jnp.einsum('bhck,bhcv->bhkv', kd_c, v_new)
        return state, o

    xs = tuple(jnp.moveaxis(t, 2, 0) for t in (qg, kd, u, w, qk, gl))
    state0 = jnp.zeros((B, H, dk, dv), jnp.float32)
    _, o = lax.scan(step, state0, xs)
    return jnp.transpose(o, (1, 0, 3, 2, 4)).reshape(B, S, H, dv)


def deltanet_branch(q, k, v, z, a, b, conv_w, a_log, dt_bias, norm_g):
    B, S, _ = q.shape
    qkv = jax.nn.silu(causal_depthwise_conv(jnp.concatenate([q, k, v], axis=-1), conv_w))
    q, k, v = jnp.split(qkv.astype(jnp.float32), 3, axis=-1)

    def heads(t):
        return t.reshape(B, S, DN_HEADS, DN_HEAD_DIM)

    q, k, v = heads(q), heads(k), heads(v)
    q = q * lax.rsqrt(jnp.sum(q * q, axis=-1, keepdims=True) + EPS) * (DN_HEAD_DIM ** -0.5)
    k = k * lax.rsqrt(jnp.sum(k * k, axis=-1, keepdims=True) + EPS)
    beta = jax.nn.sigmoid(b.astype(jnp.float32))
    g = -jnp.exp(a_log.astype(jnp.float32)) * jax.nn.softplus(
        a.astype(jnp.float32) + dt_bias.astype(jnp.float32))
    o = gated_delta_rule(q, k, v, g, beta)
    o = rmsnorm(o, norm_g) * jax.nn.silu(heads(z).astype(jnp.float32))
    return o.reshape(B, S, DN_WIDTH).astype(z.dtype)


def hybrid_mixer(h, cos, sin, w_in, gm_ln_g, gm_ln_b, gm_w_s, gm_b_s, da_lambda,
                 da_subln_g, dn_conv_w, dn_a_log, dn_dt_bias, dn_norm_g,
                 w_br_gm, w_br_da, w_br_dn, w_out, lambda_init):
    B, S, D = h.shape
    proj = jnp.einsum('bsd,de->bse', h, w_in)
    cuts = [int(i) for i in np.cumsum(IN_SPLITS)[:-1]]
    (gm_u, gm_v, da_q, da_k, da_v, dn_q, dn_k, dn_v, dn_z, dn_a, dn_b,
     gate_logits) = jnp.split(proj, cuts, axis=-1)
    y_gm = gmlp_branch(gm_u, gm_v, gm_ln_g, gm_ln_b, gm_w_s, gm_b_s)
    y_da = diff_attention(da_q, da_k, da_v, cos, sin, da_lambda, da_subln_g, lambda_init)
    y_dn = deltanet_branch(dn_q, dn_k, dn_v, dn_z, dn_a, dn_b, dn_conv_w,
                           dn_a_log, dn_dt_bias, dn_norm_g)
    gates = jax.nn.sigmoid(gate_logits.astype(jnp.float32)).astype(h.dtype)
    gates = gates.reshape(B, S, N_BRANCH, D)
    merged = (gates[:, :, 0] * jnp.einsum('bsk,kd->bsd', y_gm, w_br_gm)
              + gates[:, :, 1] * jnp.einsum('bsk,kd->bsd', y_da, w_br_da)
              + gates[:, :, 2] * jnp.einsum('bsk,kd->bsd', y_dn, w_br_dn))
    return jnp.einsum('bsd,de->bse', merged, w_out)


def swiglu(h, w_gate, w_up, w_down):
    a = jnp.einsum('bsd,df->bsf', h, w_gate)
    b = jnp.einsum('bsd,df->bsf', h, w_up)
    return jnp.einsum('bsf,fd->bsd', jax.nn.silu(a) * b, w_down)


def moe_swiglu(h, w_router, w_gate, w_up, w_down):
    B, S, D = h.shape
    T = B * S
    TK = T * TOP_K
    hf = h.reshape(T, D)
    logits = jnp.einsum('td,de->te', hf, w_router).astype(jnp.float32)
    top_logit, top_e = lax.top_k(logits, TOP_K)
    top_w = jax.nn.softmax(top_logit, axis=-1)
    flat_e = top_e.reshape(TK).astype(jnp.int32)
    flat_tok = jnp.arange(TK, dtype=jnp.int32) // TOP_K
    flat_w = top_w.reshape(TK)
    order = jnp.argsort(flat_e)
    sorted_e = flat_e[order]
    counts = jax.ops.segment_sum(jnp.ones((TK,), jnp.int32), flat_e, num_segments=N_EXPERTS)
    padded = (counts + MOE_BLOCK - 1) // MOE_BLOCK * MOE_BLOCK
    pad_end = jnp.cumsum(padded)
    pad_start = pad_end - padded
    start = jnp.cumsum(counts) - counts
    dest = pad_start[sorted_e] + jnp.arange(TK, dtype=jnp.int32) - start[sorted_e]
    P = TK + N_EXPERTS * MOE_BLOCK
    n_blk = P // MOE_BLOCK
    slot_tok = jnp.zeros((P,), jnp.int32).at[dest].set(flat_tok[order])
    slot_w = jnp.zeros((P,), jnp.float32).at[dest].set(flat_w[order])
    blk_e = jnp.minimum(
        jnp.searchsorted(pad_end, jnp.arange(n_blk, dtype=jnp.int32) * MOE_BLOCK, side='right'),
        N_EXPERTS - 1)
    xs = hf[slot_tok].reshape(n_blk, MOE_BLOCK, D)

    def expert_block(args):
        xb, e = args
        a = xb @ w_gate[e]
        b = xb @ w_up[e]
        return (jax.nn.silu(a) * b) @ w_down[e]

    ys = lax.map(expert_block, (xs, blk_e)).reshape(P, D)
    out = jnp.zeros((T, D), h.dtype).at[slot_tok].add(ys * slot_w[:, None].astype(h.dtype))
    return out.reshape(B, S, D)


def setup_inputs(seed: int = 0) -> dict:
    key = jax.random.key(seed)
    ks = jax.random.split(key, 32)
    D = D_MODEL

    def nrm(i, shape, scale):
        return jax.random.normal(ks[i], shape, jnp.float32) * scale

    x = nrm(0, (BATCH, SEQ, D), 1.0)
    c = nrm(1, (BATCH, D), 1.0)
    offsets = jax.random.randint(ks[2], (BATCH, 1), 0, 64, dtype=jnp.int32) * CHUNK
    positions = jnp.arange(SEQ, dtype=jnp.int32)[None, :] + offsets
    a_log = jnp.log(jax.random.uniform(ks[15], (DEPTH, DN_HEADS), jnp.float32, 1.0, 16.0))
    dt = jnp.exp(jax.random.uniform(ks[16], (DEPTH, DN_HEADS), jnp.float32,
                                    math.log(1e-3), math.log(1e-1)))
    dt_bias = dt + jnp.log(-jnp.expm1(-dt))
    return {
        'x': x,
        'c': c,
        'positions': positions,
        'norm1_g': 1.0 + nrm(3, (DEPTH, D), 0.05),
        'norm2_g': 1.0 + nrm(4, (DEPTH, D), 0.05),
        'w_mod': nrm(5, (DEPTH, D, 6 * D), 0.5 * D ** -0.5),
        'b_mod': nrm(6, (DEPTH, 6 * D), 0.02),
        'w_in': nrm(7, (DEPTH, D, IN_WIDTH), D ** -0.5),
        'gm_ln_g': 1.0 + nrm(8, (DEPTH, GM_WIDTH), 0.05),
        'gm_ln_b': nrm(9, (DEPTH, GM_WIDTH), 0.02),
        'gm_w_s': nrm(10, (DEPTH, GM_GROUPS, GM_BLOCK, GM_BLOCK), GM_BLOCK ** -0.5),
        'gm_b_s': 1.0 + nrm(11, (DEPTH, GM_GROUPS, GM_BLOCK), 0.1),
        'da_lambda': nrm(12, (DEPTH, 4, DA_HEAD_DIM), 0.1),
        'da_subln_g': 1.0 + nrm(13, (DEPTH, DA_V_DIM), 0.05),
        'dn_conv_w': nrm(14, (DEPTH, DN_CONV, 3 * DN_WIDTH), DN_CONV ** -0.5),
        'dn_a_log': a_log,
        'dn_dt_bias': dt_bias,
        'dn_norm_g': 1.0 + nrm(17, (DEPTH, DN_HEAD_DIM), 0.05),
        'w_br_gm': nrm(18, (DEPTH, GM_WIDTH, D), GM_WIDTH ** -0.5),
        'w_br_da': nrm(19, (DEPTH, DA_V_WIDTH, D), DA_V_WIDTH ** -0.5),
        'w_br_dn': nrm(20, (DEPTH, DN_WIDTH, D), DN_WIDTH ** -0.5),
        'w_out': nrm(21, (DEPTH, D, D), D ** -0.5),
        'ffn_w_gate': nrm(22, (N_DENSE, D, FF_DENSE), D ** -0.5),
        'ffn_w_up': nrm(23, (N_DENSE, D, FF_DENSE), D ** -0.5),
        'ffn_w_down': nrm(24, (N_DENSE, FF_DENSE, D), FF_DENSE ** -0.5),
        'moe_w_router': nrm(25, (N_MOE, D, N_EXPERTS), D ** -0.5),
        'moe_w_gate': nrm(26, (N_MOE, N_EXPERTS, D, FF_EXPERT), D ** -0.5),
        'moe_w_up': nrm(27, (N_MOE, N_EXPERTS, D, FF_EXPERT), D ** -0.5),
        'moe_w_down': nrm(28, (N_MOE, N_EXPERTS, FF_EXPERT, D), FF_EXPERT ** -0.5),
        'final_g': 1.0 + nrm(29, (D,), 0.05),
    }


def reference(x, c, positions, norm1_g, norm2_g, w_mod, b_mod, w_in, gm_ln_g, gm_ln_b,
              gm_w_s, gm_b_s, da_lambda, da_subln_g, dn_conv_w, dn_a_log, dn_dt_bias,
              dn_norm_g, w_br_gm, w_br_da, w_br_dn, w_out, ffn_w_gate, ffn_w_up,
              ffn_w_down, moe_w_router, moe_w_gate, moe_w_up, moe_w_down, final_g):
    inv_freq = ROPE_THETA ** (-jnp.arange(0, DA_HEAD_DIM, 2, dtype=jnp.float32) / DA_HEAD_DIM)
    ang = positions.astype(jnp.float32)[..., None] * inv_freq
    cos = jnp.cos(ang).astype(x.dtype)
    sin = jnp.sin(ang).astype(x.dtype)
    c_act = jax.nn.silu(c)
    for layer in range(DEPTH):
        mod = c_act @ w_mod[layer] + b_mod[layer]
        shift1, scale1, gate1, shift2, scale2, gate2 = jnp.split(mod, 6, axis=-1)
        h = rmsnorm(x, norm1_g[layer]) * (1.0 + scale1[:, None]) + shift1[:, None]
        y = hybrid_mixer(h, cos, sin, w_in[layer], gm_ln_g[layer], gm_ln_b[layer],
                         gm_w_s[layer], gm_b_s[layer], da_lambda[layer], da_subln_g[layer],
                         dn_conv_w[layer], dn_a_log[layer], dn_dt_bias[layer],
                         dn_norm_g[layer], w_br_gm[layer], w_br_da[layer], w_br_dn[layer],
                         w_out[layer], lambda_init_fn(layer))
        x = x + gate1[:, None] * y
        h = rmsnorm(x, norm2_g[layer]) * (1.0 + scale2[:, None]) + shift2[:, None]
        if layer % 2 == 0:
            f = swiglu(h, ffn_w_gate[layer // 2], ffn_w_up[layer // 2], ffn_w_down[layer // 2])
        else:
            f = moe_swiglu(h, moe_w_router[layer // 2], moe_w_gate[layer // 2],
                           moe_w_up[layer // 2], moe_w_down[layer // 2])
        x = x + gate2[:, None] * f
    return rmsnorm(x, final_g)
```

```python
import math
import numpy as np
import concourse.bass as bass
import concourse.mybir as mybir
from concourse.bass_utils import run_bass_kernel_spmd

F32 = mybir.dt.float32
BF16 = mybir.dt.bfloat16
I32 = mybir.dt.int32
AF = mybir.ActivationFunctionType
ALU = mybir.AluOpType
AX = mybir.AxisListType

SEM_LIMIT = 20000
ENGS = ("pe", "dve", "act", "pool", "sp")


class Tok:
    __slots__ = ("name", "w", "r", "dsem", "pr", "excl")

    def __init__(self, name, excl=False):
        self.name = name
        self.excl = excl
        self.w = None
        self.r = []
        self.dsem = None
        self.pr = []


class K:
    def __init__(self, nc, same=("dve", "act", "pool")):
        self.nc = nc
        self.e = {"pe": nc.tensor, "dve": nc.vector, "act": nc.scalar,
                  "pool": nc.gpsimd, "sp": nc.sync}
        self.nsem = 0
        self.sem = {}
        self.cnt = {}
        for n in ENGS:
            self._new_epoch(n)
        self.seq = {n: 0 for n in ENGS}
        self.last = {n: None for n in ENGS}
        self.sigmap = {n: [] for n in ENGS}
        self.waited = {n: {} for n in ENGS}
        self.same = set(same)
        self.rsem = {}
        self.store_recs = []
        self.ninstr = 0
        self.nwait = 0

    def _alloc(self, name):
        self.nsem += 1
        return self.nc.alloc_semaphore("%s_%d" % (name, self.nsem))

    def _new_epoch(self, n):
        self.sem[n] = self._alloc("e_" + n)
        self.cnt[n] = 0

    def _resolve(self, ev):
        if ev[0] == "c":
            _, eng, seq = ev
            lst = self.sigmap[eng]
            lo, hi = 0, len(lst)
            while lo < hi:
                mid = (lo + hi) // 2
                if lst[mid][0] >= seq:
                    hi = mid
                else:
                    lo = mid + 1
            if lo < len(lst):
                f = lst[lo]
                return f[1], f[2]
            assert self.seq[eng] >= seq and self.last[eng] is not None
            if self.cnt[eng] >= SEM_LIMIT:
                self._new_epoch(eng)
            self.cnt[eng] += 1
            self.last[eng].then_inc(self.sem[eng], 1)
            lst.append((self.seq[eng], self.sem[eng], self.cnt[eng]))
            return self.sem[eng], self.cnt[eng]
        rec = ev[1]
        return rec[0], rec[1]

    def _wait(self, eng, ev):
        if ev is None:
            return
        if ev[0] == "c" and ev[1] == eng and eng not in self.same:
            return
        sem, val = self._resolve(ev)
        w = self.waited[eng]
        if w.get(id(sem), 0) >= val:
            return
        w[id(sem)] = val
        self.e[eng].wait_ge(sem, val)
        self.nwait += 1

    def _pre(self, eng, reads, writes):
        for t in reads:
            self._wait(eng, t.w)
        for t in writes:
            self._wait(eng, t.w)
            for ev in t.r:
                self._wait(eng, ev)

    def _post(self, eng, reads, writes):
        ev = ("c", eng, self.seq[eng])
        for t in reads:
            t.r.append(ev)
            if len(t.r) > 16:
                t.r = self._compact(t.r)
        for t in writes:
            t.w = ev
            t.r = []

    @staticmethod
    def _split(reads, writes):
        xr = [t for t in reads if t.excl]
        if xr:
            writes = list(writes) + [t for t in xr if t not in writes]
            reads = [t for t in reads if not t.excl]
        return reads, writes

    def op(self, eng, fn, reads=(), writes=()):
        reads, writes = self._split(reads, writes)
        self._pre(eng, reads, writes)
        ins = fn(self.e[eng])
        self.seq[eng] += 1
        self.last[eng] = ins
        self.ninstr += 1
        self._post(eng, reads, writes)
        return ins

    def group(self, eng, fns, reads=(), writes=()):
        reads, writes = self._split(reads, writes)
        self._pre(eng, reads, writes)
        ins = None
        for fn in fns:
            ins = fn(self.e[eng])
            self.seq[eng] += 1
            self.ninstr += 1
        self.last[eng] = ins
        self._post(eng, reads, writes)
        return ins

    def _compact(self, evs):
        best = {}
        other = []
        for ev in evs:
            if ev[0] == "c":
                k = ev[1]
                if k not in best or ev[2] >= best[k][2]:
                    best[k] = ev
            else:
                if not any(o[1] is ev[1] for o in other):
                    other.append(ev)
        return list(best.values()) + other

    def dma(self, q, out, in_, reads=(), writes=(), group=False, **kw):
        for t in reads:
            self._wait(q, t.w)
        for t in writes:
            if group:
                for ev in t.pr:
                    self._wait(q, ev)
            else:
                self._wait(q, t.w)
                for ev in t.r:
                    self._wait(q, ev)
                t.pr = ([t.w] if t.w is not None else []) + list(t.r)
        ins = self.e[q].dma_start(out=out, in_=in_, **kw)
        assert len(writes) <= 1
        if writes:
            t = writes[0]
            if t.dsem is None or t.dsem[1] >= SEM_LIMIT:
                t.dsem = [self._alloc("d_" + t.name), 0]
            rec = t.dsem
        else:
            if q not in self.rsem or self.rsem[q][1] >= SEM_LIMIT:
                self.rsem[q] = [self._alloc("r_" + q), 0]
            rec = self.rsem[q]
        rec[1] += 16
        ins.then_inc(rec[0], 16)
        ev = ("d", rec)
        if reads and not any(r is rec for r in self.store_recs):
            self.store_recs.append(rec)
        if writes:
            writes[0].w = ev
            writes[0].r = []
        for s in reads:
            s.r.append(ev)
            if len(s.r) > 16:
                s.r = self._compact(s.r)
        self.ninstr += 1
        return ins

    def finish(self, toks, eng="sp"):
        for t in toks:
            self._wait(eng, t.w)
            for ev in t.r:
                self._wait(eng, ev)


D = 1024
KC = 8
S = 2048
TT = 512
NT = S // TT
INW = 7688
DEPTH = 2
EPS = 1e-6
FF_DENSE = 2816
FF_EXPERT = 3584
NEXP = 8
PI = math.pi
C1 = 6.28125
C2 = 2.0 * math.pi - 6.28125
NEG = -30000.0

COL_U, COL_V, COL_Q, COL_KK, COL_VA = 0, 512, 1024, 1536, 2048
COL_DQ, COL_DK, COL_DV, COL_DZ, COL_AB, COL_G = 2560, 3072, 3584, 4096, 4608, 4616

SP_LAYOUT = [("n1g", 8), ("n2g", 8), ("bmod", 48), ("lng", 512), ("lnb", 512), ("bs", 512),
             ("lam", 256), ("subg", 128), ("convw", 48), ("alog", 4), ("dtb", 4), ("dng", 1)]
SP_OFF = {}
_o = 0
for _n, _w in SP_LAYOUT:
    SP_OFF[_n] = (_o, _w)
    _o += _w
SP_LAYER = _o
SP_FING = 2 * SP_LAYER
SP_WR = SP_FING + 8
SP_TOTAL = SP_WR + 64

CO = {}
_o = 0
for _n, _w in [("ident", 128), ("rot", 128), ("tt", 64), ("su", 64), ("negu", 256), ("negl", 256),
               ("mstrict", 256), ("i4", 256), ("invf", 1)]:
    CO[_n] = (_o, _w)
    _o += _w
CO_TOTAL = _o


def make_consts():
    c = np.zeros((128, CO_TOTAL), np.float32)
    o = CO["ident"][0]
    c[:, o:o + 128] = np.eye(128, dtype=np.float32)
    o = CO["rot"][0]
    for m in range(128):
        if (m % 64) < 32:
            c[m + 32, o + m] = -1.0
        else:
            c[m - 32, o + m] = 1.0
    p = np.arange(64)[:, None]
    j = np.arange(64)[None, :]
    o = CO["tt"][0]
    c[:64, o:o + 64] = (p <= j)
    o = CO["su"][0]
    c[:64, o:o + 64] = (p > j)
    for h in range(4):
        o = CO["negu"][0] + h * 64
        c[:64, o:o + 64] = np.where(j > p, NEG, 0.0)
        o = CO["negl"][0] + h * 64
        c[:64, o:o + 64] = np.where(j < p, NEG, 0.0)
        o = CO["mstrict"][0] + h * 64
        c[:64, o:o + 64] = (j < p)
        o = CO["i4"][0] + h * 64
        c[:64, o:o + 64] = (j == p)
    inv_freq = (10000.0 ** (-np.arange(0, 64, 2, dtype=np.float32) / np.float32(64))).astype(np.float32)
    c[:, CO["invf"][0]] = inv_freq[np.arange(128) % 32]
    return c


def fm(v):
    v = np.asarray(v, np.float32)
    return np.ascontiguousarray(v.reshape(-1, 128).T)


def bc(v):
    v = np.asarray(v, np.float32).reshape(1, -1)
    return np.broadcast_to(v, (128, v.shape[1]))


def make_smallp(inp):
    sp = np.zeros((128, SP_TOTAL), np.float32)
    for l in range(DEPTH):
        base = l * SP_LAYER

        def put(name, arr):
            o, w = SP_OFF[name]
            assert arr.shape == (128, w), (name, arr.shape, w)
            sp[:, base + o:base + o + w] = arr
        put("n1g", fm(inp["norm1_g"][l]))
        put("n2g", fm(inp["norm2_g"][l]))
        put("bmod", fm(inp["b_mod"][l]))
        put("lng", bc(inp["gm_ln_g"][l]))
        put("lnb", bc(inp["gm_ln_b"][l]))
        put("bs", bc(inp["gm_b_s"][l].reshape(-1)))
        put("lam", bc(inp["da_lambda"][l].reshape(-1)))
        put("subg", bc(inp["da_subln_g"][l]))
        cw = np.asarray(inp["dn_conv_w"][l], np.float32)
        put("convw", np.ascontiguousarray(cw.reshape(4, 12, 128).transpose(2, 1, 0)).reshape(128, 48))
        put("alog", bc(inp["dn_a_log"][l]))
        put("dtb", bc(inp["dn_dt_bias"][l]))
        put("dng", np.asarray(inp["dn_norm_g"][l], np.float32).reshape(128, 1))
    sp[:, SP_FING:SP_FING + 8] = fm(inp["final_g"])
    wr = np.asarray(inp["moe_w_router"][0], np.float32)
    sp[:, SP_WR:SP_WR + 64] = wr.reshape(8, 128, 8).transpose(1, 0, 2).reshape(128, 64)
    return sp


def lambda_init_fn(layer):
    return 0.8 - 0.6 * math.exp(-0.3 * layer)


class Pool_:
    def __init__(self, items):
        self.items = items
        self.i = 0

    def next(self):
        it = self.items[self.i % len(self.items)]
        self.i += 1
        return it


def build(cfg=None):
    cfg = dict(cfg or {})
    NSEQ = cfg.get("nseq", 2)
    NTILE = cfg.get("ntile", NT)
    LAYERS = cfg.get("layers", DEPTH)
    STAGE = cfg.get("stage", 99)
    taps = cfg.get("taps", ())
    ACUT = cfg.get("acut", 99)

    nc = bass.Bass("TRN2", target_bir_lowering=False)
    k = K(nc, same=tuple(cfg.get("same", ("dve", "act", "pool"))))

    def din(name, shape, dt=F32):
        return nc.dram_tensor(name, list(shape), dt, kind="ExternalInput").ap()

    xT_d = din("xT", [2, D, S])
    posb_d = din("posb", [2, 128, S], I32)
    cT_d = din("cT", [128, KC * 2])
    smallp_d = din("smallp", [128, SP_TOTAL])
    consts_d = din("consts", [128, CO_TOTAL])
    wst_d = din("wst", [128, DEPTH * 4 * 128])
    w_mod_d = din("w_mod", [DEPTH, D, 6 * D])
    w_in_d = din("w_in", [DEPTH, D, INW])
    w_br_d = [din("w_br_gm", [DEPTH, 512, D]), din("w_br_da", [DEPTH, 512, D]), din("w_br_dn", [DEPTH, 512, D])]
    w_out_d = din("w_out", [DEPTH, D, D])
    ffn_g_d = din("ffn_w_gate", [1, D, FF_DENSE])
    ffn_u_d = din("ffn_w_up", [1, D, FF_DENSE])
    ffn_d_d = din("ffn_w_down", [1, FF_DENSE, D])
    moe_g_d = din("moe_w_gate", [1, NEXP, D, FF_EXPERT])
    moe_u_d = din("moe_w_up", [1, NEXP, D, FF_EXPERT])
    moe_d_d = din("moe_w_down", [1, NEXP, FF_EXPERT, D])
    out_d = nc.dram_tensor("out", [2, S, D], F32, kind="ExternalOutput").ap()
    out_tok = Tok("out")
    xs_d = nc.dram_tensor("xs_scratch", [128, KC, S], F32, kind="Internal").ap()
    t_xs = [[Tok("xs%d_%d" % (i, e)) for e in range(KC)] for i in range(NT)]

    tapd = {}

    def sb(name, shape, dt=F32):
        return nc.alloc_sbuf_tensor("s_" + name, list(shape), dt).ap()

    def sbt(name, shape, dt=F32):
        return sb(name, shape, dt), Tok(name)

    def tap(name, ap, tok):
        if name not in taps or name in tapd:
            return
        d = nc.dram_tensor("tap_" + name, list(ap.shape), ap.dtype, kind="ExternalOutput").ap()
        t = Tok("tap_" + name)
        k.dma("sp", d, ap, reads=[tok], writes=[t])
        tapd[name] = t

    def op(eng, fn, reads=(), writes=(), **kw):
        return k.op(eng, lambda e: getattr(e, fn)(**kw), reads=reads, writes=writes)

    def mm(out, lhsT, rhs, start=True, stop=True):
        return lambda e: e.matmul(out, lhsT=lhsT, rhs=rhs, start=start, stop=stop)

    def tr(out, in_, idn):
        return lambda e: e.transpose(out, in_, idn)

    def barrier():
        engs = ("pe", "dve", "act", "pool")
        evs = {n: ("c", n, k.seq[n]) for n in engs if k.last[n] is not None}
        for a in engs + ("sp",):
            for b_, ev in evs.items():
                if a != b_:
                    k._wait(a, ev)
            for rec in k.store_recs:
                k._wait(a, ("d", rec))

    consts, t_consts = sbt("consts", [128, CO_TOTAL])
    spl_t, t_spl = sbt("spl", [128, SP_LAYER])
    spg, t_spg = sbt("spg", [128, 72])
    k.dma("sp", consts, consts_d, writes=[t_consts])
    k.dma("sp", spg, smallp_d[:, SP_FING:SP_FING + 72], writes=[t_spg])

    def cst(name, rows=128):
        o, w = CO[name]
        return consts[0:rows, o:o + w]

    def spl(name):
        o, w = SP_OFF[name]
        return spl_t[:, o:o + w]

    ident = cst("ident")
    ones_f, t_ones = sbt("ones_f", [128, 128])
    ones_b, t_onesb = sbt("ones_b", [128, 128], BF16)
    nones_f, t_nones = sbt("nones_f", [128, 128])
    ident_b, t_identb = sbt("ident_b", [128, 128], BF16)
    rot_b, t_rotb = sbt("rot_b", [128, 128], BF16)
    ntt_f, t_ntt = sbt("ntt_f", [64, 64])
    op("dve", "memset", writes=[t_ones], ap=ones_f, constant=1.0)
    op("dve", "memset", writes=[t_onesb], ap=ones_b, constant=1.0)
    op("dve", "memset", writes=[t_nones], ap=nones_f, constant=-1.0)
    op("dve", "tensor_copy", reads=[t_consts], writes=[t_identb], out=ident_b, in_=ident)
    op("dve", "tensor_copy", reads=[t_consts], writes=[t_rotb], out=rot_b, in_=cst("rot"))
    op("dve", "tensor_scalar", reads=[t_consts], writes=[t_ntt], out=ntt_f, in0=cst("tt", 64), scalar1=-1.0,
       scalar2=None, op0=ALU.mult)
    TC = [t_consts, t_ones, t_onesb, t_nones, t_identb, t_rotb, t_ntt]

    psall = nc.alloc_psum_tensor("psall", [128, 8 * 512], F32).ap()
    banks = [(psall[:, i * 512:(i + 1) * 512], Tok("bank%d" % i, excl=True)) for i in range(8)]
    psA = Pool_(banks[0:4])
    psF = Pool_(banks[0:4])
    psD = Pool_(banks[4:8])
    psO = Pool_(banks[0:2])
    psT = Pool_(banks[2:4])
    ps8 = Pool_(banks)
    S_big = psall[:, 4 * 512:8 * 512]
    t_Sbig = [b_[1] for b_ in banks[4:8]]

    NSLOT = cfg.get("nslot", 8)
    wbf = Pool_([sbt("wbf%d" % i, [128, 2048], BF16) for i in range(NSLOT)])

    def load_w(srcs, a, b_):
        wb, t_wb = wbf.next()
        wbv = wb[:, 0:a * b_].rearrange("p (a b) -> p a b", b=b_)
        o = 0
        for i, (src, ai) in enumerate(srcs):
            k.dma("pool", wbv[:, o:o + ai, :], src, writes=[t_wb], group=(i > 0))
            o += ai
        assert o == a
        return wbv, t_wb

    def wrows(ap2d):
        return ap2d.rearrange("(kc p) n -> p kc n", p=128)

    cact, t_cact = sbt("cact", [128, KC * 2])
    k.dma("sp", cact, cT_d, writes=[t_cact])
    op("act", "activation", writes=[t_cact], out=cact, in_=cact, func=AF.Silu)
    cact_b, t_cactb = sbt("cact_b", [128, KC * 2], BF16)
    op("dve", "tensor_copy", reads=[t_cact], writes=[t_cactb], out=cact_b, in_=cact)
    cact3 = cact_b.rearrange("p (c b) -> p c b", b=2)
    modT, t_mod = sbt("modT", [128, DEPTH, 48, 2])
    mods, t_mods = sbt("mods", [128, DEPTH, 2, 6, 8])
    bmod_t, t_bmod = sbt("bmod_t", [128, 64])
    for l in range(LAYERS):
        k.dma("sp", bmod_t, smallp_d[:, l * SP_LAYER:l * SP_LAYER + 64], writes=[t_bmod])
        for g in range(24):
            w32, t_w32 = load_w([(wrows(w_mod_d[l])[:, :, g * 256:(g + 1) * 256], KC)], KC, 256)
            pb, t_pb = psA.next()
            fns = []
            for j in range(2):
                for kc in range(KC):
                    fns.append(mm(pb[:, j * 2:j * 2 + 2], w32[:, kc, j * 128:(j + 1) * 128], cact3[:, kc, :],
                                  start=(kc == 0), stop=(kc == KC - 1)))
            k.group("pe", fns, reads=[t_w32, t_cactb], writes=[t_pb])
            op("dve", "tensor_copy", reads=[t_pb], writes=[t_mod], out=modT[:, l, g * 2:(g + 1) * 2, :],
               in_=pb[:, 0:4].rearrange("p (j b) -> p j b", b=2))
        for b in range(2):
            op("dve", "tensor_tensor", reads=[t_bmod], writes=[t_mod], out=modT[:, l, :, b], in0=modT[:, l, :, b],
               in1=bmod_t[:, 16:64], op=ALU.add)
        for b in range(2):
            for part in range(6):
                src = modT[:, l, part * 8:(part + 1) * 8, b]
                dst = mods[:, l, b, part, :]
                if part in (1, 4):
                    go = 0 if part == 1 else 8
                    op("dve", "scalar_tensor_tensor", reads=[t_mod, t_bmod], writes=[t_mods], out=dst, in0=src, scalar=1.0,
                       in1=bmod_t[:, go:go + 8], op0=ALU.add, op1=ALU.mult)
                else:
                    op("dve", "tensor_copy", reads=[t_mod], writes=[t_mods], out=dst, in_=src)
    tap("mods", mods, t_mods)
    SHIFT1, G1S, GATE1, SHIFT2, G2S, GATE2 = 0, 1, 2, 3, 4, 5

    wsT_b, t_wst = sbt("wsT_b", [128, 4, 128], BF16)
    lay, t_lay = sbt("lay", [128, 16])
    subgs, t_subgs = sbt("subgs", [128, 128])
    halo, t_halo = sbt("halo", [128, 12, 3])
    Sst, t_S = sbt("Sst", [128, 4, 128])
    lamtmp = sbt("lamtmp", [128, 2, 64])

    rem = int(nc.sbuf_bytes_remaining)
    AW = (rem - 1024) // 4
    arena = sb("arena", [128, AW])
    ar = {"off": 0}
    tokcache = {}

    def aa(name, shape, dt=F32):
        n = 1
        for s_ in shape[1:]:
            n *= s_
        words = (n * (4 if dt in (F32, I32) else 2) + 3) // 4
        words = (words + 7) // 8 * 8
        o = ar["off"]
        assert o + words <= AW, ("arena overflow", name, o, words, AW)
        ar["off"] = o + words
        v = arena[0:shape[0], o:o + words]
        if dt != F32:
            v = v.bitcast(dt)
        v = v[:, 0:n]
        if len(shape) == 3:
            v = v.rearrange("p (a b) -> p a b", b=shape[2])
        elif len(shape) == 4:
            v = v.rearrange("p (a b c) -> p a b c", b=shape[2], c=shape[3])
        key = (name, o)
        if key not in tokcache:
            tokcache[key] = Tok(name)
        return v, tokcache[key]

    def layer_setup(l):
        k.dma("sp", spl_t, smallp_d[:, l * SP_LAYER:(l + 1) * SP_LAYER], writes=[t_spl])
        k.dma("pool", wsT_b, wst_d[:, l * 512:(l + 1) * 512].rearrange("p (g i) -> p g i", i=128), writes=[t_wst])
        op("dve", "memset", writes=[t_wst], ap=wsT_b[64:128, :, 0:64], constant=0.0)
        lam_v = spl("lam").rearrange("p (a d) -> p a d", d=64)
        pr_, t_pr = lamtmp
        op("dve", "tensor_tensor", reads=[t_spl], writes=[t_pr], out=pr_[:, 0, :], in0=lam_v[:, 0, :], in1=lam_v[:, 1, :], op=ALU.mult)
        op("dve", "tensor_tensor", reads=[t_spl], writes=[t_pr], out=pr_[:, 1, :], in0=lam_v[:, 2, :], in1=lam_v[:, 3, :], op=ALU.mult)
        op("dve", "reduce_sum", reads=[t_pr], writes=[t_lay], out=lay[:, 8:10], in_=pr_, axis=AX.X)
        op("act", "activation", reads=[], writes=[t_lay], out=lay[:, 8:10], in_=lay[:, 8:10], func=AF.Exp)
        op("dve", "scalar_tensor_tensor", reads=[], writes=[t_lay], out=lay[:, 0:1], in0=lay[:, 9:10], scalar=-lambda_init_fn(l),
           in1=lay[:, 8:9], op0=ALU.add, op1=ALU.subtract)
        op("dve", "tensor_scalar", reads=[t_spl], writes=[t_subgs], out=subgs, in0=spl("subg"), scalar1=1.0 - lambda_init_fn(l),
           scalar2=None, op0=ALU.mult)
        op("act", "activation", reads=[t_spl], writes=[t_lay], out=lay[:, 4:8], in_=spl("alog"), func=AF.Exp)
        op("dve", "tensor_scalar", reads=[], writes=[t_lay], out=lay[:, 4:8], in0=lay[:, 4:8], scalar1=-1.0, scalar2=None, op0=ALU.mult)

    def modnorm(xsrc, t_xsrc, c0, l, b, gidx, sidx, hdst, t_hd, sq_b, t_sq, rb, t_rb, tmps, h32=None):
        for c in range(KC):
            op("act", "activation", reads=[t_xsrc], writes=[t_sq], out=sq_b[:, c, :], in_=xsrc[:, c, c0:c0 + TT], func=AF.Square)
        pb, t_pb = psA.next()
        k.group("pe", [mm(pb, ones_b, sq_b[:, c, :], start=(c == 0), stop=(c == KC - 1)) for c in range(KC)],
                reads=[t_sq, t_onesb], writes=[t_pb])
        op("act", "activation", reads=[t_pb], writes=[t_rb], out=rb, in_=pb, func=AF.Sqrt, bias=eps_c, scale=1.0 / D)
        op("dve", "reciprocal", writes=[t_rb], out=rb, in_=rb)
        for c in range(KC):
            tm, t_tm = tmps.next()
            op("dve", "scalar_tensor_tensor", reads=[t_xsrc, t_mods, t_rb], writes=[t_tm], out=tm, in0=xsrc[:, c, c0:c0 + TT],
               scalar=mods[:, l, b, gidx, c:c + 1], in1=rb, op0=ALU.mult, op1=ALU.mult)
            if h32 is not None:
                op("act", "activation", reads=[t_tm, t_mods], writes=[h32[1]], out=h32[0][:, c, :], in_=tm, func=AF.Identity,
                   bias=mods[:, l, b, sidx, c:c + 1], scale=1.0)
                op("dve", "tensor_copy", reads=[h32[1]], writes=[t_hd], out=hdst[:, c, :], in_=h32[0][:, c, :])
            else:
                op("act", "activation", reads=[t_tm, t_mods], writes=[t_hd], out=hdst[:, c, :], in_=tm, func=AF.Identity,
                   bias=mods[:, l, b, sidx, c:c + 1], scale=1.0)

    epsc, t_epsc = sbt("epsc", [128, 4])
    op("dve", "memset", writes=[t_epsc], ap=epsc[:, 0:1], constant=EPS)
    op("dve", "memset", writes=[t_epsc], ap=epsc[:, 1:2], constant=1.0)
    op("dve", "memset", writes=[t_epsc], ap=epsc[:, 2:3], constant=-PI)
    eps_c = epsc[:, 0:1]
    one_c = epsc[:, 1:2]
    npi_c = epsc[:, 2:3]

    def mixer(l, b):
        barrier()
        ar["off"] = 0
        hT, t_h = aa("hT", [128, KC, TT], BF16)
        kT, t_kT = aa("kT", [128, 4, S], BF16)
        Vt, t_V = aa("Vt", [128, 16, 512], BF16)
        y_gm, t_ygm = aa("y_gm", [128, 4, TT], BF16)
        y_da, t_yda = aa("y_da", [128, 4, TT], BF16)
        y_dn, t_ydn = aa("y_dn", [128, 4, TT], BF16)
        tmps = Pool_([aa("tmpA%d" % i, [128, TT]) for i in range(3)])
        tmpb = Pool_([aa("tmpB%d" % i, [128, TT], BF16) for i in range(2)])
        st, t_st = aa("stat", [128, 32])
        P1_END = ar["off"]
        op("dve", "memset", writes=[t_halo], ap=halo, constant=0.0)
        op("dve", "memset", writes=[t_S], ap=Sst, constant=0.0)

        def wg(col0, ncols):
            return load_w([(wrows(w_in_d[l])[:, :, col0:col0 + ncols], KC)], KC, ncols)

        def proj_fm(wb, t_wb, nchunk, consume):
            for j in range(nchunk):
                pb, t_pb = psA.next()
                k.group("pe", [mm(pb, wb[:, kc, j * 128:(j + 1) * 128], hT[:, kc, :], start=(kc == 0), stop=(kc == KC - 1))
                               for kc in range(KC)], reads=[t_wb, t_h], writes=[t_pb])
                consume(j, pb, t_pb)

        def proj_tm(wb, t_wb, ncols, consume):
            for blk in range(4):
                pb, t_pb = psA.next()
                k.group("pe", [mm(pb[:, 0:ncols], hT[:, kc, blk * 128:(blk + 1) * 128], wb[:, kc, 0:ncols], start=(kc == 0),
                                  stop=(kc == KC - 1)) for kc in range(KC)], reads=[t_wb, t_h], writes=[t_pb])
                consume(blk, pb, t_pb)

        for t in range(NTILE):
            c0 = t * TT
            ar["off"] = P1_END
            xt, t_xt = aa("xt", [128, KC, TT])
            if l == 0:
                for c in range(KC):
                    k.dma("sp", xt[:, c, :], xT_d[b, c * 128:(c + 1) * 128, c0:c0 + TT], writes=[t_xt], group=(c > 0))
            else:
                for c in range(KC):
                    k.dma("sp", xt[:, c, :], xs_d[:, c, c0:c0 + TT], reads=[t_xs[t][c]], writes=[t_xt], group=(c > 0))
            rb, t_rb = aa("rb", [128, TT])
            sq_b, t_sq = aa("sq_b", [128, KC, TT], BF16)
            modnorm(xt, t_xt, 0, l, b, G1S, SHIFT1, hT, t_h, sq_b, t_sq, rb, t_rb, tmps)
            if l == 0 and t == 0:
                tap("h0", hT, t_h)
            if STAGE <= 1:
                continue
            barrier()
            ar["off"] = P1_END
            SUB0 = P1_END

            u_g, t_ug = aa("u_g", [128, 4, TT], BF16)
            vg, t_vg = aa("vg", [128, 4, 512])
            vln, t_vln = aa("vln", [128, 4, 512], BF16)
            for g2 in range(2):
                wb, t_wb = wg(COL_U + g2 * 256, 256)
                proj_fm(wb, t_wb, 2, lambda j, pb, t_pb, g2=g2: op(
                    "act", "activation", reads=[t_pb], writes=[t_ug], out=u_g[:, g2 * 2 + j, :], in_=pb, func=AF.Gelu))
            for g2 in range(2):
                wb, t_wb = wg(COL_V + g2 * 256, 256)
                proj_tm(wb, t_wb, 256, lambda blk, pb, t_pb, g2=g2: op(
                    "act", "activation", reads=[t_pb], writes=[t_vg], out=vg[:, blk, g2 * 256:(g2 + 1) * 256], in_=pb[:, 0:256], func=AF.Gelu))
            for blk in range(4):
                op("dve", "bn_stats", reads=[t_vg], writes=[t_st], out=st[:, 0:6], in_=vg[:, blk, :])
                op("dve", "bn_aggr", writes=[t_st], out=st[:, 8:10], in_=st[:, 0:6])
                op("act", "activation", writes=[t_st], out=st[:, 10:11], in_=st[:, 9:10], func=AF.Sqrt, bias=eps_c, scale=1.0)
                op("dve", "reciprocal", writes=[t_st], out=st[:, 10:11], in_=st[:, 10:11])
                tm, t_tm = tmps.next()
                op("dve", "tensor_scalar", reads=[t_vg, t_st], writes=[t_tm], out=tm, in0=vg[:, blk, :], scalar1=st[:, 8:9],
                   scalar2=st[:, 10:11], op0=ALU.subtract, op1=ALU.mult)
                op("dve", "tensor_tensor", reads=[t_spl], writes=[t_tm], out=tm, in0=tm, in1=spl("lng"), op=ALU.mult)
                op("dve", "tensor_tensor", reads=[t_spl, t_tm], writes=[t_vln], out=vln[:, blk, :], in0=tm, in1=spl("lnb"), op=ALU.add)
            for g in range(4):
                pb, t_pb = psA.next()
                k.group("pe", [mm(pb[:, blk * 128:(blk + 1) * 128], vln[:, blk, g * 128:(g + 1) * 128], wsT_b[:, g, :])
                               for blk in range(4)], reads=[t_vln, t_wst], writes=[t_pb])
                tm, t_tm = tmps.next()
                bsb = spl("bs")[:, g * 128:(g + 1) * 128].unsqueeze(1).to_broadcast([128, 4, 128])
                op("dve", "tensor_tensor", reads=[t_pb, t_spl], writes=[t_tm], out=tm.rearrange("p (a b) -> p a b", b=128),
                   in0=pb.rearrange("p (a b) -> p a b", b=128), in1=bsb, op=ALU.add)
                op("dve", "tensor_tensor", reads=[t_tm, t_ug], writes=[t_ygm], out=y_gm[:, g, :], in0=tm, in1=u_g[:, g, :], op=ALU.mult)
            if l == 0 and t == 0:
                tap("ygm", y_gm, t_ygm)
            if STAGE <= 2:
                continue
            barrier()
            ar["off"] = SUB0

            cosT, t_cos = aa("cosT", [128, TT])
            sinT, t_sin = aa("sinT", [128, TT])
            posi, t_posi = aa("posi", [128, TT], I32)
            k.dma("sp", posi, posb_d[b, :, c0:c0 + TT], writes=[t_posi])
            ang, t_ang = aa("ang", [128, TT])
            kq, t_kq = aa("kq", [128, TT])
            ki, t_ki = aa("ki", [128, TT], I32)
            op("dve", "tensor_copy", reads=[t_posi], writes=[t_ang], out=ang, in_=posi)
            op("dve", "tensor_scalar", reads=[t_consts], writes=[t_ang], out=ang, in0=ang, scalar1=cst("invf"), scalar2=None, op0=ALU.mult)
            op("dve", "tensor_scalar", reads=[t_ang], writes=[t_kq], out=kq, in0=ang, scalar1=1.0 / (2 * PI), scalar2=None, op0=ALU.mult)
            op("dve", "tensor_copy", reads=[t_kq], writes=[t_ki], out=ki, in_=kq)
            op("dve", "tensor_copy", reads=[t_ki], writes=[t_kq], out=kq, in_=ki)
            op("dve", "scalar_tensor_tensor", reads=[t_kq], writes=[t_ang], out=ang, in0=kq, scalar=-C1, in1=ang, op0=ALU.mult, op1=ALU.add)
            op("dve", "scalar_tensor_tensor", reads=[t_kq], writes=[t_ang], out=ang, in0=kq, scalar=-C2, in1=ang, op0=ALU.mult, op1=ALU.add)

            op("dve", "tensor_scalar", reads=[t_ang], writes=[t_kq], out=kq, in0=ang, scalar1=PI, scalar2=None, op0=ALU.is_gt)
            op("dve", "scalar_tensor_tensor", reads=[t_kq, t_ang], writes=[t_sin], out=sinT, in0=kq, scalar=-2 * PI, in1=ang, op0=ALU.mult, op1=ALU.add)
            op("dve", "tensor_scalar", reads=[t_sin], writes=[t_kq], out=kq, in0=sinT, scalar1=PI / 2, scalar2=None, op0=ALU.is_gt)
            op("dve", "scalar_tensor_tensor", reads=[t_kq, t_sin], writes=[t_cos], out=cosT, in0=kq, scalar=-2 * PI, in1=sinT, op0=ALU.mult, op1=ALU.add)
            op("dve", "tensor_scalar", writes=[t_cos], out=cosT, in0=cosT, scalar1=PI / 2, scalar2=None, op0=ALU.add)
            op("act", "activation", writes=[t_sin], out=sinT, in_=sinT, func=AF.Sin)
            op("act", "activation", writes=[t_cos], out=cosT, in_=cosT, func=AF.Sin)
            if l == 0 and t == 0:
                tap("cos", cosT, t_cos)
                tap("sin", sinT, t_sin)
            qT, t_qT = aa("qT", [128, 4, TT], BF16)
            PT, t_PT = aa("PT", [128, 16, 128], BF16)
            yv, t_yv = aa("yv", [128, 128])
            junk, t_junk = aa("junk", [128, 128])

            def rope_consume(dst_fn, t_dst):
                def f(j, pb, t_pb, hbase):
                    h = hbase + j
                    rw, t_rw = tmpb.next()
                    op("act", "copy", reads=[t_pb], writes=[t_rw], out=rw, in_=pb)
                    pr, t_pr = psA.next()
                    k.group("pe", [mm(pr, rot_b, rw)], reads=[t_rw, t_rotb], writes=[t_pr])
                    t1, t_t1 = tmps.next()
                    t2, t_t2 = tmps.next()
                    op("dve", "tensor_tensor", reads=[t_pb, t_cos], writes=[t_t1], out=t1, in0=pb, in1=cosT, op=ALU.mult)
                    op("dve", "tensor_tensor", reads=[t_pr, t_sin], writes=[t_t2], out=t2, in0=pr, in1=sinT, op=ALU.mult)
                    op("dve", "tensor_tensor", reads=[t_t1, t_t2], writes=[t_dst], out=dst_fn(h), in0=t1, in1=t2, op=ALU.add)
                return f
            qcons = rope_consume(lambda h: qT[:, h, :], t_qT)
            kcons = rope_consume(lambda h: kT[:, h, c0:c0 + TT], t_kT)
            for g2 in range(2 if "q" in cfg.get("parts", "qkv") else 0):
                wb, t_wb = wg(COL_Q + g2 * 256, 256)
                proj_fm(wb, t_wb, 2, lambda j, pb, t_pb, g2=g2: qcons(j, pb, t_pb, g2 * 2))
            for g2 in range(2 if "k" in cfg.get("parts", "qkv") else 0):
                wb, t_wb = wg(COL_KK + g2 * 256, 256)
                proj_fm(wb, t_wb, 2, lambda j, pb, t_pb, g2=g2: kcons(j, pb, t_pb, g2 * 2))
            for g2 in range(2 if "v" in cfg.get("parts", "qkv") else 0):
                wb, t_wb = wg(COL_VA + g2 * 256, 256)
                proj_tm(wb, t_wb, 256, lambda blk, pb, t_pb, g2=g2: op(
                    "act", "copy", reads=[t_pb], writes=[t_V], out=Vt[:, t * 4 + blk, g2 * 256:(g2 + 1) * 256], in_=pb[:, 0:256]))
            if l == 0 and t == 0:
                tap("qT", qT, t_qT)
                tap("kT", kT[:, :, 0:TT], t_kT)
            Ssb = [aa("Ssb%d" % i, [128, S]) for i in range(2)]
            Pms = [aa("Pm%d" % i, [128, S], BF16) for i in range(2)]
            asts = [aa("ast%d" % i, [128, 8]) for i in range(2)]
            rss = [aa("rs%d" % i, [128, 8]) for i in range(2)]
            ytms = [aa("ytm%d" % i, [128, 512], BF16) for i in range(2)]
            pOs = {}
            units = [(qb, h, p) for qb in range(4 if ACUT >= 2 else 0) for h in range(4) for p in range(2)]

            def stageA(ui):
                qb, h, p = units[ui]
                j = ui % 2
                gq = t * 4 + qb
                kend = (gq + 1) * 128
                Sb_, t_Sb_ = Ssb[j]
                Pj, t_Pj = Pms[j]
                aj, t_aj = asts[j]
                rs_, t_rs = rss[h % 2]
                ps_ = slice(p * 64, (p + 1) * 64)
                fns = []
                for pc in range((kend + 511) // 512):
                    n = min(512, kend - pc * 512)
                    fns.append(mm(S_big[:, pc * 512:pc * 512 + n], qT[ps_, h, qb * 128:(qb + 1) * 128], kT[ps_, h, pc * 512:pc * 512 + n]))
                k.group("pe", fns, reads=[t_qT, t_kT], writes=t_Sbig)
                op("act", "copy", reads=t_Sbig, writes=[t_Sb_], out=Sb_[:, 0:kend], in_=S_big[:, 0:kend])
                op("dve", "tensor_scalar", writes=[t_Sb_], out=Sb_[0:64, kend - 64:kend], in0=Sb_[0:64, kend - 64:kend],
                   scalar1=NEG, scalar2=None, op0=ALU.add)
                op("dve", "reduce_max", reads=[t_Sb_], writes=[t_aj], out=aj[:, 0:1], in_=Sb_[:, 0:kend], axis=AX.X)
                op("dve", "tensor_scalar", writes=[t_aj], out=aj[:, 1:2], in0=aj[:, 0:1], scalar1=-0.125, scalar2=None, op0=ALU.mult)
                op("act", "activation", reads=[t_Sb_, t_aj], writes=[t_Pj, t_rs], out=Pj[:, 0:kend], in_=Sb_[:, 0:kend],
                   func=AF.Exp, bias=aj[:, 1:2], scale=0.125, accum_out=rs_[:, p:p + 1])

            def stageB(ui):
                qb, h, p = units[ui]
                j = ui % 2
                gq = t * 4 + qb
                nkb = gq + 1
                Pj, t_Pj = Pms[j]
                rs_, t_rs = rss[h % 2]
                ytm, t_ytm = ytms[qb % 2]
                if p == 0:
                    pOs[(qb, h)] = psO.next()
                pO, t_pO = pOs[(qb, h)]
                for kb0 in range(0, nkb, 8):
                    nb = min(8, nkb - kb0)
                    ptb, t_ptb = psT.next()
                    ptv = ptb.bitcast(BF16)
                    k.group("pe", [tr(ptv[:, i * 128:(i + 1) * 128], Pj[:, (kb0 + i) * 128:(kb0 + i + 1) * 128], ident_b)
                                   for i in range(nb)], reads=[t_Pj, t_identb], writes=[t_ptb])
                    op("dve", "tensor_copy", reads=[t_ptb], writes=[t_PT], out=PT[:, kb0:kb0 + nb, :],
                       in_=ptv[:, 0:nb * 128].rearrange("p (a b) -> p a b", b=128))
                k.group("pe", [mm(pO[:, p * 128:(p + 1) * 128], PT[:, kb, :], Vt[:, kb, h * 128:(h + 1) * 128],
                                  start=(kb == 0), stop=(kb == nkb - 1)) for kb in range(nkb)], reads=[t_PT, t_V], writes=[t_pO])
                if p == 0:
                    return
                op("dve", "reciprocal", writes=[t_rs], out=rs_[:, 2:4], in_=rs_[:, 0:2])
                op("dve", "tensor_tensor", reads=[t_lay], writes=[t_rs], out=rs_[:, 3:4], in0=rs_[:, 3:4], in1=lay[:, 0:1], op=ALU.mult)
                op("dve", "tensor_scalar", reads=[t_pO, t_rs], writes=[t_yv], out=yv, in0=pO[:, 0:128], scalar1=rs_[:, 2:3], scalar2=None, op0=ALU.mult)
                op("dve", "scalar_tensor_tensor", reads=[t_pO, t_rs], writes=[t_yv], out=yv, in0=pO[:, 128:256], scalar=rs_[:, 3:4], in1=yv,
                   op0=ALU.mult, op1=ALU.add)
                op("dve", "tensor_tensor", reads=[t_yv], writes=[t_junk], out=junk, in0=yv, in1=yv, op=ALU.mult)
                op("dve", "reduce_sum", reads=[t_junk], writes=[t_rs], out=rs_[:, 4:5], in_=junk, axis=AX.X)
                op("act", "activation", writes=[t_rs], out=rs_[:, 5:6], in_=rs_[:, 4:5], func=AF.Ln, bias=eps_c, scale=1.0 / 128)
                op("act", "activation", writes=[t_rs], out=rs_[:, 5:6], in_=rs_[:, 5:6], func=AF.Exp, scale=-0.5)
                op("dve", "scalar_tensor_tensor", reads=[t_yv, t_rs, t_subgs], writes=[t_ytm], out=ytm[:, h * 128:(h + 1) * 128], in0=yv,
                   scalar=rs_[:, 5:6], in1=subgs, op0=ALU.mult, op1=ALU.mult)
                if h == 3:
                    ptb, t_ptb = psT.next()
                    ptv = ptb.bitcast(BF16)
                    k.group("pe", [tr(ptv[:, hh * 128:(hh + 1) * 128], ytm[:, hh * 128:(hh + 1) * 128], ident_b) for hh in range(4)],
                            reads=[t_ytm, t_identb], writes=[t_ptb])
                    op("dve", "tensor_copy", reads=[t_ptb], writes=[t_yda], out=y_da[:, :, qb * 128:(qb + 1) * 128],
                       in_=ptv[:, 0:512].rearrange("p (a b) -> p a b", b=128))

            for ui in range(len(units) + 1):
                if ui < len(units):
                    stageA(ui)
                if ui >= 1:
                    stageB(ui - 1)
            if l == 0 and t == 0 and ACUT >= 8:
                tap("yda", y_da, t_yda)
            if STAGE <= 3:
                continue
            barrier()
            ar["off"] = SUB0

            qn, t_qn = aa("qn", [128, 4, TT], BF16)
            kn, t_kn = aa("kn", [128, 4, TT], BF16)
            vn, t_vn = aa("vn", [128, 4, TT], BF16)
            Sb, t_Sb = aa("Sb", [128, 4, 128], BF16)
            zs, t_zs = aa("zs", [128, 4, TT], BF16)
            oT, t_oT = aa("oT", [128, 4, TT])
            cws = Pool_([aa("cw%d" % i, [128, TT + 3]) for i in range(2)])
            ab, t_ab = aa("ab", [64, 8, 8])
            gb, t_gb = aa("gb", [64, 4, 8, 4])
            dst_ = {0: (qn, t_qn), 1: (kn, t_kn), 2: (vn, t_vn)}
            op("act", "copy", reads=[t_S], writes=[t_Sb], out=Sb, in_=Sst)
            cwv = spl("convw").rearrange("p (c j) -> p c j", j=4)

            def dn_consume(j, pb, t_pb, ci0):
                ci = ci0 + j
                which, h = ci // 4, ci % 4
                dst, t_dst = dst_[which]
                cw, t_cw = cws.next()
                op("dve", "tensor_copy", reads=[t_halo], writes=[t_cw], out=cw[:, 0:3], in_=halo[:, ci, :])
                op("act", "copy", reads=[t_pb], writes=[t_cw], out=cw[:, 3:TT + 3], in_=pb)
                op("dve", "tensor_copy", reads=[t_cw], writes=[t_halo], out=halo[:, ci, :], in_=cw[:, TT:TT + 3])
                acc, t_acc = tmps.next()
                op("dve", "tensor_scalar", reads=[t_cw, t_spl], writes=[t_acc], out=acc, in0=cw[:, 3:TT + 3], scalar1=cwv[:, ci, 3:4],
                   scalar2=None, op0=ALU.mult)
                for jj in range(3):
                    op("dve", "scalar_tensor_tensor", reads=[t_cw, t_spl], writes=[t_acc], out=acc, in0=cw[:, jj:jj + TT],
                       scalar=cwv[:, ci, jj:jj + 1], in1=acc, op0=ALU.mult, op1=ALU.add)
                if which == 2:
                    op("act", "activation", reads=[t_acc], writes=[t_dst], out=dst[:, h, :], in_=acc, func=AF.Silu)
                else:
                    op("act", "activation", reads=[], writes=[t_acc], out=acc, in_=acc, func=AF.Silu)
                    sqb, t_sqb = tmpb.next()
                    op("act", "activation", reads=[t_acc], writes=[t_sqb], out=sqb, in_=acc, func=AF.Square)
                    pn, t_pn = ps8.next()
                    k.group("pe", [mm(pn, ones_b, sqb)], reads=[t_sqb, t_onesb], writes=[t_pn])
                    rn, t_rn = tmps.next()
                    op("act", "activation", reads=[t_pn], writes=[t_rn], out=rn, in_=pn, func=AF.Sqrt, bias=eps_c, scale=1.0)
                    op("dve", "reciprocal", writes=[t_rn], out=rn, in_=rn)
                    op("dve", "scalar_tensor_tensor", reads=[t_rn, t_acc], writes=[t_dst], out=dst[:, h, :], in0=acc,
                       scalar=(128.0 ** -0.5 if which == 0 else 1.0), in1=rn, op0=ALU.mult, op1=ALU.mult)
            for gi in range(6):
                wb, t_wb = wg(COL_DQ + gi * 256, 256)
                proj_fm(wb, t_wb, 2, lambda j, pb, t_pb, gi=gi: dn_consume(j, pb, t_pb, gi * 2))
            for g2 in range(2):
                wb, t_wb = wg(COL_DZ + g2 * 256, 256)
                proj_fm(wb, t_wb, 2, lambda j, pb, t_pb, g2=g2: op(
                    "act", "activation", reads=[t_pb], writes=[t_zs], out=zs[:, g2 * 2 + j, :], in_=pb, func=AF.Silu))
            wab, t_wab = wg(COL_AB, 8)
            pb, t_pb = ps8.next()
            for c in range(8):
                k.group("pe", [mm(pb[0:64, c * 8:(c + 1) * 8], hT[:, kc, c * 64:(c + 1) * 64], wab[:, kc, 0:8], start=(kc == 0),
                                  stop=(kc == KC - 1)) for kc in range(KC)], reads=[t_wab, t_h], writes=[t_pb])
            op("dve", "tensor_copy", reads=[t_pb], writes=[t_ab], out=ab, in_=pb[0:64, 0:64].rearrange("p (c e) -> p c e", e=8))
            dtb_b = spl("dtb")[0:64, :].unsqueeze(1).to_broadcast([64, 8, 4])
            nA_b = lay[0:64, 4:8].unsqueeze(1).to_broadcast([64, 8, 4])
            op("dve", "tensor_tensor", reads=[t_ab, t_spl], writes=[t_gb], out=gb[:, 3], in0=ab[:, :, 0:4], in1=dtb_b, op=ALU.add)
            op("act", "activation", writes=[t_gb], out=gb[:, 3], in_=gb[:, 3], func=AF.Exp)
            op("act", "activation", writes=[t_gb], out=gb[:, 3], in_=gb[:, 3], func=AF.Ln, bias=one_c[0:64, :], scale=1.0)
            op("dve", "tensor_tensor", reads=[t_lay], writes=[t_gb], out=gb[:, 0], in0=gb[:, 3], in1=nA_b, op=ALU.mult)
            op("act", "activation", reads=[t_ab], writes=[t_gb], out=gb[:, 1], in_=ab[:, :, 4:8], func=AF.Sigmoid)
            op("dve", "tensor_scalar", writes=[t_gb], out=gb[:, 2], in0=gb[:, 1], scalar1=-1.0, scalar2=None, op0=ALU.mult)
            if l == 0 and t == 0:
                tap("qn", qn, t_qn); tap("kn", kn, t_kn); tap("vn", vn, t_vn); tap("gb", gb, t_gb)

            def a64(name, cols=256, rows=64):
                return aa(name, [rows, cols])

            def mkset(s_):
                def b64(name, cols=256):
                    return aa(name, [64, cols], BF16)
                return {"ktok": b64("ktok" + s_, 512), "vtok": b64("vtok" + s_, 512), "R1": a64("R1" + s_), "R2": a64("R2" + s_),
                        "Dm": a64("Dm" + s_), "DTm": a64("DTm" + s_), "egs": a64("egs" + s_, 16), "glb": aa("glb" + s_, [128, 4]),
                        "egrow": aa("egrow" + s_, [128, 256]), "Pb": [b64("Pa" + s_), b64("Pb" + s_)], "Bb": [b64("Ba" + s_), b64("Bb" + s_)],
                        "Nb": [b64("Na" + s_), b64("Nb" + s_)], "rhs_w": b64("rhs_w" + s_, 512), "u_sb": a64("u_sb" + s_, 512),
                        "wT": aa("wT" + s_, [128, 256], BF16), "qgT": aa("qgT" + s_, [128, 256], BF16), "vnew": b64("vnew" + s_, 512),
                        "qkT": b64("qkT" + s_)}
            barrier()
            NW = cfg.get("nw", 4)
            BSs = [mkset("_%d" % i) for i in range(NW)]
            tt_c = cst("tt", 64); su_c = cst("su", 64); negu = cst("negu", 64); negl = cst("negl", 64)
            mstr = cst("mstrict", 64); i4 = cst("i4", 64); id64 = consts[0:64, CO["ident"][0]:CO["ident"][0] + 64]

            def v3(ap, inner):
                return ap.rearrange("p (h j) -> p h j", j=inner)


            def chunk_prep(c, BS):
                ktok, t_ktok = BS["ktok"]; vtok, t_vtok = BS["vtok"]
                R1, t_R1 = BS["R1"]; R2, t_R2 = BS["R2"]
                Dm, t_D = BS["Dm"]; DTm, t_DT = BS["DTm"]; Ds, t_Ds = Dm, t_D
                egs, t_egs = BS["egs"]; glb, t_glb = BS["glb"]; egrow, t_egrow = BS["egrow"]
                Pb = BS["Pb"]; Bb = BS["Bb"]; Nb = BS["Nb"]
                rhs_u, t_ru = vtok, t_vtok; rhs_w, t_rw_ = BS["rhs_w"]; kd, t_kd = ktok, t_ktok
                u_sb, t_u = BS["u_sb"]; wT, t_wT = BS["wT"]; qkT, t_qk = BS["qkT"]
                qgT, t_qg = BS["qgT"]; vnew, t_vnew = BS["vnew"]
                cs = slice(c * 64, (c + 1) * 64)
                gr = gb[:, 0, c, :]
                be = gb[:, 1, c, :]
                nbe = gb[:, 2, c, :]
                pk, t_pk = ps8.next()
                pkb = pk.bitcast(BF16)
                k.group("pe", [tr(pkb[0:64, h * 128:(h + 1) * 128], kn[:, h, cs], ident_b) for h in range(4)], reads=[t_kn, t_identb], writes=[t_pk])
                op("act", "copy", reads=[t_pk], writes=[t_ktok], out=ktok, in_=pkb[0:64, 0:512])
                yield
                pv, t_pv = ps8.next()
                pvb = pv.bitcast(BF16)
                k.group("pe", [tr(pvb[0:64, h * 128:(h + 1) * 128], vn[:, h, cs], ident_b) for h in range(4)], reads=[t_vn, t_identb], writes=[t_pv])
                op("dve", "tensor_copy", reads=[t_pv], writes=[t_vtok], out=vtok, in_=pvb[0:64, 0:512])
                yield
                op("dve", "tensor_tensor", reads=[t_gb, t_consts], writes=[t_R1], out=v3(R1, 64), in0=gr.unsqueeze(2).to_broadcast([64, 4, 64]),
                   in1=tt_c.unsqueeze(1).to_broadcast([64, 4, 64]), op=ALU.mult)
                yield
                op("dve", "tensor_copy", reads=[t_gb], writes=[t_R2], out=v3(R2, 64), in_=gr.unsqueeze(2).to_broadcast([64, 4, 64]))
                yield
                pg, t_pg = ps8.next()
                k.group("pe", [mm(pg[0:64, 0:256], tt_c, R2, start=True, stop=False),
                               mm(pg[0:64, 0:256], nones_f[0:64, 0:64], R1, start=False, stop=False),
                               mm(pg[0:64, 0:256], id64, negu, start=False, stop=True)],
                        reads=[t_R1, t_R2, t_consts, t_nones], writes=[t_pg])
                op("act", "activation", reads=[t_pg], writes=[t_D], out=Dm, in_=pg[0:64, 0:256], func=AF.Exp)
                yield
                pgt, t_pgt = ps8.next()
                k.group("pe", [mm(pgt[0:64, 0:256], ones_f[0:64, 0:64], R1, start=True, stop=False),
                               mm(pgt[0:64, 0:256], ntt_f, R2, start=False, stop=False),
                               mm(pgt[0:64, 0:256], id64, negl, start=False, stop=True)],
                        reads=[t_R1, t_R2, t_consts, t_ones, t_ntt], writes=[t_pgt])
                op("act", "activation", reads=[t_pgt], writes=[t_DT], out=DTm, in_=pgt[0:64, 0:256], func=AF.Exp)
                yield
                op("dve", "tensor_tensor", reads=[t_consts], writes=[t_Ds], out=Ds, in0=Dm, in1=mstr, op=ALU.mult)
                yield
                px, t_px = ps8.next()
                k.group("pe", [mm(px[:, 0:256], ones_f[0:64, :], R1), mm(px[:, 256:260], ones_f[0:64, :], gr),
                               mm(px[0:64, 264:268], tt_c, gr), mm(px[0:64, 268:272], su_c, gr)],
                        reads=[t_R1, t_gb, t_ones, t_consts], writes=[t_px])
                op("act", "activation", reads=[t_px], writes=[t_egrow], out=egrow, in_=px[:, 0:256], func=AF.Exp)
                yield
                op("act", "activation", reads=[t_px], writes=[t_glb], out=glb, in_=px[:, 256:260], func=AF.Exp)
                yield
                op("act", "activation", reads=[t_px], writes=[t_egs], out=egs[:, 0:8], in_=px[0:64, 264:272], func=AF.Exp)
                yield
                op("dve", "tensor_tensor", reads=[t_gb], writes=[t_egs], out=egs[:, 8:12], in0=egs[:, 0:4], in1=be, op=ALU.mult)
                yield
                pkk, t_pkk = ps8.next()
                k.group("pe", [mm(pkk[0:64, h * 64:(h + 1) * 64], kn[:, h, cs], kn[:, h, cs]) for h in range(4)], reads=[t_kn], writes=[t_pkk])
                P_, t_P_ = Pb[0]
                for h in range(4):
                    op("dve", "scalar_tensor_tensor", reads=[t_pkk, t_gb, t_Ds], writes=[t_P_], out=P_[:, h * 64:(h + 1) * 64],
                       in0=pkk[0:64, h * 64:(h + 1) * 64], scalar=nbe[:, h:h + 1], in1=Ds[:, h * 64:(h + 1) * 64], op0=ALU.mult, op1=ALU.mult)
                pt_, t_pt_ = ps8.next()
                pt_ = pt_.bitcast(BF16)
                k.group("pe", [tr(pt_[0:64, h * 64:(h + 1) * 64], P_[:, h * 64:(h + 1) * 64], ident_b[0:64, 0:64]) for h in range(4)],
                        reads=[t_P_, t_identb], writes=[t_pt_])
                B_, t_B_ = Bb[0]
                N_, t_N_ = Nb[0]
                op("act", "copy", reads=[t_pt_], writes=[t_B_], out=B_, in_=pt_[0:64, 0:256])
                yield
                op("dve", "tensor_tensor", reads=[t_pt_, t_consts], writes=[t_N_], out=N_, in0=pt_[0:64, 0:256], in1=i4, op=ALU.add)
                yield
                for j in range(1, 6):
                    Pn, t_Pn = Pb[j % 2]
                    Bn, t_Bn = Bb[j % 2]
                    Nn, t_Nn = Nb[j % 2]
                    pp, t_pp = ps8.next()
                    k.group("pe", [mm(pp[0:64, h * 64:(h + 1) * 64], B_[:, h * 64:(h + 1) * 64], P_[:, h * 64:(h + 1) * 64]) for h in range(4)],
                            reads=[t_B_, t_P_], writes=[t_pp])
                    op("act", "copy", reads=[t_pp], writes=[t_Pn], out=Pn, in_=pp[0:64, 0:256])
                    if j < 5:
                        pbb, t_pbb = ps8.next()
                        k.group("pe", [mm(pbb[0:64, h * 64:(h + 1) * 64], P_[:, h * 64:(h + 1) * 64], B_[:, h * 64:(h + 1) * 64]) for h in range(4)],
                                reads=[t_B_, t_P_], writes=[t_pbb])
                        op("dve", "tensor_copy", reads=[t_pbb], writes=[t_Bn], out=Bn, in_=pbb[0:64, 0:256])
                    pn2, t_pn2 = ps8.next()
                    k.group("pe", [mm(pn2[0:64, h * 64:(h + 1) * 64], Pn[:, h * 64:(h + 1) * 64], N_[:, h * 64:(h + 1) * 64]) for h in range(4)],
                            reads=[t_Pn, t_N_], writes=[t_pn2])
                    op("dve", "tensor_tensor", reads=[t_pn2, t_N_], writes=[t_Nn], out=Nn, in0=pn2[0:64, 0:256], in1=N_, op=ALU.add)
                    P_, t_P_, B_, t_B_, N_, t_N_ = Pn, t_Pn, Bn, t_Bn, Nn, t_Nn
                op("dve", "tensor_tensor", reads=[t_vtok, t_gb], writes=[t_ru], out=v3(rhs_u, 128), in0=v3(vtok, 128),
                   in1=be.unsqueeze(2).to_broadcast([64, 4, 128]), op=ALU.mult)
                yield
                op("dve", "tensor_tensor", reads=[t_ktok, t_egs], writes=[t_rw_], out=v3(rhs_w, 128), in0=v3(ktok, 128),
                   in1=egs[:, 8:12].unsqueeze(2).to_broadcast([64, 4, 128]), op=ALU.mult)
                yield
                op("dve", "tensor_tensor", reads=[t_ktok, t_egs], writes=[t_kd], out=v3(kd, 128), in0=v3(ktok, 128),
                   in1=egs[:, 4:8].unsqueeze(2).to_broadcast([64, 4, 128]), op=ALU.mult)
                yield
                pu, t_pu = ps8.next()
                k.group("pe", [mm(pu[0:64, h * 128:(h + 1) * 128], N_[:, h * 64:(h + 1) * 64], rhs_u[:, h * 128:(h + 1) * 128]) for h in range(4)],
                        reads=[t_N_, t_ru], writes=[t_pu])
                op("act", "copy", reads=[t_pu], writes=[t_u], out=u_sb, in_=pu[0:64, :])
                yield
                pw, t_pw = ps8.next()
                k.group("pe", [mm(pw[:, h * 64:(h + 1) * 64], rhs_w[:, h * 128:(h + 1) * 128], N_[:, h * 64:(h + 1) * 64]) for h in range(4)],
                        reads=[t_N_, t_rw_], writes=[t_pw])
                op("dve", "tensor_copy", reads=[t_pw], writes=[t_wT], out=wT, in_=pw[:, 0:256])
                yield
                pq, t_pq = ps8.next()
                k.group("pe", [mm(pq[0:64, h * 64:(h + 1) * 64], kn[:, h, cs], qn[:, h, cs]) for h in range(4)], reads=[t_kn, t_qn], writes=[t_pq])
                op("dve", "tensor_tensor", reads=[t_pq, t_DT], writes=[t_qk], out=qkT, in0=pq[0:64, 0:256], in1=DTm, op=ALU.mult)
                yield
                op("dve", "tensor_tensor", reads=[t_qn, t_egrow], writes=[t_qg], out=v3(qgT, 64), in0=qn[:, :, cs], in1=v3(egrow, 64), op=ALU.mult)
                yield

            def chunk_recur(c, BS):
                ktok, t_ktok = BS["ktok"]; vtok, t_vtok = BS["vtok"]
                R1, t_R1 = BS["R1"]; R2, t_R2 = BS["R2"]
                Dm, t_D = BS["Dm"]; DTm, t_DT = BS["DTm"]; Ds, t_Ds = Dm, t_D
                egs, t_egs = BS["egs"]; glb, t_glb = BS["glb"]; egrow, t_egrow = BS["egrow"]
                Pb = BS["Pb"]; Bb = BS["Bb"]; Nb = BS["Nb"]
                rhs_u, t_ru = vtok, t_vtok; rhs_w, t_rw_ = BS["rhs_w"]; kd, t_kd = ktok, t_ktok
                u_sb, t_u = BS["u_sb"]; wT, t_wT = BS["wT"]; qkT, t_qk = BS["qkT"]
                qgT, t_qg = BS["qgT"]; vnew, t_vnew = BS["vnew"]
                cs = slice(c * 64, (c + 1) * 64)
                gr = gb[:, 0, c, :]
                be = gb[:, 1, c, :]
                nbe = gb[:, 2, c, :]
                pws, t_pws = ps8.next()
                k.group("pe", [mm(pws[0:64, h * 128:(h + 1) * 128], wT[:, h * 64:(h + 1) * 64], Sb[:, h, :]) for h in range(4)],
                        reads=[t_wT, t_Sb], writes=[t_pws])
                op("dve", "tensor_tensor", reads=[t_u, t_pws], writes=[t_vnew], out=vnew, in0=u_sb, in1=pws[0:64, :], op=ALU.subtract)
                po, t_po = ps8.next()
                fns = []
                for h in range(4):
                    fns.append(mm(po[:, h * 64:(h + 1) * 64], Sb[:, h, :], qgT[:, h * 64:(h + 1) * 64], start=True, stop=False))
                    fns.append(mm(po[:, h * 64:(h + 1) * 64], vnew[:, h * 128:(h + 1) * 128], qkT[:, h * 64:(h + 1) * 64], start=False, stop=True))
                k.group("pe", fns, reads=[t_Sb, t_qg, t_vnew, t_qk], writes=[t_po])
                op("act", "copy", reads=[t_po], writes=[t_oT], out=oT[:, :, cs], in_=v3(po[:, 0:256], 64))
                pds, t_pds = ps8.next()
                k.group("pe", [mm(pds[:, h * 128:(h + 1) * 128], kd[:, h * 128:(h + 1) * 128], vnew[:, h * 128:(h + 1) * 128]) for h in range(4)],
                        reads=[t_kd, t_vnew], writes=[t_pds])
                for h in range(4):
                    op("dve", "scalar_tensor_tensor", reads=[t_pds, t_glb], writes=[t_S], out=Sst[:, h, :], in0=Sst[:, h, :], scalar=glb[:, h:h + 1],
                       in1=pds[:, h * 128:(h + 1) * 128], op0=ALU.mult, op1=ALU.add)
                op("act", "copy", reads=[t_S], writes=[t_Sb], out=Sb, in_=Sst)

            for c0_ in range(0, 8, NW):
                gens = [chunk_prep(c0_ + i, BSs[i]) for i in range(NW)]
                alive = list(gens)
                while alive:
                    for g_ in list(alive):
                        try:
                            next(g_)
                        except StopIteration:
                            alive.remove(g_)
                for i in range(NW):
                    chunk_recur(c0_ + i, BSs[i])
            if l == 0 and t == 0:
                tap("oT", oT, t_oT)
            for h in range(4):
                sqb, t_sqb = tmpb.next()
                op("act", "activation", reads=[t_oT], writes=[t_sqb], out=sqb, in_=oT[:, h, :], func=AF.Square)
                pn, t_pn = ps8.next()
                k.group("pe", [mm(pn, ones_b, sqb)], reads=[t_sqb, t_onesb], writes=[t_pn])
                rn, t_rn = tmps.next()
                op("act", "activation", reads=[t_pn], writes=[t_rn], out=rn, in_=pn, func=AF.Sqrt, bias=eps_c, scale=1.0 / 128)
                op("dve", "reciprocal", writes=[t_rn], out=rn, in_=rn)
                op("dve", "scalar_tensor_tensor", reads=[t_oT, t_spl], writes=[t_rn], out=rn, in0=oT[:, h, :], scalar=spl("dng"), in1=rn,
                   op0=ALU.mult, op1=ALU.mult)
                op("dve", "tensor_tensor", reads=[t_rn, t_zs], writes=[t_ydn], out=y_dn[:, h, :], in0=rn, in1=zs[:, h, :], op=ALU.mult)
            if l == 0 and t == 0:
                tap("ydn", y_dn, t_ydn)
            if STAGE <= 4:
                continue
            barrier()
            ar["off"] = SUB0

            merged, t_mg = aa("merged", [128, KC, TT], BF16)
            sgs = Pool_([aa("sg%d" % i, [128, TT]) for i in range(3)])
            macc, t_macc = aa("macc", [128, TT])
            xins = Pool_([aa("xin%d" % i, [128, TT]) for i in range(3)])
            ybr = [(y_gm, t_ygm), (y_da, t_yda), (y_dn, t_ydn)]
            for d in range(KC):
                for br in range(3):
                    wgt, t_wgt = load_w([(wrows(w_in_d[l])[:, :, COL_G + br * 1024 + d * 128:COL_G + br * 1024 + (d + 1) * 128], KC)], KC, 128)
                    pg, t_pg = ps8.next()
                    k.group("pe", [mm(pg, wgt[:, kc, :], hT[:, kc, :], start=(kc == 0), stop=(kc == KC - 1)) for kc in range(KC)],
                            reads=[t_wgt, t_h], writes=[t_pg])
                    sg, t_sg = sgs.next()
                    op("act", "activation", reads=[t_pg], writes=[t_sg], out=sg, in_=pg, func=AF.Sigmoid)
                    pp, t_pp = ps8.next()
                    yb, t_yb = ybr[br]
                    wbr, t_wbr = load_w([(wrows(w_br_d[br][l])[:, :, d * 128:(d + 1) * 128], 4)], 4, 128)
                    k.group("pe", [mm(pp, wbr[:, kc, :], yb[:, kc, :], start=(kc == 0), stop=(kc == 3)) for kc in range(4)],
                            reads=[t_wbr, t_yb], writes=[t_pp])
                    if br == 0:
                        op("dve", "tensor_tensor", reads=[t_pp, t_sg], writes=[t_macc], out=macc, in0=pp, in1=sg, op=ALU.mult)
                    else:
                        op("dve", "tensor_tensor", reads=[t_pp], writes=[t_sg], out=sg, in0=pp, in1=sg, op=ALU.mult)
                        if br == 1:
                            op("dve", "tensor_tensor", reads=[t_sg], writes=[t_macc], out=macc, in0=macc, in1=sg, op=ALU.add)
                        else:
                            op("dve", "tensor_tensor", reads=[t_sg, t_macc], writes=[t_mg], out=merged[:, d, :], in0=macc, in1=sg, op=ALU.add)
            if l == 0 and t == 0:
                tap("merged", merged, t_mg)
            for g4 in range(4):
                wo, t_wo = load_w([(wrows(w_out_d[l])[:, :, g4 * 256:(g4 + 1) * 256], KC)], KC, 256)
                for j in range(2):
                    e_ = g4 * 2 + j
                    po, t_po = ps8.next()
                    k.group("pe", [mm(po, wo[:, dc, j * 128:(j + 1) * 128], merged[:, dc, :], start=(dc == 0), stop=(dc == KC - 1))
                                   for dc in range(KC)], reads=[t_wo, t_mg], writes=[t_po])
                    xin, t_xin = xins.next()
                    if l == 0:
                        k.dma("sp", xin, xT_d[b, e_ * 128:(e_ + 1) * 128, c0:c0 + TT], writes=[t_xin])
                    else:
                        k.dma("sp", xin, xs_d[:, e_, c0:c0 + TT], reads=[t_xs[t][e_]], writes=[t_xin])
                    op("dve", "scalar_tensor_tensor", reads=[t_po, t_mods], writes=[t_xin], out=xin, in0=po,
                       scalar=mods[:, l, b, GATE1, e_:e_ + 1], in1=xin, op0=ALU.mult, op1=ALU.add)
                    k.dma("sp", xs_d[:, e_, c0:c0 + TT], xin, reads=[t_xin], writes=[t_xs[t][e_]])
            barrier()


    def ffn(l, b, last):
        barrier()
        ar["off"] = 0
        xT, t_x = aa("xT", [128, KC, S])
        rb, t_rb = aa("rb", [128, TT])
        sq_b, t_sq = aa("sq_b", [128, KC, TT], BF16)
        tmps = Pool_([aa("tmpA%d" % i, [128, TT]) for i in range(3)])
        FMARK = ar["off"]
        h2, t_h2 = aa("h2", [128, KC, S], BF16)
        moe = (l % 2 == 1)
        for t in range(NTILE):
            k.dma("sp", xT[:, :, t * TT:(t + 1) * TT], xs_d[:, :, t * TT:(t + 1) * TT], reads=t_xs[t], writes=[t_x], group=(t > 0))
        if moe:
            h32, t_h32 = aa("h32", [128, KC, TT])
            WT, t_WT = aa("WT", [8, S])
            rhe, t_rhe = aa("rhe", [8, TT])
            Wb0, t_Wb0 = aa("Wbc0", [128, S], BF16)
            Wb1, t_Wb1 = aa("Wbc1", [128, S], BF16)
            Wbcs = [Wb0, Wb1]; t_Wbcs = [t_Wb0, t_Wb1]
            lg, t_lg = aa("lg", [128, 64])
            wr3 = spg[:, 8:72].rearrange("p (c e) -> p c e", e=8)
        for t in range(NTILE):
            modnorm(xT, t_x, t * TT, l, b, G2S, SHIFT2, h2[:, :, t * TT:(t + 1) * TT], t_h2, sq_b, t_sq, rb, t_rb, tmps,
                    h32=((h32, t_h32) if moe else None))
            if moe:
                for blk in range(4):
                    gblk = t * 4 + blk
                    pl, t_pl = ps8.next()
                    k.group("pe", [mm(pl[:, 0:8], h32[:, kc, blk * 128:(blk + 1) * 128], wr3[:, kc, :], start=(kc == 0), stop=(kc == KC - 1))
                                   for kc in range(KC)], reads=[t_h32, t_spg], writes=[t_pl])
                    L = lg[:, 0:8]; E1 = lg[:, 8:16]; M2 = lg[:, 16:24]; E2 = lg[:, 24:32]; Wt = lg[:, 32:40]
                    m1 = lg[:, 40:41]; m2 = lg[:, 41:42]; dd = lg[:, 42:43]; w1 = lg[:, 43:44]; w2 = lg[:, 44:45]
                    op("dve", "tensor_copy", reads=[t_pl], writes=[t_lg], out=L, in_=pl[:, 0:8])
                    op("dve", "reduce_max", writes=[t_lg], out=m1, in_=L, axis=AX.X)
                    op("dve", "tensor_scalar", writes=[t_lg], out=E1, in0=L, scalar1=m1, scalar2=None, op0=ALU.is_equal)
                    op("dve", "scalar_tensor_tensor", writes=[t_lg], out=M2, in0=E1, scalar=-1e30, in1=L, op0=ALU.mult, op1=ALU.add)
                    op("dve", "reduce_max", writes=[t_lg], out=m2, in_=M2, axis=AX.X)
                    op("dve", "tensor_scalar", writes=[t_lg], out=E2, in0=M2, scalar1=m2, scalar2=None, op0=ALU.is_equal)
                    op("dve", "tensor_tensor", writes=[t_lg], out=dd, in0=m2, in1=m1, op=ALU.subtract)
                    op("act", "activation", writes=[t_lg], out=dd, in_=dd, func=AF.Exp)
                    op("dve", "tensor_scalar", writes=[t_lg], out=w1, in0=dd, scalar1=1.0, scalar2=None, op0=ALU.add)
                    op("dve", "reciprocal", writes=[t_lg], out=w1, in_=w1)
                    op("dve", "tensor_tensor", writes=[t_lg], out=w2, in0=dd, in1=w1, op=ALU.mult)
                    op("dve", "tensor_scalar", writes=[t_lg], out=Wt, in0=E1, scalar1=w1, scalar2=None, op0=ALU.mult)
                    op("dve", "scalar_tensor_tensor", writes=[t_lg], out=Wt, in0=E2, scalar=w2, in1=Wt, op0=ALU.mult, op1=ALU.add)
                    pt_, t_pt_ = ps8.next()
                    k.group("pe", [tr(pt_[0:8, 0:128], Wt, ident)], reads=[t_lg, t_consts], writes=[t_pt_])
                    op("act", "copy", reads=[t_pt_], writes=[t_WT], out=WT[:, gblk * 128:(gblk + 1) * 128], in_=pt_[0:8, 0:128])
        if moe and b == 0:
            tap("WT", WT, t_WT)
        if b == 0:
            tap("h2_%d" % l, h2, t_h2)

        hids = Pool_([aa("hid%d" % i, [128, 2, TT], BF16) for i in range(2)])
        pend = {"f": None}

        def expert_pass(wg_d, wu_d, wd_d, F, scale_by=None):
            for fg in range(F // 256):
                wgb, t_wgb = load_w([(wrows(wg_d)[:, :, fg * 256:(fg + 1) * 256], KC)], KC, 256)
                wub, t_wub = load_w([(wrows(wu_d)[:, :, fg * 256:(fg + 1) * 256], KC)], KC, 256)
                wdb, t_wdb = load_w([(wrows(wd_d[fg * 256:(fg + 1) * 256, :]), 2)], 2, D)
                for t in range(NTILE):
                    tc_ = slice(t * TT, (t + 1) * TT)
                    hid, t_hid = hids.next()
                    for j in range(2):
                        pa, t_pa = psF.next()
                        k.group("pe", [mm(pa, wgb[:, kc, j * 128:(j + 1) * 128], h2[:, kc, tc_], start=(kc == 0), stop=(kc == KC - 1))
                                       for kc in range(KC)], reads=[t_wgb, t_h2], writes=[t_pa])
                        pu_, t_pu_ = psF.next()
                        k.group("pe", [mm(pu_, wub[:, kc, j * 128:(j + 1) * 128], h2[:, kc, tc_], start=(kc == 0), stop=(kc == KC - 1))
                                       for kc in range(KC)], reads=[t_wub, t_h2], writes=[t_pu_])
                        sa, t_sa = tmps.next()
                        op("act", "activation", reads=[t_pa], writes=[t_sa], out=sa, in_=pa, func=AF.Silu)
                        if scale_by is not None:
                            op("dve", "tensor_tensor", reads=[scale_by[1]], writes=[t_sa], out=sa, in0=sa, in1=scale_by[0][:, tc_], op=ALU.mult)
                        op("dve", "tensor_tensor", reads=[t_pu_, t_sa], writes=[t_hid], out=hid[:, j, :], in0=pu_, in1=sa, op=ALU.mult)
                    if pend["f"] is not None:
                        pend["f"]()

                    def down(wdb=wdb, t_wdb=t_wdb, hid=hid, t_hid=t_hid, tc_=tc_):
                        for d in range(KC):
                            pd, t_pd = psD.next()
                            k.group("pe", [mm(pd, wdb[:, j, d * 128:(d + 1) * 128], hid[:, j, :], start=(j == 0), stop=(j == 1)) for j in range(2)],
                                    reads=[t_wdb, t_hid], writes=[t_pd])
                            op("dve", "scalar_tensor_tensor", reads=[t_pd, t_mods], writes=[t_x], out=xT[:, d, tc_], in0=pd,
                               scalar=mods[:, l, b, GATE2, d:d + 1], in1=xT[:, d, tc_], op0=ALU.mult, op1=ALU.add)
                    pend["f"] = down

        def flush():
            if pend["f"] is not None:
                pend["f"]()
                pend["f"] = None

        if not moe:
            expert_pass(ffn_g_d[0], ffn_u_d[0], ffn_d_d[0], FF_DENSE)
            flush()
        else:
            for e_ in range(cfg.get("nexp", NEXP)):
                for t in range(NTILE):
                    op("dve", "tensor_scalar", reads=[t_WT, t_consts], writes=[t_rhe], out=rhe, in0=WT[:, t * TT:(t + 1) * TT],
                       scalar1=consts[0:8, CO["ident"][0] + e_:CO["ident"][0] + e_ + 1], scalar2=None, op0=ALU.mult)
                    pw, t_pw = ps8.next()
                    k.group("pe", [mm(pw, ones_f[0:8, :], rhe)], reads=[t_rhe, t_ones], writes=[t_pw])
                    op("act", "copy", reads=[t_pw], writes=[t_Wbcs[e_ % 2]], out=Wbcs[e_ % 2][:, t * TT:(t + 1) * TT], in_=pw)
                expert_pass(moe_g_d[0, e_], moe_u_d[0, e_], moe_d_d[0, e_], FF_EXPERT, scale_by=(Wbcs[e_ % 2], t_Wbcs[e_ % 2]))
            flush()
        if b == 0:
            tap("xffn_%d" % l, xT, t_x)
        if not last:
            for t in range(NTILE):
                for c in range(KC):
                    k.dma("sp", xs_d[:, c, t * TT:(t + 1) * TT], xT[:, c, t * TT:(t + 1) * TT], reads=[t_x], writes=[t_xs[t][c]])
            return
        barrier()
        ar["off"] = FMARK
        xn, t_xn = aa("xn", [128, KC, TT])
        otm = Pool_([aa("otm%d" % i, [128, D]) for i in range(2)])
        for t in range(NTILE):
            for c in range(KC):
                op("act", "activation", reads=[t_x], writes=[t_sq], out=sq_b[:, c, :], in_=xT[:, c, t * TT:(t + 1) * TT], func=AF.Square)
            pb, t_pb = ps8.next()
            k.group("pe", [mm(pb, ones_b, sq_b[:, c, :], start=(c == 0), stop=(c == KC - 1)) for c in range(KC)],
                    reads=[t_sq, t_onesb], writes=[t_pb])
            op("act", "activation", reads=[t_pb], writes=[t_rb], out=rb, in_=pb, func=AF.Sqrt, bias=eps_c, scale=1.0 / D)
            op("dve", "reciprocal", writes=[t_rb], out=rb, in_=rb)
            for c in range(KC):
                op("dve", "scalar_tensor_tensor", reads=[t_x, t_spg, t_rb], writes=[t_xn], out=xn[:, c, :], in0=xT[:, c, t * TT:(t + 1) * TT],
                   scalar=spg[:, c:c + 1], in1=rb, op0=ALU.mult, op1=ALU.mult)
            for blk in range(4):
                ot, t_ot = otm.next()
                for half in range(2):
                    pt_, t_pt_ = ps8.next()
                    k.group("pe", [tr(pt_[:, i * 128:(i + 1) * 128], xn[:, half * 4 + i, blk * 128:(blk + 1) * 128], ident) for i in range(4)],
                            reads=[t_xn, t_consts], writes=[t_pt_])
                    if half == 0:
                        op("act", "copy", reads=[t_pt_], writes=[t_ot], out=ot[:, 0:512], in_=pt_)
                    else:
                        op("dve", "tensor_copy", reads=[t_pt_], writes=[t_ot], out=ot[:, 512:1024], in_=pt_)
                r0 = t * TT + blk * 128
                k.dma("sp", out_d[b, r0:r0 + 128, :], ot, reads=[t_ot], writes=[out_tok])

    for b in range(NSEQ):
        for l in range(LAYERS):
            layer_setup(l)
            mixer(l, b)
            if STAGE >= 6:
                ffn(l, b, last=(l == LAYERS - 1))

    fin = [out_tok] + list(tapd.values())
    if STAGE < 99:
        z, t_z = sbt("zz", [128, 8])
        op("dve", "memset", writes=[t_z], ap=z, constant=0.0)
        k.dma("sp", out_d[0, 0:128, 0:8], z, reads=[t_z], writes=[out_tok])
    k.finish(fin)
    nc._k = k
    return nc


def prep_core_inputs(inp, core):
    x = np.asarray(inp["x"], np.float32)
    b0 = 2 * core
    xT = np.ascontiguousarray(x[b0:b0 + 2].transpose(0, 2, 1))
    pos = np.asarray(inp["positions"], np.int32)[b0:b0 + 2]
    posb = np.ascontiguousarray(np.broadcast_to(pos[:, None, :], (2, 128, S)))
    c = np.asarray(inp["c"], np.float32)[b0:b0 + 2]
    cT = np.ascontiguousarray(c.reshape(2, KC, 128).transpose(2, 1, 0)).reshape(128, KC * 2)
    return {"xT": xT, "posb": posb, "cT": cT}


def shared_inputs(inp):
    ws = np.asarray(inp["gm_w_s"], np.float32)
    wst = np.ascontiguousarray(ws.transpose(3, 0, 1, 2)).reshape(128, DEPTH * 4 * 128)
    sh = {"smallp": make_smallp(inp), "consts": make_consts(), "wst": wst}
    for n in ["w_mod", "w_in", "w_br_gm", "w_br_da", "w_br_dn", "w_out", "ffn_w_gate", "ffn_w_up",
              "ffn_w_down", "moe_w_gate", "moe_w_up", "moe_w_down"]:
        sh[n] = np.ascontiguousarray(np.asarray(inp[n], np.float32))
    return sh


def kernel(**inputs):
    nc = build()
    sh = shared_inputs(inputs)
    in_maps = []
    for core in range(8):
        m = dict(sh)
        m.update(prep_core_inputs(inputs, core))
        in_maps.append(m)
    res = run_bass_kernel_spmd(nc, in_maps, core_ids=list(range(8)))
    return np.concatenate([np.asarray(r["out"], np.float32) for r in res.results], axis=0)
```

```python
import math
import numpy as np
import concourse.bass as bass
import concourse.mybir as mybir
from concourse.bass_utils import run_bass_kernel_spmd

F32 = mybir.dt.float32
BF16 = mybir.dt.bfloat16
I32 = mybir.dt.int32
AF = mybir.ActivationFunctionType
ALU = mybir.AluOpType
AX = mybir.AxisListType

SEM_LIMIT = 20000
ENGS = ("pe", "dve", "act", "pool", "sp")


class Tok:
    __slots__ = ("name", "w", "r", "dsem", "pr", "excl")

    def __init__(self, name, excl=False):
        self.name = name
        self.excl = excl
        self.w = None
        self.r = []
        self.dsem = None
        self.pr = []


class K:
    def __init__(self, nc, same=("dve", "act", "pool"), eager=True):
        self.nc = nc
        self.eager = eager
        self.e = {"pe": nc.tensor, "dve": nc.vector, "act": nc.scalar,
                  "pool": nc.gpsimd, "sp": nc.sync}
        self.nsem = 0
        self.sem = {}
        self.cnt = {}
        for n in ENGS:
            self._new_epoch(n)
        self.seq = {n: 0 for n in ENGS}
        self.last = {n: None for n in ENGS}
        self.sigmap = {n: [] for n in ENGS}
        self.waited = {n: {} for n in ENGS}
        self.same = set(same)
        self.rsem = {}
        self.store_recs = []
        self.ninstr = 0
        self.nwait = 0

    def _alloc(self, name):
        self.nsem += 1
        return self.nc.alloc_semaphore("%s_%d" % (name, self.nsem))

    def _new_epoch(self, n):
        self.sem[n] = self._alloc("e_" + n)
        self.cnt[n] = 0

    def _resolve(self, ev):
        if ev[0] == "c":
            _, eng, seq = ev
            lst = self.sigmap[eng]
            lo, hi = 0, len(lst)
            while lo < hi:
                mid = (lo + hi) // 2
                if lst[mid][0] >= seq:
                    hi = mid
                else:
                    lo = mid + 1
            if lo < len(lst):
                f = lst[lo]
                return f[1], f[2]
            assert self.seq[eng] >= seq and self.last[eng] is not None
            if self.cnt[eng] >= SEM_LIMIT:
                self._new_epoch(eng)
            self.cnt[eng] += 1
            self.last[eng].then_inc(self.sem[eng], 1)
            lst.append((self.seq[eng], self.sem[eng], self.cnt[eng]))
            return self.sem[eng], self.cnt[eng]
        rec = ev[1]
        return rec[0], rec[1]

    def _wait(self, eng, ev):
        if ev is None:
            return
        if ev[0] == "c" and ev[1] == eng and eng not in self.same:
            return
        sem, val = self._resolve(ev)
        w = self.waited[eng]
        if w.get(id(sem), 0) >= val:
            return
        w[id(sem)] = val
        self.e[eng].wait_ge(sem, val)
        self.nwait += 1

    def _pre(self, eng, reads, writes):
        for t in reads:
            self._wait(eng, t.w)
        for t in writes:
            self._wait(eng, t.w)
            for ev in t.r:
                self._wait(eng, ev)

    def _signal_last(self, eng):
        if self.cnt[eng] >= SEM_LIMIT:
            self._new_epoch(eng)
        self.cnt[eng] += 1
        self.last[eng].then_inc(self.sem[eng], 1)
        self.sigmap[eng].append((self.seq[eng], self.sem[eng], self.cnt[eng]))

    def _post(self, eng, reads, writes):
        if self.eager and (reads or writes):
            self._signal_last(eng)
        ev = ("c", eng, self.seq[eng])
        for t in reads:
            t.r.append(ev)
            if len(t.r) > 16:
                t.r = self._compact(t.r)
        for t in writes:
            t.w = ev
            t.r = []

    @staticmethod
    def _split(reads, writes):
        xr = [t for t in reads if t.excl]
        if xr:
            writes = list(writes) + [t for t in xr if t not in writes]
            reads = [t for t in reads if not t.excl]
        return reads, writes

    def op(self, eng, fn, reads=(), writes=()):
        reads, writes = self._split(reads, writes)
        self._pre(eng, reads, writes)
        ins = fn(self.e[eng])
        self.seq[eng] += 1
        self.last[eng] = ins
        self.ninstr += 1
        self._post(eng, reads, writes)
        return ins

    def group(self, eng, fns, reads=(), writes=()):
        reads, writes = self._split(reads, writes)
        self._pre(eng, reads, writes)
        ins = None
        for fn in fns:
            ins = fn(self.e[eng])
            self.seq[eng] += 1
            self.ninstr += 1
        self.last[eng] = ins
        self._post(eng, reads, writes)
        return ins

    def _compact(self, evs):
        best = {}
        other = []
        for ev in evs:
            if ev[0] == "c":
                k = ev[1]
                if k not in best or ev[2] >= best[k][2]:
                    best[k] = ev
            else:
                if not any(o[1] is ev[1] for o in other):
                    other.append(ev)
        return list(best.values()) + other

    def dma(self, q, out, in_, reads=(), writes=(), group=False, **kw):
        for t in reads:
            self._wait(q, t.w)
        for t in writes:
            if group:
                for ev in t.pr:
                    self._wait(q, ev)
            else:
                self._wait(q, t.w)
                for ev in t.r:
                    self._wait(q, ev)
                t.pr = ([t.w] if t.w is not None else []) + list(t.r)
        ins = self.e[q].dma_start(out=out, in_=in_, **kw)
        assert len(writes) <= 1
        if writes:
            t = writes[0]
            if t.dsem is None or t.dsem[1] >= SEM_LIMIT:
                t.dsem = [self._alloc("d_" + t.name), 0]
            rec = t.dsem
        else:
            if q not in self.rsem or self.rsem[q][1] >= SEM_LIMIT:
                self.rsem[q] = [self._alloc("r_" + q), 0]
            rec = self.rsem[q]
        rec[1] += 16
        ins.then_inc(rec[0], 16)
        ev = ("d", rec)
        if reads and not any(r is rec for r in self.store_recs):
            self.store_recs.append(rec)
        if writes:
            writes[0].w = ev
            writes[0].r = []
        for s in reads:
            s.r.append(ev)
            if len(s.r) > 16:
                s.r = self._compact(s.r)
        self.ninstr += 1
        return ins

    def finish(self, toks, eng="sp"):
        for t in toks:
            self._wait(eng, t.w)
            for ev in t.r:
                self._wait(eng, ev)


D = 1024
KC = 8
S = 2048
TT = 512
NT = S // TT
INW = 7688
DEPTH = 2
EPS = 1e-6
FF_DENSE = 2816
FF_EXPERT = 3584
NEXP = 8
PI = math.pi
C1 = 6.28125
C2 = 2.0 * math.pi - 6.28125
NEG = -30000.0

COL_U, COL_V, COL_Q, COL_KK, COL_VA = 0, 512, 1024, 1536, 2048
COL_DQ, COL_DK, COL_DV, COL_DZ, COL_AB, COL_G = 2560, 3072, 3584, 4096, 4608, 4616

SP_LAYOUT = [("n1g", 8), ("n2g", 8), ("bmod", 48), ("lng", 512), ("lnb", 512), ("bs", 512),
             ("lam", 256), ("subg", 128), ("convw", 48), ("alog", 4), ("dtb", 4), ("dng", 1)]
SP_OFF = {}
_o = 0
for _n, _w in SP_LAYOUT:
    SP_OFF[_n] = (_o, _w)
    _o += _w
SP_LAYER = _o
SP_FING = 2 * SP_LAYER
SP_WR = SP_FING + 8
SP_TOTAL = SP_WR + 64

CO = {}
_o = 0
for _n, _w in [("ident", 128), ("rot", 128), ("tt", 64), ("su", 64), ("negu", 256), ("negl", 256),
               ("mstrict", 256), ("i4", 256), ("invf", 1)]:
    CO[_n] = (_o, _w)
    _o += _w
CO_TOTAL = _o


def make_consts():
    c = np.zeros((128, CO_TOTAL), np.float32)
    o = CO["ident"][0]
    c[:, o:o + 128] = np.eye(128, dtype=np.float32)
    o = CO["rot"][0]
    for m in range(128):
        if (m % 64) < 32:
            c[m + 32, o + m] = -1.0
        else:
            c[m - 32, o + m] = 1.0
    p = np.arange(64)[:, None]
    j = np.arange(64)[None, :]
    o = CO["tt"][0]
    c[:64, o:o + 64] = (p <= j)
    o = CO["su"][0]
    c[:64, o:o + 64] = (p > j)
    for h in range(4):
        o = CO["negu"][0] + h * 64
        c[:64, o:o + 64] = np.where(j > p, NEG, 0.0)
        o = CO["negl"][0] + h * 64
        c[:64, o:o + 64] = np.where(j < p, NEG, 0.0)
        o = CO["mstrict"][0] + h * 64
        c[:64, o:o + 64] = (j < p)
        o = CO["i4"][0] + h * 64
        c[:64, o:o + 64] = (j == p)
    inv_freq = (10000.0 ** (-np.arange(0, 64, 2, dtype=np.float32) / np.float32(64))).astype(np.float32)
    c[:, CO["invf"][0]] = inv_freq[np.arange(128) % 32]
    return c


def fm(v):
    v = np.asarray(v, np.float32)
    return np.ascontiguousarray(v.reshape(-1, 128).T)


def bc(v):
    v = np.asarray(v, np.float32).reshape(1, -1)
    return np.broadcast_to(v, (128, v.shape[1]))


def make_smallp(inp):
    sp = np.zeros((128, SP_TOTAL), np.float32)
    for l in range(DEPTH):
        base = l * SP_LAYER

        def put(name, arr):
            o, w = SP_OFF[name]
            assert arr.shape == (128, w), (name, arr.shape, w)
            sp[:, base + o:base + o + w] = arr
        put("n1g", fm(inp["norm1_g"][l]))
        put("n2g", fm(inp["norm2_g"][l]))
        put("bmod", fm(inp["b_mod"][l]))
        put("lng", bc(inp["gm_ln_g"][l]))
        put("lnb", bc(inp["gm_ln_b"][l]))
        put("bs", bc(inp["gm_b_s"][l].reshape(-1)))
        put("lam", bc(inp["da_lambda"][l].reshape(-1)))
        put("subg", bc(inp["da_subln_g"][l]))
        cw = np.asarray(inp["dn_conv_w"][l], np.float32)
        put("convw", np.ascontiguousarray(cw.reshape(4, 12, 128).transpose(2, 1, 0)).reshape(128, 48))
        put("alog", bc(inp["dn_a_log"][l]))
        put("dtb", bc(inp["dn_dt_bias"][l]))
        put("dng", np.asarray(inp["dn_norm_g"][l], np.float32).reshape(128, 1))
    sp[:, SP_FING:SP_FING + 8] = fm(inp["final_g"])
    wr = np.asarray(inp["moe_w_router"][0], np.float32)
    sp[:, SP_WR:SP_WR + 64] = wr.reshape(8, 128, 8).transpose(1, 0, 2).reshape(128, 64)
    return sp


def lambda_init_fn(layer):
    return 0.8 - 0.6 * math.exp(-0.3 * layer)


class Pool_:
    def __init__(self, items):
        self.items = items
        self.i = 0

    def next(self):
        it = self.items[self.i % len(self.items)]
        self.i += 1
        return it


def build(cfg=None):
    cfg = dict(cfg or {})
    NSEQ = cfg.get("nseq", 2)
    NTILE = cfg.get("ntile", NT)
    LAYERS = cfg.get("layers", DEPTH)
    STAGE = cfg.get("stage", 99)
    taps = cfg.get("taps", ())
    ACUT = cfg.get("acut", 99)

    nc = bass.Bass("TRN2", target_bir_lowering=False)
    k = K(nc, same=tuple(cfg.get("same", ("dve", "act", "pool"))), eager=cfg.get("eager", True))

    def din(name, shape, dt=F32):
        return nc.dram_tensor(name, list(shape), dt, kind="ExternalInput").ap()

    xT_d = din("xT", [2, D, S])
    posb_d = din("posb", [2, 128, S], I32)
    cT_d = din("cT", [128, KC * 2])
    smallp_d = din("smallp", [128, SP_TOTAL])
    consts_d = din("consts", [128, CO_TOTAL])
    wst_d = din("wst", [128, DEPTH * 4 * 128])
    w_mod_d = din("w_mod", [DEPTH, D, 6 * D])
    w_in_d = din("w_in", [DEPTH, D, INW])
    w_br_d = [din("w_br_gm", [DEPTH, 512, D]), din("w_br_da", [DEPTH, 512, D]), din("w_br_dn", [DEPTH, 512, D])]
    w_out_d = din("w_out", [DEPTH, D, D])
    ffn_g_d = din("ffn_w_gate", [1, D, FF_DENSE])
    ffn_u_d = din("ffn_w_up", [1, D, FF_DENSE])
    ffn_d_d = din("ffn_w_down", [1, FF_DENSE, D])
    moe_g_d = din("moe_w_gate", [1, NEXP, D, FF_EXPERT])
    moe_u_d = din("moe_w_up", [1, NEXP, D, FF_EXPERT])
    moe_d_d = din("moe_w_down", [1, NEXP, FF_EXPERT, D])
    out_d = nc.dram_tensor("out", [2, S, D], F32, kind="ExternalOutput").ap()
    out_tok = Tok("out")
    xs_d = nc.dram_tensor("xs_scratch", [128, KC, S], F32, kind="Internal").ap()
    t_xs = [[Tok("xs%d_%d" % (i, e)) for e in range(KC)] for i in range(NT)]

    tapd = {}

    def sb(name, shape, dt=F32):
        return nc.alloc_sbuf_tensor("s_" + name, list(shape), dt).ap()

    def sbt(name, shape, dt=F32):
        return sb(name, shape, dt), Tok(name)

    def tap(name, ap, tok):
        if name not in taps or name in tapd:
            return
        d = nc.dram_tensor("tap_" + name, list(ap.shape), ap.dtype, kind="ExternalOutput").ap()
        t = Tok("tap_" + name)
        k.dma("sp", d, ap, reads=[tok], writes=[t])
        tapd[name] = t

    def op(eng, fn, reads=(), writes=(), **kw):
        return k.op(eng, lambda e: getattr(e, fn)(**kw), reads=reads, writes=writes)

    def mm(out, lhsT, rhs, start=True, stop=True):
        return lambda e: e.matmul(out, lhsT=lhsT, rhs=rhs, start=start, stop=stop)

    def tr(out, in_, idn):
        return lambda e: e.transpose(out, in_, idn)

    def barrier():
        engs = ("pe", "dve", "act", "pool")
        evs = {n: ("c", n, k.seq[n]) for n in engs if k.last[n] is not None}
        for a in engs + ("sp",):
            for b_, ev in evs.items():
                if a != b_:
                    k._wait(a, ev)
            for rec in k.store_recs:
                k._wait(a, ("d", rec))

    consts, t_consts = sbt("consts", [128, CO_TOTAL])
    spl_t, t_spl = sbt("spl", [128, SP_LAYER])
    spg, t_spg = sbt("spg", [128, 72])
    k.dma("sp", consts, consts_d, writes=[t_consts])
    k.dma("sp", spg, smallp_d[:, SP_FING:SP_FING + 72], writes=[t_spg])

    def cst(name, rows=128):
        o, w = CO[name]
        return consts[0:rows, o:o + w]

    def spl(name):
        o, w = SP_OFF[name]
        return spl_t[:, o:o + w]

    ident = cst("ident")
    ones_f, t_ones = sbt("ones_f", [128, 128])
    ones_b, t_onesb = sbt("ones_b", [128, 128], BF16)
    nones_f, t_nones = sbt("nones_f", [128, 128])
    ident_b, t_identb = sbt("ident_b", [128, 128], BF16)
    rot_b, t_rotb = sbt("rot_b", [128, 128], BF16)
    ntt_f, t_ntt = sbt("ntt_f", [64, 64])
    op("dve", "memset", writes=[t_ones], ap=ones_f, constant=1.0)
    op("dve", "memset", writes=[t_onesb], ap=ones_b, constant=1.0)
    op("dve", "memset", writes=[t_nones], ap=nones_f, constant=-1.0)
    op("dve", "tensor_copy", reads=[t_consts], writes=[t_identb], out=ident_b, in_=ident)
    op("dve", "tensor_copy", reads=[t_consts], writes=[t_rotb], out=rot_b, in_=cst("rot"))
    op("dve", "tensor_scalar", reads=[t_consts], writes=[t_ntt], out=ntt_f, in0=cst("tt", 64), scalar1=-1.0,
       scalar2=None, op0=ALU.mult)
    TC = [t_consts, t_ones, t_onesb, t_nones, t_identb, t_rotb, t_ntt]

    psall = nc.alloc_psum_tensor("psall", [128, 8 * 512], F32).ap()
    banks = [(psall[:, i * 512:(i + 1) * 512], Tok("bank%d" % i, excl=True)) for i in range(8)]
    psA = Pool_(banks[0:4])
    psF = Pool_(banks[0:4])
    psD = Pool_(banks[4:8])
    psS = Pool_(banks[4:6])
    psO = Pool_(banks[0:2])
    psT = Pool_(banks[2:4])
    ps8 = Pool_(banks)
    psDN = Pool_(banks[6:8]) if not cfg.get("dn8") else ps8
    S_big = psall[:, 4 * 512:8 * 512]
    t_Sbig = [b_[1] for b_ in banks[4:8]]

    NSLOT = cfg.get("nslot", 7)
    wbf = Pool_([sbt("wbf%d" % i, [128, 2048], BF16) for i in range(NSLOT)])

    def load_w(srcs, a, b_):
        wb, t_wb = wbf.next()
        wbv = wb[:, 0:a * b_].rearrange("p (a b) -> p a b", b=b_)
        o = 0
        for i, (src, ai) in enumerate(srcs):
            k.dma("pool", wbv[:, o:o + ai, :], src, writes=[t_wb], group=(i > 0))
            o += ai
        assert o == a
        return wbv, t_wb

    def wrows(ap2d):
        return ap2d.rearrange("(kc p) n -> p kc n", p=128)

    cact, t_cact = sbt("cact", [128, KC * 2])
    k.dma("sp", cact, cT_d, writes=[t_cact])
    op("act", "activation", writes=[t_cact], out=cact, in_=cact, func=AF.Silu)
    cact_b, t_cactb = sbt("cact_b", [128, KC * 2], BF16)
    op("dve", "tensor_copy", reads=[t_cact], writes=[t_cactb], out=cact_b, in_=cact)
    cact3 = cact_b.rearrange("p (c b) -> p c b", b=2)
    modT, t_mod = sbt("modT", [128, DEPTH, 48, 2])
    mods, t_mods = sbt("mods", [128, DEPTH, 2, 6, 8])
    bmod_t, t_bmod = sbt("bmod_t", [128, 64])
    for l in range(LAYERS):
        k.dma("sp", bmod_t, smallp_d[:, l * SP_LAYER:l * SP_LAYER + 64], writes=[t_bmod])
        for g in range(24):
            w32, t_w32 = load_w([(wrows(w_mod_d[l])[:, :, g * 256:(g + 1) * 256], KC)], KC, 256)
            pb, t_pb = psA.next()
            fns = []
            for j in range(2):
                for kc in range(KC):
                    fns.append(mm(pb[:, j * 2:j * 2 + 2], w32[:, kc, j * 128:(j + 1) * 128], cact3[:, kc, :],
                                  start=(kc == 0), stop=(kc == KC - 1)))
            k.group("pe", fns, reads=[t_w32, t_cactb], writes=[t_pb])
            op("dve", "tensor_copy", reads=[t_pb], writes=[t_mod], out=modT[:, l, g * 2:(g + 1) * 2, :],
               in_=pb[:, 0:4].rearrange("p (j b) -> p j b", b=2))
        for b in range(2):
            op("dve", "tensor_tensor", reads=[t_bmod], writes=[t_mod], out=modT[:, l, :, b], in0=modT[:, l, :, b],
               in1=bmod_t[:, 16:64], op=ALU.add)
        for b in range(2):
            for part in range(6):
                src = modT[:, l, part * 8:(part + 1) * 8, b]
                dst = mods[:, l, b, part, :]
                if part in (1, 4):
                    go = 0 if part == 1 else 8
                    op("dve", "scalar_tensor_tensor", reads=[t_mod, t_bmod], writes=[t_mods], out=dst, in0=src, scalar=1.0,
                       in1=bmod_t[:, go:go + 8], op0=ALU.add, op1=ALU.mult)
                else:
                    op("dve", "tensor_copy", reads=[t_mod], writes=[t_mods], out=dst, in_=src)
    tap("mods", mods, t_mods)
    SHIFT1, G1S, GATE1, SHIFT2, G2S, GATE2 = 0, 1, 2, 3, 4, 5

    wsT_b, t_wst = sbt("wsT_b", [128, 4, 128], BF16)
    lay, t_lay = sbt("lay", [128, 16])
    subgs, t_subgs = sbt("subgs", [128, 128])
    halo, t_halo = sbt("halo", [128, 12, 3])
    Sst, t_S = sbt("Sst", [128, 4, 128])
    lamtmp = sbt("lamtmp", [128, 2, 64])

    rem = int(nc.sbuf_bytes_remaining)
    AW = (rem - 1024) // 4
    arena = sb("arena", [128, AW])
    ar = {"off": 0}
    tokcache = {}

    def aa(name, shape, dt=F32):
        n = 1
        for s_ in shape[1:]:
            n *= s_
        words = (n * (4 if dt in (F32, I32) else 2) + 3) // 4
        words = (words + 7) // 8 * 8
        o = ar["off"]
        assert o + words <= AW, ("arena overflow", name, o, words, AW)
        ar["off"] = o + words
        v = arena[0:shape[0], o:o + words]
        if dt != F32:
            v = v.bitcast(dt)
        v = v[:, 0:n]
        if len(shape) == 3:
            v = v.rearrange("p (a b) -> p a b", b=shape[2])
        elif len(shape) == 4:
            v = v.rearrange("p (a b c) -> p a b c", b=shape[2], c=shape[3])
        key = (name, o)
        if key not in tokcache:
            tokcache[key] = Tok(name)
        return v, tokcache[key]

    def layer_setup(l):
        k.dma("sp", spl_t, smallp_d[:, l * SP_LAYER:(l + 1) * SP_LAYER], writes=[t_spl])
        k.dma("pool", wsT_b, wst_d[:, l * 512:(l + 1) * 512].rearrange("p (g i) -> p g i", i=128), writes=[t_wst])
        op("dve", "memset", writes=[t_wst], ap=wsT_b[64:128, :, 0:64], constant=0.0)
        lam_v = spl("lam").rearrange("p (a d) -> p a d", d=64)
        pr_, t_pr = lamtmp
        op("dve", "tensor_tensor", reads=[t_spl], writes=[t_pr], out=pr_[:, 0, :], in0=lam_v[:, 0, :], in1=lam_v[:, 1, :], op=ALU.mult)
        op("dve", "tensor_tensor", reads=[t_spl], writes=[t_pr], out=pr_[:, 1, :], in0=lam_v[:, 2, :], in1=lam_v[:, 3, :], op=ALU.mult)
        op("dve", "reduce_sum", reads=[t_pr], writes=[t_lay], out=lay[:, 8:10], in_=pr_, axis=AX.X)
        op("act", "activation", reads=[], writes=[t_lay], out=lay[:, 8:10], in_=lay[:, 8:10], func=AF.Exp)
        op("dve", "scalar_tensor_tensor", reads=[], writes=[t_lay], out=lay[:, 0:1], in0=lay[:, 9:10], scalar=-lambda_init_fn(l),
           in1=lay[:, 8:9], op0=ALU.add, op1=ALU.subtract)
        op("dve", "tensor_scalar", reads=[t_spl], writes=[t_subgs], out=subgs, in0=spl("subg"), scalar1=1.0 - lambda_init_fn(l),
           scalar2=None, op0=ALU.mult)
        op("act", "activation", reads=[t_spl], writes=[t_lay], out=lay[:, 4:8], in_=spl("alog"), func=AF.Exp)
        op("dve", "tensor_scalar", reads=[], writes=[t_lay], out=lay[:, 4:8], in0=lay[:, 4:8], scalar1=-1.0, scalar2=None, op0=ALU.mult)

    def modnorm(xsrc, t_xsrc, c0, l, b, gidx, sidx, hdst, t_hd, sq_b, t_sq, rb, t_rb, tmps, h32=None):
        for c in range(KC):
            op("act", "activation", reads=[t_xsrc], writes=[t_sq], out=sq_b[:, c, :], in_=xsrc[:, c, c0:c0 + TT], func=AF.Square)
        pb, t_pb = psA.next()
        k.group("pe", [mm(pb, ones_b, sq_b[:, c, :], start=(c == 0), stop=(c == KC - 1)) for c in range(KC)],
                reads=[t_sq, t_onesb], writes=[t_pb])
        op("act", "activation", reads=[t_pb], writes=[t_rb], out=rb, in_=pb, func=AF.Sqrt, bias=eps_c, scale=1.0 / D)
        op("dve", "reciprocal", writes=[t_rb], out=rb, in_=rb)
        for c in range(KC):
            tm, t_tm = tmps.next()
            op("dve", "scalar_tensor_tensor", reads=[t_xsrc, t_mods, t_rb], writes=[t_tm], out=tm, in0=xsrc[:, c, c0:c0 + TT],
               scalar=mods[:, l, b, gidx, c:c + 1], in1=rb, op0=ALU.mult, op1=ALU.mult)
            if h32 is not None:
                op("act", "activation", reads=[t_tm, t_mods], writes=[h32[1]], out=h32[0][:, c, :], in_=tm, func=AF.Identity,
                   bias=mods[:, l, b, sidx, c:c + 1], scale=1.0)
                op("dve", "tensor_copy", reads=[h32[1]], writes=[t_hd], out=hdst[:, c, :], in_=h32[0][:, c, :])
            else:
                op("act", "activation", reads=[t_tm, t_mods], writes=[t_hd], out=hdst[:, c, :], in_=tm, func=AF.Identity,
                   bias=mods[:, l, b, sidx, c:c + 1], scale=1.0)

    epsc, t_epsc = sbt("epsc", [128, 4])
    op("dve", "memset", writes=[t_epsc], ap=epsc[:, 0:1], constant=EPS)
    op("dve", "memset", writes=[t_epsc], ap=epsc[:, 1:2], constant=1.0)
    op("dve", "memset", writes=[t_epsc], ap=epsc[:, 2:3], constant=-PI)
    eps_c = epsc[:, 0:1]
    one_c = epsc[:, 1:2]
    npi_c = epsc[:, 2:3]

    def mixer(l, b):
        barrier()
        ar["off"] = 0
        hT, t_h = aa("hT", [128, KC, TT], BF16)
        kT, t_kT = aa("kT", [128, 4, S], BF16)
        Vt, t_V = aa("Vt", [128, 16, 512], BF16)
        y_gm, t_ygm = aa("y_gm", [128, 4, TT], BF16)
        y_da, t_yda = aa("y_da", [128, 4, TT], BF16)
        y_dn, t_ydn = aa("y_dn", [128, 4, TT], BF16)
        tmps = Pool_([aa("tmpA%d" % i, [128, TT]) for i in range(3)])
        tmpb = Pool_([aa("tmpB%d" % i, [128, TT], BF16) for i in range(2)])
        st, t_st = aa("stat", [128, 32])
        P1_END = ar["off"]
        op("dve", "memset", writes=[t_halo], ap=halo, constant=0.0)
        op("dve", "memset", writes=[t_S], ap=Sst, constant=0.0)

        def wg(col0, ncols):
            return load_w([(wrows(w_in_d[l])[:, :, col0:col0 + ncols], KC)], KC, ncols)

        def proj_fm(wb, t_wb, nchunk, consume):
            for j in range(nchunk):
                pb, t_pb = psA.next()
                k.group("pe", [mm(pb, wb[:, kc, j * 128:(j + 1) * 128], hT[:, kc, :], start=(kc == 0), stop=(kc == KC - 1))
                               for kc in range(KC)], reads=[t_wb, t_h], writes=[t_pb])
                consume(j, pb, t_pb)

        def proj_tm(wb, t_wb, ncols, consume):
            for blk in range(4):
                pb, t_pb = psA.next()
                k.group("pe", [mm(pb[:, 0:ncols], hT[:, kc, blk * 128:(blk + 1) * 128], wb[:, kc, 0:ncols], start=(kc == 0),
                                  stop=(kc == KC - 1)) for kc in range(KC)], reads=[t_wb, t_h], writes=[t_pb])
                consume(blk, pb, t_pb)

        for t in range(NTILE):
            c0 = t * TT
            ar["off"] = P1_END
            xt, t_xt = aa("xt", [128, KC, TT])
            if l == 0:
                for c in range(KC):
                    k.dma("sp", xt[:, c, :], xT_d[b, c * 128:(c + 1) * 128, c0:c0 + TT], writes=[t_xt], group=(c > 0))
            else:
                for c in range(KC):
                    k.dma("sp", xt[:, c, :], xs_d[:, c, c0:c0 + TT], reads=[t_xs[t][c]], writes=[t_xt], group=(c > 0))
            rb, t_rb = aa("rb", [128, TT])
            sq_b, t_sq = aa("sq_b", [128, KC, TT], BF16)
            modnorm(xt, t_xt, 0, l, b, G1S, SHIFT1, hT, t_h, sq_b, t_sq, rb, t_rb, tmps)
            if l == 0 and t == 0:
                tap("h0", hT, t_h)
            if STAGE <= 1:
                continue
            barrier()
            ar["off"] = P1_END
            SUB0 = P1_END

            u_g, t_ug = aa("u_g", [128, 4, TT], BF16)
            vg, t_vg = aa("vg", [128, 4, 512])
            vln, t_vln = aa("vln", [128, 4, 512], BF16)
            for g2 in range(2):
                wb, t_wb = wg(COL_U + g2 * 256, 256)
                proj_fm(wb, t_wb, 2, lambda j, pb, t_pb, g2=g2: op(
                    "act", "activation", reads=[t_pb], writes=[t_ug], out=u_g[:, g2 * 2 + j, :], in_=pb, func=AF.Gelu))
            for g2 in range(2):
                wb, t_wb = wg(COL_V + g2 * 256, 256)
                proj_tm(wb, t_wb, 256, lambda blk, pb, t_pb, g2=g2: op(
                    "act", "activation", reads=[t_pb], writes=[t_vg], out=vg[:, blk, g2 * 256:(g2 + 1) * 256], in_=pb[:, 0:256], func=AF.Gelu))
            for blk in range(4):
                op("dve", "bn_stats", reads=[t_vg], writes=[t_st], out=st[:, 0:6], in_=vg[:, blk, :])
                op("dve", "bn_aggr", writes=[t_st], out=st[:, 8:10], in_=st[:, 0:6])
                op("act", "activation", writes=[t_st], out=st[:, 10:11], in_=st[:, 9:10], func=AF.Sqrt, bias=eps_c, scale=1.0)
                op("dve", "reciprocal", writes=[t_st], out=st[:, 10:11], in_=st[:, 10:11])
                tm, t_tm = tmps.next()
                op("dve", "tensor_scalar", reads=[t_vg, t_st], writes=[t_tm], out=tm, in0=vg[:, blk, :], scalar1=st[:, 8:9],
                   scalar2=st[:, 10:11], op0=ALU.subtract, op1=ALU.mult)
                op("dve", "tensor_tensor", reads=[t_spl], writes=[t_tm], out=tm, in0=tm, in1=spl("lng"), op=ALU.mult)
                op("dve", "tensor_tensor", reads=[t_spl, t_tm], writes=[t_vln], out=vln[:, blk, :], in0=tm, in1=spl("lnb"), op=ALU.add)
            for g in range(4):
                pb, t_pb = psA.next()
                k.group("pe", [mm(pb[:, blk * 128:(blk + 1) * 128], vln[:, blk, g * 128:(g + 1) * 128], wsT_b[:, g, :])
                               for blk in range(4)], reads=[t_vln, t_wst], writes=[t_pb])
                tm, t_tm = tmps.next()
                bsb = spl("bs")[:, g * 128:(g + 1) * 128].unsqueeze(1).to_broadcast([128, 4, 128])
                op("dve", "tensor_tensor", reads=[t_pb, t_spl], writes=[t_tm], out=tm.rearrange("p (a b) -> p a b", b=128),
                   in0=pb.rearrange("p (a b) -> p a b", b=128), in1=bsb, op=ALU.add)
                op("dve", "tensor_tensor", reads=[t_tm, t_ug], writes=[t_ygm], out=y_gm[:, g, :], in0=tm, in1=u_g[:, g, :], op=ALU.mult)
            if l == 0 and t == 0:
                tap("ygm", y_gm, t_ygm)
            if STAGE <= 2:
                continue
            barrier()
            ar["off"] = SUB0

            cosT, t_cos = aa("cosT", [128, TT])
            sinT, t_sin = aa("sinT", [128, TT])
            qT, t_qT = aa("qT", [128, 4, TT], BF16)
            PT, t_PT = aa("PT", [128, 16, 128], BF16)
            yv, t_yv = aa("yv", [128, 128])
            junk, t_junk = aa("junk", [128, 128])
            Ssb = [aa("Ssb%d" % i, [128, S]) for i in range(2)]
            Pms = [aa("Pm%d" % i, [128, S], BF16) for i in range(2)]
            asts = [aa("ast%d" % i, [128, 8]) for i in range(2)]
            rss = [aa("rs%d" % i, [128, 8]) for i in range(2)]
            ytms = [aa("ytm%d" % i, [128, 512], BF16) for i in range(2)]
            pOs = {}
            qn, t_qn = aa("qn", [128, 4, TT], BF16)
            kn, t_kn = aa("kn", [128, 4, TT], BF16)
            vn, t_vn = aa("vn", [128, 4, TT], BF16)
            Sb, t_Sb = aa("Sb", [128, 4, 128], BF16)
            zs, t_zs = aa("zs", [128, 4, TT], BF16)
            oT, t_oT = aa("oT", [128, 4, TT])
            ab, t_ab = aa("ab", [64, 8, 8])
            gb, t_gb = aa("gb", [64, 4, 8, 4])
            dst_ = {0: (qn, t_qn), 1: (kn, t_kn), 2: (vn, t_vn)}
            op("act", "copy", reads=[t_S], writes=[t_Sb], out=Sb, in_=Sst)
            cwv = spl("convw").rearrange("p (c j) -> p c j", j=4)
            OV0 = ar["off"]
            posi, t_posi = aa("posi", [128, TT], I32)
            k.dma("sp", posi, posb_d[b, :, c0:c0 + TT], writes=[t_posi])
            ang, t_ang = aa("ang", [128, TT])
            kq, t_kq = aa("kq", [128, TT])
            ki, t_ki = aa("ki", [128, TT], I32)
            op("dve", "tensor_copy", reads=[t_posi], writes=[t_ang], out=ang, in_=posi)
            op("dve", "tensor_scalar", reads=[t_consts], writes=[t_ang], out=ang, in0=ang, scalar1=cst("invf"), scalar2=None, op0=ALU.mult)
            op("dve", "tensor_scalar", reads=[t_ang], writes=[t_kq], out=kq, in0=ang, scalar1=1.0 / (2 * PI), scalar2=None, op0=ALU.mult)
            op("dve", "tensor_copy", reads=[t_kq], writes=[t_ki], out=ki, in_=kq)
            op("dve", "tensor_copy", reads=[t_ki], writes=[t_kq], out=kq, in_=ki)
            op("dve", "scalar_tensor_tensor", reads=[t_kq], writes=[t_ang], out=ang, in0=kq, scalar=-C1, in1=ang, op0=ALU.mult, op1=ALU.add)
            op("dve", "scalar_tensor_tensor", reads=[t_kq], writes=[t_ang], out=ang, in0=kq, scalar=-C2, in1=ang, op0=ALU.mult, op1=ALU.add)

            op("dve", "tensor_scalar", reads=[t_ang], writes=[t_kq], out=kq, in0=ang, scalar1=PI, scalar2=None, op0=ALU.is_gt)
            op("dve", "scalar_tensor_tensor", reads=[t_kq, t_ang], writes=[t_sin], out=sinT, in0=kq, scalar=-2 * PI, in1=ang, op0=ALU.mult, op1=ALU.add)
            op("dve", "tensor_scalar", reads=[t_sin], writes=[t_kq], out=kq, in0=sinT, scalar1=PI / 2, scalar2=None, op0=ALU.is_gt)
            op("dve", "scalar_tensor_tensor", reads=[t_kq, t_sin], writes=[t_cos], out=cosT, in0=kq, scalar=-2 * PI, in1=sinT, op0=ALU.mult, op1=ALU.add)
            op("dve", "tensor_scalar", writes=[t_cos], out=cosT, in0=cosT, scalar1=PI / 2, scalar2=None, op0=ALU.add)
            op("act", "activation", writes=[t_sin], out=sinT, in_=sinT, func=AF.Sin)
            op("act", "activation", writes=[t_cos], out=cosT, in_=cosT, func=AF.Sin)
            if l == 0 and t == 0:
                tap("cos", cosT, t_cos)
                tap("sin", sinT, t_sin)
            cws = Pool_([aa("cw%d" % i, [128, TT + 3]) for i in range(2)])

            def rope_consume(dst_fn, t_dst):
                def f(j, pb, t_pb, hbase):
                    h = hbase + j
                    rw, t_rw = tmpb.next()
                    op("act", "copy", reads=[t_pb], writes=[t_rw], out=rw, in_=pb)
                    pr, t_pr = psA.next()
                    k.group("pe", [mm(pr, rot_b, rw)], reads=[t_rw, t_rotb], writes=[t_pr])
                    t1, t_t1 = tmps.next()
                    t2, t_t2 = tmps.next()
                    op("dve", "tensor_tensor", reads=[t_pb, t_cos], writes=[t_t1], out=t1, in0=pb, in1=cosT, op=ALU.mult)
                    op("dve", "tensor_tensor", reads=[t_pr, t_sin], writes=[t_t2], out=t2, in0=pr, in1=sinT, op=ALU.mult)
                    op("dve", "tensor_tensor", reads=[t_t1, t_t2], writes=[t_dst], out=dst_fn(h), in0=t1, in1=t2, op=ALU.add)
                return f
            qcons = rope_consume(lambda h: qT[:, h, :], t_qT)
            kcons = rope_consume(lambda h: kT[:, h, c0:c0 + TT], t_kT)
            for g2 in range(2 if "q" in cfg.get("parts", "qkv") else 0):
                wb, t_wb = wg(COL_Q + g2 * 256, 256)
                proj_fm(wb, t_wb, 2, lambda j, pb, t_pb, g2=g2: qcons(j, pb, t_pb, g2 * 2))
            for g2 in range(2 if "k" in cfg.get("parts", "qkv") else 0):
                wb, t_wb = wg(COL_KK + g2 * 256, 256)
                proj_fm(wb, t_wb, 2, lambda j, pb, t_pb, g2=g2: kcons(j, pb, t_pb, g2 * 2))
            for g2 in range(2 if "v" in cfg.get("parts", "qkv") else 0):
                wb, t_wb = wg(COL_VA + g2 * 256, 256)
                proj_tm(wb, t_wb, 256, lambda blk, pb, t_pb, g2=g2: op(
                    "act", "copy", reads=[t_pb], writes=[t_V], out=Vt[:, t * 4 + blk, g2 * 256:(g2 + 1) * 256], in_=pb[:, 0:256]))
            if l == 0 and t == 0:
                tap("qT", qT, t_qT)
                tap("kT", kT[:, :, 0:TT], t_kT)

            def dn_consume(j, pb, t_pb, ci0):
                ci = ci0 + j
                which, h = ci // 4, ci % 4
                dst, t_dst = dst_[which]
                cw, t_cw = cws.next()
                op("dve", "tensor_copy", reads=[t_halo], writes=[t_cw], out=cw[:, 0:3], in_=halo[:, ci, :])
                op("act", "copy", reads=[t_pb], writes=[t_cw], out=cw[:, 3:TT + 3], in_=pb)
                op("dve", "tensor_copy", reads=[t_cw], writes=[t_halo], out=halo[:, ci, :], in_=cw[:, TT:TT + 3])
                acc, t_acc = tmps.next()
                op("dve", "tensor_scalar", reads=[t_cw, t_spl], writes=[t_acc], out=acc, in0=cw[:, 3:TT + 3], scalar1=cwv[:, ci, 3:4],
                   scalar2=None, op0=ALU.mult)
                for jj in range(3):
                    op("dve", "scalar_tensor_tensor", reads=[t_cw, t_spl], writes=[t_acc], out=acc, in0=cw[:, jj:jj + TT],
                       scalar=cwv[:, ci, jj:jj + 1], in1=acc, op0=ALU.mult, op1=ALU.add)
                if which == 2:
                    op("act", "activation", reads=[t_acc], writes=[t_dst], out=dst[:, h, :], in_=acc, func=AF.Silu)
                else:
                    op("act", "activation", reads=[], writes=[t_acc], out=acc, in_=acc, func=AF.Silu)
                    sqb, t_sqb = tmpb.next()
                    op("act", "activation", reads=[t_acc], writes=[t_sqb], out=sqb, in_=acc, func=AF.Square)
                    pn, t_pn = ps8.next()
                    k.group("pe", [mm(pn, ones_b, sqb)], reads=[t_sqb, t_onesb], writes=[t_pn])
                    rn, t_rn = tmps.next()
                    op("act", "activation", reads=[t_pn], writes=[t_rn], out=rn, in_=pn, func=AF.Sqrt, bias=eps_c, scale=1.0)
                    op("dve", "reciprocal", writes=[t_rn], out=rn, in_=rn)
                    op("dve", "scalar_tensor_tensor", reads=[t_rn, t_acc], writes=[t_dst], out=dst[:, h, :], in0=acc,
                       scalar=(128.0 ** -0.5 if which == 0 else 1.0), in1=rn, op0=ALU.mult, op1=ALU.mult)
            for gi in range(6):
                wb, t_wb = wg(COL_DQ + gi * 256, 256)
                proj_fm(wb, t_wb, 2, lambda j, pb, t_pb, gi=gi: dn_consume(j, pb, t_pb, gi * 2))
            for g2 in range(2):
                wb, t_wb = wg(COL_DZ + g2 * 256, 256)
                proj_fm(wb, t_wb, 2, lambda j, pb, t_pb, g2=g2: op(
                    "act", "activation", reads=[t_pb], writes=[t_zs], out=zs[:, g2 * 2 + j, :], in_=pb, func=AF.Silu))
            wab, t_wab = wg(COL_AB, 8)
            pb, t_pb = ps8.next()
            for c in range(8):
                k.group("pe", [mm(pb[0:64, c * 8:(c + 1) * 8], hT[:, kc, c * 64:(c + 1) * 64], wab[:, kc, 0:8], start=(kc == 0),
                                  stop=(kc == KC - 1)) for kc in range(KC)], reads=[t_wab, t_h], writes=[t_pb])
            op("dve", "tensor_copy", reads=[t_pb], writes=[t_ab], out=ab, in_=pb[0:64, 0:64].rearrange("p (c e) -> p c e", e=8))
            dtb_b = spl("dtb")[0:64, :].unsqueeze(1).to_broadcast([64, 8, 4])
            nA_b = lay[0:64, 4:8].unsqueeze(1).to_broadcast([64, 8, 4])
            op("dve", "tensor_tensor", reads=[t_ab, t_spl], writes=[t_gb], out=gb[:, 3], in0=ab[:, :, 0:4], in1=dtb_b, op=ALU.add)
            op("act", "activation", writes=[t_gb], out=gb[:, 3], in_=gb[:, 3], func=AF.Exp)
            op("act", "activation", writes=[t_gb], out=gb[:, 3], in_=gb[:, 3], func=AF.Ln, bias=one_c[0:64, :], scale=1.0)
            op("dve", "tensor_tensor", reads=[t_lay], writes=[t_gb], out=gb[:, 0], in0=gb[:, 3], in1=nA_b, op=ALU.mult)
            op("act", "activation", reads=[t_ab], writes=[t_gb], out=gb[:, 1], in_=ab[:, :, 4:8], func=AF.Sigmoid)
            op("dve", "tensor_scalar", writes=[t_gb], out=gb[:, 2], in0=gb[:, 1], scalar1=-1.0, scalar2=None, op0=ALU.mult)
            if l == 0 and t == 0:
                tap("qn", qn, t_qn); tap("kn", kn, t_kn); tap("vn", vn, t_vn); tap("gb", gb, t_gb)

            barrier()
            ar["off"] = OV0
            def a64(name, cols=256, rows=64):
                return aa(name, [rows, cols])

            def mkset(s_):
                def b64(name, cols=256):
                    return aa(name, [64, cols], BF16)
                return {"ktok": b64("ktok" + s_, 512), "vtok": b64("vtok" + s_, 512), "R1": a64("R1" + s_), "R2": a64("R2" + s_),
                        "Dm": a64("Dm" + s_), "DTm": a64("DTm" + s_), "egs": a64("egs" + s_, 16), "glb": aa("glb" + s_, [128, 4]),
                        "egrow": aa("egrow" + s_, [128, 256]), "Pb": [b64("Pa" + s_), b64("Pb" + s_)], "Bb": [b64("Ba" + s_), b64("Bb" + s_)],
                        "Nb": [b64("Na" + s_), b64("Nb" + s_)], "rhs_w": b64("rhs_w" + s_, 512), "u_sb": a64("u_sb" + s_, 512),
                        "wT": aa("wT" + s_, [128, 256], BF16), "qgT": aa("qgT" + s_, [128, 256], BF16), "vnew": b64("vnew" + s_, 512),
                        "qkT": b64("qkT" + s_)}
            NW = cfg.get("nw", 2)
            BSs = [mkset("_%d" % i) for i in range(NW)]
            tt_c = cst("tt", 64); su_c = cst("su", 64); negu = cst("negu", 64); negl = cst("negl", 64)
            mstr = cst("mstrict", 64); i4 = cst("i4", 64); id64 = consts[0:64, CO["ident"][0]:CO["ident"][0] + 64]
            units = [(qb, h, p) for qb in range(4 if ACUT >= 2 else 0) for h in range(4) for p in range(2)]

            def stageA(ui):
                qb, h, p = units[ui]
                j = ui % 2
                gq = t * 4 + qb
                kend = (gq + 1) * 128
                Sb_, t_Sb_ = Ssb[j]
                Pj, t_Pj = Pms[j]
                aj, t_aj = asts[j]
                rs_, t_rs = rss[h % 2]
                ps_ = slice(p * 64, (p + 1) * 64)
                for pc in range((kend + 511) // 512):
                    n = min(512, kend - pc * 512)
                    sbk, t_sbk = psS.next()
                    k.group("pe", [mm(sbk[:, 0:n], qT[ps_, h, qb * 128:(qb + 1) * 128], kT[ps_, h, pc * 512:pc * 512 + n])],
                            reads=[t_qT, t_kT], writes=[t_sbk])
                    op("act", "copy", reads=[t_sbk], writes=[t_Sb_], out=Sb_[:, pc * 512:pc * 512 + n], in_=sbk[:, 0:n])
                op("dve", "tensor_scalar", writes=[t_Sb_], out=Sb_[0:64, kend - 64:kend], in0=Sb_[0:64, kend - 64:kend],
                   scalar1=NEG, scalar2=None, op0=ALU.add)
                op("dve", "reduce_max", reads=[t_Sb_], writes=[t_aj], out=aj[:, 0:1], in_=Sb_[:, 0:kend], axis=AX.X)
                op("dve", "tensor_scalar", writes=[t_aj], out=aj[:, 1:2], in0=aj[:, 0:1], scalar1=-0.125, scalar2=None, op0=ALU.mult)
                op("act", "activation", reads=[t_Sb_, t_aj], writes=[t_Pj, t_rs], out=Pj[:, 0:kend], in_=Sb_[:, 0:kend],
                   func=AF.Exp, bias=aj[:, 1:2], scale=0.125, accum_out=rs_[:, p:p + 1])

            def stageB(ui):
                qb, h, p = units[ui]
                j = ui % 2
                gq = t * 4 + qb
                nkb = gq + 1
                Pj, t_Pj = Pms[j]
                rs_, t_rs = rss[h % 2]
                ytm, t_ytm = ytms[qb % 2]
                if p == 0:
                    pOs[(qb, h)] = psO.next()
                pO, t_pO = pOs[(qb, h)]
                for kb0 in range(0, nkb, 8):
                    nb = min(8, nkb - kb0)
                    ptb, t_ptb = psT.next()
                    ptv = ptb.bitcast(BF16)
                    k.group("pe", [tr(ptv[:, i * 128:(i + 1) * 128], Pj[:, (kb0 + i) * 128:(kb0 + i + 1) * 128], ident_b)
                                   for i in range(nb)], reads=[t_Pj, t_identb], writes=[t_ptb])
                    op("dve", "tensor_copy", reads=[t_ptb], writes=[t_PT], out=PT[:, kb0:kb0 + nb, :],
                       in_=ptv[:, 0:nb * 128].rearrange("p (a b) -> p a b", b=128))
                k.group("pe", [mm(pO[:, p * 128:(p + 1) * 128], PT[:, kb, :], Vt[:, kb, h * 128:(h + 1) * 128],
                                  start=(kb == 0), stop=(kb == nkb - 1)) for kb in range(nkb)], reads=[t_PT, t_V], writes=[t_pO])
                if p == 0:
                    return
                op("dve", "reciprocal", writes=[t_rs], out=rs_[:, 2:4], in_=rs_[:, 0:2])
                op("dve", "tensor_tensor", reads=[t_lay], writes=[t_rs], out=rs_[:, 3:4], in0=rs_[:, 3:4], in1=lay[:, 0:1], op=ALU.mult)
                op("dve", "tensor_scalar", reads=[t_pO, t_rs], writes=[t_yv], out=yv, in0=pO[:, 0:128], scalar1=rs_[:, 2:3], scalar2=None, op0=ALU.mult)
                op("dve", "scalar_tensor_tensor", reads=[t_pO, t_rs], writes=[t_yv], out=yv, in0=pO[:, 128:256], scalar=rs_[:, 3:4], in1=yv,
                   op0=ALU.mult, op1=ALU.add)
                op("dve", "tensor_tensor", reads=[t_yv], writes=[t_junk], out=junk, in0=yv, in1=yv, op=ALU.mult)
                op("dve", "reduce_sum", reads=[t_junk], writes=[t_rs], out=rs_[:, 4:5], in_=junk, axis=AX.X)
                op("act", "activation", writes=[t_rs], out=rs_[:, 5:6], in_=rs_[:, 4:5], func=AF.Ln, bias=eps_c, scale=1.0 / 128)
                op("act", "activation", writes=[t_rs], out=rs_[:, 5:6], in_=rs_[:, 5:6], func=AF.Exp, scale=-0.5)
                op("dve", "scalar_tensor_tensor", reads=[t_yv, t_rs, t_subgs], writes=[t_ytm], out=ytm[:, h * 128:(h + 1) * 128], in0=yv,
                   scalar=rs_[:, 5:6], in1=subgs, op0=ALU.mult, op1=ALU.mult)
                if h == 3:
                    ptb, t_ptb = psT.next()
                    ptv = ptb.bitcast(BF16)
                    k.group("pe", [tr(ptv[:, hh * 128:(hh + 1) * 128], ytm[:, hh * 128:(hh + 1) * 128], ident_b) for hh in range(4)],
                            reads=[t_ytm, t_identb], writes=[t_ptb])
                    op("dve", "tensor_copy", reads=[t_ptb], writes=[t_yda], out=y_da[:, :, qb * 128:(qb + 1) * 128],
                       in_=ptv[:, 0:512].rearrange("p (a b) -> p a b", b=128))


            def v3(ap, inner):
                return ap.rearrange("p (h j) -> p h j", j=inner)


            def chunk_prep(c, BS):
                ktok, t_ktok = BS["ktok"]; vtok, t_vtok = BS["vtok"]
                R1, t_R1 = BS["R1"]; R2, t_R2 = BS["R2"]
                Dm, t_D = BS["Dm"]; DTm, t_DT = BS["DTm"]; Ds, t_Ds = Dm, t_D
                egs, t_egs = BS["egs"]; glb, t_glb = BS["glb"]; egrow, t_egrow = BS["egrow"]
                Pb = BS["Pb"]; Bb = BS["Bb"]; Nb = BS["Nb"]
                rhs_u, t_ru = vtok, t_vtok; rhs_w, t_rw_ = BS["rhs_w"]; kd, t_kd = ktok, t_ktok
                u_sb, t_u = BS["u_sb"]; wT, t_wT = BS["wT"]; qkT, t_qk = BS["qkT"]
                qgT, t_qg = BS["qgT"]; vnew, t_vnew = BS["vnew"]
                cs = slice(c * 64, (c + 1) * 64)
                gr = gb[:, 0, c, :]
                be = gb[:, 1, c, :]
                nbe = gb[:, 2, c, :]
                pk, t_pk = psDN.next()
                pkb = pk.bitcast(BF16)
                k.group("pe", [tr(pkb[0:64, h * 128:(h + 1) * 128], kn[:, h, cs], ident_b) for h in range(4)], reads=[t_kn, t_identb], writes=[t_pk])
                op("act", "copy", reads=[t_pk], writes=[t_ktok], out=ktok, in_=pkb[0:64, 0:512])
                yield
                pv, t_pv = psDN.next()
                pvb = pv.bitcast(BF16)
                k.group("pe", [tr(pvb[0:64, h * 128:(h + 1) * 128], vn[:, h, cs], ident_b) for h in range(4)], reads=[t_vn, t_identb], writes=[t_pv])
                op("dve", "tensor_copy", reads=[t_pv], writes=[t_vtok], out=vtok, in_=pvb[0:64, 0:512])
                yield
                op("dve", "tensor_tensor", reads=[t_gb, t_consts], writes=[t_R1], out=v3(R1, 64), in0=gr.unsqueeze(2).to_broadcast([64, 4, 64]),
                   in1=tt_c.unsqueeze(1).to_broadcast([64, 4, 64]), op=ALU.mult)
                yield
                op("dve", "tensor_copy", reads=[t_gb], writes=[t_R2], out=v3(R2, 64), in_=gr.unsqueeze(2).to_broadcast([64, 4, 64]))
                yield
                pg, t_pg = psDN.next()
                k.group("pe", [mm(pg[0:64, 0:256], tt_c, R2, start=True, stop=False),
                               mm(pg[0:64, 0:256], nones_f[0:64, 0:64], R1, start=False, stop=False),
                               mm(pg[0:64, 0:256], id64, negu, start=False, stop=True)],
                        reads=[t_R1, t_R2, t_consts, t_nones], writes=[t_pg])
                op("act", "activation", reads=[t_pg], writes=[t_D], out=Dm, in_=pg[0:64, 0:256], func=AF.Exp)
                yield
                pgt, t_pgt = psDN.next()
                k.group("pe", [mm(pgt[0:64, 0:256], ones_f[0:64, 0:64], R1, start=True, stop=False),
                               mm(pgt[0:64, 0:256], ntt_f, R2, start=False, stop=False),
                               mm(pgt[0:64, 0:256], id64, negl, start=False, stop=True)],
                        reads=[t_R1, t_R2, t_consts, t_ones, t_ntt], writes=[t_pgt])
                op("act", "activation", reads=[t_pgt], writes=[t_DT], out=DTm, in_=pgt[0:64, 0:256], func=AF.Exp)
                yield
                op("dve", "tensor_tensor", reads=[t_consts], writes=[t_Ds], out=Ds, in0=Dm, in1=mstr, op=ALU.mult)
                yield
                px, t_px = psDN.next()
                k.group("pe", [mm(px[:, 0:256], ones_f[0:64, :], R1), mm(px[:, 256:260], ones_f[0:64, :], gr),
                               mm(px[0:64, 264:268], tt_c, gr), mm(px[0:64, 268:272], su_c, gr)],
                        reads=[t_R1, t_gb, t_ones, t_consts], writes=[t_px])
                op("act", "activation", reads=[t_px], writes=[t_egrow], out=egrow, in_=px[:, 0:256], func=AF.Exp)
                op("act", "activation", reads=[t_px], writes=[t_glb], out=glb, in_=px[:, 256:260], func=AF.Exp)
                op("act", "activation", reads=[t_px], writes=[t_egs], out=egs[:, 0:8], in_=px[0:64, 264:272], func=AF.Exp)
                yield
                op("dve", "tensor_tensor", reads=[t_gb], writes=[t_egs], out=egs[:, 8:12], in0=egs[:, 0:4], in1=be, op=ALU.mult)
                yield
                pkk, t_pkk = psDN.next()
                k.group("pe", [mm(pkk[0:64, h * 64:(h + 1) * 64], kn[:, h, cs], kn[:, h, cs]) for h in range(4)], reads=[t_kn], writes=[t_pkk])
                P_, t_P_ = Pb[0]
                for h in range(4):
                    op("dve", "scalar_tensor_tensor", reads=[t_pkk, t_gb, t_Ds], writes=[t_P_], out=P_[:, h * 64:(h + 1) * 64],
                       in0=pkk[0:64, h * 64:(h + 1) * 64], scalar=nbe[:, h:h + 1], in1=Ds[:, h * 64:(h + 1) * 64], op0=ALU.mult, op1=ALU.mult)
                pt_, t_pt_ = psDN.next()
                pt_ = pt_.bitcast(BF16)
                k.group("pe", [tr(pt_[0:64, h * 64:(h + 1) * 64], P_[:, h * 64:(h + 1) * 64], ident_b[0:64, 0:64]) for h in range(4)],
                        reads=[t_P_, t_identb], writes=[t_pt_])
                B_, t_B_ = Bb[0]
                N_, t_N_ = Nb[0]
                op("act", "copy", reads=[t_pt_], writes=[t_B_], out=B_, in_=pt_[0:64, 0:256])
                op("dve", "tensor_tensor", reads=[t_pt_, t_consts], writes=[t_N_], out=N_, in0=pt_[0:64, 0:256], in1=i4, op=ALU.add)
                yield
                for j in range(1, 6):
                    Pn, t_Pn = Pb[j % 2]
                    Bn, t_Bn = Bb[j % 2]
                    Nn, t_Nn = Nb[j % 2]
                    pp, t_pp = psDN.next()
                    k.group("pe", [mm(pp[0:64, h * 64:(h + 1) * 64], B_[:, h * 64:(h + 1) * 64], P_[:, h * 64:(h + 1) * 64]) for h in range(4)],
                            reads=[t_B_, t_P_], writes=[t_pp])
                    op("act", "copy", reads=[t_pp], writes=[t_Pn], out=Pn, in_=pp[0:64, 0:256])
                    if j < 5:
                        pbb, t_pbb = psDN.next()
                        k.group("pe", [mm(pbb[0:64, h * 64:(h + 1) * 64], P_[:, h * 64:(h + 1) * 64], B_[:, h * 64:(h + 1) * 64]) for h in range(4)],
                                reads=[t_B_, t_P_], writes=[t_pbb])
                        op("dve", "tensor_copy", reads=[t_pbb], writes=[t_Bn], out=Bn, in_=pbb[0:64, 0:256])
                    pn2, t_pn2 = psDN.next()
                    k.group("pe", [mm(pn2[0:64, h * 64:(h + 1) * 64], Pn[:, h * 64:(h + 1) * 64], N_[:, h * 64:(h + 1) * 64]) for h in range(4)],
                            reads=[t_Pn, t_N_], writes=[t_pn2])
                    op("dve", "tensor_tensor", reads=[t_pn2, t_N_], writes=[t_Nn], out=Nn, in0=pn2[0:64, 0:256], in1=N_, op=ALU.add)
                    P_, t_P_, B_, t_B_, N_, t_N_ = Pn, t_Pn, Bn, t_Bn, Nn, t_Nn
                op("dve", "tensor_tensor", reads=[t_vtok, t_gb], writes=[t_ru], out=v3(rhs_u, 128), in0=v3(vtok, 128),
                   in1=be.unsqueeze(2).to_broadcast([64, 4, 128]), op=ALU.mult)
                yield
                op("dve", "tensor_tensor", reads=[t_ktok, t_egs], writes=[t_rw_], out=v3(rhs_w, 128), in0=v3(ktok, 128),
                   in1=egs[:, 8:12].unsqueeze(2).to_broadcast([64, 4, 128]), op=ALU.mult)
                yield
                op("dve", "tensor_tensor", reads=[t_ktok, t_egs], writes=[t_kd], out=v3(kd, 128), in0=v3(ktok, 128),
                   in1=egs[:, 4:8].unsqueeze(2).to_broadcast([64, 4, 128]), op=ALU.mult)
                yield
                pu, t_pu = psDN.next()
                k.group("pe", [mm(pu[0:64, h * 128:(h + 1) * 128], N_[:, h * 64:(h + 1) * 64], rhs_u[:, h * 128:(h + 1) * 128]) for h in range(4)],
                        reads=[t_N_, t_ru], writes=[t_pu])
                op("act", "copy", reads=[t_pu], writes=[t_u], out=u_sb, in_=pu[0:64, :])
                yield
                pw, t_pw = psDN.next()
                k.group("pe", [mm(pw[:, h * 64:(h + 1) * 64], rhs_w[:, h * 128:(h + 1) * 128], N_[:, h * 64:(h + 1) * 64]) for h in range(4)],
                        reads=[t_N_, t_rw_], writes=[t_pw])
                op("dve", "tensor_copy", reads=[t_pw], writes=[t_wT], out=wT, in_=pw[:, 0:256])
                yield
                pq, t_pq = psDN.next()
                k.group("pe", [mm(pq[0:64, h * 64:(h + 1) * 64], kn[:, h, cs], qn[:, h, cs]) for h in range(4)], reads=[t_kn, t_qn], writes=[t_pq])
                op("dve", "tensor_tensor", reads=[t_pq, t_DT], writes=[t_qk], out=qkT, in0=pq[0:64, 0:256], in1=DTm, op=ALU.mult)
                yield
                op("dve", "tensor_tensor", reads=[t_qn, t_egrow], writes=[t_qg], out=v3(qgT, 64), in0=qn[:, :, cs], in1=v3(egrow, 64), op=ALU.mult)
                yield

            def chunk_recur(c, BS):
                ktok, t_ktok = BS["ktok"]; vtok, t_vtok = BS["vtok"]
                R1, t_R1 = BS["R1"]; R2, t_R2 = BS["R2"]
                Dm, t_D = BS["Dm"]; DTm, t_DT = BS["DTm"]; Ds, t_Ds = Dm, t_D
                egs, t_egs = BS["egs"]; glb, t_glb = BS["glb"]; egrow, t_egrow = BS["egrow"]
                Pb = BS["Pb"]; Bb = BS["Bb"]; Nb = BS["Nb"]
                rhs_u, t_ru = vtok, t_vtok; rhs_w, t_rw_ = BS["rhs_w"]; kd, t_kd = ktok, t_ktok
                u_sb, t_u = BS["u_sb"]; wT, t_wT = BS["wT"]; qkT, t_qk = BS["qkT"]
                qgT, t_qg = BS["qgT"]; vnew, t_vnew = BS["vnew"]
                cs = slice(c * 64, (c + 1) * 64)
                gr = gb[:, 0, c, :]
                be = gb[:, 1, c, :]
                nbe = gb[:, 2, c, :]
                pws, t_pws = psDN.next()
                k.group("pe", [mm(pws[0:64, h * 128:(h + 1) * 128], wT[:, h * 64:(h + 1) * 64], Sb[:, h, :]) for h in range(4)],
                        reads=[t_wT, t_Sb], writes=[t_pws])
                op("dve", "tensor_tensor", reads=[t_u, t_pws], writes=[t_vnew], out=vnew, in0=u_sb, in1=pws[0:64, :], op=ALU.subtract)
                po, t_po = psDN.next()
                fns = []
                for h in range(4):
                    fns.append(mm(po[:, h * 64:(h + 1) * 64], Sb[:, h, :], qgT[:, h * 64:(h + 1) * 64], start=True, stop=False))
                    fns.append(mm(po[:, h * 64:(h + 1) * 64], vnew[:, h * 128:(h + 1) * 128], qkT[:, h * 64:(h + 1) * 64], start=False, stop=True))
                k.group("pe", fns, reads=[t_Sb, t_qg, t_vnew, t_qk], writes=[t_po])
                op("act", "copy", reads=[t_po], writes=[t_oT], out=oT[:, :, cs], in_=v3(po[:, 0:256], 64))
                pds, t_pds = psDN.next()
                k.group("pe", [mm(pds[:, h * 128:(h + 1) * 128], kd[:, h * 128:(h + 1) * 128], vnew[:, h * 128:(h + 1) * 128]) for h in range(4)],
                        reads=[t_kd, t_vnew], writes=[t_pds])
                for h in range(4):
                    op("dve", "scalar_tensor_tensor", reads=[t_pds, t_glb], writes=[t_S], out=Sst[:, h, :], in0=Sst[:, h, :], scalar=glb[:, h:h + 1],
                       in1=pds[:, h * 128:(h + 1) * 128], op0=ALU.mult, op1=ALU.add)
                op("act", "copy", reads=[t_S], writes=[t_Sb], out=Sb, in_=Sst)


            def att_gen():
                for ui in range(len(units) + 1):
                    if ui < len(units):
                        stageA(ui)
                        yield
                    if ui >= 1:
                        stageB(ui - 1)
                        yield

            def dn_gen():
                for c0_ in range(0, 8, NW):
                    gens = [chunk_prep(c0_ + i, BSs[i]) for i in range(NW)]
                    alive = list(gens)
                    while alive:
                        for g_ in list(alive):
                            try:
                                next(g_)
                            except StopIteration:
                                alive.remove(g_)
                            yield
                    for i in range(NW):
                        chunk_recur(c0_ + i, BSs[i])
                        yield

            RATIO = cfg.get("dn_ratio", 6)
            streams = [[att_gen(), 1], [dn_gen(), RATIO]]
            while streams:
                for st_ in list(streams):
                    for _ in range(st_[1]):
                        try:
                            next(st_[0])
                        except StopIteration:
                            streams.remove(st_)
                            break

            if l == 0 and t == 0 and ACUT >= 8:
                tap("yda", y_da, t_yda)
            if l == 0 and t == 0:
                tap("oT", oT, t_oT)
            for h in range(4):
                sqb, t_sqb = tmpb.next()
                op("act", "activation", reads=[t_oT], writes=[t_sqb], out=sqb, in_=oT[:, h, :], func=AF.Square)
                pn, t_pn = ps8.next()
                k.group("pe", [mm(pn, ones_b, sqb)], reads=[t_sqb, t_onesb], writes=[t_pn])
                rn, t_rn = tmps.next()
                op("act", "activation", reads=[t_pn], writes=[t_rn], out=rn, in_=pn, func=AF.Sqrt, bias=eps_c, scale=1.0 / 128)
                op("dve", "reciprocal", writes=[t_rn], out=rn, in_=rn)
                op("dve", "scalar_tensor_tensor", reads=[t_oT, t_spl], writes=[t_rn], out=rn, in0=oT[:, h, :], scalar=spl("dng"), in1=rn,
                   op0=ALU.mult, op1=ALU.mult)
                op("dve", "tensor_tensor", reads=[t_rn, t_zs], writes=[t_ydn], out=y_dn[:, h, :], in0=rn, in1=zs[:, h, :], op=ALU.mult)
            if l == 0 and t == 0:
                tap("ydn", y_dn, t_ydn)
            if STAGE <= 4:
                continue
            barrier()
            ar["off"] = SUB0
            merged, t_mg = aa("merged", [128, KC, TT], BF16)
            sgs = Pool_([aa("sg%d" % i, [128, TT]) for i in range(3)])
            macc, t_macc = aa("macc", [128, TT])
            xins = Pool_([aa("xin%d" % i, [128, TT]) for i in range(3)])
            ybr = [(y_gm, t_ygm), (y_da, t_yda), (y_dn, t_ydn)]
            for d in range(KC):
                for br in range(3):
                    wgt, t_wgt = load_w([(wrows(w_in_d[l])[:, :, COL_G + br * 1024 + d * 128:COL_G + br * 1024 + (d + 1) * 128], KC)], KC, 128)
                    pg, t_pg = ps8.next()
                    k.group("pe", [mm(pg, wgt[:, kc, :], hT[:, kc, :], start=(kc == 0), stop=(kc == KC - 1)) for kc in range(KC)],
                            reads=[t_wgt, t_h], writes=[t_pg])
                    sg, t_sg = sgs.next()
                    op("act", "activation", reads=[t_pg], writes=[t_sg], out=sg, in_=pg, func=AF.Sigmoid)
                    pp, t_pp = ps8.next()
                    yb, t_yb = ybr[br]
                    wbr, t_wbr = load_w([(wrows(w_br_d[br][l])[:, :, d * 128:(d + 1) * 128], 4)], 4, 128)
                    k.group("pe", [mm(pp, wbr[:, kc, :], yb[:, kc, :], start=(kc == 0), stop=(kc == 3)) for kc in range(4)],
                            reads=[t_wbr, t_yb], writes=[t_pp])
                    if br == 0:
                        op("dve", "tensor_tensor", reads=[t_pp, t_sg], writes=[t_macc], out=macc, in0=pp, in1=sg, op=ALU.mult)
                    else:
                        op("dve", "tensor_tensor", reads=[t_pp], writes=[t_sg], out=sg, in0=pp, in1=sg, op=ALU.mult)
                        if br == 1:
                            op("dve", "tensor_tensor", reads=[t_sg], writes=[t_macc], out=macc, in0=macc, in1=sg, op=ALU.add)
                        else:
                            op("dve", "tensor_tensor", reads=[t_sg, t_macc], writes=[t_mg], out=merged[:, d, :], in0=macc, in1=sg, op=ALU.add)
            if l == 0 and t == 0:
                tap("merged", merged, t_mg)
            for g4 in range(4):
                wo, t_wo = load_w([(wrows(w_out_d[l])[:, :, g4 * 256:(g4 + 1) * 256], KC)], KC, 256)
                for j in range(2):
                    e_ = g4 * 2 + j
                    po, t_po = ps8.next()
                    k.group("pe", [mm(po, wo[:, dc, j * 128:(j + 1) * 128], merged[:, dc, :], start=(dc == 0), stop=(dc == KC - 1))
                                   for dc in range(KC)], reads=[t_wo, t_mg], writes=[t_po])
                    xin, t_xin = xins.next()
                    if l == 0:
                        k.dma("sp", xin, xT_d[b, e_ * 128:(e_ + 1) * 128, c0:c0 + TT], writes=[t_xin])
                    else:
                        k.dma("sp", xin, xs_d[:, e_, c0:c0 + TT], reads=[t_xs[t][e_]], writes=[t_xin])
                    op("dve", "scalar_tensor_tensor", reads=[t_po, t_mods], writes=[t_xin], out=xin, in0=po,
                       scalar=mods[:, l, b, GATE1, e_:e_ + 1], in1=xin, op0=ALU.mult, op1=ALU.add)
                    k.dma("sp", xs_d[:, e_, c0:c0 + TT], xin, reads=[t_xin], writes=[t_xs[t][e_]])
            barrier()


    def ffn(l, b, last):
        barrier()
        ar["off"] = 0
        xT, t_x = aa("xT", [128, KC, S])
        rb, t_rb = aa("rb", [128, TT])
        sq_b, t_sq = aa("sq_b", [128, KC, TT], BF16)
        tmps = Pool_([aa("tmpA%d" % i, [128, TT]) for i in range(3)])
        FMARK = ar["off"]
        h2, t_h2 = aa("h2", [128, KC, S], BF16)
        moe = (l % 2 == 1)
        for t in range(NTILE):
            k.dma("sp", xT[:, :, t * TT:(t + 1) * TT], xs_d[:, :, t * TT:(t + 1) * TT], reads=t_xs[t], writes=[t_x], group=(t > 0))
        if moe:
            h32, t_h32 = aa("h32", [128, KC, TT])
            WT, t_WT = aa("WT", [8, S])
            rhe, t_rhe = aa("rhe", [8, TT])
            Wb0, t_Wb0 = aa("Wbc0", [128, S], BF16)
            Wb1, t_Wb1 = aa("Wbc1", [128, S], BF16)
            Wbcs = [Wb0, Wb1]; t_Wbcs = [t_Wb0, t_Wb1]
            lg, t_lg = aa("lg", [128, 64])
            wr3 = spg[:, 8:72].rearrange("p (c e) -> p c e", e=8)
        for t in range(NTILE):
            modnorm(xT, t_x, t * TT, l, b, G2S, SHIFT2, h2[:, :, t * TT:(t + 1) * TT], t_h2, sq_b, t_sq, rb, t_rb, tmps,
                    h32=((h32, t_h32) if moe else None))
            if moe:
                for blk in range(4):
                    gblk = t * 4 + blk
                    pl, t_pl = ps8.next()
                    k.group("pe", [mm(pl[:, 0:8], h32[:, kc, blk * 128:(blk + 1) * 128], wr3[:, kc, :], start=(kc == 0), stop=(kc == KC - 1))
                                   for kc in range(KC)], reads=[t_h32, t_spg], writes=[t_pl])
                    L = lg[:, 0:8]; E1 = lg[:, 8:16]; M2 = lg[:, 16:24]; E2 = lg[:, 24:32]; Wt = lg[:, 32:40]
                    m1 = lg[:, 40:41]; m2 = lg[:, 41:42]; dd = lg[:, 42:43]; w1 = lg[:, 43:44]; w2 = lg[:, 44:45]
                    op("dve", "tensor_copy", reads=[t_pl], writes=[t_lg], out=L, in_=pl[:, 0:8])
                    op("dve", "reduce_max", writes=[t_lg], out=m1, in_=L, axis=AX.X)
                    op("dve", "tensor_scalar", writes=[t_lg], out=E1, in0=L, scalar1=m1, scalar2=None, op0=ALU.is_equal)
                    op("dve", "scalar_tensor_tensor", writes=[t_lg], out=M2, in0=E1, scalar=-1e30, in1=L, op0=ALU.mult, op1=ALU.add)
                    op("dve", "reduce_max", writes=[t_lg], out=m2, in_=M2, axis=AX.X)
                    op("dve", "tensor_scalar", writes=[t_lg], out=E2, in0=M2, scalar1=m2, scalar2=None, op0=ALU.is_equal)
                    op("dve", "tensor_tensor", writes=[t_lg], out=dd, in0=m2, in1=m1, op=ALU.subtract)
                    op("act", "activation", writes=[t_lg], out=dd, in_=dd, func=AF.Exp)
                    op("dve", "tensor_scalar", writes=[t_lg], out=w1, in0=dd, scalar1=1.0, scalar2=None, op0=ALU.add)
                    op("dve", "reciprocal", writes=[t_lg], out=w1, in_=w1)
                    op("dve", "tensor_tensor", writes=[t_lg], out=w2, in0=dd, in1=w1, op=ALU.mult)
                    op("dve", "tensor_scalar", writes=[t_lg], out=Wt, in0=E1, scalar1=w1, scalar2=None, op0=ALU.mult)
                    op("dve", "scalar_tensor_tensor", writes=[t_lg], out=Wt, in0=E2, scalar=w2, in1=Wt, op0=ALU.mult, op1=ALU.add)
                    pt_, t_pt_ = ps8.next()
                    k.group("pe", [tr(pt_[0:8, 0:128], Wt, ident)], reads=[t_lg, t_consts], writes=[t_pt_])
                    op("act", "copy", reads=[t_pt_], writes=[t_WT], out=WT[:, gblk * 128:(gblk + 1) * 128], in_=pt_[0:8, 0:128])
        if moe and b == 0:
            tap("WT", WT, t_WT)
        if b == 0:
            tap("h2_%d" % l, h2, t_h2)

        hids = Pool_([aa("hid%d" % i, [128, 2, TT], BF16) for i in range(2)])
        pend = {"f": None}

        def expert_pass(wg_d, wu_d, wd_d, F, scale_by=None):
            for fg in range(F // 256):
                wgb, t_wgb = load_w([(wrows(wg_d)[:, :, fg * 256:(fg + 1) * 256], KC)], KC, 256)
                wub, t_wub = load_w([(wrows(wu_d)[:, :, fg * 256:(fg + 1) * 256], KC)], KC, 256)
                wdb, t_wdb = load_w([(wrows(wd_d[fg * 256:(fg + 1) * 256, :]), 2)], 2, D)
                for t in range(NTILE):
                    tc_ = slice(t * TT, (t + 1) * TT)
                    hid, t_hid = hids.next()
                    for j in range(2):
                        pa, t_pa = psF.next()
                        k.group("pe", [mm(pa, wgb[:, kc, j * 128:(j + 1) * 128], h2[:, kc, tc_], start=(kc == 0), stop=(kc == KC - 1))
                                       for kc in range(KC)], reads=[t_wgb, t_h2], writes=[t_pa])
                        pu_, t_pu_ = psF.next()
                        k.group("pe", [mm(pu_, wub[:, kc, j * 128:(j + 1) * 128], h2[:, kc, tc_], start=(kc == 0), stop=(kc == KC - 1))
                                       for kc in range(KC)], reads=[t_wub, t_h2], writes=[t_pu_])
                        sa, t_sa = tmps.next()
                        op("act", "activation", reads=[t_pa], writes=[t_sa], out=sa, in_=pa, func=AF.Silu)
                        if scale_by is not None:
                            op("dve", "tensor_tensor", reads=[scale_by[1]], writes=[t_sa], out=sa, in0=sa, in1=scale_by[0][:, tc_], op=ALU.mult)
                        op("dve", "tensor_tensor", reads=[t_pu_, t_sa], writes=[t_hid], out=hid[:, j, :], in0=pu_, in1=sa, op=ALU.mult)
                    if pend["f"] is not None:
                        pend["f"]()

                    def down(wdb=wdb, t_wdb=t_wdb, hid=hid, t_hid=t_hid, tc_=tc_):
                        for d in range(KC):
                            pd, t_pd = psD.next()
                            k.group("pe", [mm(pd, wdb[:, j, d * 128:(d + 1) * 128], hid[:, j, :], start=(j == 0), stop=(j == 1)) for j in range(2)],
                                    reads=[t_wdb, t_hid], writes=[t_pd])
                            op("dve", "scalar_tensor_tensor", reads=[t_pd, t_mods], writes=[t_x], out=xT[:, d, tc_], in0=pd,
                               scalar=mods[:, l, b, GATE2, d:d + 1], in1=xT[:, d, tc_], op0=ALU.mult, op1=ALU.add)
                    pend["f"] = down

        def flush():
            if pend["f"] is not None:
                pend["f"]()
                pend["f"] = None

        if not moe:
            expert_pass(ffn_g_d[0], ffn_u_d[0], ffn_d_d[0], FF_DENSE)
            flush()
        else:
            for e_ in range(cfg.get("nexp", NEXP)):
                for t in range(NTILE):
                    op("dve", "tensor_scalar", reads=[t_WT, t_consts], writes=[t_rhe], out=rhe, in0=WT[:, t * TT:(t + 1) * TT],
                       scalar1=consts[0:8, CO["ident"][0] + e_:CO["ident"][0] + e_ + 1], scalar2=None, op0=ALU.mult)
                    pw, t_pw = ps8.next()
                    k.group("pe", [mm(pw, ones_f[0:8, :], rhe)], reads=[t_rhe, t_ones], writes=[t_pw])
                    op("act", "copy", reads=[t_pw], writes=[t_Wbcs[e_ % 2]], out=Wbcs[e_ % 2][:, t * TT:(t + 1) * TT], in_=pw)
                expert_pass(moe_g_d[0, e_], moe_u_d[0, e_], moe_d_d[0, e_], FF_EXPERT, scale_by=(Wbcs[e_ % 2], t_Wbcs[e_ % 2]))
            flush()
        if b == 0:
            tap("xffn_%d" % l, xT, t_x)
        if not last:
            for t in range(NTILE):
                for c in range(KC):
                    k.dma("sp", xs_d[:, c, t * TT:(t + 1) * TT], xT[:, c, t * TT:(t + 1) * TT], reads=[t_x], writes=[t_xs[t][c]])
            return
        barrier()
        ar["off"] = FMARK
        xn, t_xn = aa("xn", [128, KC, TT])
        otm = Pool_([aa("otm%d" % i, [128, D]) for i in range(2)])
        for t in range(NTILE):
            for c in range(KC):
                op("act", "activation", reads=[t_x], writes=[t_sq], out=sq_b[:, c, :], in_=xT[:, c, t * TT:(t + 1) * TT], func=AF.Square)
            pb, t_pb = ps8.next()
            k.group("pe", [mm(pb, ones_b, sq_b[:, c, :], start=(c == 0), stop=(c == KC - 1)) for c in range(KC)],
                    reads=[t_sq, t_onesb], writes=[t_pb])
            op("act", "activation", reads=[t_pb], writes=[t_rb], out=rb, in_=pb, func=AF.Sqrt, bias=eps_c, scale=1.0 / D)
            op("dve", "reciprocal", writes=[t_rb], out=rb, in_=rb)
            for c in range(KC):
                op("dve", "scalar_tensor_tensor", reads=[t_x, t_spg, t_rb], writes=[t_xn], out=xn[:, c, :], in0=xT[:, c, t * TT:(t + 1) * TT],
                   scalar=spg[:, c:c + 1], in1=rb, op0=ALU.mult, op1=ALU.mult)
            for blk in range(4):
                ot, t_ot = otm.next()
                for half in range(2):
                    pt_, t_pt_ = ps8.next()
                    k.group("pe", [tr(pt_[:, i * 128:(i + 1) * 128], xn[:, half * 4 + i, blk * 128:(blk + 1) * 128], ident) for i in range(4)],
                            reads=[t_xn, t_consts], writes=[t_pt_])
                    if half == 0:
                        op("act", "copy", reads=[t_pt_], writes=[t_ot], out=ot[:, 0:512], in_=pt_)
                    else:
                        op("dve", "tensor_copy", reads=[t_pt_], writes=[t_ot], out=ot[:, 512:1024], in_=pt_)
                r0 = t * TT + blk * 128
                k.dma("sp", out_d[b, r0:r0 + 128, :], ot, reads=[t_ot], writes=[out_tok])

    for b in range(NSEQ):
        for l in range(LAYERS):
            layer_setup(l)
            mixer(l, b)
            if STAGE >= 6:
                ffn(l, b, last=(l == LAYERS - 1))

    fin = [out_tok] + list(tapd.values())
    if STAGE < 99:
        z, t_z = sbt("zz", [128, 8])
        op("dve", "memset", writes=[t_z], ap=z, constant=0.0)
        k.dma("sp", out_d[0, 0:128, 0:8], z, reads=[t_z], writes=[out_tok])
    k.finish(fin)
    nc._k = k
    return nc


def prep_core_inputs(inp, core):
    x = np.asarray(inp["x"], np.float32)
    b0 = 2 * core
    xT = np.ascontiguousarray(x[b0:b0 + 2].transpose(0, 2, 1))
    pos = np.asarray(inp["positions"], np.int32)[b0:b0 + 2]
    posb = np.ascontiguousarray(np.broadcast_to(pos[:, None, :], (2, 128, S)))
    c = np.asarray(inp["c"], np.float32)[b0:b0 + 2]
    cT = np.ascontiguousarray(c.reshape(2, KC, 128).transpose(2, 1, 0)).reshape(128, KC * 2)
    return {"xT": xT, "posb": posb, "cT": cT}


def shared_inputs(inp):
    ws = np.asarray(inp["gm_w_s"], np.float32)
    wst = np.ascontiguousarray(ws.transpose(3, 0, 1, 2)).reshape(128, DEPTH * 4 * 128)
    sh = {"smallp": make_smallp(inp), "consts": make_consts(), "wst": wst}
    for n in ["w_mod", "w_in", "w_br_gm", "w_br_da", "w_br_dn", "w_out", "ffn_w_gate", "ffn_w_up",
              "ffn_w_down", "moe_w_gate", "moe_w_up", "moe_w_down"]:
        sh[n] = np.ascontiguousarray(np.asarray(inp[n], np.float32))
    return sh


def kernel(**inputs):
    nc = build()
    sh = shared_inputs(inputs)
    in_maps = []
    for core in range(8):
        m = dict(sh)
        m.update(prep_core_inputs(inputs, core))
        in_maps.append(m)
    res = run_bass_kernel_spmd(nc, in_maps, core_ids=list(range(8)))
    return np.concatenate([np.asarray(r["out"], np.float32) for r in res.results], axis=0)
```

```python
import math
import numpy as np
import concourse.bass as bass
import concourse.mybir as mybir
from concourse.bass_utils import run_bass_kernel_spmd

F32 = mybir.dt.float32
BF16 = mybir.dt.bfloat16
I32 = mybir.dt.int32
AF = mybir.ActivationFunctionType
ALU = mybir.AluOpType
AX = mybir.AxisListType

SEM_LIMIT = 20000
ENGS = ("pe", "dve", "act", "pool", "sp")


class Tok:
    __slots__ = ("name", "w", "r", "dsem", "pr", "excl")

    def __init__(self, name, excl=False):
        self.name = name
        self.excl = excl
        self.w = None
        self.r = []
        self.dsem = None
        self.pr = []


class K:
    def __init__(self, nc, same=("dve", "act", "pool"), eager=True):
        self.nc = nc
        self.eager = eager
        self.e = {"pe": nc.tensor, "dve": nc.vector, "act": nc.scalar,
                  "pool": nc.gpsimd, "sp": nc.sync}
        self.nsem = 0
        self.sem = {}
        self.cnt = {}
        for n in ENGS:
            self._new_epoch(n)
        self.seq = {n: 0 for n in ENGS}
        self.last = {n: None for n in ENGS}
        self.sigmap = {n: [] for n in ENGS}
        self.waited = {n: {} for n in ENGS}
        self.same = set(same)
        self.rsem = {}
        self.store_recs = []
        self.ninstr = 0
        self.nwait = 0

    def _alloc(self, name):
        self.nsem += 1
        return self.nc.alloc_semaphore("%s_%d" % (name, self.nsem))

    def _new_epoch(self, n):
        self.sem[n] = self._alloc("e_" + n)
        self.cnt[n] = 0

    def _resolve(self, ev):
        if ev[0] == "c":
            _, eng, seq = ev
            lst = self.sigmap[eng]
            lo, hi = 0, len(lst)
            while lo < hi:
                mid = (lo + hi) // 2
                if lst[mid][0] >= seq:
                    hi = mid
                else:
                    lo = mid + 1
            if lo < len(lst):
                f = lst[lo]
                return f[1], f[2]
            assert self.seq[eng] >= seq and self.last[eng] is not None
            if self.cnt[eng] >= SEM_LIMIT:
                self._new_epoch(eng)
            self.cnt[eng] += 1
            self.last[eng].then_inc(self.sem[eng], 1)
            lst.append((self.seq[eng], self.sem[eng], self.cnt[eng]))
            return self.sem[eng], self.cnt[eng]
        rec = ev[1]
        return rec[0], rec[1]

    def _wait(self, eng, ev):
        if ev is None:
            return
        if ev[0] == "c" and ev[1] == eng and eng not in self.same:
            return
        sem, val = self._resolve(ev)
        w = self.waited[eng]
        if w.get(id(sem), 0) >= val:
            return
        w[id(sem)] = val
        self.e[eng].wait_ge(sem, val)
        self.nwait += 1

    def _pre(self, eng, reads, writes):
        for t in reads:
            self._wait(eng, t.w)
        for t in writes:
            self._wait(eng, t.w)
            for ev in t.r:
                self._wait(eng, ev)

    def _signal_last(self, eng):
        if self.cnt[eng] >= SEM_LIMIT:
            self._new_epoch(eng)
        self.cnt[eng] += 1
        self.last[eng].then_inc(self.sem[eng], 1)
        self.sigmap[eng].append((self.seq[eng], self.sem[eng], self.cnt[eng]))

    def _post(self, eng, reads, writes):
        if self.eager and (reads or writes):
            self._signal_last(eng)
        ev = ("c", eng, self.seq[eng])
        for t in reads:
            t.r.append(ev)
            if len(t.r) > 16:
                t.r = self._compact(t.r)
        for t in writes:
            t.w = ev
            t.r = []

    @staticmethod
    def _split(reads, writes):
        xr = [t for t in reads if t.excl]
        if xr:
            writes = list(writes) + [t for t in xr if t not in writes]
            reads = [t for t in reads if not t.excl]
        return reads, writes

    def op(self, eng, fn, reads=(), writes=()):
        reads, writes = self._split(reads, writes)
        self._pre(eng, reads, writes)
        ins = fn(self.e[eng])
        self.seq[eng] += 1
        self.last[eng] = ins
        self.ninstr += 1
        self._post(eng, reads, writes)
        return ins

    def group(self, eng, fns, reads=(), writes=()):
        reads, writes = self._split(reads, writes)
        self._pre(eng, reads, writes)
        ins = None
        for fn in fns:
            ins = fn(self.e[eng])
            self.seq[eng] += 1
            self.ninstr += 1
        self.last[eng] = ins
        self._post(eng, reads, writes)
        return ins

    def _compact(self, evs):
        best = {}
        other = []
        for ev in evs:
            if ev[0] == "c":
                k = ev[1]
                if k not in best or ev[2] >= best[k][2]:
                    best[k] = ev
            else:
                if not any(o[1] is ev[1] for o in other):
                    other.append(ev)
        return list(best.values()) + other

    def dma(self, q, out, in_, reads=(), writes=(), group=False, **kw):
        for t in reads:
            self._wait(q, t.w)
        for t in writes:
            if group:
                for ev in t.pr:
                    self._wait(q, ev)
            else:
                self._wait(q, t.w)
                for ev in t.r:
                    self._wait(q, ev)
                t.pr = ([t.w] if t.w is not None else []) + list(t.r)
        ins = self.e[q].dma_start(out=out, in_=in_, **kw)
        assert len(writes) <= 1
        if writes:
            t = writes[0]
            if t.dsem is None or t.dsem[1] >= SEM_LIMIT:
                t.dsem = [self._alloc("d_" + t.name), 0]
            rec = t.dsem
        else:
            if q not in self.rsem or self.rsem[q][1] >= SEM_LIMIT:
                self.rsem[q] = [self._alloc("r_" + q), 0]
            rec = self.rsem[q]
        rec[1] += 16
        ins.then_inc(rec[0], 16)
        ev = ("d", rec)
        if reads and not any(r is rec for r in self.store_recs):
            self.store_recs.append(rec)
        if writes:
            writes[0].w = ev
            writes[0].r = []
        for s in reads:
            s.r.append(ev)
            if len(s.r) > 16:
                s.r = self._compact(s.r)
        self.ninstr += 1
        return ins

    def finish(self, toks, eng="sp"):
        for t in toks:
            self._wait(eng, t.w)
            for ev in t.r:
                self._wait(eng, ev)


D = 1024
KC = 8
S = 2048
TT = 512
NT = S // TT
INW = 7688
DEPTH = 2
EPS = 1e-6
FF_DENSE = 2816
FF_EXPERT = 3584
NEXP = 8
PI = math.pi
C1 = 6.28125
C2 = 2.0 * math.pi - 6.28125
NEG = -30000.0

COL_U, COL_V, COL_Q, COL_KK, COL_VA = 0, 512, 1024, 1536, 2048
COL_DQ, COL_DK, COL_DV, COL_DZ, COL_AB, COL_G = 2560, 3072, 3584, 4096, 4608, 4616

SP_LAYOUT = [("n1g", 8), ("n2g", 8), ("bmod", 48), ("lng", 512), ("lnb", 512), ("bs", 512),
             ("lam", 256), ("subg", 128), ("convw", 48), ("alog", 4), ("dtb", 4), ("dng", 1)]
SP_OFF = {}
_o = 0
for _n, _w in SP_LAYOUT:
    SP_OFF[_n] = (_o, _w)
    _o += _w
SP_LAYER = _o
SP_FING = 2 * SP_LAYER
SP_WR = SP_FING + 8
SP_TOTAL = SP_WR + 64

CO = {}
_o = 0
for _n, _w in [("ident", 128), ("rot", 128), ("tt", 64), ("su", 64), ("negu", 256), ("negl", 256),
               ("mstrict", 256), ("i4", 256), ("invf", 1)]:
    CO[_n] = (_o, _w)
    _o += _w
CO_TOTAL = _o


def make_consts():
    c = np.zeros((128, CO_TOTAL), np.float32)
    o = CO["ident"][0]
    c[:, o:o + 128] = np.eye(128, dtype=np.float32)
    o = CO["rot"][0]
    for m in range(128):
        if (m % 64) < 32:
            c[m + 32, o + m] = -1.0
        else:
            c[m - 32, o + m] = 1.0
    p = np.arange(64)[:, None]
    j = np.arange(64)[None, :]
    o = CO["tt"][0]
    c[:64, o:o + 64] = (p <= j)
    o = CO["su"][0]
    c[:64, o:o + 64] = (p > j)
    for h in range(4):
        o = CO["negu"][0] + h * 64
        c[:64, o:o + 64] = np.where(j > p, NEG, 0.0)
        o = CO["negl"][0] + h * 64
        c[:64, o:o + 64] = np.where(j < p, NEG, 0.0)
        o = CO["mstrict"][0] + h * 64
        c[:64, o:o + 64] = (j < p)
        o = CO["i4"][0] + h * 64
        c[:64, o:o + 64] = (j == p)
    inv_freq = (10000.0 ** (-np.arange(0, 64, 2, dtype=np.float32) / np.float32(64))).astype(np.float32)
    c[:, CO["invf"][0]] = inv_freq[np.arange(128) % 32]
    return c


def fm(v):
    v = np.asarray(v, np.float32)
    return np.ascontiguousarray(v.reshape(-1, 128).T)


def bc(v):
    v = np.asarray(v, np.float32).reshape(1, -1)
    return np.broadcast_to(v, (128, v.shape[1]))


def make_smallp(inp):
    sp = np.zeros((128, SP_TOTAL), np.float32)
    for l in range(DEPTH):
        base = l * SP_LAYER

        def put(name, arr):
            o, w = SP_OFF[name]
            assert arr.shape == (128, w), (name, arr.shape, w)
            sp[:, base + o:base + o + w] = arr
        put("n1g", fm(inp["norm1_g"][l]))
        put("n2g", fm(inp["norm2_g"][l]))
        put("bmod", fm(inp["b_mod"][l]))
        put("lng", bc(inp["gm_ln_g"][l]))
        put("lnb", bc(inp["gm_ln_b"][l]))
        put("bs", bc(inp["gm_b_s"][l].reshape(-1)))
        put("lam", bc(inp["da_lambda"][l].reshape(-1)))
        put("subg", bc(inp["da_subln_g"][l]))
        cw = np.asarray(inp["dn_conv_w"][l], np.float32)
        put("convw", np.ascontiguousarray(cw.reshape(4, 12, 128).transpose(2, 1, 0)).reshape(128, 48))
        put("alog", bc(inp["dn_a_log"][l]))
        put("dtb", bc(inp["dn_dt_bias"][l]))
        put("dng", np.asarray(inp["dn_norm_g"][l], np.float32).reshape(128, 1))
    sp[:, SP_FING:SP_FING + 8] = fm(inp["final_g"])
    wr = np.asarray(inp["moe_w_router"][0], np.float32)
    sp[:, SP_WR:SP_WR + 64] = wr.reshape(8, 128, 8).transpose(1, 0, 2).reshape(128, 64)
    return sp


def lambda_init_fn(layer):
    return 0.8 - 0.6 * math.exp(-0.3 * layer)


class Pool_:
    def __init__(self, items):
        self.items = items
        self.i = 0

    def next(self):
        it = self.items[self.i % len(self.items)]
        self.i += 1
        return it


def build(cfg=None):
    cfg = dict(cfg or {})
    NSEQ = cfg.get("nseq", 2)
    NTILE = cfg.get("ntile", NT)
    LAYERS = cfg.get("layers", DEPTH)
    STAGE = cfg.get("stage", 99)
    taps = cfg.get("taps", ())
    ACUT = cfg.get("acut", 99)

    nc = bass.Bass("TRN2", target_bir_lowering=False)
    k = K(nc, same=tuple(cfg.get("same", ("dve", "act", "pool"))), eager=cfg.get("eager", True))

    def din(name, shape, dt=F32):
        return nc.dram_tensor(name, list(shape), dt, kind="ExternalInput").ap()

    xT_d = din("xT", [2, D, S])
    posb_d = din("posb", [2, 128, S], I32)
    cT_d = din("cT", [128, KC * 2])
    smallp_d = din("smallp", [128, SP_TOTAL])
    consts_d = din("consts", [128, CO_TOTAL])
    wst_d = din("wst", [128, DEPTH * 4 * 128])
    w_mod_d = din("w_mod", [DEPTH, D, 6 * D])
    w_in_d = din("w_in", [DEPTH, D, INW])
    w_br_d = [din("w_br_gm", [DEPTH, 512, D]), din("w_br_da", [DEPTH, 512, D]), din("w_br_dn", [DEPTH, 512, D])]
    w_out_d = din("w_out", [DEPTH, D, D])
    ffn_g_d = din("ffn_w_gate", [1, D, FF_DENSE])
    ffn_u_d = din("ffn_w_up", [1, D, FF_DENSE])
    ffn_d_d = din("ffn_w_down", [1, FF_DENSE, D])
    moe_g_d = din("moe_w_gate", [1, NEXP, D, FF_EXPERT])
    moe_u_d = din("moe_w_up", [1, NEXP, D, FF_EXPERT])
    moe_d_d = din("moe_w_down", [1, NEXP, FF_EXPERT, D])
    out_d = nc.dram_tensor("out", [2, S, D], F32, kind="ExternalOutput").ap()
    out_tok = Tok("out")
    xs_d = nc.dram_tensor("xs_scratch", [128, KC, S], F32, kind="Internal").ap()
    t_xs = [[Tok("xs%d_%d" % (i, e)) for e in range(KC)] for i in range(NT)]

    tapd = {}

    def sb(name, shape, dt=F32):
        return nc.alloc_sbuf_tensor("s_" + name, list(shape), dt).ap()

    def sbt(name, shape, dt=F32):
        return sb(name, shape, dt), Tok(name)

    def tap(name, ap, tok):
        if name not in taps or name in tapd:
            return
        d = nc.dram_tensor("tap_" + name, list(ap.shape), ap.dtype, kind="ExternalOutput").ap()
        t = Tok("tap_" + name)
        k.dma("sp", d, ap, reads=[tok], writes=[t])
        tapd[name] = t

    def op(eng, fn, reads=(), writes=(), **kw):
        return k.op(eng, lambda e: getattr(e, fn)(**kw), reads=reads, writes=writes)

    def mm(out, lhsT, rhs, start=True, stop=True):
        return lambda e: e.matmul(out, lhsT=lhsT, rhs=rhs, start=start, stop=stop)

    def tr(out, in_, idn):
        return lambda e: e.transpose(out, in_, idn)

    def barrier():
        engs = ("pe", "dve", "act", "pool")
        evs = {n: ("c", n, k.seq[n]) for n in engs if k.last[n] is not None}
        for a in engs + ("sp",):
            for b_, ev in evs.items():
                if a != b_:
                    k._wait(a, ev)
            for rec in k.store_recs:
                k._wait(a, ("d", rec))

    consts, t_consts = sbt("consts", [128, CO_TOTAL])
    spl_t, t_spl = sbt("spl", [128, SP_LAYER])
    spg, t_spg = sbt("spg", [128, 72])
    k.dma("sp", consts, consts_d, writes=[t_consts])
    k.dma("sp", spg, smallp_d[:, SP_FING:SP_FING + 72], writes=[t_spg])

    def cst(name, rows=128):
        o, w = CO[name]
        return consts[0:rows, o:o + w]

    def spl(name):
        o, w = SP_OFF[name]
        return spl_t[:, o:o + w]

    ident = cst("ident")
    ones_f, t_ones = sbt("ones_f", [128, 128])
    ones_b, t_onesb = sbt("ones_b", [128, 128], BF16)
    nones_f, t_nones = sbt("nones_f", [128, 128])
    ident_b, t_identb = sbt("ident_b", [128, 128], BF16)
    rot_b, t_rotb = sbt("rot_b", [128, 128], BF16)
    ntt_f, t_ntt = sbt("ntt_f", [64, 64])
    op("dve", "memset", writes=[t_ones], ap=ones_f, constant=1.0)
    op("dve", "memset", writes=[t_onesb], ap=ones_b, constant=1.0)
    op("dve", "memset", writes=[t_nones], ap=nones_f, constant=-1.0)
    op("dve", "tensor_copy", reads=[t_consts], writes=[t_identb], out=ident_b, in_=ident)
    op("dve", "tensor_copy", reads=[t_consts], writes=[t_rotb], out=rot_b, in_=cst("rot"))
    op("dve", "tensor_scalar", reads=[t_consts], writes=[t_ntt], out=ntt_f, in0=cst("tt", 64), scalar1=-1.0,
       scalar2=None, op0=ALU.mult)
    TC = [t_consts, t_ones, t_onesb, t_nones, t_identb, t_rotb, t_ntt]

    psall = nc.alloc_psum_tensor("psall", [128, 8 * 512], F32).ap()
    banks = [(psall[:, i * 512:(i + 1) * 512], Tok("bank%d" % i, excl=True)) for i in range(8)]
    psA = Pool_(banks[0:4])
    psF = Pool_(banks[0:4])
    psD = Pool_(banks[4:8])
    psS = Pool_(banks[4:6])
    psO = Pool_(banks[0:2])
    psT = Pool_(banks[2:4])
    ps8 = Pool_(banks)
    psDN = Pool_(banks[6:8]) if not cfg.get("dn8") else ps8
    S_big = psall[:, 4 * 512:8 * 512]
    t_Sbig = [b_[1] for b_ in banks[4:8]]

    NSLOT = cfg.get("nslot", 7)
    wbf = Pool_([sbt("wbf%d" % i, [128, 2048], BF16) for i in range(NSLOT)])

    def load_w(srcs, a, b_):
        wb, t_wb = wbf.next()
        wbv = wb[:, 0:a * b_].rearrange("p (a b) -> p a b", b=b_)
        o = 0
        for i, (src, ai) in enumerate(srcs):
            k.dma("pool", wbv[:, o:o + ai, :], src, writes=[t_wb], group=(i > 0))
            o += ai
        assert o == a
        return wbv, t_wb

    def wrows(ap2d):
        return ap2d.rearrange("(kc p) n -> p kc n", p=128)

    cact, t_cact = sbt("cact", [128, KC * 2])
    k.dma("sp", cact, cT_d, writes=[t_cact])
    op("act", "activation", writes=[t_cact], out=cact, in_=cact, func=AF.Silu)
    cact_b, t_cactb = sbt("cact_b", [128, KC * 2], BF16)
    op("dve", "tensor_copy", reads=[t_cact], writes=[t_cactb], out=cact_b, in_=cact)
    cact3 = cact_b.rearrange("p (c b) -> p c b", b=2)
    modT, t_mod = sbt("modT", [128, DEPTH, 48, 2])
    mods, t_mods = sbt("mods", [128, DEPTH, 2, 6, 8])
    bmod_t, t_bmod = sbt("bmod_t", [128, 64])
    for l in range(LAYERS):
        k.dma("sp", bmod_t, smallp_d[:, l * SP_LAYER:l * SP_LAYER + 64], writes=[t_bmod])
        for g in range(24):
            w32, t_w32 = load_w([(wrows(w_mod_d[l])[:, :, g * 256:(g + 1) * 256], KC)], KC, 256)
            pb, t_pb = psA.next()
            fns = []
            for j in range(2):
                for kc in range(KC):
                    fns.append(mm(pb[:, j * 2:j * 2 + 2], w32[:, kc, j * 128:(j + 1) * 128], cact3[:, kc, :],
                                  start=(kc == 0), stop=(kc == KC - 1)))
            k.group("pe", fns, reads=[t_w32, t_cactb], writes=[t_pb])
            op("dve", "tensor_copy", reads=[t_pb], writes=[t_mod], out=modT[:, l, g * 2:(g + 1) * 2, :],
               in_=pb[:, 0:4].rearrange("p (j b) -> p j b", b=2))
        for b in range(2):
            op("dve", "tensor_tensor", reads=[t_bmod], writes=[t_mod], out=modT[:, l, :, b], in0=modT[:, l, :, b],
               in1=bmod_t[:, 16:64], op=ALU.add)
        for b in range(2):
            for part in range(6):
                src = modT[:, l, part * 8:(part + 1) * 8, b]
                dst = mods[:, l, b, part, :]
                if part in (1, 4):
                    go = 0 if part == 1 else 8
                    op("dve", "scalar_tensor_tensor", reads=[t_mod, t_bmod], writes=[t_mods], out=dst, in0=src, scalar=1.0,
                       in1=bmod_t[:, go:go + 8], op0=ALU.add, op1=ALU.mult)
                else:
                    op("dve", "tensor_copy", reads=[t_mod], writes=[t_mods], out=dst, in_=src)
    tap("mods", mods, t_mods)
    SHIFT1, G1S, GATE1, SHIFT2, G2S, GATE2 = 0, 1, 2, 3, 4, 5

    wsT_b, t_wst = sbt("wsT_b", [128, 4, 128], BF16)
    lay, t_lay = sbt("lay", [128, 16])
    subgs, t_subgs = sbt("subgs", [128, 128])
    halo, t_halo = sbt("halo", [128, 12, 3])
    Sst, t_S = sbt("Sst", [128, 4, 128])
    lamtmp = sbt("lamtmp", [128, 2, 64])

    rem = int(nc.sbuf_bytes_remaining)
    AW = (rem - 1024) // 4
    arena = sb("arena", [128, AW])
    ar = {"off": 0}
    tokcache = {}

    def aa(name, shape, dt=F32):
        n = 1
        for s_ in shape[1:]:
            n *= s_
        words = (n * (4 if dt in (F32, I32) else 2) + 3) // 4
        words = (words + 7) // 8 * 8
        o = ar["off"]
        assert o + words <= AW, ("arena overflow", name, o, words, AW)
        ar["off"] = o + words
        v = arena[0:shape[0], o:o + words]
        if dt != F32:
            v = v.bitcast(dt)
        v = v[:, 0:n]
        if len(shape) == 3:
            v = v.rearrange("p (a b) -> p a b", b=shape[2])
        elif len(shape) == 4:
            v = v.rearrange("p (a b c) -> p a b c", b=shape[2], c=shape[3])
        key = (name, o)
        if key not in tokcache:
            tokcache[key] = Tok(name)
        return v, tokcache[key]

    def layer_setup(l):
        k.dma("sp", spl_t, smallp_d[:, l * SP_LAYER:(l + 1) * SP_LAYER], writes=[t_spl])
        k.dma("pool", wsT_b, wst_d[:, l * 512:(l + 1) * 512].rearrange("p (g i) -> p g i", i=128), writes=[t_wst])
        op("dve", "memset", writes=[t_wst], ap=wsT_b[64:128, :, 0:64], constant=0.0)
        lam_v = spl("lam").rearrange("p (a d) -> p a d", d=64)
        pr_, t_pr = lamtmp
        op("dve", "tensor_tensor", reads=[t_spl], writes=[t_pr], out=pr_[:, 0, :], in0=lam_v[:, 0, :], in1=lam_v[:, 1, :], op=ALU.mult)
        op("dve", "tensor_tensor", reads=[t_spl], writes=[t_pr], out=pr_[:, 1, :], in0=lam_v[:, 2, :], in1=lam_v[:, 3, :], op=ALU.mult)
        op("dve", "reduce_sum", reads=[t_pr], writes=[t_lay], out=lay[:, 8:10], in_=pr_, axis=AX.X)
        op("act", "activation", reads=[], writes=[t_lay], out=lay[:, 8:10], in_=lay[:, 8:10], func=AF.Exp)
        op("dve", "scalar_tensor_tensor", reads=[], writes=[t_lay], out=lay[:, 0:1], in0=lay[:, 9:10], scalar=-lambda_init_fn(l),
           in1=lay[:, 8:9], op0=ALU.add, op1=ALU.subtract)
        op("dve", "tensor_scalar", reads=[t_spl], writes=[t_subgs], out=subgs, in0=spl("subg"), scalar1=1.0 - lambda_init_fn(l),
           scalar2=None, op0=ALU.mult)
        op("act", "activation", reads=[t_spl], writes=[t_lay], out=lay[:, 4:8], in_=spl("alog"), func=AF.Exp)
        op("dve", "tensor_scalar", reads=[], writes=[t_lay], out=lay[:, 4:8], in0=lay[:, 4:8], scalar1=-1.0, scalar2=None, op0=ALU.mult)

    def modnorm(xsrc, t_xsrc, c0, l, b, gidx, sidx, hdst, t_hd, sq_b, t_sq, rb, t_rb, tmps, h32=None):
        for c in range(KC):
            op("act", "activation", reads=[t_xsrc], writes=[t_sq], out=sq_b[:, c, :], in_=xsrc[:, c, c0:c0 + TT], func=AF.Square)
        pb, t_pb = psA.next()
        k.group("pe", [mm(pb, ones_b, sq_b[:, c, :], start=(c == 0), stop=(c == KC - 1)) for c in range(KC)],
                reads=[t_sq, t_onesb], writes=[t_pb])
        op("act", "activation", reads=[t_pb], writes=[t_rb], out=rb, in_=pb, func=AF.Sqrt, bias=eps_c, scale=1.0 / D)
        op("dve", "reciprocal", writes=[t_rb], out=rb, in_=rb)
        for c in range(KC):
            tm, t_tm = tmps.next()
            op("dve", "scalar_tensor_tensor", reads=[t_xsrc, t_mods, t_rb], writes=[t_tm], out=tm, in0=xsrc[:, c, c0:c0 + TT],
               scalar=mods[:, l, b, gidx, c:c + 1], in1=rb, op0=ALU.mult, op1=ALU.mult)
            if h32 is not None:
                op("act", "activation", reads=[t_tm, t_mods], writes=[h32[1]], out=h32[0][:, c, :], in_=tm, func=AF.Identity,
                   bias=mods[:, l, b, sidx, c:c + 1], scale=1.0)
                op("dve", "tensor_copy", reads=[h32[1]], writes=[t_hd], out=hdst[:, c, :], in_=h32[0][:, c, :])
            else:
                op("act", "activation", reads=[t_tm, t_mods], writes=[t_hd], out=hdst[:, c, :], in_=tm, func=AF.Identity,
                   bias=mods[:, l, b, sidx, c:c + 1], scale=1.0)

    epsc, t_epsc = sbt("epsc", [128, 4])
    op("dve", "memset", writes=[t_epsc], ap=epsc[:, 0:1], constant=EPS)
    op("dve", "memset", writes=[t_epsc], ap=epsc[:, 1:2], constant=1.0)
    op("dve", "memset", writes=[t_epsc], ap=epsc[:, 2:3], constant=-PI)
    eps_c = epsc[:, 0:1]
    one_c = epsc[:, 1:2]
    npi_c = epsc[:, 2:3]

    def mixer(l, b):
        barrier()
        ar["off"] = 0
        hT, t_h = aa("hT", [128, KC, TT], BF16)
        kT, t_kT = aa("kT", [128, 4, S], BF16)
        Vt, t_V = aa("Vt", [128, 16, 512], BF16)
        y_gm, t_ygm = aa("y_gm", [128, 4, TT], BF16)
        y_da, t_yda = aa("y_da", [128, 4, TT], BF16)
        y_dn, t_ydn = aa("y_dn", [128, 4, TT], BF16)
        tmps = Pool_([aa("tmpA%d" % i, [128, TT]) for i in range(3)])
        tmpb = Pool_([aa("tmpB%d" % i, [128, TT], BF16) for i in range(2)])
        st, t_st = aa("stat", [128, 32])
        P1_END = ar["off"]
        op("dve", "memset", writes=[t_halo], ap=halo, constant=0.0)
        op("dve", "memset", writes=[t_S], ap=Sst, constant=0.0)

        def wg(col0, ncols):
            return load_w([(wrows(w_in_d[l])[:, :, col0:col0 + ncols], KC)], KC, ncols)

        def proj_fm(wb, t_wb, nchunk, consume):
            for j in range(nchunk):
                pb, t_pb = psA.next()
                k.group("pe", [mm(pb, wb[:, kc, j * 128:(j + 1) * 128], hT[:, kc, :], start=(kc == 0), stop=(kc == KC - 1))
                               for kc in range(KC)], reads=[t_wb, t_h], writes=[t_pb])
                consume(j, pb, t_pb)

        def proj_tm(wb, t_wb, ncols, consume):
            for blk in range(4):
                pb, t_pb = psA.next()
                k.group("pe", [mm(pb[:, 0:ncols], hT[:, kc, blk * 128:(blk + 1) * 128], wb[:, kc, 0:ncols], start=(kc == 0),
                                  stop=(kc == KC - 1)) for kc in range(KC)], reads=[t_wb, t_h], writes=[t_pb])
                consume(blk, pb, t_pb)

        for t in range(NTILE):
            c0 = t * TT
            ar["off"] = P1_END
            xt, t_xt = aa("xt", [128, KC, TT])
            if l == 0:
                for c in range(KC):
                    k.dma("sp", xt[:, c, :], xT_d[b, c * 128:(c + 1) * 128, c0:c0 + TT], writes=[t_xt], group=(c > 0))
            else:
                for c in range(KC):
                    k.dma("sp", xt[:, c, :], xs_d[:, c, c0:c0 + TT], reads=[t_xs[t][c]], writes=[t_xt], group=(c > 0))
            rb, t_rb = aa("rb", [128, TT])
            sq_b, t_sq = aa("sq_b", [128, KC, TT], BF16)
            modnorm(xt, t_xt, 0, l, b, G1S, SHIFT1, hT, t_h, sq_b, t_sq, rb, t_rb, tmps)
            if l == 0 and t == 0:
                tap("h0", hT, t_h)
            if STAGE <= 1:
                continue
            barrier()
            ar["off"] = P1_END
            SUB0 = P1_END

            u_g, t_ug = aa("u_g", [128, 4, TT], BF16)
            vg, t_vg = aa("vg", [128, 4, 512])
            vln, t_vln = aa("vln", [128, 4, 512], BF16)
            for g2 in range(2):
                wb, t_wb = wg(COL_U + g2 * 256, 256)
                proj_fm(wb, t_wb, 2, lambda j, pb, t_pb, g2=g2: op(
                    "act", "activation", reads=[t_pb], writes=[t_ug], out=u_g[:, g2 * 2 + j, :], in_=pb, func=AF.Gelu))
            for g2 in range(2):
                wb, t_wb = wg(COL_V + g2 * 256, 256)
                proj_tm(wb, t_wb, 256, lambda blk, pb, t_pb, g2=g2: op(
                    "act", "activation", reads=[t_pb], writes=[t_vg], out=vg[:, blk, g2 * 256:(g2 + 1) * 256], in_=pb[:, 0:256], func=AF.Gelu))
            for blk in range(4):
                op("dve", "bn_stats", reads=[t_vg], writes=[t_st], out=st[:, 0:6], in_=vg[:, blk, :])
                op("dve", "bn_aggr", writes=[t_st], out=st[:, 8:10], in_=st[:, 0:6])
                op("act", "activation", writes=[t_st], out=st[:, 10:11], in_=st[:, 9:10], func=AF.Sqrt, bias=eps_c, scale=1.0)
                op("dve", "reciprocal", writes=[t_st], out=st[:, 10:11], in_=st[:, 10:11])
                tm, t_tm = tmps.next()
                op("dve", "tensor_scalar", reads=[t_vg, t_st], writes=[t_tm], out=tm, in0=vg[:, blk, :], scalar1=st[:, 8:9],
                   scalar2=st[:, 10:11], op0=ALU.subtract, op1=ALU.mult)
                op("dve", "tensor_tensor", reads=[t_spl], writes=[t_tm], out=tm, in0=tm, in1=spl("lng"), op=ALU.mult)
                op("dve", "tensor_tensor", reads=[t_spl, t_tm], writes=[t_vln], out=vln[:, blk, :], in0=tm, in1=spl("lnb"), op=ALU.add)
            for g in range(4):
                pb, t_pb = psA.next()
                k.group("pe", [mm(pb[:, blk * 128:(blk + 1) * 128], vln[:, blk, g * 128:(g + 1) * 128], wsT_b[:, g, :])
                               for blk in range(4)], reads=[t_vln, t_wst], writes=[t_pb])
                tm, t_tm = tmps.next()
                bsb = spl("bs")[:, g * 128:(g + 1) * 128].unsqueeze(1).to_broadcast([128, 4, 128])
                op("dve", "tensor_tensor", reads=[t_pb, t_spl], writes=[t_tm], out=tm.rearrange("p (a b) -> p a b", b=128),
                   in0=pb.rearrange("p (a b) -> p a b", b=128), in1=bsb, op=ALU.add)
                op("dve", "tensor_tensor", reads=[t_tm, t_ug], writes=[t_ygm], out=y_gm[:, g, :], in0=tm, in1=u_g[:, g, :], op=ALU.mult)
            if l == 0 and t == 0:
                tap("ygm", y_gm, t_ygm)
            if STAGE <= 2:
                continue
            barrier()
            ar["off"] = SUB0

            cosT, t_cos = aa("cosT", [128, TT])
            sinT, t_sin = aa("sinT", [128, TT])
            qT, t_qT = aa("qT", [128, 4, TT], BF16)
            PT, t_PT = aa("PT", [128, 16, 128], BF16)
            yv, t_yv = aa("yv", [128, 128])
            junk, t_junk = aa("junk", [128, 128])
            Ssb = [aa("Ssb%d" % i, [128, S]) for i in range(2)]
            Pms = [aa("Pm%d" % i, [128, S], BF16) for i in range(2)]
            asts = [aa("ast%d" % i, [128, 8]) for i in range(2)]
            rss = [aa("rs%d" % i, [128, 8]) for i in range(2)]
            ytms = [aa("ytm%d" % i, [128, 512], BF16) for i in range(2)]
            pOs = {}
            qn, t_qn = aa("qn", [128, 4, TT], BF16)
            kn, t_kn = aa("kn", [128, 4, TT], BF16)
            vn, t_vn = aa("vn", [128, 4, TT], BF16)
            Sb, t_Sb = aa("Sb", [128, 4, 128], BF16)
            zs, t_zs = aa("zs", [128, 4, TT], BF16)
            oT, t_oT = aa("oT", [128, 4, TT])
            ab, t_ab = aa("ab", [64, 8, 8])
            gb, t_gb = aa("gb", [64, 4, 8, 4])
            dst_ = {0: (qn, t_qn), 1: (kn, t_kn), 2: (vn, t_vn)}
            op("act", "copy", reads=[t_S], writes=[t_Sb], out=Sb, in_=Sst)
            cwv = spl("convw").rearrange("p (c j) -> p c j", j=4)
            OV0 = ar["off"]
            posi, t_posi = aa("posi", [128, TT], I32)
            k.dma("sp", posi, posb_d[b, :, c0:c0 + TT], writes=[t_posi])
            ang, t_ang = aa("ang", [128, TT])
            kq, t_kq = aa("kq", [128, TT])
            ki, t_ki = aa("ki", [128, TT], I32)
            op("dve", "tensor_copy", reads=[t_posi], writes=[t_ang], out=ang, in_=posi)
            op("dve", "tensor_scalar", reads=[t_consts], writes=[t_ang], out=ang, in0=ang, scalar1=cst("invf"), scalar2=None, op0=ALU.mult)
            op("dve", "tensor_scalar", reads=[t_ang], writes=[t_kq], out=kq, in0=ang, scalar1=1.0 / (2 * PI), scalar2=None, op0=ALU.mult)
            op("dve", "tensor_copy", reads=[t_kq], writes=[t_ki], out=ki, in_=kq)
            op("dve", "tensor_copy", reads=[t_ki], writes=[t_kq], out=kq, in_=ki)
            op("dve", "scalar_tensor_tensor", reads=[t_kq], writes=[t_ang], out=ang, in0=kq, scalar=-C1, in1=ang, op0=ALU.mult, op1=ALU.add)
            op("dve", "scalar_tensor_tensor", reads=[t_kq], writes=[t_ang], out=ang, in0=kq, scalar=-C2, in1=ang, op0=ALU.mult, op1=ALU.add)

            op("dve", "tensor_scalar", reads=[t_ang], writes=[t_kq], out=kq, in0=ang, scalar1=PI, scalar2=None, op0=ALU.is_gt)
            op("dve", "scalar_tensor_tensor", reads=[t_kq, t_ang], writes=[t_sin], out=sinT, in0=kq, scalar=-2 * PI, in1=ang, op0=ALU.mult, op1=ALU.add)
            op("dve", "tensor_scalar", reads=[t_sin], writes=[t_kq], out=kq, in0=sinT, scalar1=PI / 2, scalar2=None, op0=ALU.is_gt)
            op("dve", "scalar_tensor_tensor", reads=[t_kq, t_sin], writes=[t_cos], out=cosT, in0=kq, scalar=-2 * PI, in1=sinT, op0=ALU.mult, op1=ALU.add)
            op("dve", "tensor_scalar", writes=[t_cos], out=cosT, in0=cosT, scalar1=PI / 2, scalar2=None, op0=ALU.add)
            op("act", "activation", writes=[t_sin], out=sinT, in_=sinT, func=AF.Sin)
            op("act", "activation", writes=[t_cos], out=cosT, in_=cosT, func=AF.Sin)
            if l == 0 and t == 0:
                tap("cos", cosT, t_cos)
                tap("sin", sinT, t_sin)
            cws = Pool_([aa("cw%d" % i, [128, TT + 3]) for i in range(2)])

            def rope_consume(dst_fn, t_dst):
                def f(j, pb, t_pb, hbase):
                    h = hbase + j
                    rw, t_rw = tmpb.next()
                    op("act", "copy", reads=[t_pb], writes=[t_rw], out=rw, in_=pb)
                    pr, t_pr = psA.next()
                    k.group("pe", [mm(pr, rot_b, rw)], reads=[t_rw, t_rotb], writes=[t_pr])
                    t1, t_t1 = tmps.next()
                    t2, t_t2 = tmps.next()
                    op("dve", "tensor_tensor", reads=[t_pb, t_cos], writes=[t_t1], out=t1, in0=pb, in1=cosT, op=ALU.mult)
                    op("dve", "tensor_tensor", reads=[t_pr, t_sin], writes=[t_t2], out=t2, in0=pr, in1=sinT, op=ALU.mult)
                    op("dve", "tensor_tensor", reads=[t_t1, t_t2], writes=[t_dst], out=dst_fn(h), in0=t1, in1=t2, op=ALU.add)
                return f
            qcons = rope_consume(lambda h: qT[:, h, :], t_qT)
            kcons = rope_consume(lambda h: kT[:, h, c0:c0 + TT], t_kT)
            for g2 in range(2 if "q" in cfg.get("parts", "qkv") else 0):
                wb, t_wb = wg(COL_Q + g2 * 256, 256)
                proj_fm(wb, t_wb, 2, lambda j, pb, t_pb, g2=g2: qcons(j, pb, t_pb, g2 * 2))
            for g2 in range(2 if "k" in cfg.get("parts", "qkv") else 0):
                wb, t_wb = wg(COL_KK + g2 * 256, 256)
                proj_fm(wb, t_wb, 2, lambda j, pb, t_pb, g2=g2: kcons(j, pb, t_pb, g2 * 2))
            for g2 in range(2 if "v" in cfg.get("parts", "qkv") else 0):
                wb, t_wb = wg(COL_VA + g2 * 256, 256)
                proj_tm(wb, t_wb, 256, lambda blk, pb, t_pb, g2=g2: op(
                    "act", "copy", reads=[t_pb], writes=[t_V], out=Vt[:, t * 4 + blk, g2 * 256:(g2 + 1) * 256], in_=pb[:, 0:256]))
            if l == 0 and t == 0:
                tap("qT", qT, t_qT)
                tap("kT", kT[:, :, 0:TT], t_kT)

            def dn_consume(j, pb, t_pb, ci0):
                ci = ci0 + j
                which, h = ci // 4, ci % 4
                dst, t_dst = dst_[which]
                cw, t_cw = cws.next()
                op("dve", "tensor_copy", reads=[t_halo], writes=[t_cw], out=cw[:, 0:3], in_=halo[:, ci, :])
                op("act", "copy", reads=[t_pb], writes=[t_cw], out=cw[:, 3:TT + 3], in_=pb)
                op("dve", "tensor_copy", reads=[t_cw], writes=[t_halo], out=halo[:, ci, :], in_=cw[:, TT:TT + 3])
                acc, t_acc = tmps.next()
                op("dve", "tensor_scalar", reads=[t_cw, t_spl], writes=[t_acc], out=acc, in0=cw[:, 3:TT + 3], scalar1=cwv[:, ci, 3:4],
                   scalar2=None, op0=ALU.mult)
                for jj in range(3):
                    op("dve", "scalar_tensor_tensor", reads=[t_cw, t_spl], writes=[t_acc], out=acc, in0=cw[:, jj:jj + TT],
                       scalar=cwv[:, ci, jj:jj + 1], in1=acc, op0=ALU.mult, op1=ALU.add)
                if which == 2:
                    op("act", "activation", reads=[t_acc], writes=[t_dst], out=dst[:, h, :], in_=acc, func=AF.Silu)
                else:
                    op("act", "activation", reads=[], writes=[t_acc], out=acc, in_=acc, func=AF.Silu)
                    sqb, t_sqb = tmpb.next()
                    op("act", "activation", reads=[t_acc], writes=[t_sqb], out=sqb, in_=acc, func=AF.Square)
                    pn, t_pn = ps8.next()
                    k.group("pe", [mm(pn, ones_b, sqb)], reads=[t_sqb, t_onesb], writes=[t_pn])
                    rn, t_rn = tmps.next()
                    op("act", "activation", reads=[t_pn], writes=[t_rn], out=rn, in_=pn, func=AF.Sqrt, bias=eps_c, scale=1.0)
                    op("dve", "reciprocal", writes=[t_rn], out=rn, in_=rn)
                    op("dve", "scalar_tensor_tensor", reads=[t_rn, t_acc], writes=[t_dst], out=dst[:, h, :], in0=acc,
                       scalar=(128.0 ** -0.5 if which == 0 else 1.0), in1=rn, op0=ALU.mult, op1=ALU.mult)
            for gi in range(6):
                wb, t_wb = wg(COL_DQ + gi * 256, 256)
                proj_fm(wb, t_wb, 2, lambda j, pb, t_pb, gi=gi: dn_consume(j, pb, t_pb, gi * 2))
            for g2 in range(2):
                wb, t_wb = wg(COL_DZ + g2 * 256, 256)
                proj_fm(wb, t_wb, 2, lambda j, pb, t_pb, g2=g2: op(
                    "act", "activation", reads=[t_pb], writes=[t_zs], out=zs[:, g2 * 2 + j, :], in_=pb, func=AF.Silu))
            wab, t_wab = wg(COL_AB, 8)
            pb, t_pb = ps8.next()
            for c in range(8):
                k.group("pe", [mm(pb[0:64, c * 8:(c + 1) * 8], hT[:, kc, c * 64:(c + 1) * 64], wab[:, kc, 0:8], start=(kc == 0),
                                  stop=(kc == KC - 1)) for kc in range(KC)], reads=[t_wab, t_h], writes=[t_pb])
            op("dve", "tensor_copy", reads=[t_pb], writes=[t_ab], out=ab, in_=pb[0:64, 0:64].rearrange("p (c e) -> p c e", e=8))
            dtb_b = spl("dtb")[0:64, :].unsqueeze(1).to_broadcast([64, 8, 4])
            nA_b = lay[0:64, 4:8].unsqueeze(1).to_broadcast([64, 8, 4])
            op("dve", "tensor_tensor", reads=[t_ab, t_spl], writes=[t_gb], out=gb[:, 3], in0=ab[:, :, 0:4], in1=dtb_b, op=ALU.add)
            op("act", "activation", writes=[t_gb], out=gb[:, 3], in_=gb[:, 3], func=AF.Exp)
            op("act", "activation", writes=[t_gb], out=gb[:, 3], in_=gb[:, 3], func=AF.Ln, bias=one_c[0:64, :], scale=1.0)
            op("dve", "tensor_tensor", reads=[t_lay], writes=[t_gb], out=gb[:, 0], in0=gb[:, 3], in1=nA_b, op=ALU.mult)
            op("act", "activation", reads=[t_ab], writes=[t_gb], out=gb[:, 1], in_=ab[:, :, 4:8], func=AF.Sigmoid)
            op("dve", "tensor_scalar", writes=[t_gb], out=gb[:, 2], in0=gb[:, 1], scalar1=-1.0, scalar2=None, op0=ALU.mult)
            if l == 0 and t == 0:
                tap("qn", qn, t_qn); tap("kn", kn, t_kn); tap("vn", vn, t_vn); tap("gb", gb, t_gb)

            barrier()
            ar["off"] = OV0
            def a64(name, cols=256, rows=64):
                return aa(name, [rows, cols])

            def mkset(s_):
                def b64(name, cols=256):
                    return aa(name, [64, cols], BF16)
                return {"ktok": b64("ktok" + s_, 512), "vtok": b64("vtok" + s_, 512), "R1": a64("R1" + s_), "R2": a64("R2" + s_),
                        "Dm": a64("Dm" + s_), "DTm": a64("DTm" + s_), "egs": a64("egs" + s_, 16), "glb": aa("glb" + s_, [128, 4]),
                        "egrow": aa("egrow" + s_, [128, 256]), "Pb": [b64("Pa" + s_), b64("Pb" + s_)], "Bb": [b64("Ba" + s_), b64("Bb" + s_)],
                        "Nb": [b64("Na" + s_), b64("Nb" + s_)], "rhs_w": b64("rhs_w" + s_, 512), "u_sb": a64("u_sb" + s_, 512),
                        "wT": aa("wT" + s_, [128, 256], BF16), "qgT": aa("qgT" + s_, [128, 256], BF16), "vnew": b64("vnew" + s_, 512),
                        "qkT": b64("qkT" + s_)}
            NW = cfg.get("nw", 2)
            BSs = [mkset("_%d" % i) for i in range(NW)]
            tt_c = cst("tt", 64); su_c = cst("su", 64); negu = cst("negu", 64); negl = cst("negl", 64)
            mstr = cst("mstrict", 64); i4 = cst("i4", 64); id64 = consts[0:64, CO["ident"][0]:CO["ident"][0] + 64]
            units = [(qb, h, p) for qb in range(4 if ACUT >= 2 else 0) for h in range(4) for p in range(2)]

            def stageA(ui):
                qb, h, p = units[ui]
                j = ui % 2
                gq = t * 4 + qb
                kend = (gq + 1) * 128
                Sb_, t_Sb_ = Ssb[j]
                Pj, t_Pj = Pms[j]
                aj, t_aj = asts[j]
                rs_, t_rs = rss[h % 2]
                ps_ = slice(p * 64, (p + 1) * 64)
                for pc in range((kend + 511) // 512):
                    n = min(512, kend - pc * 512)
                    sbk, t_sbk = psS.next()
                    k.group("pe", [mm(sbk[:, 0:n], qT[ps_, h, qb * 128:(qb + 1) * 128], kT[ps_, h, pc * 512:pc * 512 + n])],
                            reads=[t_qT, t_kT], writes=[t_sbk])
                    op("act", "copy", reads=[t_sbk], writes=[t_Sb_], out=Sb_[:, pc * 512:pc * 512 + n], in_=sbk[:, 0:n])
                op("dve", "tensor_scalar", writes=[t_Sb_], out=Sb_[0:64, kend - 64:kend], in0=Sb_[0:64, kend - 64:kend],
                   scalar1=NEG, scalar2=None, op0=ALU.add)
                op("dve", "reduce_max", reads=[t_Sb_], writes=[t_aj], out=aj[:, 0:1], in_=Sb_[:, 0:kend], axis=AX.X)
                op("dve", "tensor_scalar", writes=[t_aj], out=aj[:, 1:2], in0=aj[:, 0:1], scalar1=-0.125, scalar2=None, op0=ALU.mult)
                op("act", "activation", reads=[t_Sb_, t_aj], writes=[t_Pj, t_rs], out=Pj[:, 0:kend], in_=Sb_[:, 0:kend],
                   func=AF.Exp, bias=aj[:, 1:2], scale=0.125, accum_out=rs_[:, p:p + 1])

            def stageB(ui):
                qb, h, p = units[ui]
                j = ui % 2
                gq = t * 4 + qb
                nkb = gq + 1
                Pj, t_Pj = Pms[j]
                rs_, t_rs = rss[h % 2]
                ytm, t_ytm = ytms[qb % 2]
                if p == 0:
                    pOs[(qb, h)] = psO.next()
                pO, t_pO = pOs[(qb, h)]
                for kb0 in range(0, nkb, 8):
                    nb = min(8, nkb - kb0)
                    ptb, t_ptb = psT.next()
                    ptv = ptb.bitcast(BF16)
                    k.group("pe", [tr(ptv[:, i * 128:(i + 1) * 128], Pj[:, (kb0 + i) * 128:(kb0 + i + 1) * 128], ident_b)
                                   for i in range(nb)], reads=[t_Pj, t_identb], writes=[t_ptb])
                    op("dve", "tensor_copy", reads=[t_ptb], writes=[t_PT], out=PT[:, kb0:kb0 + nb, :],
                       in_=ptv[:, 0:nb * 128].rearrange("p (a b) -> p a b", b=128))
                k.group("pe", [mm(pO[:, p * 128:(p + 1) * 128], PT[:, kb, :], Vt[:, kb, h * 128:(h + 1) * 128],
                                  start=(kb == 0), stop=(kb == nkb - 1)) for kb in range(nkb)], reads=[t_PT, t_V], writes=[t_pO])
                if p == 0:
                    return
                op("dve", "reciprocal", writes=[t_rs], out=rs_[:, 2:4], in_=rs_[:, 0:2])
                op("dve", "tensor_tensor", reads=[t_lay], writes=[t_rs], out=rs_[:, 3:4], in0=rs_[:, 3:4], in1=lay[:, 0:1], op=ALU.mult)
                op("dve", "tensor_scalar", reads=[t_pO, t_rs], writes=[t_yv], out=yv, in0=pO[:, 0:128], scalar1=rs_[:, 2:3], scalar2=None, op0=ALU.mult)
                op("dve", "scalar_tensor_tensor", reads=[t_pO, t_rs], writes=[t_yv], out=yv, in0=pO[:, 128:256], scalar=rs_[:, 3:4], in1=yv,
                   op0=ALU.mult, op1=ALU.add)
                op("dve", "tensor_tensor", reads=[t_yv], writes=[t_junk], out=junk, in0=yv, in1=yv, op=ALU.mult)
                op("dve", "reduce_sum", reads=[t_junk], writes=[t_rs], out=rs_[:, 4:5], in_=junk, axis=AX.X)
                op("act", "activation", writes=[t_rs], out=rs_[:, 5:6], in_=rs_[:, 4:5], func=AF.Ln, bias=eps_c, scale=1.0 / 128)
                op("act", "activation", writes=[t_rs], out=rs_[:, 5:6], in_=rs_[:, 5:6], func=AF.Exp, scale=-0.5)
                op("dve", "scalar_tensor_tensor", reads=[t_yv, t_rs, t_subgs], writes=[t_ytm], out=ytm[:, h * 128:(h + 1) * 128], in0=yv,
                   scalar=rs_[:, 5:6], in1=subgs, op0=ALU.mult, op1=ALU.mult)
                if h == 3:
                    ptb, t_ptb = psT.next()
                    ptv = ptb.bitcast(BF16)
                    k.group("pe", [tr(ptv[:, hh * 128:(hh + 1) * 128], ytm[:, hh * 128:(hh + 1) * 128], ident_b) for hh in range(4)],
                            reads=[t_ytm, t_identb], writes=[t_ptb])
                    op("dve", "tensor_copy", reads=[t_ptb], writes=[t_yda], out=y_da[:, :, qb * 128:(qb + 1) * 128],
                       in_=ptv[:, 0:512].rearrange("p (a b) -> p a b", b=128))


            def v3(ap, inner):
                return ap.rearrange("p (h j) -> p h j", j=inner)


            def chunk_prep(c, BS):
                ktok, t_ktok = BS["ktok"]; vtok, t_vtok = BS["vtok"]
                R1, t_R1 = BS["R1"]; R2, t_R2 = BS["R2"]
                Dm, t_D = BS["Dm"]; DTm, t_DT = BS["DTm"]; Ds, t_Ds = Dm, t_D
                egs, t_egs = BS["egs"]; glb, t_glb = BS["glb"]; egrow, t_egrow = BS["egrow"]
                Pb = BS["Pb"]; Bb = BS["Bb"]; Nb = BS["Nb"]
                rhs_u, t_ru = vtok, t_vtok; rhs_w, t_rw_ = BS["rhs_w"]; kd, t_kd = ktok, t_ktok
                u_sb, t_u = BS["u_sb"]; wT, t_wT = BS["wT"]; qkT, t_qk = BS["qkT"]
                qgT, t_qg = BS["qgT"]; vnew, t_vnew = BS["vnew"]
                cs = slice(c * 64, (c + 1) * 64)
                gr = gb[:, 0, c, :]
                be = gb[:, 1, c, :]
                nbe = gb[:, 2, c, :]
                pk, t_pk = psDN.next()
                pkb = pk.bitcast(BF16)
                k.group("pe", [tr(pkb[0:64, h * 128:(h + 1) * 128], kn[:, h, cs], ident_b) for h in range(4)], reads=[t_kn, t_identb], writes=[t_pk])
                op("act", "copy", reads=[t_pk], writes=[t_ktok], out=ktok, in_=pkb[0:64, 0:512])
                yield
                pv, t_pv = psDN.next()
                pvb = pv.bitcast(BF16)
                k.group("pe", [tr(pvb[0:64, h * 128:(h + 1) * 128], vn[:, h, cs], ident_b) for h in range(4)], reads=[t_vn, t_identb], writes=[t_pv])
                op("dve", "tensor_copy", reads=[t_pv], writes=[t_vtok], out=vtok, in_=pvb[0:64, 0:512])
                yield
                op("dve", "tensor_tensor", reads=[t_gb, t_consts], writes=[t_R1], out=v3(R1, 64), in0=gr.unsqueeze(2).to_broadcast([64, 4, 64]),
                   in1=tt_c.unsqueeze(1).to_broadcast([64, 4, 64]), op=ALU.mult)
                yield
                op("dve", "tensor_copy", reads=[t_gb], writes=[t_R2], out=v3(R2, 64), in_=gr.unsqueeze(2).to_broadcast([64, 4, 64]))
                yield
                pg, t_pg = psDN.next()
                k.group("pe", [mm(pg[0:64, 0:256], tt_c, R2, start=True, stop=False),
                               mm(pg[0:64, 0:256], nones_f[0:64, 0:64], R1, start=False, stop=False),
                               mm(pg[0:64, 0:256], id64, negu, start=False, stop=True)],
                        reads=[t_R1, t_R2, t_consts, t_nones], writes=[t_pg])
                op("act", "activation", reads=[t_pg], writes=[t_D], out=Dm, in_=pg[0:64, 0:256], func=AF.Exp)
                yield
                pgt, t_pgt = psDN.next()
                k.group("pe", [mm(pgt[0:64, 0:256], ones_f[0:64, 0:64], R1, start=True, stop=False),
                               mm(pgt[0:64, 0:256], ntt_f, R2, start=False, stop=False),
                               mm(pgt[0:64, 0:256], id64, negl, start=False, stop=True)],
                        reads=[t_R1, t_R2, t_consts, t_ones, t_ntt], writes=[t_pgt])
                op("act", "activation", reads=[t_pgt], writes=[t_DT], out=DTm, in_=pgt[0:64, 0:256], func=AF.Exp)
                yield
                op("dve", "tensor_tensor", reads=[t_consts], writes=[t_Ds], out=Ds, in0=Dm, in1=mstr, op=ALU.mult)
                yield
                px, t_px = psDN.next()
                k.group("pe", [mm(px[:, 0:256], ones_f[0:64, :], R1), mm(px[:, 256:260], ones_f[0:64, :], gr),
                               mm(px[0:64, 264:268], tt_c, gr), mm(px[0:64, 268:272], su_c, gr)],
                        reads=[t_R1, t_gb, t_ones, t_consts], writes=[t_px])
                op("act", "activation", reads=[t_px], writes=[t_egrow], out=egrow, in_=px[:, 0:256], func=AF.Exp)
                op("act", "activation", reads=[t_px], writes=[t_glb], out=glb, in_=px[:, 256:260], func=AF.Exp)
                op("act", "activation", reads=[t_px], writes=[t_egs], out=egs[:, 0:8], in_=px[0:64, 264:272], func=AF.Exp)
                yield
                op("dve", "tensor_tensor", reads=[t_gb], writes=[t_egs], out=egs[:, 8:12], in0=egs[:, 0:4], in1=be, op=ALU.mult)
                yield
                pkk, t_pkk = psDN.next()
                k.group("pe", [mm(pkk[0:64, h * 64:(h + 1) * 64], kn[:, h, cs], kn[:, h, cs]) for h in range(4)], reads=[t_kn], writes=[t_pkk])
                P_, t_P_ = Pb[0]
                for h in range(4):
                    op("dve", "scalar_tensor_tensor", reads=[t_pkk, t_gb, t_Ds], writes=[t_P_], out=P_[:, h * 64:(h + 1) * 64],
                       in0=pkk[0:64, h * 64:(h + 1) * 64], scalar=nbe[:, h:h + 1], in1=Ds[:, h * 64:(h + 1) * 64], op0=ALU.mult, op1=ALU.mult)
                pt_, t_pt_ = psDN.next()
                pt_ = pt_.bitcast(BF16)
                k.group("pe", [tr(pt_[0:64, h * 64:(h + 1) * 64], P_[:, h * 64:(h + 1) * 64], ident_b[0:64, 0:64]) for h in range(4)],
                        reads=[t_P_, t_identb], writes=[t_pt_])
                B_, t_B_ = Bb[0]
                N_, t_N_ = Nb[0]
                op("act", "copy", reads=[t_pt_], writes=[t_B_], out=B_, in_=pt_[0:64, 0:256])
                op("dve", "tensor_tensor", reads=[t_pt_, t_consts], writes=[t_N_], out=N_, in0=pt_[0:64, 0:256], in1=i4, op=ALU.add)
                yield
                for j in range(1, 6):
                    Pn, t_Pn = Pb[j % 2]
                    Bn, t_Bn = Bb[j % 2]
                    Nn, t_Nn = Nb[j % 2]
                    pp, t_pp = psDN.next()
                    k.group("pe", [mm(pp[0:64, h * 64:(h + 1) * 64], B_[:, h * 64:(h + 1) * 64], P_[:, h * 64:(h + 1) * 64]) for h in range(4)],
                            reads=[t_B_, t_P_], writes=[t_pp])
                    op("act", "copy", reads=[t_pp], writes=[t_Pn], out=Pn, in_=pp[0:64, 0:256])
                    if j < 5:
                        pbb, t_pbb = psDN.next()
                        k.group("pe", [mm(pbb[0:64, h * 64:(h + 1) * 64], P_[:, h * 64:(h + 1) * 64], B_[:, h * 64:(h + 1) * 64]) for h in range(4)],
                                reads=[t_B_, t_P_], writes=[t_pbb])
                        op("dve", "tensor_copy", reads=[t_pbb], writes=[t_Bn], out=Bn, in_=pbb[0:64, 0:256])
                    pn2, t_pn2 = psDN.next()
                    k.group("pe", [mm(pn2[0:64, h * 64:(h + 1) * 64], Pn[:, h * 64:(h + 1) * 64], N_[:, h * 64:(h + 1) * 64]) for h in range(4)],
                            reads=[t_Pn, t_N_], writes=[t_pn2])
                    op("dve", "tensor_tensor", reads=[t_pn2, t_N_], writes=[t_Nn], out=Nn, in0=pn2[0:64, 0:256], in1=N_, op=ALU.add)
                    P_, t_P_, B_, t_B_, N_, t_N_ = Pn, t_Pn, Bn, t_Bn, Nn, t_Nn
                op("dve", "tensor_tensor", reads=[t_vtok, t_gb], writes=[t_ru], out=v3(rhs_u, 128), in0=v3(vtok, 128),
                   in1=be.unsqueeze(2).to_broadcast([64, 4, 128]), op=ALU.mult)
                yield
                op("dve", "tensor_tensor", reads=[t_ktok, t_egs], writes=[t_rw_], out=v3(rhs_w, 128), in0=v3(ktok, 128),
                   in1=egs[:, 8:12].unsqueeze(2).to_broadcast([64, 4, 128]), op=ALU.mult)
                yield
                op("dve", "tensor_tensor", reads=[t_ktok, t_egs], writes=[t_kd], out=v3(kd, 128), in0=v3(ktok, 128),
                   in1=egs[:, 4:8].unsqueeze(2).to_broadcast([64, 4, 128]), op=ALU.mult)
                yield
                pu, t_pu = psDN.next()
                k.group("pe", [mm(pu[0:64, h * 128:(h + 1) * 128], N_[:, h * 64:(h + 1) * 64], rhs_u[:, h * 128:(h + 1) * 128]) for h in range(4)],
                        reads=[t_N_, t_ru], writes=[t_pu])
                op("act", "copy", reads=[t_pu], writes=[t_u], out=u_sb, in_=pu[0:64, :])
                yield
                pw, t_pw = psDN.next()
                k.group("pe", [mm(pw[:, h * 64:(h + 1) * 64], rhs_w[:, h * 128:(h + 1) * 128], N_[:, h * 64:(h + 1) * 64]) for h in range(4)],
                        reads=[t_N_, t_rw_], writes=[t_pw])
                op("dve", "tensor_copy", reads=[t_pw], writes=[t_wT], out=wT, in_=pw[:, 0:256])
                yield
                pq, t_pq = psDN.next()
                k.group("pe", [mm(pq[0:64, h * 64:(h + 1) * 64], kn[:, h, cs], qn[:, h, cs]) for h in range(4)], reads=[t_kn, t_qn], writes=[t_pq])
                op("dve", "tensor_tensor", reads=[t_pq, t_DT], writes=[t_qk], out=qkT, in0=pq[0:64, 0:256], in1=DTm, op=ALU.mult)
                yield
                op("dve", "tensor_tensor", reads=[t_qn, t_egrow], writes=[t_qg], out=v3(qgT, 64), in0=qn[:, :, cs], in1=v3(egrow, 64), op=ALU.mult)
                yield

            def chunk_recur(c, BS):
                ktok, t_ktok = BS["ktok"]; vtok, t_vtok = BS["vtok"]
                R1, t_R1 = BS["R1"]; R2, t_R2 = BS["R2"]
                Dm, t_D = BS["Dm"]; DTm, t_DT = BS["DTm"]; Ds, t_Ds = Dm, t_D
                egs, t_egs = BS["egs"]; glb, t_glb = BS["glb"]; egrow, t_egrow = BS["egrow"]
                Pb = BS["Pb"]; Bb = BS["Bb"]; Nb = BS["Nb"]
                rhs_u, t_ru = vtok, t_vtok; rhs_w, t_rw_ = BS["rhs_w"]; kd, t_kd = ktok, t_ktok
                u_sb, t_u = BS["u_sb"]; wT, t_wT = BS["wT"]; qkT, t_qk = BS["qkT"]
                qgT, t_qg = BS["qgT"]; vnew, t_vnew = BS["vnew"]
                cs = slice(c * 64, (c + 1) * 64)
                gr = gb[:, 0, c, :]
                be = gb[:, 1, c, :]
                nbe = gb[:, 2, c, :]
                pws, t_pws = psDN.next()
                k.group("pe", [mm(pws[0:64, h * 128:(h + 1) * 128], wT[:, h * 64:(h + 1) * 64], Sb[:, h, :]) for h in range(4)],
                        reads=[t_wT, t_Sb], writes=[t_pws])
                op("dve", "tensor_tensor", reads=[t_u, t_pws], writes=[t_vnew], out=vnew, in0=u_sb, in1=pws[0:64, :], op=ALU.subtract)
                po, t_po = psDN.next()
                fns = []
                for h in range(4):
                    fns.append(mm(po[:, h * 64:(h + 1) * 64], Sb[:, h, :], qgT[:, h * 64:(h + 1) * 64], start=True, stop=False))
                    fns.append(mm(po[:, h * 64:(h + 1) * 64], vnew[:, h * 128:(h + 1) * 128], qkT[:, h * 64:(h + 1) * 64], start=False, stop=True))
                k.group("pe", fns, reads=[t_Sb, t_qg, t_vnew, t_qk], writes=[t_po])
                op("act", "copy", reads=[t_po], writes=[t_oT], out=oT[:, :, cs], in_=v3(po[:, 0:256], 64))
                pds, t_pds = psDN.next()
                k.group("pe", [mm(pds[:, h * 128:(h + 1) * 128], kd[:, h * 128:(h + 1) * 128], vnew[:, h * 128:(h + 1) * 128]) for h in range(4)],
                        reads=[t_kd, t_vnew], writes=[t_pds])
                for h in range(4):
                    op("dve", "scalar_tensor_tensor", reads=[t_pds, t_glb], writes=[t_S], out=Sst[:, h, :], in0=Sst[:, h, :], scalar=glb[:, h:h + 1],
                       in1=pds[:, h * 128:(h + 1) * 128], op0=ALU.mult, op1=ALU.add)
                op("act", "copy", reads=[t_S], writes=[t_Sb], out=Sb, in_=Sst)


            def att_gen():
                for ui in range(len(units) + 1):
                    if ui < len(units):
                        stageA(ui)
                        yield
                    if ui >= 1:
                        stageB(ui - 1)
                        yield

            def dn_gen():
                for c0_ in range(0, 8, NW):
                    gens = [chunk_prep(c0_ + i, BSs[i]) for i in range(NW)]
                    alive = list(gens)
                    while alive:
                        for g_ in list(alive):
                            try:
                                next(g_)
                            except StopIteration:
                                alive.remove(g_)
                            yield
                    for i in range(NW):
                        chunk_recur(c0_ + i, BSs[i])
                        yield

            RATIO = cfg.get("dn_ratio", 6)
            streams = [[att_gen(), 1], [dn_gen(), RATIO]]
            while streams:
                for st_ in list(streams):
                    for _ in range(st_[1]):
                        try:
                            next(st_[0])
                        except StopIteration:
                            streams.remove(st_)
                            break

            if l == 0 and t == 0 and ACUT >= 8:
                tap("yda", y_da, t_yda)
            if l == 0 and t == 0:
                tap("oT", oT, t_oT)
            for h in range(4):
                sqb, t_sqb = tmpb.next()
                op("act", "activation", reads=[t_oT], writes=[t_sqb], out=sqb, in_=oT[:, h, :], func=AF.Square)
                pn, t_pn = ps8.next()
                k.group("pe", [mm(pn, ones_b, sqb)], reads=[t_sqb, t_onesb], writes=[t_pn])
                rn, t_rn = tmps.next()
                op("act", "activation", reads=[t_pn], writes=[t_rn], out=rn, in_=pn, func=AF.Sqrt, bias=eps_c, scale=1.0 / 128)
                op("dve", "reciprocal", writes=[t_rn], out=rn, in_=rn)
                op("dve", "scalar_tensor_tensor", reads=[t_oT, t_spl], writes=[t_rn], out=rn, in0=oT[:, h, :], scalar=spl("dng"), in1=rn,
                   op0=ALU.mult, op1=ALU.mult)
                op("dve", "tensor_tensor", reads=[t_rn, t_zs], writes=[t_ydn], out=y_dn[:, h, :], in0=rn, in1=zs[:, h, :], op=ALU.mult)
            if l == 0 and t == 0:
                tap("ydn", y_dn, t_ydn)
            if STAGE <= 4:
                continue
            barrier()
            ar["off"] = SUB0
            merged, t_mg = aa("merged", [128, KC, TT], BF16)
            sgs = Pool_([aa("sg%d" % i, [128, TT]) for i in range(3)])
            macc, t_macc = aa("macc", [128, TT])
            xins = Pool_([aa("xin%d" % i, [128, TT]) for i in range(3)])
            ybr = [(y_gm, t_ygm), (y_da, t_yda), (y_dn, t_ydn)]
            for d in range(KC):
                for br in range(3):
                    wgt, t_wgt = load_w([(wrows(w_in_d[l])[:, :, COL_G + br * 1024 + d * 128:COL_G + br * 1024 + (d + 1) * 128], KC)], KC, 128)
                    pg, t_pg = ps8.next()
                    k.group("pe", [mm(pg, wgt[:, kc, :], hT[:, kc, :], start=(kc == 0), stop=(kc == KC - 1)) for kc in range(KC)],
                            reads=[t_wgt, t_h], writes=[t_pg])
                    sg, t_sg = sgs.next()
                    op("act", "activation", reads=[t_pg], writes=[t_sg], out=sg, in_=pg, func=AF.Sigmoid)
                    pp, t_pp = ps8.next()
                    yb, t_yb = ybr[br]
                    wbr, t_wbr = load_w([(wrows(w_br_d[br][l])[:, :, d * 128:(d + 1) * 128], 4)], 4, 128)
                    k.group("pe", [mm(pp, wbr[:, kc, :], yb[:, kc, :], start=(kc == 0), stop=(kc == 3)) for kc in range(4)],
                            reads=[t_wbr, t_yb], writes=[t_pp])
                    if br == 0:
                        op("dve", "tensor_tensor", reads=[t_pp, t_sg], writes=[t_macc], out=macc, in0=pp, in1=sg, op=ALU.mult)
                    else:
                        op("dve", "tensor_tensor", reads=[t_pp], writes=[t_sg], out=sg, in0=pp, in1=sg, op=ALU.mult)
                        if br == 1:
                            op("dve", "tensor_tensor", reads=[t_sg], writes=[t_macc], out=macc, in0=macc, in1=sg, op=ALU.add)
                        else:
                            op("dve", "tensor_tensor", reads=[t_sg, t_macc], writes=[t_mg], out=merged[:, d, :], in0=macc, in1=sg, op=ALU.add)
            if l == 0 and t == 0:
                tap("merged", merged, t_mg)
            for g4 in range(4):
                wo, t_wo = load_w([(wrows(w_out_d[l])[:, :, g4 * 256:(g4 + 1) * 256], KC)], KC, 256)
                for j in range(2):
                    e_ = g4 * 2 + j
                    po, t_po = ps8.next()
                    k.group("pe", [mm(po, wo[:, dc, j * 128:(j + 1) * 128], merged[:, dc, :], start=(dc == 0), stop=(dc == KC - 1))
                                   for dc in range(KC)], reads=[t_wo, t_mg], writes=[t_po])
                    xin, t_xin = xins.next()
                    if l == 0:
                        k.dma("sp", xin, xT_d[b, e_ * 128:(e_ + 1) * 128, c0:c0 + TT], writes=[t_xin])
                    else:
                        k.dma("sp", xin, xs_d[:, e_, c0:c0 + TT], reads=[t_xs[t][e_]], writes=[t_xin])
                    op("dve", "scalar_tensor_tensor", reads=[t_po, t_mods], writes=[t_xin], out=xin, in0=po,
                       scalar=mods[:, l, b, GATE1, e_:e_ + 1], in1=xin, op0=ALU.mult, op1=ALU.add)
                    k.dma("sp", xs_d[:, e_, c0:c0 + TT], xin, reads=[t_xin], writes=[t_xs[t][e_]])
            barrier()


    def ffn(l, b, last):
        barrier()
        ar["off"] = 0
        xT, t_x = aa("xT", [128, KC, S])
        rb, t_rb = aa("rb", [128, TT])
        sq_b, t_sq = aa("sq_b", [128, KC, TT], BF16)
        tmps = Pool_([aa("tmpA%d" % i, [128, TT]) for i in range(3)])
        FMARK = ar["off"]
        h2, t_h2 = aa("h2", [128, KC, S], BF16)
        moe = (l % 2 == 1)
        for t in range(NTILE):
            k.dma("sp", xT[:, :, t * TT:(t + 1) * TT], xs_d[:, :, t * TT:(t + 1) * TT], reads=t_xs[t], writes=[t_x], group=(t > 0))
        if moe:
            h32, t_h32 = aa("h32", [128, KC, TT])
            WT, t_WT = aa("WT", [8, S])
            rhe, t_rhe = aa("rhe", [8, TT])
            Wb0, t_Wb0 = aa("Wbc0", [128, S], BF16)
            Wb1, t_Wb1 = aa("Wbc1", [128, S], BF16)
            Wbcs = [Wb0, Wb1]; t_Wbcs = [t_Wb0, t_Wb1]
            lg, t_lg = aa("lg", [128, 64])
            wr3 = spg[:, 8:72].rearrange("p (c e) -> p c e", e=8)
        for t in range(NTILE):
            modnorm(xT, t_x, t * TT, l, b, G2S, SHIFT2, h2[:, :, t * TT:(t + 1) * TT], t_h2, sq_b, t_sq, rb, t_rb, tmps,
                    h32=((h32, t_h32) if moe else None))
            if moe:
                for blk in range(4):
                    gblk = t * 4 + blk
                    pl, t_pl = ps8.next()
                    k.group("pe", [mm(pl[:, 0:8], h32[:, kc, blk * 128:(blk + 1) * 128], wr3[:, kc, :], start=(kc == 0), stop=(kc == KC - 1))
                                   for kc in range(KC)], reads=[t_h32, t_spg], writes=[t_pl])
                    L = lg[:, 0:8]; E1 = lg[:, 8:16]; M2 = lg[:, 16:24]; E2 = lg[:, 24:32]; Wt = lg[:, 32:40]
                    m1 = lg[:, 40:41]; m2 = lg[:, 41:42]; dd = lg[:, 42:43]; w1 = lg[:, 43:44]; w2 = lg[:, 44:45]
                    op("dve", "tensor_copy", reads=[t_pl], writes=[t_lg], out=L, in_=pl[:, 0:8])
                    op("dve", "reduce_max", writes=[t_lg], out=m1, in_=L, axis=AX.X)
                    op("dve", "tensor_scalar", writes=[t_lg], out=E1, in0=L, scalar1=m1, scalar2=None, op0=ALU.is_equal)
                    op("dve", "scalar_tensor_tensor", writes=[t_lg], out=M2, in0=E1, scalar=-1e30, in1=L, op0=ALU.mult, op1=ALU.add)
                    op("dve", "reduce_max", writes=[t_lg], out=m2, in_=M2, axis=AX.X)
                    op("dve", "tensor_scalar", writes=[t_lg], out=E2, in0=M2, scalar1=m2, scalar2=None, op0=ALU.is_equal)
                    op("dve", "tensor_tensor", writes=[t_lg], out=dd, in0=m2, in1=m1, op=ALU.subtract)
                    op("act", "activation", writes=[t_lg], out=dd, in_=dd, func=AF.Exp)
                    op("dve", "tensor_scalar", writes=[t_lg], out=w1, in0=dd, scalar1=1.0, scalar2=None, op0=ALU.add)
                    op("dve", "reciprocal", writes=[t_lg], out=w1, in_=w1)
                    op("dve", "tensor_tensor", writes=[t_lg], out=w2, in0=dd, in1=w1, op=ALU.mult)
                    op("dve", "tensor_scalar", writes=[t_lg], out=Wt, in0=E1, scalar1=w1, scalar2=None, op0=ALU.mult)
                    op("dve", "scalar_tensor_tensor", writes=[t_lg], out=Wt, in0=E2, scalar=w2, in1=Wt, op0=ALU.mult, op1=ALU.add)
                    pt_, t_pt_ = ps8.next()
                    k.group("pe", [tr(pt_[0:8, 0:128], Wt, ident)], reads=[t_lg, t_consts], writes=[t_pt_])
                    op("act", "copy", reads=[t_pt_], writes=[t_WT], out=WT[:, gblk * 128:(gblk + 1) * 128], in_=pt_[0:8, 0:128])
        if moe and b == 0:
            tap("WT", WT, t_WT)
        if b == 0:
            tap("h2_%d" % l, h2, t_h2)

        hids = Pool_([aa("hid%d" % i, [128, 2, TT], BF16) for i in range(2)])
        pend = {"f": None}

        def expert_pass(wg_d, wu_d, wd_d, F, scale_by=None):
            for fg in range(F // 256):
                wgb, t_wgb = load_w([(wrows(wg_d)[:, :, fg * 256:(fg + 1) * 256], KC)], KC, 256)
                wub, t_wub = load_w([(wrows(wu_d)[:, :, fg * 256:(fg + 1) * 256], KC)], KC, 256)
                wdb, t_wdb = load_w([(wrows(wd_d[fg * 256:(fg + 1) * 256, :]), 2)], 2, D)
                for t in range(NTILE):
                    tc_ = slice(t * TT, (t + 1) * TT)
                    hid, t_hid = hids.next()
                    pg_ = pend["f"]

                    def adv():
                        if pend["f"] is not None:
                            try:
                                next(pend["f"])
                            except StopIteration:
                                pend["f"] = None
                    for j in range(2):
                        pa, t_pa = psF.next()
                        k.group("pe", [mm(pa, wgb[:, kc, j * 128:(j + 1) * 128], h2[:, kc, tc_], start=(kc == 0), stop=(kc == KC - 1))
                                       for kc in range(KC)], reads=[t_wgb, t_h2], writes=[t_pa])
                        adv()
                        pu_, t_pu_ = psF.next()
                        k.group("pe", [mm(pu_, wub[:, kc, j * 128:(j + 1) * 128], h2[:, kc, tc_], start=(kc == 0), stop=(kc == KC - 1))
                                       for kc in range(KC)], reads=[t_wub, t_h2], writes=[t_pu_])
                        adv()
                        sa, t_sa = tmps.next()
                        op("act", "activation", reads=[t_pa], writes=[t_sa], out=sa, in_=pa, func=AF.Silu)
                        if scale_by is not None:
                            op("dve", "tensor_tensor", reads=[scale_by[1]], writes=[t_sa], out=sa, in0=sa, in1=scale_by[0][:, tc_], op=ALU.mult)
                        op("dve", "tensor_tensor", reads=[t_pu_, t_sa], writes=[t_hid], out=hid[:, j, :], in0=pu_, in1=sa, op=ALU.mult)
                    while pend["f"] is not None:
                        adv()

                    def down(wdb=wdb, t_wdb=t_wdb, hid=hid, t_hid=t_hid, tc_=tc_):
                        for d in range(KC):
                            pd, t_pd = psD.next()
                            k.group("pe", [mm(pd, wdb[:, j, d * 128:(d + 1) * 128], hid[:, j, :], start=(j == 0), stop=(j == 1)) for j in range(2)],
                                    reads=[t_wdb, t_hid], writes=[t_pd])
                            op("dve", "scalar_tensor_tensor", reads=[t_pd, t_mods], writes=[t_x], out=xT[:, d, tc_], in0=pd,
                               scalar=mods[:, l, b, GATE2, d:d + 1], in1=xT[:, d, tc_], op0=ALU.mult, op1=ALU.add)
                            if d % 2 == 1:
                                yield
                    pend["f"] = down()

        def flush():
            while pend["f"] is not None:
                try:
                    next(pend["f"])
                except StopIteration:
                    pend["f"] = None

        if not moe:
            expert_pass(ffn_g_d[0], ffn_u_d[0], ffn_d_d[0], FF_DENSE)
            flush()
        else:
            for e_ in range(cfg.get("nexp", NEXP)):
                for t in range(NTILE):
                    op("dve", "tensor_scalar", reads=[t_WT, t_consts], writes=[t_rhe], out=rhe, in0=WT[:, t * TT:(t + 1) * TT],
                       scalar1=consts[0:8, CO["ident"][0] + e_:CO["ident"][0] + e_ + 1], scalar2=None, op0=ALU.mult)
                    pw, t_pw = ps8.next()
                    k.group("pe", [mm(pw, ones_f[0:8, :], rhe)], reads=[t_rhe, t_ones], writes=[t_pw])
                    op("act", "copy", reads=[t_pw], writes=[t_Wbcs[e_ % 2]], out=Wbcs[e_ % 2][:, t * TT:(t + 1) * TT], in_=pw)
                expert_pass(moe_g_d[0, e_], moe_u_d[0, e_], moe_d_d[0, e_], FF_EXPERT, scale_by=(Wbcs[e_ % 2], t_Wbcs[e_ % 2]))
            flush()
        if b == 0:
            tap("xffn_%d" % l, xT, t_x)
        if not last:
            for t in range(NTILE):
                for c in range(KC):
                    k.dma("sp", xs_d[:, c, t * TT:(t + 1) * TT], xT[:, c, t * TT:(t + 1) * TT], reads=[t_x], writes=[t_xs[t][c]])
            return
        barrier()
        ar["off"] = FMARK
        xn, t_xn = aa("xn", [128, KC, TT])
        otm = Pool_([aa("otm%d" % i, [128, D]) for i in range(2)])
        for t in range(NTILE):
            for c in range(KC):
                op("act", "activation", reads=[t_x], writes=[t_sq], out=sq_b[:, c, :], in_=xT[:, c, t * TT:(t + 1) * TT], func=AF.Square)
            pb, t_pb = ps8.next()
            k.group("pe", [mm(pb, ones_b, sq_b[:, c, :], start=(c == 0), stop=(c == KC - 1)) for c in range(KC)],
                    reads=[t_sq, t_onesb], writes=[t_pb])
            op("act", "activation", reads=[t_pb], writes=[t_rb], out=rb, in_=pb, func=AF.Sqrt, bias=eps_c, scale=1.0 / D)
            op("dve", "reciprocal", writes=[t_rb], out=rb, in_=rb)
            for c in range(KC):
                op("dve", "scalar_tensor_tensor", reads=[t_x, t_spg, t_rb], writes=[t_xn], out=xn[:, c, :], in0=xT[:, c, t * TT:(t + 1) * TT],
                   scalar=spg[:, c:c + 1], in1=rb, op0=ALU.mult, op1=ALU.mult)
            for blk in range(4):
                ot, t_ot = otm.next()
                for half in range(2):
                    pt_, t_pt_ = ps8.next()
                    k.group("pe", [tr(pt_[:, i * 128:(i + 1) * 128], xn[:, half * 4 + i, blk * 128:(blk + 1) * 128], ident) for i in range(4)],
                            reads=[t_xn, t_consts], writes=[t_pt_])
                    if half == 0:
                        op("act", "copy", reads=[t_pt_], writes=[t_ot], out=ot[:, 0:512], in_=pt_)
                    else:
                        op("dve", "tensor_copy", reads=[t_pt_], writes=[t_ot], out=ot[:, 512:1024], in_=pt_)
                r0 = t * TT + blk * 128
                k.dma("sp", out_d[b, r0:r0 + 128, :], ot, reads=[t_ot], writes=[out_tok])

    for b in range(NSEQ):
        for l in range(LAYERS):
            layer_setup(l)
            mixer(l, b)
            if STAGE >= 6:
                ffn(l, b, last=(l == LAYERS - 1))

    fin = [out_tok] + list(tapd.values())
    if STAGE < 99:
        z, t_z = sbt("zz", [128, 8])
        op("dve", "memset", writes=[t_z], ap=z, constant=0.0)
        k.dma("sp", out_d[0, 0:128, 0:8], z, reads=[t_z], writes=[out_tok])
    k.finish(fin)
    nc._k = k
    return nc


def prep_core_inputs(inp, core):
    x = np.asarray(inp["x"], np.float32)
    b0 = 2 * core
    xT = np.ascontiguousarray(x[b0:b0 + 2].transpose(0, 2, 1))
    pos = np.asarray(inp["positions"], np.int32)[b0:b0 + 2]
    posb = np.ascontiguousarray(np.broadcast_to(pos[:, None, :], (2, 128, S)))
    c = np.asarray(inp["c"], np.float32)[b0:b0 + 2]
    cT = np.ascontiguousarray(c.reshape(2, KC, 128).transpose(2, 1, 0)).reshape(128, KC * 2)
    return {"xT": xT, "posb": posb, "cT": cT}


def shared_inputs(inp):
    ws = np.asarray(inp["gm_w_s"], np.float32)
    wst = np.ascontiguousarray(ws.transpose(3, 0, 1, 2)).reshape(128, DEPTH * 4 * 128)
    sh = {"smallp": make_smallp(inp), "consts": make_consts(), "wst": wst}
    for n in ["w_mod", "w_in", "w_br_gm", "w_br_da", "w_br_dn", "w_out", "ffn_w_gate", "ffn_w_up",
              "ffn_w_down", "moe_w_gate", "moe_w_up", "moe_w_down"]:
        sh[n] = np.ascontiguousarray(np.asarray(inp[n], np.float32))
    return sh


def kernel(**inputs):
    nc = build()
    sh = shared_inputs(inputs)
    in_maps = []
    for core in range(8):
        m = dict(sh)
        m.update(prep_core_inputs(inputs, core))
        in_maps.append(m)
    res = run_bass_kernel_spmd(nc, in_maps, core_ids=list(range(8)))
    return np.concatenate([np.asarray(r["out"], np.float32) for r in res.results], axis=0)
```

```python
import math
import numpy as np
import concourse.bass as bass
import concourse.mybir as mybir
from concourse.bass_utils import run_bass_kernel_spmd

F32 = mybir.dt.float32
BF16 = mybir.dt.bfloat16
I32 = mybir.dt.int32
AF = mybir.ActivationFunctionType
ALU = mybir.AluOpType
AX = mybir.AxisListType

SEM_LIMIT = 20000
ENGS = ("pe", "dve", "act", "pool", "sp")


class Tok:
    __slots__ = ("name", "w", "r", "dsem", "pr", "excl")

    def __init__(self, name, excl=False):
        self.name = name
        self.excl = excl
        self.w = None
        self.r = []
        self.dsem = None
        self.pr = []


class K:
    def __init__(self, nc, same=("dve", "act", "pool"), eager=True):
        self.nc = nc
        self.eager = eager
        self.embed = True
        self._collect = None
        self.e = {"pe": nc.tensor, "dve": nc.vector, "act": nc.scalar,
                  "pool": nc.gpsimd, "sp": nc.sync}
        self.nsem = 0
        self.sem = {}
        self.cnt = {}
        for n in ENGS:
            self._new_epoch(n)
        self.seq = {n: 0 for n in ENGS}
        self.last = {n: None for n in ENGS}
        self.sigmap = {n: [] for n in ENGS}
        self.waited = {n: {} for n in ENGS}
        self.same = set(same)
        self.rsem = {}
        self.store_recs = []
        self.ninstr = 0
        self.nwait = 0

    def _alloc(self, name):
        self.nsem += 1
        return self.nc.alloc_semaphore("%s_%d" % (name, self.nsem))

    def _new_epoch(self, n):
        self.sem[n] = self._alloc("e_" + n)
        self.cnt[n] = 0

    def _resolve(self, ev):
        if ev[0] == "c":
            _, eng, seq = ev
            lst = self.sigmap[eng]
            lo, hi = 0, len(lst)
            while lo < hi:
                mid = (lo + hi) // 2
                if lst[mid][0] >= seq:
                    hi = mid
                else:
                    lo = mid + 1
            if lo < len(lst):
                f = lst[lo]
                return f[1], f[2]
            assert self.seq[eng] >= seq and self.last[eng] is not None
            if self.cnt[eng] >= SEM_LIMIT:
                self._new_epoch(eng)
            self.cnt[eng] += 1
            self.last[eng].then_inc(self.sem[eng], 1)
            lst.append((self.seq[eng], self.sem[eng], self.cnt[eng]))
            return self.sem[eng], self.cnt[eng]
        rec = ev[1]
        return rec[0], rec[1]

    def _wait(self, eng, ev):
        if ev is None:
            return
        if ev[0] == "c" and ev[1] == eng and eng not in self.same:
            return
        sem, val = self._resolve(ev)
        w = self.waited[eng]
        if w.get(id(sem), 0) >= val:
            return
        w[id(sem)] = val
        if self._collect is not None:
            self._collect.append((sem, val))
            return
        self.e[eng].wait_ge(sem, val)
        self.nwait += 1

    def _pre(self, eng, reads, writes):
        for t in reads:
            self._wait(eng, t.w)
        for t in writes:
            self._wait(eng, t.w)
            for ev in t.r:
                self._wait(eng, ev)

    def _signal_last(self, eng):
        if self.cnt[eng] >= SEM_LIMIT:
            self._new_epoch(eng)
        self.cnt[eng] += 1
        self.last[eng].then_inc(self.sem[eng], 1)
        self.sigmap[eng].append((self.seq[eng], self.sem[eng], self.cnt[eng]))

    def _post(self, eng, reads, writes):
        if self.eager and (reads or writes):
            self._signal_last(eng)
        ev = ("c", eng, self.seq[eng])
        for t in reads:
            t.r.append(ev)
            if len(t.r) > 16:
                t.r = self._compact(t.r)
        for t in writes:
            t.w = ev
            t.r = []

    @staticmethod
    def _split(reads, writes):
        xr = [t for t in reads if t.excl]
        if xr:
            writes = list(writes) + [t for t in xr if t not in writes]
            reads = [t for t in reads if not t.excl]
        return reads, writes

    def _pre_embed(self, eng, reads, writes):
        if not self.embed:
            self._pre(eng, reads, writes)
            return None
        self._collect = []
        self._pre(eng, reads, writes)
        waits, self._collect = self._collect, None
        for sem, val in waits[:-1]:
            self.e[eng].wait_ge(sem, val)
            self.nwait += 1
        return waits[-1] if waits else None

    def op(self, eng, fn, reads=(), writes=()):
        reads, writes = self._split(reads, writes)
        w_ = self._pre_embed(eng, reads, writes)
        ins = fn(self.e[eng])
        if w_ is not None:
            ins._wait_ge(w_[0], w_[1])
        self.seq[eng] += 1
        self.last[eng] = ins
        self.ninstr += 1
        self._post(eng, reads, writes)
        return ins

    def group(self, eng, fns, reads=(), writes=()):
        reads, writes = self._split(reads, writes)
        w_ = self._pre_embed(eng, reads, writes)
        ins = None
        for i_, fn in enumerate(fns):
            ins = fn(self.e[eng])
            if i_ == 0 and w_ is not None:
                ins._wait_ge(w_[0], w_[1])
            self.seq[eng] += 1
            self.ninstr += 1
        self.last[eng] = ins
        self._post(eng, reads, writes)
        return ins

    def _compact(self, evs):
        best = {}
        other = []
        for ev in evs:
            if ev[0] == "c":
                k = ev[1]
                if k not in best or ev[2] >= best[k][2]:
                    best[k] = ev
            else:
                if not any(o[1] is ev[1] for o in other):
                    other.append(ev)
        return list(best.values()) + other

    def dma(self, q, out, in_, reads=(), writes=(), group=False, **kw):
        for t in reads:
            self._wait(q, t.w)
        for t in writes:
            if group:
                for ev in t.pr:
                    self._wait(q, ev)
            else:
                self._wait(q, t.w)
                for ev in t.r:
                    self._wait(q, ev)
                t.pr = ([t.w] if t.w is not None else []) + list(t.r)
        ins = self.e[q].dma_start(out=out, in_=in_, **kw)
        assert len(writes) <= 1
        if writes:
            t = writes[0]
            if t.dsem is None or t.dsem[1] >= SEM_LIMIT:
                t.dsem = [self._alloc("d_" + t.name), 0]
            rec = t.dsem
        else:
            if q not in self.rsem or self.rsem[q][1] >= SEM_LIMIT:
                self.rsem[q] = [self._alloc("r_" + q), 0]
            rec = self.rsem[q]
        rec[1] += 16
        ins.then_inc(rec[0], 16)
        ev = ("d", rec)
        if reads and not any(r is rec for r in self.store_recs):
            self.store_recs.append(rec)
        if writes:
            writes[0].w = ev
            writes[0].r = []
        for s in reads:
            s.r.append(ev)
            if len(s.r) > 16:
                s.r = self._compact(s.r)
        self.ninstr += 1
        return ins

    def finish(self, toks, eng="sp"):
        for t in toks:
            self._wait(eng, t.w)
            for ev in t.r:
                self._wait(eng, ev)


D = 1024
KC = 8
S = 2048
TT = 512
NT = S // TT
INW = 7688
DEPTH = 2
EPS = 1e-6
FF_DENSE = 2816
FF_EXPERT = 3584
NEXP = 8
PI = math.pi
C1 = 6.28125
C2 = 2.0 * math.pi - 6.28125
NEG = -30000.0

COL_U, COL_V, COL_Q, COL_KK, COL_VA = 0, 512, 1024, 1536, 2048
COL_DQ, COL_DK, COL_DV, COL_DZ, COL_AB, COL_G = 2560, 3072, 3584, 4096, 4608, 4616

SP_LAYOUT = [("n1g", 8), ("n2g", 8), ("bmod", 48), ("lng", 512), ("lnb", 512), ("bs", 512),
             ("lam", 256), ("subg", 128), ("convw", 48), ("alog", 4), ("dtb", 4), ("dng", 1)]
SP_OFF = {}
_o = 0
for _n, _w in SP_LAYOUT:
    SP_OFF[_n] = (_o, _w)
    _o += _w
SP_LAYER = _o
SP_FING = 2 * SP_LAYER
SP_WR = SP_FING + 8
SP_TOTAL = SP_WR + 64

CO = {}
_o = 0
for _n, _w in [("ident", 128), ("rot", 128), ("tt", 64), ("su", 64), ("negu", 256), ("negl", 256),
               ("mstrict", 256), ("i4", 256), ("invf", 1)]:
    CO[_n] = (_o, _w)
    _o += _w
CO_TOTAL = _o


def make_consts():
    c = np.zeros((128, CO_TOTAL), np.float32)
    o = CO["ident"][0]
    c[:, o:o + 128] = np.eye(128, dtype=np.float32)
    o = CO["rot"][0]
    for m in range(128):
        if (m % 64) < 32:
            c[m + 32, o + m] = -1.0
        else:
            c[m - 32, o + m] = 1.0
    p = np.arange(64)[:, None]
    j = np.arange(64)[None, :]
    o = CO["tt"][0]
    c[:64, o:o + 64] = (p <= j)
    o = CO["su"][0]
    c[:64, o:o + 64] = (p > j)
    for h in range(4):
        o = CO["negu"][0] + h * 64
        c[:64, o:o + 64] = np.where(j > p, NEG, 0.0)
        o = CO["negl"][0] + h * 64
        c[:64, o:o + 64] = np.where(j < p, NEG, 0.0)
        o = CO["mstrict"][0] + h * 64
        c[:64, o:o + 64] = (j < p)
        o = CO["i4"][0] + h * 64
        c[:64, o:o + 64] = (j == p)
    inv_freq = (10000.0 ** (-np.arange(0, 64, 2, dtype=np.float32) / np.float32(64))).astype(np.float32)
    c[:, CO["invf"][0]] = inv_freq[np.arange(128) % 32]
    return c


def fm(v):
    v = np.asarray(v, np.float32)
    return np.ascontiguousarray(v.reshape(-1, 128).T)


def bc(v):
    v = np.asarray(v, np.float32).reshape(1, -1)
    return np.broadcast_to(v, (128, v.shape[1]))


def make_smallp(inp):
    sp = np.zeros((128, SP_TOTAL), np.float32)
    for l in range(DEPTH):
        base = l * SP_LAYER

        def put(name, arr):
            o, w = SP_OFF[name]
            assert arr.shape == (128, w), (name, arr.shape, w)
            sp[:, base + o:base + o + w] = arr
        put("n1g", fm(inp["norm1_g"][l]))
        put("n2g", fm(inp["norm2_g"][l]))
        put("bmod", fm(inp["b_mod"][l]))
        put("lng", bc(inp["gm_ln_g"][l]))
        put("lnb", bc(inp["gm_ln_b"][l]))
        put("bs", bc(inp["gm_b_s"][l].reshape(-1)))
        put("lam", bc(inp["da_lambda"][l].reshape(-1)))
        put("subg", bc(inp["da_subln_g"][l]))
        cw = np.asarray(inp["dn_conv_w"][l], np.float32)
        put("convw", np.ascontiguousarray(cw.reshape(4, 12, 128).transpose(2, 1, 0)).reshape(128, 48))
        put("alog", bc(inp["dn_a_log"][l]))
        put("dtb", bc(inp["dn_dt_bias"][l]))
        put("dng", np.asarray(inp["dn_norm_g"][l], np.float32).reshape(128, 1))
    sp[:, SP_FING:SP_FING + 8] = fm(inp["final_g"])
    wr = np.asarray(inp["moe_w_router"][0], np.float32)
    sp[:, SP_WR:SP_WR + 64] = wr.reshape(8, 128, 8).transpose(1, 0, 2).reshape(128, 64)
    return sp


def lambda_init_fn(layer):
    return 0.8 - 0.6 * math.exp(-0.3 * layer)


class Pool_:
    def __init__(self, items):
        self.items = items
        self.i = 0

    def next(self):
        it = self.items[self.i % len(self.items)]
        self.i += 1
        return it


def build(cfg=None):
    cfg = dict(cfg or {})
    NSEQ = cfg.get("nseq", 2)
    NTILE = cfg.get("ntile", NT)
    LAYERS = cfg.get("layers", DEPTH)
    STAGE = cfg.get("stage", 99)
    taps = cfg.get("taps", ())
    ACUT = cfg.get("acut", 99)

    nc = bass.Bass("TRN2", target_bir_lowering=False)
    k = K(nc, same=tuple(cfg.get("same", ("dve", "act", "pool"))), eager=cfg.get("eager", True))
    k.embed = cfg.get("embed", True)

    def din(name, shape, dt=F32):
        return nc.dram_tensor(name, list(shape), dt, kind="ExternalInput").ap()

    xT_d = din("xT", [2, D, S])
    posb_d = din("posb", [2, 128, S], I32)
    cT_d = din("cT", [128, KC * 2])
    smallp_d = din("smallp", [128, SP_TOTAL])
    consts_d = din("consts", [128, CO_TOTAL])
    wst_d = din("wst", [128, DEPTH * 4 * 128])
    w_mod_d = din("w_mod", [DEPTH, D, 6 * D])
    w_in_d = din("w_in", [DEPTH, D, INW])
    w_br_d = [din("w_br_gm", [DEPTH, 512, D]), din("w_br_da", [DEPTH, 512, D]), din("w_br_dn", [DEPTH, 512, D])]
    w_out_d = din("w_out", [DEPTH, D, D])
    ffn_g_d = din("ffn_w_gate", [1, D, FF_DENSE])
    ffn_u_d = din("ffn_w_up", [1, D, FF_DENSE])
    ffn_d_d = din("ffn_w_down", [1, FF_DENSE, D])
    moe_g_d = din("moe_w_gate", [1, NEXP, D, FF_EXPERT])
    moe_u_d = din("moe_w_up", [1, NEXP, D, FF_EXPERT])
    moe_d_d = din("moe_w_down", [1, NEXP, FF_EXPERT, D])
    out_d = nc.dram_tensor("out", [2, S, D], F32, kind="ExternalOutput").ap()
    out_tok = Tok("out")
    xs_d = nc.dram_tensor("xs_scratch", [128, KC, S], F32, kind="Internal").ap()
    t_xs = [[Tok("xs%d_%d" % (i, e)) for e in range(KC)] for i in range(NT)]

    tapd = {}

    def sb(name, shape, dt=F32):
        return nc.alloc_sbuf_tensor("s_" + name, list(shape), dt).ap()

    def sbt(name, shape, dt=F32):
        return sb(name, shape, dt), Tok(name)

    def tap(name, ap, tok):
        if name not in taps or name in tapd:
            return
        d = nc.dram_tensor("tap_" + name, list(ap.shape), ap.dtype, kind="ExternalOutput").ap()
        t = Tok("tap_" + name)
        k.dma("sp", d, ap, reads=[tok], writes=[t])
        tapd[name] = t

    def op(eng, fn, reads=(), writes=(), **kw):
        return k.op(eng, lambda e: getattr(e, fn)(**kw), reads=reads, writes=writes)

    def mm(out, lhsT, rhs, start=True, stop=True):
        return lambda e: e.matmul(out, lhsT=lhsT, rhs=rhs, start=start, stop=stop)

    def tr(out, in_, idn):
        return lambda e: e.transpose(out, in_, idn)

    def barrier():
        engs = ("pe", "dve", "act", "pool")
        evs = {n: ("c", n, k.seq[n]) for n in engs if k.last[n] is not None}
        for a in engs + ("sp",):
            for b_, ev in evs.items():
                if a != b_:
                    k._wait(a, ev)
            for rec in k.store_recs:
                k._wait(a, ("d", rec))

    consts, t_consts = sbt("consts", [128, CO_TOTAL])
    spl_t, t_spl = sbt("spl", [128, SP_LAYER])
    spg, t_spg = sbt("spg", [128, 72])
    k.dma("sp", consts, consts_d, writes=[t_consts])
    k.dma("sp", spg, smallp_d[:, SP_FING:SP_FING + 72], writes=[t_spg])

    def cst(name, rows=128):
        o, w = CO[name]
        return consts[0:rows, o:o + w]

    def spl(name):
        o, w = SP_OFF[name]
        return spl_t[:, o:o + w]

    ident = cst("ident")
    ones_f, t_ones = sbt("ones_f", [128, 128])
    ones_b, t_onesb = sbt("ones_b", [128, 128], BF16)
    nones_f, t_nones = sbt("nones_f", [128, 128])
    ident_b, t_identb = sbt("ident_b", [128, 128], BF16)
    rot_b, t_rotb = sbt("rot_b", [128, 128], BF16)
    ntt_f, t_ntt = sbt("ntt_f", [64, 64])
    op("dve", "memset", writes=[t_ones], ap=ones_f, constant=1.0)
    op("dve", "memset", writes=[t_onesb], ap=ones_b, constant=1.0)
    op("dve", "memset", writes=[t_nones], ap=nones_f, constant=-1.0)
    op("dve", "tensor_copy", reads=[t_consts], writes=[t_identb], out=ident_b, in_=ident)
    op("dve", "tensor_copy", reads=[t_consts], writes=[t_rotb], out=rot_b, in_=cst("rot"))
    op("dve", "tensor_scalar", reads=[t_consts], writes=[t_ntt], out=ntt_f, in0=cst("tt", 64), scalar1=-1.0,
       scalar2=None, op0=ALU.mult)
    TC = [t_consts, t_ones, t_onesb, t_nones, t_identb, t_rotb, t_ntt]

    psall = nc.alloc_psum_tensor("psall", [128, 8 * 512], F32).ap()
    banks = [(psall[:, i * 512:(i + 1) * 512], Tok("bank%d" % i, excl=True)) for i in range(8)]
    psA = Pool_(banks[0:4])
    psF = Pool_(banks[0:4])
    psD = Pool_(banks[4:8])
    psS = Pool_(banks[4:6])
    psO = Pool_(banks[0:2])
    psT = Pool_(banks[2:4])
    ps8 = Pool_(banks)
    psDN = Pool_(banks[6:8]) if not cfg.get("dn8") else ps8
    S_big = psall[:, 4 * 512:8 * 512]
    t_Sbig = [b_[1] for b_ in banks[4:8]]

    NSLOT = cfg.get("nslot", 7)
    wbf = Pool_([sbt("wbf%d" % i, [128, 2048], BF16) for i in range(NSLOT)])

    def load_w(srcs, a, b_):
        wb, t_wb = wbf.next()
        wbv = wb[:, 0:a * b_].rearrange("p (a b) -> p a b", b=b_)
        o = 0
        for i, (src, ai) in enumerate(srcs):
            k.dma("pool", wbv[:, o:o + ai, :], src, writes=[t_wb], group=(i > 0))
            o += ai
        assert o == a
        return wbv, t_wb

    def wrows(ap2d):
        return ap2d.rearrange("(kc p) n -> p kc n", p=128)

    cact, t_cact = sbt("cact", [128, KC * 2])
    k.dma("sp", cact, cT_d, writes=[t_cact])
    op("act", "activation", writes=[t_cact], out=cact, in_=cact, func=AF.Silu)
    cact_b, t_cactb = sbt("cact_b", [128, KC * 2], BF16)
    op("dve", "tensor_copy", reads=[t_cact], writes=[t_cactb], out=cact_b, in_=cact)
    cact3 = cact_b.rearrange("p (c b) -> p c b", b=2)
    modT, t_mod = sbt("modT", [128, DEPTH, 48, 2])
    mods, t_mods = sbt("mods", [128, DEPTH, 2, 6, 8])
    bmod_t, t_bmod = sbt("bmod_t", [128, 64])
    for l in range(LAYERS):
        k.dma("sp", bmod_t, smallp_d[:, l * SP_LAYER:l * SP_LAYER + 64], writes=[t_bmod])
        for g in range(24):
            w32, t_w32 = load_w([(wrows(w_mod_d[l])[:, :, g * 256:(g + 1) * 256], KC)], KC, 256)
            pb, t_pb = psA.next()
            fns = []
            for j in range(2):
                for kc in range(KC):
                    fns.append(mm(pb[:, j * 2:j * 2 + 2], w32[:, kc, j * 128:(j + 1) * 128], cact3[:, kc, :],
                                  start=(kc == 0), stop=(kc == KC - 1)))
            k.group("pe", fns, reads=[t_w32, t_cactb], writes=[t_pb])
            op("dve", "tensor_copy", reads=[t_pb], writes=[t_mod], out=modT[:, l, g * 2:(g + 1) * 2, :],
               in_=pb[:, 0:4].rearrange("p (j b) -> p j b", b=2))
        for b in range(2):
            op("dve", "tensor_tensor", reads=[t_bmod], writes=[t_mod], out=modT[:, l, :, b], in0=modT[:, l, :, b],
               in1=bmod_t[:, 16:64], op=ALU.add)
        for b in range(2):
            for part in range(6):
                src = modT[:, l, part * 8:(part + 1) * 8, b]
                dst = mods[:, l, b, part, :]
                if part in (1, 4):
                    go = 0 if part == 1 else 8
                    op("dve", "scalar_tensor_tensor", reads=[t_mod, t_bmod], writes=[t_mods], out=dst, in0=src, scalar=1.0,
                       in1=bmod_t[:, go:go + 8], op0=ALU.add, op1=ALU.mult)
                else:
                    op("dve", "tensor_copy", reads=[t_mod], writes=[t_mods], out=dst, in_=src)
    tap("mods", mods, t_mods)
    SHIFT1, G1S, GATE1, SHIFT2, G2S, GATE2 = 0, 1, 2, 3, 4, 5

    wsT_b, t_wst = sbt("wsT_b", [128, 4, 128], BF16)
    lay, t_lay = sbt("lay", [128, 16])
    subgs, t_subgs = sbt("subgs", [128, 128])
    halo, t_halo = sbt("halo", [128, 12, 3])
    Sst, t_S = sbt("Sst", [128, 4, 128])
    lamtmp = sbt("lamtmp", [128, 2, 64])

    rem = int(nc.sbuf_bytes_remaining)
    AW = (rem - 1024) // 4
    arena = sb("arena", [128, AW])
    ar = {"off": 0}
    tokcache = {}

    def aa(name, shape, dt=F32):
        n = 1
        for s_ in shape[1:]:
            n *= s_
        words = (n * (4 if dt in (F32, I32) else 2) + 3) // 4
        words = (words + 7) // 8 * 8
        o = ar["off"]
        assert o + words <= AW, ("arena overflow", name, o, words, AW)
        ar["off"] = o + words
        v = arena[0:shape[0], o:o + words]
        if dt != F32:
            v = v.bitcast(dt)
        v = v[:, 0:n]
        if len(shape) == 3:
            v = v.rearrange("p (a b) -> p a b", b=shape[2])
        elif len(shape) == 4:
            v = v.rearrange("p (a b c) -> p a b c", b=shape[2], c=shape[3])
        key = (name, o)
        if key not in tokcache:
            tokcache[key] = Tok(name)
        return v, tokcache[key]

    def layer_setup(l):
        k.dma("sp", spl_t, smallp_d[:, l * SP_LAYER:(l + 1) * SP_LAYER], writes=[t_spl])
        k.dma("pool", wsT_b, wst_d[:, l * 512:(l + 1) * 512].rearrange("p (g i) -> p g i", i=128), writes=[t_wst])
        op("dve", "memset", writes=[t_wst], ap=wsT_b[64:128, :, 0:64], constant=0.0)
        lam_v = spl("lam").rearrange("p (a d) -> p a d", d=64)
        pr_, t_pr = lamtmp
        op("dve", "tensor_tensor", reads=[t_spl], writes=[t_pr], out=pr_[:, 0, :], in0=lam_v[:, 0, :], in1=lam_v[:, 1, :], op=ALU.mult)
        op("dve", "tensor_tensor", reads=[t_spl], writes=[t_pr], out=pr_[:, 1, :], in0=lam_v[:, 2, :], in1=lam_v[:, 3, :], op=ALU.mult)
        op("dve", "reduce_sum", reads=[t_pr], writes=[t_lay], out=lay[:, 8:10], in_=pr_, axis=AX.X)
        op("act", "activation", reads=[], writes=[t_lay], out=lay[:, 8:10], in_=lay[:, 8:10], func=AF.Exp)
        op("dve", "scalar_tensor_tensor", reads=[], writes=[t_lay], out=lay[:, 0:1], in0=lay[:, 9:10], scalar=-lambda_init_fn(l),
           in1=lay[:, 8:9], op0=ALU.add, op1=ALU.subtract)
        op("dve", "tensor_scalar", reads=[t_spl], writes=[t_subgs], out=subgs, in0=spl("subg"), scalar1=1.0 - lambda_init_fn(l),
           scalar2=None, op0=ALU.mult)
        op("act", "activation", reads=[t_spl], writes=[t_lay], out=lay[:, 4:8], in_=spl("alog"), func=AF.Exp)
        op("dve", "tensor_scalar", reads=[], writes=[t_lay], out=lay[:, 4:8], in0=lay[:, 4:8], scalar1=-1.0, scalar2=None, op0=ALU.mult)

    def modnorm(xsrc, t_xsrc, c0, l, b, gidx, sidx, hdst, t_hd, sq_b, t_sq, rb, t_rb, tmps, h32=None):
        for c in range(KC):
            op("act", "activation", reads=[t_xsrc], writes=[t_sq], out=sq_b[:, c, :], in_=xsrc[:, c, c0:c0 + TT], func=AF.Square)
        pb, t_pb = psA.next()
        k.group("pe", [mm(pb, ones_b, sq_b[:, c, :], start=(c == 0), stop=(c == KC - 1)) for c in range(KC)],
                reads=[t_sq, t_onesb], writes=[t_pb])
        op("act", "activation", reads=[t_pb], writes=[t_rb], out=rb, in_=pb, func=AF.Sqrt, bias=eps_c, scale=1.0 / D)
        op("dve", "reciprocal", writes=[t_rb], out=rb, in_=rb)
        for c in range(KC):
            tm, t_tm = tmps.next()
            op("dve", "scalar_tensor_tensor", reads=[t_xsrc, t_mods, t_rb], writes=[t_tm], out=tm, in0=xsrc[:, c, c0:c0 + TT],
               scalar=mods[:, l, b, gidx, c:c + 1], in1=rb, op0=ALU.mult, op1=ALU.mult)
            if h32 is not None:
                op("act", "activation", reads=[t_tm, t_mods], writes=[h32[1]], out=h32[0][:, c, :], in_=tm, func=AF.Identity,
                   bias=mods[:, l, b, sidx, c:c + 1], scale=1.0)
                op("dve", "tensor_copy", reads=[h32[1]], writes=[t_hd], out=hdst[:, c, :], in_=h32[0][:, c, :])
            else:
                op("act", "activation", reads=[t_tm, t_mods], writes=[t_hd], out=hdst[:, c, :], in_=tm, func=AF.Identity,
                   bias=mods[:, l, b, sidx, c:c + 1], scale=1.0)

    epsc, t_epsc = sbt("epsc", [128, 4])
    op("dve", "memset", writes=[t_epsc], ap=epsc[:, 0:1], constant=EPS)
    op("dve", "memset", writes=[t_epsc], ap=epsc[:, 1:2], constant=1.0)
    op("dve", "memset", writes=[t_epsc], ap=epsc[:, 2:3], constant=-PI)
    eps_c = epsc[:, 0:1]
    one_c = epsc[:, 1:2]
    npi_c = epsc[:, 2:3]

    def mixer(l, b):
        barrier()
        ar["off"] = 0
        hT, t_h = aa("hT", [128, KC, TT], BF16)
        kT, t_kT = aa("kT", [128, 4, S], BF16)
        Vt, t_V = aa("Vt", [128, 16, 512], BF16)
        y_gm, t_ygm = aa("y_gm", [128, 4, TT], BF16)
        y_da, t_yda = aa("y_da", [128, 4, TT], BF16)
        y_dn, t_ydn = aa("y_dn", [128, 4, TT], BF16)
        tmps = Pool_([aa("tmpA%d" % i, [128, TT]) for i in range(3)])
        tmpb = Pool_([aa("tmpB%d" % i, [128, TT], BF16) for i in range(2)])
        st, t_st = aa("stat", [128, 32])
        P1_END = ar["off"]
        op("dve", "memset", writes=[t_halo], ap=halo, constant=0.0)
        op("dve", "memset", writes=[t_S], ap=Sst, constant=0.0)

        def wg(col0, ncols):
            return load_w([(wrows(w_in_d[l])[:, :, col0:col0 + ncols], KC)], KC, ncols)

        def proj_fm(wb, t_wb, nchunk, consume):
            for j in range(nchunk):
                pb, t_pb = psA.next()
                k.group("pe", [mm(pb, wb[:, kc, j * 128:(j + 1) * 128], hT[:, kc, :], start=(kc == 0), stop=(kc == KC - 1))
                               for kc in range(KC)], reads=[t_wb, t_h], writes=[t_pb])
                consume(j, pb, t_pb)

        def proj_tm(wb, t_wb, ncols, consume):
            for blk in range(4):
                pb, t_pb = psA.next()
                k.group("pe", [mm(pb[:, 0:ncols], hT[:, kc, blk * 128:(blk + 1) * 128], wb[:, kc, 0:ncols], start=(kc == 0),
                                  stop=(kc == KC - 1)) for kc in range(KC)], reads=[t_wb, t_h], writes=[t_pb])
                consume(blk, pb, t_pb)

        for t in range(NTILE):
            c0 = t * TT
            ar["off"] = P1_END
            xt, t_xt = aa("xt", [128, KC, TT])
            if l == 0:
                for c in range(KC):
                    k.dma("sp", xt[:, c, :], xT_d[b, c * 128:(c + 1) * 128, c0:c0 + TT], writes=[t_xt], group=(c > 0))
            else:
                for c in range(KC):
                    k.dma("sp", xt[:, c, :], xs_d[:, c, c0:c0 + TT], reads=[t_xs[t][c]], writes=[t_xt], group=(c > 0))
            rb, t_rb = aa("rb", [128, TT])
            sq_b, t_sq = aa("sq_b", [128, KC, TT], BF16)
            modnorm(xt, t_xt, 0, l, b, G1S, SHIFT1, hT, t_h, sq_b, t_sq, rb, t_rb, tmps)
            if l == 0 and t == 0:
                tap("h0", hT, t_h)
            if STAGE <= 1:
                continue
            barrier()
            ar["off"] = P1_END
            SUB0 = P1_END

            u_g, t_ug = aa("u_g", [128, 4, TT], BF16)
            vg, t_vg = aa("vg", [128, 4, 512])
            vln, t_vln = aa("vln", [128, 4, 512], BF16)
            for g2 in range(2):
                wb, t_wb = wg(COL_U + g2 * 256, 256)
                proj_fm(wb, t_wb, 2, lambda j, pb, t_pb, g2=g2: op(
                    "act", "activation", reads=[t_pb], writes=[t_ug], out=u_g[:, g2 * 2 + j, :], in_=pb, func=AF.Gelu))
            for g2 in range(2):
                wb, t_wb = wg(COL_V + g2 * 256, 256)
                proj_tm(wb, t_wb, 256, lambda blk, pb, t_pb, g2=g2: op(
                    "act", "activation", reads=[t_pb], writes=[t_vg], out=vg[:, blk, g2 * 256:(g2 + 1) * 256], in_=pb[:, 0:256], func=AF.Gelu))
            for blk in range(4):
                op("dve", "bn_stats", reads=[t_vg], writes=[t_st], out=st[:, 0:6], in_=vg[:, blk, :])
                op("dve", "bn_aggr", writes=[t_st], out=st[:, 8:10], in_=st[:, 0:6])
                op("act", "activation", writes=[t_st], out=st[:, 10:11], in_=st[:, 9:10], func=AF.Sqrt, bias=eps_c, scale=1.0)
                op("dve", "reciprocal", writes=[t_st], out=st[:, 10:11], in_=st[:, 10:11])
                tm, t_tm = tmps.next()
                op("dve", "tensor_scalar", reads=[t_vg, t_st], writes=[t_tm], out=tm, in0=vg[:, blk, :], scalar1=st[:, 8:9],
                   scalar2=st[:, 10:11], op0=ALU.subtract, op1=ALU.mult)
                op("dve", "tensor_tensor", reads=[t_spl], writes=[t_tm], out=tm, in0=tm, in1=spl("lng"), op=ALU.mult)
                op("dve", "tensor_tensor", reads=[t_spl, t_tm], writes=[t_vln], out=vln[:, blk, :], in0=tm, in1=spl("lnb"), op=ALU.add)
            for g in range(4):
                pb, t_pb = psA.next()
                k.group("pe", [mm(pb[:, blk * 128:(blk + 1) * 128], vln[:, blk, g * 128:(g + 1) * 128], wsT_b[:, g, :])
                               for blk in range(4)], reads=[t_vln, t_wst], writes=[t_pb])
                tm, t_tm = tmps.next()
                bsb = spl("bs")[:, g * 128:(g + 1) * 128].unsqueeze(1).to_broadcast([128, 4, 128])
                op("dve", "tensor_tensor", reads=[t_pb, t_spl], writes=[t_tm], out=tm.rearrange("p (a b) -> p a b", b=128),
                   in0=pb.rearrange("p (a b) -> p a b", b=128), in1=bsb, op=ALU.add)
                op("dve", "tensor_tensor", reads=[t_tm, t_ug], writes=[t_ygm], out=y_gm[:, g, :], in0=tm, in1=u_g[:, g, :], op=ALU.mult)
            if l == 0 and t == 0:
                tap("ygm", y_gm, t_ygm)
            if STAGE <= 2:
                continue
            barrier()
            ar["off"] = SUB0

            cosT, t_cos = aa("cosT", [128, TT])
            sinT, t_sin = aa("sinT", [128, TT])
            qT, t_qT = aa("qT", [128, 4, TT], BF16)
            PT, t_PT = aa("PT", [128, 16, 128], BF16)
            yv, t_yv = aa("yv", [128, 128])
            junk, t_junk = aa("junk", [128, 128])
            Ssb = [aa("Ssb%d" % i, [128, S]) for i in range(2)]
            Pms = [aa("Pm%d" % i, [128, S], BF16) for i in range(2)]
            asts = [aa("ast%d" % i, [128, 8]) for i in range(2)]
            rss = [aa("rs%d" % i, [128, 8]) for i in range(2)]
            ytms = [aa("ytm%d" % i, [128, 512], BF16) for i in range(2)]
            pOs = {}
            qn, t_qn = aa("qn", [128, 4, TT], BF16)
            kn, t_kn = aa("kn", [128, 4, TT], BF16)
            vn, t_vn = aa("vn", [128, 4, TT], BF16)
            Sb, t_Sb = aa("Sb", [128, 4, 128], BF16)
            zs, t_zs = aa("zs", [128, 4, TT], BF16)
            oT, t_oT = aa("oT", [128, 4, TT])
            ab, t_ab = aa("ab", [64, 8, 8])
            gb, t_gb = aa("gb", [64, 4, 8, 4])
            dst_ = {0: (qn, t_qn), 1: (kn, t_kn), 2: (vn, t_vn)}
            op("act", "copy", reads=[t_S], writes=[t_Sb], out=Sb, in_=Sst)
            cwv = spl("convw").rearrange("p (c j) -> p c j", j=4)
            OV0 = ar["off"]
            posi, t_posi = aa("posi", [128, TT], I32)
            k.dma("sp", posi, posb_d[b, :, c0:c0 + TT], writes=[t_posi])
            ang, t_ang = aa("ang", [128, TT])
            kq, t_kq = aa("kq", [128, TT])
            ki, t_ki = aa("ki", [128, TT], I32)
            op("dve", "tensor_copy", reads=[t_posi], writes=[t_ang], out=ang, in_=posi)
            op("dve", "tensor_scalar", reads=[t_consts], writes=[t_ang], out=ang, in0=ang, scalar1=cst("invf"), scalar2=None, op0=ALU.mult)
            op("dve", "tensor_scalar", reads=[t_ang], writes=[t_kq], out=kq, in0=ang, scalar1=1.0 / (2 * PI), scalar2=None, op0=ALU.mult)
            op("dve", "tensor_copy", reads=[t_kq], writes=[t_ki], out=ki, in_=kq)
            op("dve", "tensor_copy", reads=[t_ki], writes=[t_kq], out=kq, in_=ki)
            op("dve", "scalar_tensor_tensor", reads=[t_kq], writes=[t_ang], out=ang, in0=kq, scalar=-C1, in1=ang, op0=ALU.mult, op1=ALU.add)
            op("dve", "scalar_tensor_tensor", reads=[t_kq], writes=[t_ang], out=ang, in0=kq, scalar=-C2, in1=ang, op0=ALU.mult, op1=ALU.add)

            op("dve", "tensor_scalar", reads=[t_ang], writes=[t_kq], out=kq, in0=ang, scalar1=PI, scalar2=None, op0=ALU.is_gt)
            op("dve", "scalar_tensor_tensor", reads=[t_kq, t_ang], writes=[t_sin], out=sinT, in0=kq, scalar=-2 * PI, in1=ang, op0=ALU.mult, op1=ALU.add)
            op("dve", "tensor_scalar", reads=[t_sin], writes=[t_kq], out=kq, in0=sinT, scalar1=PI / 2, scalar2=None, op0=ALU.is_gt)
            op("dve", "scalar_tensor_tensor", reads=[t_kq, t_sin], writes=[t_cos], out=cosT, in0=kq, scalar=-2 * PI, in1=sinT, op0=ALU.mult, op1=ALU.add)
            op("dve", "tensor_scalar", writes=[t_cos], out=cosT, in0=cosT, scalar1=PI / 2, scalar2=None, op0=ALU.add)
            op("act", "activation", writes=[t_sin], out=sinT, in_=sinT, func=AF.Sin)
            op("act", "activation", writes=[t_cos], out=cosT, in_=cosT, func=AF.Sin)
            if l == 0 and t == 0:
                tap("cos", cosT, t_cos)
                tap("sin", sinT, t_sin)
            cws = Pool_([aa("cw%d" % i, [128, TT + 3]) for i in range(2)])

            def rope_consume(dst_fn, t_dst):
                def f(j, pb, t_pb, hbase):
                    h = hbase + j
                    rw, t_rw = tmpb.next()
                    op("act", "copy", reads=[t_pb], writes=[t_rw], out=rw, in_=pb)
                    pr, t_pr = psA.next()
                    k.group("pe", [mm(pr, rot_b, rw)], reads=[t_rw, t_rotb], writes=[t_pr])
                    t1, t_t1 = tmps.next()
                    t2, t_t2 = tmps.next()
                    op("dve", "tensor_tensor", reads=[t_pb, t_cos], writes=[t_t1], out=t1, in0=pb, in1=cosT, op=ALU.mult)
                    op("dve", "tensor_tensor", reads=[t_pr, t_sin], writes=[t_t2], out=t2, in0=pr, in1=sinT, op=ALU.mult)
                    op("dve", "tensor_tensor", reads=[t_t1, t_t2], writes=[t_dst], out=dst_fn(h), in0=t1, in1=t2, op=ALU.add)
                return f
            qcons = rope_consume(lambda h: qT[:, h, :], t_qT)
            kcons = rope_consume(lambda h: kT[:, h, c0:c0 + TT], t_kT)
            for g2 in range(2 if "q" in cfg.get("parts", "qkv") else 0):
                wb, t_wb = wg(COL_Q + g2 * 256, 256)
                proj_fm(wb, t_wb, 2, lambda j, pb, t_pb, g2=g2: qcons(j, pb, t_pb, g2 * 2))
            for g2 in range(2 if "k" in cfg.get("parts", "qkv") else 0):
                wb, t_wb = wg(COL_KK + g2 * 256, 256)
                proj_fm(wb, t_wb, 2, lambda j, pb, t_pb, g2=g2: kcons(j, pb, t_pb, g2 * 2))
            for g2 in range(2 if "v" in cfg.get("parts", "qkv") else 0):
                wb, t_wb = wg(COL_VA + g2 * 256, 256)
                proj_tm(wb, t_wb, 256, lambda blk, pb, t_pb, g2=g2: op(
                    "act", "copy", reads=[t_pb], writes=[t_V], out=Vt[:, t * 4 + blk, g2 * 256:(g2 + 1) * 256], in_=pb[:, 0:256]))
            if l == 0 and t == 0:
                tap("qT", qT, t_qT)
                tap("kT", kT[:, :, 0:TT], t_kT)

            def dn_consume(j, pb, t_pb, ci0):
                ci = ci0 + j
                which, h = ci // 4, ci % 4
                dst, t_dst = dst_[which]
                cw, t_cw = cws.next()
                op("dve", "tensor_copy", reads=[t_halo], writes=[t_cw], out=cw[:, 0:3], in_=halo[:, ci, :])
                op("act", "copy", reads=[t_pb], writes=[t_cw], out=cw[:, 3:TT + 3], in_=pb)
                op("dve", "tensor_copy", reads=[t_cw], writes=[t_halo], out=halo[:, ci, :], in_=cw[:, TT:TT + 3])
                acc, t_acc = tmps.next()
                op("dve", "tensor_scalar", reads=[t_cw, t_spl], writes=[t_acc], out=acc, in0=cw[:, 3:TT + 3], scalar1=cwv[:, ci, 3:4],
                   scalar2=None, op0=ALU.mult)
                for jj in range(3):
                    op("dve", "scalar_tensor_tensor", reads=[t_cw, t_spl], writes=[t_acc], out=acc, in0=cw[:, jj:jj + TT],
                       scalar=cwv[:, ci, jj:jj + 1], in1=acc, op0=ALU.mult, op1=ALU.add)
                if which == 2:
                    op("act", "activation", reads=[t_acc], writes=[t_dst], out=dst[:, h, :], in_=acc, func=AF.Silu)
                else:
                    op("act", "activation", reads=[], writes=[t_acc], out=acc, in_=acc, func=AF.Silu)
                    sqb, t_sqb = tmpb.next()
                    op("act", "activation", reads=[t_acc], writes=[t_sqb], out=sqb, in_=acc, func=AF.Square)
                    pn, t_pn = ps8.next()
                    k.group("pe", [mm(pn, ones_b, sqb)], reads=[t_sqb, t_onesb], writes=[t_pn])
                    rn, t_rn = tmps.next()
                    op("act", "activation", reads=[t_pn], writes=[t_rn], out=rn, in_=pn, func=AF.Sqrt, bias=eps_c, scale=1.0)
                    op("dve", "reciprocal", writes=[t_rn], out=rn, in_=rn)
                    op("dve", "scalar_tensor_tensor", reads=[t_rn, t_acc], writes=[t_dst], out=dst[:, h, :], in0=acc,
                       scalar=(128.0 ** -0.5 if which == 0 else 1.0), in1=rn, op0=ALU.mult, op1=ALU.mult)
            for gi in range(6):
                wb, t_wb = wg(COL_DQ + gi * 256, 256)
                proj_fm(wb, t_wb, 2, lambda j, pb, t_pb, gi=gi: dn_consume(j, pb, t_pb, gi * 2))
            for g2 in range(2):
                wb, t_wb = wg(COL_DZ + g2 * 256, 256)
                proj_fm(wb, t_wb, 2, lambda j, pb, t_pb, g2=g2: op(
                    "act", "activation", reads=[t_pb], writes=[t_zs], out=zs[:, g2 * 2 + j, :], in_=pb, func=AF.Silu))
            wab, t_wab = wg(COL_AB, 8)
            pb, t_pb = ps8.next()
            for c in range(8):
                k.group("pe", [mm(pb[0:64, c * 8:(c + 1) * 8], hT[:, kc, c * 64:(c + 1) * 64], wab[:, kc, 0:8], start=(kc == 0),
                                  stop=(kc == KC - 1)) for kc in range(KC)], reads=[t_wab, t_h], writes=[t_pb])
            op("dve", "tensor_copy", reads=[t_pb], writes=[t_ab], out=ab, in_=pb[0:64, 0:64].rearrange("p (c e) -> p c e", e=8))
            dtb_b = spl("dtb")[0:64, :].unsqueeze(1).to_broadcast([64, 8, 4])
            nA_b = lay[0:64, 4:8].unsqueeze(1).to_broadcast([64, 8, 4])
            op("dve", "tensor_tensor", reads=[t_ab, t_spl], writes=[t_gb], out=gb[:, 3], in0=ab[:, :, 0:4], in1=dtb_b, op=ALU.add)
            op("act", "activation", writes=[t_gb], out=gb[:, 3], in_=gb[:, 3], func=AF.Exp)
            op("act", "activation", writes=[t_gb], out=gb[:, 3], in_=gb[:, 3], func=AF.Ln, bias=one_c[0:64, :], scale=1.0)
            op("dve", "tensor_tensor", reads=[t_lay], writes=[t_gb], out=gb[:, 0], in0=gb[:, 3], in1=nA_b, op=ALU.mult)
            op("act", "activation", reads=[t_ab], writes=[t_gb], out=gb[:, 1], in_=ab[:, :, 4:8], func=AF.Sigmoid)
            op("dve", "tensor_scalar", writes=[t_gb], out=gb[:, 2], in0=gb[:, 1], scalar1=-1.0, scalar2=None, op0=ALU.mult)
            if l == 0 and t == 0:
                tap("qn", qn, t_qn); tap("kn", kn, t_kn); tap("vn", vn, t_vn); tap("gb", gb, t_gb)

            barrier()
            ar["off"] = OV0
            def a64(name, cols=256, rows=64):
                return aa(name, [rows, cols])

            def mkset(s_):
                def b64(name, cols=256):
                    return aa(name, [64, cols], BF16)
                return {"ktok": b64("ktok" + s_, 512), "vtok": b64("vtok" + s_, 512), "R1": a64("R1" + s_), "R2": a64("R2" + s_),
                        "Dm": a64("Dm" + s_), "DTm": a64("DTm" + s_), "egs": a64("egs" + s_, 16), "glb": aa("glb" + s_, [128, 4]),
                        "egrow": aa("egrow" + s_, [128, 256]), "Pb": [b64("Pa" + s_), b64("Pb" + s_)], "Bb": [b64("Ba" + s_), b64("Bb" + s_)],
                        "Nb": [b64("Na" + s_), b64("Nb" + s_)], "rhs_w": b64("rhs_w" + s_, 512), "u_sb": a64("u_sb" + s_, 512),
                        "wT": aa("wT" + s_, [128, 256], BF16), "qgT": aa("qgT" + s_, [128, 256], BF16), "vnew": b64("vnew" + s_, 512),
                        "qkT": b64("qkT" + s_)}
            NW = cfg.get("nw", 2)
            BSs = [mkset("_%d" % i) for i in range(NW)]
            tt_c = cst("tt", 64); su_c = cst("su", 64); negu = cst("negu", 64); negl = cst("negl", 64)
            mstr = cst("mstrict", 64); i4 = cst("i4", 64); id64 = consts[0:64, CO["ident"][0]:CO["ident"][0] + 64]
            units = [(qb, h, p) for qb in range(4 if ACUT >= 2 else 0) for h in range(4) for p in range(2)]

            def stageA(ui):
                qb, h, p = units[ui]
                j = ui % 2
                gq = t * 4 + qb
                kend = (gq + 1) * 128
                Sb_, t_Sb_ = Ssb[j]
                Pj, t_Pj = Pms[j]
                aj, t_aj = asts[j]
                rs_, t_rs = rss[h % 2]
                ps_ = slice(p * 64, (p + 1) * 64)
                for pc in range((kend + 511) // 512):
                    n = min(512, kend - pc * 512)
                    sbk, t_sbk = psS.next()
                    k.group("pe", [mm(sbk[:, 0:n], qT[ps_, h, qb * 128:(qb + 1) * 128], kT[ps_, h, pc * 512:pc * 512 + n])],
                            reads=[t_qT, t_kT], writes=[t_sbk])
                    op("act", "copy", reads=[t_sbk], writes=[t_Sb_], out=Sb_[:, pc * 512:pc * 512 + n], in_=sbk[:, 0:n])
                op("dve", "tensor_scalar", writes=[t_Sb_], out=Sb_[0:64, kend - 64:kend], in0=Sb_[0:64, kend - 64:kend],
                   scalar1=NEG, scalar2=None, op0=ALU.add)
                op("dve", "reduce_max", reads=[t_Sb_], writes=[t_aj], out=aj[:, 0:1], in_=Sb_[:, 0:kend], axis=AX.X)
                op("dve", "tensor_scalar", writes=[t_aj], out=aj[:, 1:2], in0=aj[:, 0:1], scalar1=-0.125, scalar2=None, op0=ALU.mult)
                op("act", "activation", reads=[t_Sb_, t_aj], writes=[t_Pj, t_rs], out=Pj[:, 0:kend], in_=Sb_[:, 0:kend],
                   func=AF.Exp, bias=aj[:, 1:2], scale=0.125, accum_out=rs_[:, p:p + 1])

            def stageB(ui):
                qb, h, p = units[ui]
                j = ui % 2
                gq = t * 4 + qb
                nkb = gq + 1
                Pj, t_Pj = Pms[j]
                rs_, t_rs = rss[h % 2]
                ytm, t_ytm = ytms[qb % 2]
                if p == 0:
                    pOs[(qb, h)] = psO.next()
                pO, t_pO = pOs[(qb, h)]
                for kb0 in range(0, nkb, 8):
                    nb = min(8, nkb - kb0)
                    ptb, t_ptb = psT.next()
                    ptv = ptb.bitcast(BF16)
                    k.group("pe", [tr(ptv[:, i * 128:(i + 1) * 128], Pj[:, (kb0 + i) * 128:(kb0 + i + 1) * 128], ident_b)
                                   for i in range(nb)], reads=[t_Pj, t_identb], writes=[t_ptb])
                    op("dve", "tensor_copy", reads=[t_ptb], writes=[t_PT], out=PT[:, kb0:kb0 + nb, :],
                       in_=ptv[:, 0:nb * 128].rearrange("p (a b) -> p a b", b=128))
                k.group("pe", [mm(pO[:, p * 128:(p + 1) * 128], PT[:, kb, :], Vt[:, kb, h * 128:(h + 1) * 128],
                                  start=(kb == 0), stop=(kb == nkb - 1)) for kb in range(nkb)], reads=[t_PT, t_V], writes=[t_pO])
                if p == 0:
                    return
                op("dve", "reciprocal", writes=[t_rs], out=rs_[:, 2:4], in_=rs_[:, 0:2])
                op("dve", "tensor_tensor", reads=[t_lay], writes=[t_rs], out=rs_[:, 3:4], in0=rs_[:, 3:4], in1=lay[:, 0:1], op=ALU.mult)
                op("dve", "tensor_scalar", reads=[t_pO, t_rs], writes=[t_yv], out=yv, in0=pO[:, 0:128], scalar1=rs_[:, 2:3], scalar2=None, op0=ALU.mult)
                op("dve", "scalar_tensor_tensor", reads=[t_pO, t_rs], writes=[t_yv], out=yv, in0=pO[:, 128:256], scalar=rs_[:, 3:4], in1=yv,
                   op0=ALU.mult, op1=ALU.add)
                op("dve", "tensor_tensor", reads=[t_yv], writes=[t_junk], out=junk, in0=yv, in1=yv, op=ALU.mult)
                op("dve", "reduce_sum", reads=[t_junk], writes=[t_rs], out=rs_[:, 4:5], in_=junk, axis=AX.X)
                op("act", "activation", writes=[t_rs], out=rs_[:, 5:6], in_=rs_[:, 4:5], func=AF.Ln, bias=eps_c, scale=1.0 / 128)
                op("act", "activation", writes=[t_rs], out=rs_[:, 5:6], in_=rs_[:, 5:6], func=AF.Exp, scale=-0.5)
                op("dve", "scalar_tensor_tensor", reads=[t_yv, t_rs, t_subgs], writes=[t_ytm], out=ytm[:, h * 128:(h + 1) * 128], in0=yv,
                   scalar=rs_[:, 5:6], in1=subgs, op0=ALU.mult, op1=ALU.mult)
                if h == 3:
                    ptb, t_ptb = psT.next()
                    ptv = ptb.bitcast(BF16)
                    k.group("pe", [tr(ptv[:, hh * 128:(hh + 1) * 128], ytm[:, hh * 128:(hh + 1) * 128], ident_b) for hh in range(4)],
                            reads=[t_ytm, t_identb], writes=[t_ptb])
                    op("dve", "tensor_copy", reads=[t_ptb], writes=[t_yda], out=y_da[:, :, qb * 128:(qb + 1) * 128],
                       in_=ptv[:, 0:512].rearrange("p (a b) -> p a b", b=128))


            def v3(ap, inner):
                return ap.rearrange("p (h j) -> p h j", j=inner)


            def chunk_prep(c, BS):
                ktok, t_ktok = BS["ktok"]; vtok, t_vtok = BS["vtok"]
                R1, t_R1 = BS["R1"]; R2, t_R2 = BS["R2"]
                Dm, t_D = BS["Dm"]; DTm, t_DT = BS["DTm"]; Ds, t_Ds = Dm, t_D
                egs, t_egs = BS["egs"]; glb, t_glb = BS["glb"]; egrow, t_egrow = BS["egrow"]
                Pb = BS["Pb"]; Bb = BS["Bb"]; Nb = BS["Nb"]
                rhs_u, t_ru = vtok, t_vtok; rhs_w, t_rw_ = BS["rhs_w"]; kd, t_kd = ktok, t_ktok
                u_sb, t_u = BS["u_sb"]; wT, t_wT = BS["wT"]; qkT, t_qk = BS["qkT"]
                qgT, t_qg = BS["qgT"]; vnew, t_vnew = BS["vnew"]
                cs = slice(c * 64, (c + 1) * 64)
                gr = gb[:, 0, c, :]
                be = gb[:, 1, c, :]
                nbe = gb[:, 2, c, :]
                pk, t_pk = psDN.next()
                pkb = pk.bitcast(BF16)
                k.group("pe", [tr(pkb[0:64, h * 128:(h + 1) * 128], kn[:, h, cs], ident_b) for h in range(4)], reads=[t_kn, t_identb], writes=[t_pk])
                op("act", "copy", reads=[t_pk], writes=[t_ktok], out=ktok, in_=pkb[0:64, 0:512])
                yield
                pv, t_pv = psDN.next()
                pvb = pv.bitcast(BF16)
                k.group("pe", [tr(pvb[0:64, h * 128:(h + 1) * 128], vn[:, h, cs], ident_b) for h in range(4)], reads=[t_vn, t_identb], writes=[t_pv])
                op("dve", "tensor_copy", reads=[t_pv], writes=[t_vtok], out=vtok, in_=pvb[0:64, 0:512])
                yield
                op("dve", "tensor_tensor", reads=[t_gb, t_consts], writes=[t_R1], out=v3(R1, 64), in0=gr.unsqueeze(2).to_broadcast([64, 4, 64]),
                   in1=tt_c.unsqueeze(1).to_broadcast([64, 4, 64]), op=ALU.mult)
                yield
                op("dve", "tensor_copy", reads=[t_gb], writes=[t_R2], out=v3(R2, 64), in_=gr.unsqueeze(2).to_broadcast([64, 4, 64]))
                yield
                pg, t_pg = psDN.next()
                k.group("pe", [mm(pg[0:64, 0:256], tt_c, R2, start=True, stop=False),
                               mm(pg[0:64, 0:256], nones_f[0:64, 0:64], R1, start=False, stop=False),
                               mm(pg[0:64, 0:256], id64, negu, start=False, stop=True)],
                        reads=[t_R1, t_R2, t_consts, t_nones], writes=[t_pg])
                op("act", "activation", reads=[t_pg], writes=[t_D], out=Dm, in_=pg[0:64, 0:256], func=AF.Exp)
                yield
                pgt, t_pgt = psDN.next()
                k.group("pe", [mm(pgt[0:64, 0:256], ones_f[0:64, 0:64], R1, start=True, stop=False),
                               mm(pgt[0:64, 0:256], ntt_f, R2, start=False, stop=False),
                               mm(pgt[0:64, 0:256], id64, negl, start=False, stop=True)],
                        reads=[t_R1, t_R2, t_consts, t_ones, t_ntt], writes=[t_pgt])
                op("act", "activation", reads=[t_pgt], writes=[t_DT], out=DTm, in_=pgt[0:64, 0:256], func=AF.Exp)
                yield
                op("dve", "tensor_tensor", reads=[t_consts], writes=[t_Ds], out=Ds, in0=Dm, in1=mstr, op=ALU.mult)
                yield
                px, t_px = psDN.next()
                k.group("pe", [mm(px[:, 0:256], ones_f[0:64, :], R1), mm(px[:, 256:260], ones_f[0:64, :], gr),
                               mm(px[0:64, 264:268], tt_c, gr), mm(px[0:64, 268:272], su_c, gr)],
                        reads=[t_R1, t_gb, t_ones, t_consts], writes=[t_px])
                op("act", "activation", reads=[t_px], writes=[t_egrow], out=egrow, in_=px[:, 0:256], func=AF.Exp)
                op("act", "activation", reads=[t_px], writes=[t_glb], out=glb, in_=px[:, 256:260], func=AF.Exp)
                op("act", "activation", reads=[t_px], writes=[t_egs], out=egs[:, 0:8], in_=px[0:64, 264:272], func=AF.Exp)
                yield
                op("dve", "tensor_tensor", reads=[t_gb], writes=[t_egs], out=egs[:, 8:12], in0=egs[:, 0:4], in1=be, op=ALU.mult)
                yield
                pkk, t_pkk = psDN.next()
                k.group("pe", [mm(pkk[0:64, h * 64:(h + 1) * 64], kn[:, h, cs], kn[:, h, cs]) for h in range(4)], reads=[t_kn], writes=[t_pkk])
                P_, t_P_ = Pb[0]
                for h in range(4):
                    op("dve", "scalar_tensor_tensor", reads=[t_pkk, t_gb, t_Ds], writes=[t_P_], out=P_[:, h * 64:(h + 1) * 64],
                       in0=pkk[0:64, h * 64:(h + 1) * 64], scalar=nbe[:, h:h + 1], in1=Ds[:, h * 64:(h + 1) * 64], op0=ALU.mult, op1=ALU.mult)
                pt_, t_pt_ = psDN.next()
                pt_ = pt_.bitcast(BF16)
                k.group("pe", [tr(pt_[0:64, h * 64:(h + 1) * 64], P_[:, h * 64:(h + 1) * 64], ident_b[0:64, 0:64]) for h in range(4)],
                        reads=[t_P_, t_identb], writes=[t_pt_])
                B_, t_B_ = Bb[0]
                N_, t_N_ = Nb[0]
                op("act", "copy", reads=[t_pt_], writes=[t_B_], out=B_, in_=pt_[0:64, 0:256])
                op("dve", "tensor_tensor", reads=[t_pt_, t_consts], writes=[t_N_], out=N_, in0=pt_[0:64, 0:256], in1=i4, op=ALU.add)
                yield
                for j in range(1, 6):
                    Pn, t_Pn = Pb[j % 2]
                    Bn, t_Bn = Bb[j % 2]
                    Nn, t_Nn = Nb[j % 2]
                    pp, t_pp = psDN.next()
                    k.group("pe", [mm(pp[0:64, h * 64:(h + 1) * 64], B_[:, h * 64:(h + 1) * 64], P_[:, h * 64:(h + 1) * 64]) for h in range(4)],
                            reads=[t_B_, t_P_], writes=[t_pp])
                    op("act", "copy", reads=[t_pp], writes=[t_Pn], out=Pn, in_=pp[0:64, 0:256])
                    if j < 5:
                        pbb, t_pbb = psDN.next()
                        k.group("pe", [mm(pbb[0:64, h * 64:(h + 1) * 64], P_[:, h * 64:(h + 1) * 64], B_[:, h * 64:(h + 1) * 64]) for h in range(4)],
                                reads=[t_B_, t_P_], writes=[t_pbb])
                        op("dve", "tensor_copy", reads=[t_pbb], writes=[t_Bn], out=Bn, in_=pbb[0:64, 0:256])
                    pn2, t_pn2 = psDN.next()
                    k.group("pe", [mm(pn2[0:64, h * 64:(h + 1) * 64], Pn[:, h * 64:(h + 1) * 64], N_[:, h * 64:(h + 1) * 64]) for h in range(4)],
                            reads=[t_Pn, t_N_], writes=[t_pn2])
                    op("dve", "tensor_tensor", reads=[t_pn2, t_N_], writes=[t_Nn], out=Nn, in0=pn2[0:64, 0:256], in1=N_, op=ALU.add)
                    P_, t_P_, B_, t_B_, N_, t_N_ = Pn, t_Pn, Bn, t_Bn, Nn, t_Nn
                op("dve", "tensor_tensor", reads=[t_vtok, t_gb], writes=[t_ru], out=v3(rhs_u, 128), in0=v3(vtok, 128),
                   in1=be.unsqueeze(2).to_broadcast([64, 4, 128]), op=ALU.mult)
                yield
                op("dve", "tensor_tensor", reads=[t_ktok, t_egs], writes=[t_rw_], out=v3(rhs_w, 128), in0=v3(ktok, 128),
                   in1=egs[:, 8:12].unsqueeze(2).to_broadcast([64, 4, 128]), op=ALU.mult)
                yield
                op("dve", "tensor_tensor", reads=[t_ktok, t_egs], writes=[t_kd], out=v3(kd, 128), in0=v3(ktok, 128),
                   in1=egs[:, 4:8].unsqueeze(2).to_broadcast([64, 4, 128]), op=ALU.mult)
                yield
                pu, t_pu = psDN.next()
                k.group("pe", [mm(pu[0:64, h * 128:(h + 1) * 128], N_[:, h * 64:(h + 1) * 64], rhs_u[:, h * 128:(h + 1) * 128]) for h in range(4)],
                        reads=[t_N_, t_ru], writes=[t_pu])
                op("act", "copy", reads=[t_pu], writes=[t_u], out=u_sb, in_=pu[0:64, :])
                yield
                pw, t_pw = psDN.next()
                k.group("pe", [mm(pw[:, h * 64:(h + 1) * 64], rhs_w[:, h * 128:(h + 1) * 128], N_[:, h * 64:(h + 1) * 64]) for h in range(4)],
                        reads=[t_N_, t_rw_], writes=[t_pw])
                op("dve", "tensor_copy", reads=[t_pw], writes=[t_wT], out=wT, in_=pw[:, 0:256])
                yield
                pq, t_pq = psDN.next()
                k.group("pe", [mm(pq[0:64, h * 64:(h + 1) * 64], kn[:, h, cs], qn[:, h, cs]) for h in range(4)], reads=[t_kn, t_qn], writes=[t_pq])
                op("dve", "tensor_tensor", reads=[t_pq, t_DT], writes=[t_qk], out=qkT, in0=pq[0:64, 0:256], in1=DTm, op=ALU.mult)
                yield
                op("dve", "tensor_tensor", reads=[t_qn, t_egrow], writes=[t_qg], out=v3(qgT, 64), in0=qn[:, :, cs], in1=v3(egrow, 64), op=ALU.mult)
                yield

            def chunk_recur(c, BS):
                ktok, t_ktok = BS["ktok"]; vtok, t_vtok = BS["vtok"]
                R1, t_R1 = BS["R1"]; R2, t_R2 = BS["R2"]
                Dm, t_D = BS["Dm"]; DTm, t_DT = BS["DTm"]; Ds, t_Ds = Dm, t_D
                egs, t_egs = BS["egs"]; glb, t_glb = BS["glb"]; egrow, t_egrow = BS["egrow"]
                Pb = BS["Pb"]; Bb = BS["Bb"]; Nb = BS["Nb"]
                rhs_u, t_ru = vtok, t_vtok; rhs_w, t_rw_ = BS["rhs_w"]; kd, t_kd = ktok, t_ktok
                u_sb, t_u = BS["u_sb"]; wT, t_wT = BS["wT"]; qkT, t_qk = BS["qkT"]
                qgT, t_qg = BS["qgT"]; vnew, t_vnew = BS["vnew"]
                cs = slice(c * 64, (c + 1) * 64)
                gr = gb[:, 0, c, :]
                be = gb[:, 1, c, :]
                nbe = gb[:, 2, c, :]
                pws, t_pws = psDN.next()
                k.group("pe", [mm(pws[0:64, h * 128:(h + 1) * 128], wT[:, h * 64:(h + 1) * 64], Sb[:, h, :]) for h in range(4)],
                        reads=[t_wT, t_Sb], writes=[t_pws])
                op("dve", "tensor_tensor", reads=[t_u, t_pws], writes=[t_vnew], out=vnew, in0=u_sb, in1=pws[0:64, :], op=ALU.subtract)
                po, t_po = psDN.next()
                fns = []
                for h in range(4):
                    fns.append(mm(po[:, h * 64:(h + 1) * 64], Sb[:, h, :], qgT[:, h * 64:(h + 1) * 64], start=True, stop=False))
                    fns.append(mm(po[:, h * 64:(h + 1) * 64], vnew[:, h * 128:(h + 1) * 128], qkT[:, h * 64:(h + 1) * 64], start=False, stop=True))
                k.group("pe", fns, reads=[t_Sb, t_qg, t_vnew, t_qk], writes=[t_po])
                op("act", "copy", reads=[t_po], writes=[t_oT], out=oT[:, :, cs], in_=v3(po[:, 0:256], 64))
                pds, t_pds = psDN.next()
                k.group("pe", [mm(pds[:, h * 128:(h + 1) * 128], kd[:, h * 128:(h + 1) * 128], vnew[:, h * 128:(h + 1) * 128]) for h in range(4)],
                        reads=[t_kd, t_vnew], writes=[t_pds])
                for h in range(4):
                    op("dve", "scalar_tensor_tensor", reads=[t_pds, t_glb], writes=[t_S], out=Sst[:, h, :], in0=Sst[:, h, :], scalar=glb[:, h:h + 1],
                       in1=pds[:, h * 128:(h + 1) * 128], op0=ALU.mult, op1=ALU.add)
                op("act", "copy", reads=[t_S], writes=[t_Sb], out=Sb, in_=Sst)


            def att_gen():
                for ui in range(len(units) + 1):
                    if ui < len(units):
                        stageA(ui)
                        yield
                    if ui >= 1:
                        stageB(ui - 1)
                        yield

            def dn_gen():
                for c0_ in range(0, 8, NW):
                    gens = [chunk_prep(c0_ + i, BSs[i]) for i in range(NW)]
                    alive = list(gens)
                    while alive:
                        for g_ in list(alive):
                            try:
                                next(g_)
                            except StopIteration:
                                alive.remove(g_)
                            yield
                    for i in range(NW):
                        chunk_recur(c0_ + i, BSs[i])
                        yield

            RATIO = cfg.get("dn_ratio", 6)
            streams = [[att_gen(), 1], [dn_gen(), RATIO]]
            while streams:
                for st_ in list(streams):
                    for _ in range(st_[1]):
                        try:
                            next(st_[0])
                        except StopIteration:
                            streams.remove(st_)
                            break

            if l == 0 and t == 0 and ACUT >= 8:
                tap("yda", y_da, t_yda)
            if l == 0 and t == 0:
                tap("oT", oT, t_oT)
            for h in range(4):
                sqb, t_sqb = tmpb.next()
                op("act", "activation", reads=[t_oT], writes=[t_sqb], out=sqb, in_=oT[:, h, :], func=AF.Square)
                pn, t_pn = ps8.next()
                k.group("pe", [mm(pn, ones_b, sqb)], reads=[t_sqb, t_onesb], writes=[t_pn])
                rn, t_rn = tmps.next()
                op("act", "activation", reads=[t_pn], writes=[t_rn], out=rn, in_=pn, func=AF.Sqrt, bias=eps_c, scale=1.0 / 128)
                op("dve", "reciprocal", writes=[t_rn], out=rn, in_=rn)
                op("dve", "scalar_tensor_tensor", reads=[t_oT, t_spl], writes=[t_rn], out=rn, in0=oT[:, h, :], scalar=spl("dng"), in1=rn,
                   op0=ALU.mult, op1=ALU.mult)
                op("dve", "tensor_tensor", reads=[t_rn, t_zs], writes=[t_ydn], out=y_dn[:, h, :], in0=rn, in1=zs[:, h, :], op=ALU.mult)
            if l == 0 and t == 0:
                tap("ydn", y_dn, t_ydn)
            if STAGE <= 4:
                continue
            barrier()
            ar["off"] = SUB0
            merged, t_mg = aa("merged", [128, KC, TT], BF16)
            sgs = Pool_([aa("sg%d" % i, [128, TT]) for i in range(3)])
            macc, t_macc = aa("macc", [128, TT])
            xins = Pool_([aa("xin%d" % i, [128, TT]) for i in range(3)])
            ybr = [(y_gm, t_ygm), (y_da, t_yda), (y_dn, t_ydn)]
            for d in range(KC):
                for br in range(3):
                    wgt, t_wgt = load_w([(wrows(w_in_d[l])[:, :, COL_G + br * 1024 + d * 128:COL_G + br * 1024 + (d + 1) * 128], KC)], KC, 128)
                    pg, t_pg = ps8.next()
                    k.group("pe", [mm(pg, wgt[:, kc, :], hT[:, kc, :], start=(kc == 0), stop=(kc == KC - 1)) for kc in range(KC)],
                            reads=[t_wgt, t_h], writes=[t_pg])
                    sg, t_sg = sgs.next()
                    op("act", "activation", reads=[t_pg], writes=[t_sg], out=sg, in_=pg, func=AF.Sigmoid)
                    pp, t_pp = ps8.next()
                    yb, t_yb = ybr[br]
                    wbr, t_wbr = load_w([(wrows(w_br_d[br][l])[:, :, d * 128:(d + 1) * 128], 4)], 4, 128)
                    k.group("pe", [mm(pp, wbr[:, kc, :], yb[:, kc, :], start=(kc == 0), stop=(kc == 3)) for kc in range(4)],
                            reads=[t_wbr, t_yb], writes=[t_pp])
                    if br == 0:
                        op("dve", "tensor_tensor", reads=[t_pp, t_sg], writes=[t_macc], out=macc, in0=pp, in1=sg, op=ALU.mult)
                    else:
                        op("dve", "tensor_tensor", reads=[t_pp], writes=[t_sg], out=sg, in0=pp, in1=sg, op=ALU.mult)
                        if br == 1:
                            op("dve", "tensor_tensor", reads=[t_sg], writes=[t_macc], out=macc, in0=macc, in1=sg, op=ALU.add)
                        else:
                            op("dve", "tensor_tensor", reads=[t_sg, t_macc], writes=[t_mg], out=merged[:, d, :], in0=macc, in1=sg, op=ALU.add)
            if l == 0 and t == 0:
                tap("merged", merged, t_mg)
            for g4 in range(4):
                wo, t_wo = load_w([(wrows(w_out_d[l])[:, :, g4 * 256:(g4 + 1) * 256], KC)], KC, 256)
                for j in range(2):
                    e_ = g4 * 2 + j
                    po, t_po = ps8.next()
                    k.group("pe", [mm(po, wo[:, dc, j * 128:(j + 1) * 128], merged[:, dc, :], start=(dc == 0), stop=(dc == KC - 1))
                                   for dc in range(KC)], reads=[t_wo, t_mg], writes=[t_po])
                    xin, t_xin = xins.next()
                    if l == 0:
                        k.dma("sp", xin, xT_d[b, e_ * 128:(e_ + 1) * 128, c0:c0 + TT], writes=[t_xin])
                    else:
                        k.dma("sp", xin, xs_d[:, e_, c0:c0 + TT], reads=[t_xs[t][e_]], writes=[t_xin])
                    op("dve", "scalar_tensor_tensor", reads=[t_po, t_mods], writes=[t_xin], out=xin, in0=po,
                       scalar=mods[:, l, b, GATE1, e_:e_ + 1], in1=xin, op0=ALU.mult, op1=ALU.add)
                    k.dma("sp", xs_d[:, e_, c0:c0 + TT], xin, reads=[t_xin], writes=[t_xs[t][e_]])
            barrier()


    def ffn(l, b, last):
        barrier()
        ar["off"] = 0
        xT, t_x = aa("xT", [128, KC, S])
        rb, t_rb = aa("rb", [128, TT])
        sq_b, t_sq = aa("sq_b", [128, KC, TT], BF16)
        tmps = Pool_([aa("tmpA%d" % i, [128, TT]) for i in range(3)])
        FMARK = ar["off"]
        h2, t_h2 = aa("h2", [128, KC, S], BF16)
        moe = (l % 2 == 1)
        for t in range(NTILE):
            k.dma("sp", xT[:, :, t * TT:(t + 1) * TT], xs_d[:, :, t * TT:(t + 1) * TT], reads=t_xs[t], writes=[t_x], group=(t > 0))
        if moe:
            h32, t_h32 = aa("h32", [128, KC, TT])
            WT, t_WT = aa("WT", [8, S])
            rhe, t_rhe = aa("rhe", [8, TT])
            Wb0, t_Wb0 = aa("Wbc0", [128, S], BF16)
            Wb1, t_Wb1 = aa("Wbc1", [128, S], BF16)
            Wbcs = [Wb0, Wb1]; t_Wbcs = [t_Wb0, t_Wb1]
            lg, t_lg = aa("lg", [128, 64])
            wr3 = spg[:, 8:72].rearrange("p (c e) -> p c e", e=8)
        for t in range(NTILE):
            modnorm(xT, t_x, t * TT, l, b, G2S, SHIFT2, h2[:, :, t * TT:(t + 1) * TT], t_h2, sq_b, t_sq, rb, t_rb, tmps,
                    h32=((h32, t_h32) if moe else None))
            if moe:
                for blk in range(4):
                    gblk = t * 4 + blk
                    pl, t_pl = ps8.next()
                    k.group("pe", [mm(pl[:, 0:8], h32[:, kc, blk * 128:(blk + 1) * 128], wr3[:, kc, :], start=(kc == 0), stop=(kc == KC - 1))
                                   for kc in range(KC)], reads=[t_h32, t_spg], writes=[t_pl])
                    L = lg[:, 0:8]; E1 = lg[:, 8:16]; M2 = lg[:, 16:24]; E2 = lg[:, 24:32]; Wt = lg[:, 32:40]
                    m1 = lg[:, 40:41]; m2 = lg[:, 41:42]; dd = lg[:, 42:43]; w1 = lg[:, 43:44]; w2 = lg[:, 44:45]
                    op("dve", "tensor_copy", reads=[t_pl], writes=[t_lg], out=L, in_=pl[:, 0:8])
                    op("dve", "reduce_max", writes=[t_lg], out=m1, in_=L, axis=AX.X)
                    op("dve", "tensor_scalar", writes=[t_lg], out=E1, in0=L, scalar1=m1, scalar2=None, op0=ALU.is_equal)
                    op("dve", "scalar_tensor_tensor", writes=[t_lg], out=M2, in0=E1, scalar=-1e30, in1=L, op0=ALU.mult, op1=ALU.add)
                    op("dve", "reduce_max", writes=[t_lg], out=m2, in_=M2, axis=AX.X)
                    op("dve", "tensor_scalar", writes=[t_lg], out=E2, in0=M2, scalar1=m2, scalar2=None, op0=ALU.is_equal)
                    op("dve", "tensor_tensor", writes=[t_lg], out=dd, in0=m2, in1=m1, op=ALU.subtract)
                    op("act", "activation", writes=[t_lg], out=dd, in_=dd, func=AF.Exp)
                    op("dve", "tensor_scalar", writes=[t_lg], out=w1, in0=dd, scalar1=1.0, scalar2=None, op0=ALU.add)
                    op("dve", "reciprocal", writes=[t_lg], out=w1, in_=w1)
                    op("dve", "tensor_tensor", writes=[t_lg], out=w2, in0=dd, in1=w1, op=ALU.mult)
                    op("dve", "tensor_scalar", writes=[t_lg], out=Wt, in0=E1, scalar1=w1, scalar2=None, op0=ALU.mult)
                    op("dve", "scalar_tensor_tensor", writes=[t_lg], out=Wt, in0=E2, scalar=w2, in1=Wt, op0=ALU.mult, op1=ALU.add)
                    pt_, t_pt_ = ps8.next()
                    k.group("pe", [tr(pt_[0:8, 0:128], Wt, ident)], reads=[t_lg, t_consts], writes=[t_pt_])
                    op("act", "copy", reads=[t_pt_], writes=[t_WT], out=WT[:, gblk * 128:(gblk + 1) * 128], in_=pt_[0:8, 0:128])
        if moe and b == 0:
            tap("WT", WT, t_WT)
        if b == 0:
            tap("h2_%d" % l, h2, t_h2)

        hids = Pool_([aa("hid%d" % i, [128, 2, TT], BF16) for i in range(2)])
        pend = {"f": None}

        def expert_pass(wg_d, wu_d, wd_d, F, scale_by=None):
            for fg in range(F // 256):
                wgb, t_wgb = load_w([(wrows(wg_d)[:, :, fg * 256:(fg + 1) * 256], KC)], KC, 256)
                wub, t_wub = load_w([(wrows(wu_d)[:, :, fg * 256:(fg + 1) * 256], KC)], KC, 256)
                wdb, t_wdb = load_w([(wrows(wd_d[fg * 256:(fg + 1) * 256, :]), 2)], 2, D)
                for t in range(NTILE):
                    tc_ = slice(t * TT, (t + 1) * TT)
                    hid, t_hid = hids.next()
                    pg_ = pend["f"]

                    def adv():
                        if pend["f"] is not None:
                            try:
                                next(pend["f"])
                            except StopIteration:
                                pend["f"] = None
                    for j in range(2):
                        pa, t_pa = psF.next()
                        k.group("pe", [mm(pa, wgb[:, kc, j * 128:(j + 1) * 128], h2[:, kc, tc_], start=(kc == 0), stop=(kc == KC - 1))
                                       for kc in range(KC)], reads=[t_wgb, t_h2], writes=[t_pa])
                        adv()
                        pu_, t_pu_ = psF.next()
                        k.group("pe", [mm(pu_, wub[:, kc, j * 128:(j + 1) * 128], h2[:, kc, tc_], start=(kc == 0), stop=(kc == KC - 1))
                                       for kc in range(KC)], reads=[t_wub, t_h2], writes=[t_pu_])
                        adv()
                        sa, t_sa = tmps.next()
                        op("act", "activation", reads=[t_pa], writes=[t_sa], out=sa, in_=pa, func=AF.Silu)
                        if scale_by is not None:
                            op("dve", "tensor_tensor", reads=[scale_by[1]], writes=[t_sa], out=sa, in0=sa, in1=scale_by[0][:, tc_], op=ALU.mult)
                        op("dve", "tensor_tensor", reads=[t_pu_, t_sa], writes=[t_hid], out=hid[:, j, :], in0=pu_, in1=sa, op=ALU.mult)
                    while pend["f"] is not None:
                        adv()

                    def down(wdb=wdb, t_wdb=t_wdb, hid=hid, t_hid=t_hid, tc_=tc_):
                        for d in range(KC):
                            pd, t_pd = psD.next()
                            k.group("pe", [mm(pd, wdb[:, j, d * 128:(d + 1) * 128], hid[:, j, :], start=(j == 0), stop=(j == 1)) for j in range(2)],
                                    reads=[t_wdb, t_hid], writes=[t_pd])
                            op("dve", "scalar_tensor_tensor", reads=[t_pd, t_mods], writes=[t_x], out=xT[:, d, tc_], in0=pd,
                               scalar=mods[:, l, b, GATE2, d:d + 1], in1=xT[:, d, tc_], op0=ALU.mult, op1=ALU.add)
                            if d % 2 == 1:
                                yield
                    pend["f"] = down()

        def flush():
            while pend["f"] is not None:
                try:
                    next(pend["f"])
                except StopIteration:
                    pend["f"] = None

        if not moe:
            expert_pass(ffn_g_d[0], ffn_u_d[0], ffn_d_d[0], FF_DENSE)
            flush()
        else:
            for e_ in range(cfg.get("nexp", NEXP)):
                for t in range(NTILE):
                    op("dve", "tensor_scalar", reads=[t_WT, t_consts], writes=[t_rhe], out=rhe, in0=WT[:, t * TT:(t + 1) * TT],
                       scalar1=consts[0:8, CO["ident"][0] + e_:CO["ident"][0] + e_ + 1], scalar2=None, op0=ALU.mult)
                    pw, t_pw = ps8.next()
                    k.group("pe", [mm(pw, ones_f[0:8, :], rhe)], reads=[t_rhe, t_ones], writes=[t_pw])
                    op("act", "copy", reads=[t_pw], writes=[t_Wbcs[e_ % 2]], out=Wbcs[e_ % 2][:, t * TT:(t + 1) * TT], in_=pw)
                expert_pass(moe_g_d[0, e_], moe_u_d[0, e_], moe_d_d[0, e_], FF_EXPERT, scale_by=(Wbcs[e_ % 2], t_Wbcs[e_ % 2]))
            flush()
        if b == 0:
            tap("xffn_%d" % l, xT, t_x)
        if not last:
            for t in range(NTILE):
                for c in range(KC):
                    k.dma("sp", xs_d[:, c, t * TT:(t + 1) * TT], xT[:, c, t * TT:(t + 1) * TT], reads=[t_x], writes=[t_xs[t][c]])
            return
        barrier()
        ar["off"] = FMARK
        xn, t_xn = aa("xn", [128, KC, TT])
        otm = Pool_([aa("otm%d" % i, [128, D]) for i in range(2)])
        for t in range(NTILE):
            for c in range(KC):
                op("act", "activation", reads=[t_x], writes=[t_sq], out=sq_b[:, c, :], in_=xT[:, c, t * TT:(t + 1) * TT], func=AF.Square)
            pb, t_pb = ps8.next()
            k.group("pe", [mm(pb, ones_b, sq_b[:, c, :], start=(c == 0), stop=(c == KC - 1)) for c in range(KC)],
                    reads=[t_sq, t_onesb], writes=[t_pb])
            op("act", "activation", reads=[t_pb], writes=[t_rb], out=rb, in_=pb, func=AF.Sqrt, bias=eps_c, scale=1.0 / D)
            op("dve", "reciprocal", writes=[t_rb], out=rb, in_=rb)
            for c in range(KC):
                op("dve", "scalar_tensor_tensor", reads=[t_x, t_spg, t_rb], writes=[t_xn], out=xn[:, c, :], in0=xT[:, c, t * TT:(t + 1) * TT],
                   scalar=spg[:, c:c + 1], in1=rb, op0=ALU.mult, op1=ALU.mult)
            for blk in range(4):
                ot, t_ot = otm.next()
                for half in range(2):
                    pt_, t_pt_ = ps8.next()
                    k.group("pe", [tr(pt_[:, i * 128:(i + 1) * 128], xn[:, half * 4 + i, blk * 128:(blk + 1) * 128], ident) for i in range(4)],
                            reads=[t_xn, t_consts], writes=[t_pt_])
                    if half == 0:
                        op("act", "copy", reads=[t_pt_], writes=[t_ot], out=ot[:, 0:512], in_=pt_)
                    else:
                        op("dve", "tensor_copy", reads=[t_pt_], writes=[t_ot], out=ot[:, 512:1024], in_=pt_)
                r0 = t * TT + blk * 128
                k.dma("sp", out_d[b, r0:r0 + 128, :], ot, reads=[t_ot], writes=[out_tok])

    for b in range(NSEQ):
        for l in range(LAYERS):
            layer_setup(l)
            mixer(l, b)
            if STAGE >= 6:
                ffn(l, b, last=(l == LAYERS - 1))

    fin = [out_tok] + list(tapd.values())
    if STAGE < 99:
        z, t_z = sbt("zz", [128, 8])
        op("dve", "memset", writes=[t_z], ap=z, constant=0.0)
        k.dma("sp", out_d[0, 0:128, 0:8], z, reads=[t_z], writes=[out_tok])
    k.finish(fin)
    nc._k = k
    return nc


def prep_core_inputs(inp, core):
    x = np.asarray(inp["x"], np.float32)
    b0 = 2 * core
    xT = np.ascontiguousarray(x[b0:b0 + 2].transpose(0, 2, 1))
    pos = np.asarray(inp["positions"], np.int32)[b0:b0 + 2]
    posb = np.ascontiguousarray(np.broadcast_to(pos[:, None, :], (2, 128, S)))
    c = np.asarray(inp["c"], np.float32)[b0:b0 + 2]
    cT = np.ascontiguousarray(c.reshape(2, KC, 128).transpose(2, 1, 0)).reshape(128, KC * 2)
    return {"xT": xT, "posb": posb, "cT": cT}


def shared_inputs(inp):
    ws = np.asarray(inp["gm_w_s"], np.float32)
    wst = np.ascontiguousarray(ws.transpose(3, 0, 1, 2)).reshape(128, DEPTH * 4 * 128)
    sh = {"smallp": make_smallp(inp), "consts": make_consts(), "wst": wst}
    for n in ["w_mod", "w_in", "w_br_gm", "w_br_da", "w_br_dn", "w_out", "ffn_w_gate", "ffn_w_up",
              "ffn_w_down", "moe_w_gate", "moe_w_up", "moe_w_down"]:
        sh[n] = np.ascontiguousarray(np.asarray(inp[n], np.float32))
    return sh


def kernel(**inputs):
    nc = build()
    sh = shared_inputs(inputs)
    in_maps = []
    for core in range(8):
        m = dict(sh)
        m.update(prep_core_inputs(inputs, core))
        in_maps.append(m)
    res = run_bass_kernel_spmd(nc, in_maps, core_ids=list(range(8)))
    return np.concatenate([np.asarray(r["out"], np.float32) for r in res.results], axis=0)
```

```python
import math
import numpy as np
import concourse.bass as bass
import concourse.mybir as mybir
from concourse.bass_utils import run_bass_kernel_spmd

F32 = mybir.dt.float32
BF16 = mybir.dt.bfloat16
I32 = mybir.dt.int32
AF = mybir.ActivationFunctionType
ALU = mybir.AluOpType
AX = mybir.AxisListType

SEM_LIMIT = 20000
ENGS = ("pe", "dve", "act", "pool", "sp")


class Tok:
    __slots__ = ("name", "w", "r", "dsem", "pr", "excl")

    def __init__(self, name, excl=False):
        self.name = name
        self.excl = excl
        self.w = None
        self.r = []
        self.dsem = None
        self.pr = []


class K:
    def __init__(self, nc, same=("dve", "act", "pool"), eager=True):
        self.nc = nc
        self.eager = eager
        self.embed = True
        self._collect = None
        self.e = {"pe": nc.tensor, "dve": nc.vector, "act": nc.scalar,
                  "pool": nc.gpsimd, "sp": nc.sync}
        self.nsem = 0
        self.sem = {}
        self.cnt = {}
        for n in ENGS:
            self._new_epoch(n)
        self.seq = {n: 0 for n in ENGS}
        self.last = {n: None for n in ENGS}
        self.sigmap = {n: [] for n in ENGS}
        self.waited = {n: {} for n in ENGS}
        self.same = set(same)
        self.rsem = {}
        self.store_recs = []
        self.ninstr = 0
        self.nwait = 0

    def _alloc(self, name):
        self.nsem += 1
        return self.nc.alloc_semaphore("%s_%d" % (name, self.nsem))

    def _new_epoch(self, n):
        self.sem[n] = self._alloc("e_" + n)
        self.cnt[n] = 0

    def _resolve(self, ev):
        if ev[0] == "c":
            _, eng, seq = ev
            lst = self.sigmap[eng]
            lo, hi = 0, len(lst)
            while lo < hi:
                mid = (lo + hi) // 2
                if lst[mid][0] >= seq:
                    hi = mid
                else:
                    lo = mid + 1
            if lo < len(lst):
                f = lst[lo]
                return f[1], f[2]
            assert self.seq[eng] >= seq and self.last[eng] is not None
            if self.cnt[eng] >= SEM_LIMIT:
                self._new_epoch(eng)
            self.cnt[eng] += 1
            self.last[eng].then_inc(self.sem[eng], 1)
            lst.append((self.seq[eng], self.sem[eng], self.cnt[eng]))
            return self.sem[eng], self.cnt[eng]
        rec = ev[1]
        return rec[0], rec[1]

    def _wait(self, eng, ev):
        if ev is None:
            return
        if ev[0] == "c" and ev[1] == eng and eng not in self.same:
            return
        sem, val = self._resolve(ev)
        w = self.waited[eng]
        if w.get(id(sem), 0) >= val:
            return
        w[id(sem)] = val
        if self._collect is not None:
            self._collect.append((sem, val))
            return
        self.e[eng].wait_ge(sem, val)
        self.nwait += 1

    def _pre(self, eng, reads, writes):
        for t in reads:
            self._wait(eng, t.w)
        for t in writes:
            self._wait(eng, t.w)
            for ev in t.r:
                self._wait(eng, ev)

    def _signal_last(self, eng):
        if self.cnt[eng] >= SEM_LIMIT:
            self._new_epoch(eng)
        self.cnt[eng] += 1
        self.last[eng].then_inc(self.sem[eng], 1)
        self.sigmap[eng].append((self.seq[eng], self.sem[eng], self.cnt[eng]))

    def _post(self, eng, reads, writes):
        if self.eager and (reads or writes):
            self._signal_last(eng)
        ev = ("c", eng, self.seq[eng])
        for t in reads:
            t.r.append(ev)
            if len(t.r) > 16:
                t.r = self._compact(t.r)
        for t in writes:
            t.w = ev
            t.r = []

    @staticmethod
    def _split(reads, writes):
        xr = [t for t in reads if t.excl]
        if xr:
            writes = list(writes) + [t for t in xr if t not in writes]
            reads = [t for t in reads if not t.excl]
        return reads, writes

    def _pre_embed(self, eng, reads, writes):
        if not self.embed:
            self._pre(eng, reads, writes)
            return None
        self._collect = []
        self._pre(eng, reads, writes)
        waits, self._collect = self._collect, None
        for sem, val in waits[:-1]:
            self.e[eng].wait_ge(sem, val)
            self.nwait += 1
        return waits[-1] if waits else None

    def op(self, eng, fn, reads=(), writes=()):
        reads, writes = self._split(reads, writes)
        w_ = self._pre_embed(eng, reads, writes)
        ins = fn(self.e[eng])
        if w_ is not None:
            ins._wait_ge(w_[0], w_[1])
        self.seq[eng] += 1
        self.last[eng] = ins
        self.ninstr += 1
        self._post(eng, reads, writes)
        return ins

    def group(self, eng, fns, reads=(), writes=()):
        reads, writes = self._split(reads, writes)
        w_ = self._pre_embed(eng, reads, writes)
        ins = None
        for i_, fn in enumerate(fns):
            ins = fn(self.e[eng])
            if i_ == 0 and w_ is not None:
                ins._wait_ge(w_[0], w_[1])
            self.seq[eng] += 1
            self.ninstr += 1
        self.last[eng] = ins
        self._post(eng, reads, writes)
        return ins

    def _compact(self, evs):
        best = {}
        other = []
        for ev in evs:
            if ev[0] == "c":
                k = ev[1]
                if k not in best or ev[2] >= best[k][2]:
                    best[k] = ev
            else:
                if not any(o[1] is ev[1] for o in other):
                    other.append(ev)
        return list(best.values()) + other

    def dma(self, q, out, in_, reads=(), writes=(), group=False, **kw):
        for t in reads:
            self._wait(q, t.w)
        for t in writes:
            if group:
                for ev in t.pr:
                    self._wait(q, ev)
            else:
                self._wait(q, t.w)
                for ev in t.r:
                    self._wait(q, ev)
                t.pr = ([t.w] if t.w is not None else []) + list(t.r)
        ins = self.e[q].dma_start(out=out, in_=in_, **kw)
        assert len(writes) <= 1
        if writes:
            t = writes[0]
            if t.dsem is None or t.dsem[1] >= SEM_LIMIT:
                t.dsem = [self._alloc("d_" + t.name), 0]
            rec = t.dsem
        else:
            if q not in self.rsem or self.rsem[q][1] >= SEM_LIMIT:
                self.rsem[q] = [self._alloc("r_" + q), 0]
            rec = self.rsem[q]
        rec[1] += 16
        ins.then_inc(rec[0], 16)
        ev = ("d", rec)
        if reads and not any(r is rec for r in self.store_recs):
            self.store_recs.append(rec)
        if writes:
            writes[0].w = ev
            writes[0].r = []
        for s in reads:
            s.r.append(ev)
            if len(s.r) > 16:
                s.r = self._compact(s.r)
        self.ninstr += 1
        return ins

    def finish(self, toks, eng="sp"):
        for t in toks:
            self._wait(eng, t.w)
            for ev in t.r:
                self._wait(eng, ev)


D = 1024
KC = 8
S = 2048
TT = 512
NT = S // TT
INW = 7688
DEPTH = 2
EPS = 1e-6
FF_DENSE = 2816
FF_EXPERT = 3584
NEXP = 8
PI = math.pi
C1 = 6.28125
C2 = 2.0 * math.pi - 6.28125
NEG = -30000.0

COL_U, COL_V, COL_Q, COL_KK, COL_VA = 0, 512, 1024, 1536, 2048
COL_DQ, COL_DK, COL_DV, COL_DZ, COL_AB, COL_G = 2560, 3072, 3584, 4096, 4608, 4616

SP_LAYOUT = [("n1g", 8), ("n2g", 8), ("bmod", 48), ("lng", 512), ("lnb", 512), ("bs", 512),
             ("lam", 256), ("subg", 128), ("convw", 48), ("alog", 4), ("dtb", 4), ("dng", 1)]
SP_OFF = {}
_o = 0
for _n, _w in SP_LAYOUT:
    SP_OFF[_n] = (_o, _w)
    _o += _w
SP_LAYER = _o
SP_FING = 2 * SP_LAYER
SP_WR = SP_FING + 8
SP_TOTAL = SP_WR + 64

CO = {}
_o = 0
for _n, _w in [("ident", 128), ("rot", 128), ("tt", 64), ("su", 64), ("negu", 256), ("negl", 256),
               ("mstrict", 256), ("i4", 256), ("invf", 1)]:
    CO[_n] = (_o, _w)
    _o += _w
CO_TOTAL = _o


def make_consts():
    c = np.zeros((128, CO_TOTAL), np.float32)
    o = CO["ident"][0]
    c[:, o:o + 128] = np.eye(128, dtype=np.float32)
    o = CO["rot"][0]
    for m in range(128):
        if (m % 64) < 32:
            c[m + 32, o + m] = -1.0
        else:
            c[m - 32, o + m] = 1.0
    p = np.arange(64)[:, None]
    j = np.arange(64)[None, :]
    o = CO["tt"][0]
    c[:64, o:o + 64] = (p <= j)
    o = CO["su"][0]
    c[:64, o:o + 64] = (p > j)
    for h in range(4):
        o = CO["negu"][0] + h * 64
        c[:64, o:o + 64] = np.where(j > p, NEG, 0.0)
        o = CO["negl"][0] + h * 64
        c[:64, o:o + 64] = np.where(j < p, NEG, 0.0)
        o = CO["mstrict"][0] + h * 64
        c[:64, o:o + 64] = (j < p)
        o = CO["i4"][0] + h * 64
        c[:64, o:o + 64] = (j == p)
    inv_freq = (10000.0 ** (-np.arange(0, 64, 2, dtype=np.float32) / np.float32(64))).astype(np.float32)
    c[:, CO["invf"][0]] = inv_freq[np.arange(128) % 32]
    return c


def fm(v):
    v = np.asarray(v, np.float32)
    return np.ascontiguousarray(v.reshape(-1, 128).T)


def bc(v):
    v = np.asarray(v, np.float32).reshape(1, -1)
    return np.broadcast_to(v, (128, v.shape[1]))


def make_smallp(inp):
    sp = np.zeros((128, SP_TOTAL), np.float32)
    for l in range(DEPTH):
        base = l * SP_LAYER

        def put(name, arr):
            o, w = SP_OFF[name]
            assert arr.shape == (128, w), (name, arr.shape, w)
            sp[:, base + o:base + o + w] = arr
        put("n1g", fm(inp["norm1_g"][l]))
        put("n2g", fm(inp["norm2_g"][l]))
        put("bmod", fm(inp["b_mod"][l]))
        put("lng", bc(inp["gm_ln_g"][l]))
        put("lnb", bc(inp["gm_ln_b"][l]))
        put("bs", bc(inp["gm_b_s"][l].reshape(-1)))
        put("lam", bc(inp["da_lambda"][l].reshape(-1)))
        put("subg", bc(inp["da_subln_g"][l]))
        cw = np.asarray(inp["dn_conv_w"][l], np.float32)
        put("convw", np.ascontiguousarray(cw.reshape(4, 12, 128).transpose(2, 1, 0)).reshape(128, 48))
        put("alog", bc(inp["dn_a_log"][l]))
        put("dtb", bc(inp["dn_dt_bias"][l]))
        put("dng", np.asarray(inp["dn_norm_g"][l], np.float32).reshape(128, 1))
    sp[:, SP_FING:SP_FING + 8] = fm(inp["final_g"])
    wr = np.asarray(inp["moe_w_router"][0], np.float32)
    sp[:, SP_WR:SP_WR + 64] = wr.reshape(8, 128, 8).transpose(1, 0, 2).reshape(128, 64)
    return sp


def lambda_init_fn(layer):
    return 0.8 - 0.6 * math.exp(-0.3 * layer)


class Pool_:
    def __init__(self, items):
        self.items = items
        self.i = 0

    def next(self):
        it = self.items[self.i % len(self.items)]
        self.i += 1
        return it


def build(cfg=None):
    cfg = dict(cfg or {})
    NSEQ = cfg.get("nseq", 2)
    NTILE = cfg.get("ntile", NT)
    LAYERS = cfg.get("layers", DEPTH)
    STAGE = cfg.get("stage", 99)
    taps = cfg.get("taps", ())
    ACUT = cfg.get("acut", 99)

    nc = bass.Bass("TRN2", target_bir_lowering=False)
    k = K(nc, same=tuple(cfg.get("same", ("dve", "act", "pool"))), eager=cfg.get("eager", True))
    k.embed = cfg.get("embed", True)
    k.trans = cfg.get("trans", True)

    def din(name, shape, dt=F32):
        return nc.dram_tensor(name, list(shape), dt, kind="ExternalInput").ap()

    xT_d = din("xT", [2, D, S])
    posb_d = din("posb", [2, 128, S], I32)
    cT_d = din("cT", [128, KC * 2])
    smallp_d = din("smallp", [128, SP_TOTAL])
    consts_d = din("consts", [128, CO_TOTAL])
    wst_d = din("wst", [128, DEPTH * 4 * 128])
    w_mod_d = din("w_mod", [DEPTH, D, 6 * D])
    w_in_d = din("w_in", [DEPTH, D, INW])
    w_br_d = [din("w_br_gm", [DEPTH, 512, D]), din("w_br_da", [DEPTH, 512, D]), din("w_br_dn", [DEPTH, 512, D])]
    w_out_d = din("w_out", [DEPTH, D, D])
    ffn_g_d = din("ffn_w_gate", [1, D, FF_DENSE])
    ffn_u_d = din("ffn_w_up", [1, D, FF_DENSE])
    ffn_d_d = din("ffn_w_down", [1, FF_DENSE, D])
    moe_g_d = din("moe_w_gate", [1, NEXP, D, FF_EXPERT])
    moe_u_d = din("moe_w_up", [1, NEXP, D, FF_EXPERT])
    moe_d_d = din("moe_w_down", [1, NEXP, FF_EXPERT, D])
    out_d = nc.dram_tensor("out", [2, S, D], F32, kind="ExternalOutput").ap()
    out_tok = Tok("out")
    xs_d = nc.dram_tensor("xs_scratch", [128, KC, S], F32, kind="Internal").ap()
    t_xs = [[Tok("xs%d_%d" % (i, e)) for e in range(KC)] for i in range(NT)]

    tapd = {}

    def sb(name, shape, dt=F32):
        return nc.alloc_sbuf_tensor("s_" + name, list(shape), dt).ap()

    def sbt(name, shape, dt=F32):
        return sb(name, shape, dt), Tok(name)

    def tap(name, ap, tok):
        if name not in taps or name in tapd:
            return
        d = nc.dram_tensor("tap_" + name, list(ap.shape), ap.dtype, kind="ExternalOutput").ap()
        t = Tok("tap_" + name)
        k.dma("sp", d, ap, reads=[tok], writes=[t])
        tapd[name] = t

    def op(eng, fn, reads=(), writes=(), **kw):
        return k.op(eng, lambda e: getattr(e, fn)(**kw), reads=reads, writes=writes)

    def mm(out, lhsT, rhs, start=True, stop=True):
        return lambda e: e.matmul(out, lhsT=lhsT, rhs=rhs, start=start, stop=stop)

    def tr(out, in_, idn):
        return lambda e: e.transpose(out, in_, idn)

    fence = {"gen": 0, "evs": []}
    LAZY = cfg.get("lazy_fence", True)

    def barrier():
        engs = ("pe", "dve", "act", "pool")
        evs = {n: ("c", n, k.seq[n]) for n in engs if k.last[n] is not None}
        if LAZY:
            fence["evs"] = list(evs.values()) + [("d", rec) for rec in k.store_recs]
            fence["gen"] += 1
            return
        for a in engs + ("sp",):
            for b_, ev in evs.items():
                if a != b_:
                    k._wait(a, ev)
            for rec in k.store_recs:
                k._wait(a, ("d", rec))

    consts, t_consts = sbt("consts", [128, CO_TOTAL])
    spl_t, t_spl = sbt("spl", [128, SP_LAYER])
    spg, t_spg = sbt("spg", [128, 72])
    k.dma("sp", consts, consts_d, writes=[t_consts])
    k.dma("sp", spg, smallp_d[:, SP_FING:SP_FING + 72], writes=[t_spg])

    def cst(name, rows=128):
        o, w = CO[name]
        return consts[0:rows, o:o + w]

    def spl(name):
        o, w = SP_OFF[name]
        return spl_t[:, o:o + w]

    ident = cst("ident")
    ones_f, t_ones = sbt("ones_f", [128, 128])
    ones_b, t_onesb = sbt("ones_b", [128, 128], BF16)
    nones_f, t_nones = sbt("nones_f", [128, 128])
    ident_b, t_identb = sbt("ident_b", [128, 128], BF16)
    rot_b, t_rotb = sbt("rot_b", [128, 128], BF16)
    ntt_f, t_ntt = sbt("ntt_f", [64, 64])
    op("dve", "memset", writes=[t_ones], ap=ones_f, constant=1.0)
    op("dve", "memset", writes=[t_onesb], ap=ones_b, constant=1.0)
    op("dve", "memset", writes=[t_nones], ap=nones_f, constant=-1.0)
    op("dve", "tensor_copy", reads=[t_consts], writes=[t_identb], out=ident_b, in_=ident)
    op("dve", "tensor_copy", reads=[t_consts], writes=[t_rotb], out=rot_b, in_=cst("rot"))
    op("dve", "tensor_scalar", reads=[t_consts], writes=[t_ntt], out=ntt_f, in0=cst("tt", 64), scalar1=-1.0,
       scalar2=None, op0=ALU.mult)
    TC = [t_consts, t_ones, t_onesb, t_nones, t_identb, t_rotb, t_ntt]

    psall = nc.alloc_psum_tensor("psall", [128, 8 * 512], F32).ap()
    banks = [(psall[:, i * 512:(i + 1) * 512], Tok("bank%d" % i, excl=True)) for i in range(8)]
    psA = Pool_(banks[0:4])
    psF = Pool_(banks[0:4])
    psD = Pool_(banks[4:8])
    psS = Pool_(banks[4:6])
    psO = Pool_(banks[0:2])
    psT = Pool_(banks[2:4])
    ps8 = Pool_(banks)
    psDN = Pool_(banks[6:8]) if not cfg.get("dn8") else ps8
    S_big = psall[:, 4 * 512:8 * 512]
    t_Sbig = [b_[1] for b_ in banks[4:8]]

    NSLOT = cfg.get("nslot", 7)
    wbf = Pool_([sbt("wbf%d" % i, [128, 2048], BF16) for i in range(NSLOT)])

    def load_w(srcs, a, b_):
        wb, t_wb = wbf.next()
        wbv = wb[:, 0:a * b_].rearrange("p (a b) -> p a b", b=b_)
        o = 0
        for i, (src, ai) in enumerate(srcs):
            k.dma("pool", wbv[:, o:o + ai, :], src, writes=[t_wb], group=(i > 0))
            o += ai
        assert o == a
        return wbv, t_wb

    def wrows(ap2d):
        return ap2d.rearrange("(kc p) n -> p kc n", p=128)

    cact, t_cact = sbt("cact", [128, KC * 2])
    k.dma("sp", cact, cT_d, writes=[t_cact])
    op("act", "activation", writes=[t_cact], out=cact, in_=cact, func=AF.Silu)
    cact_b, t_cactb = sbt("cact_b", [128, KC * 2], BF16)
    op("dve", "tensor_copy", reads=[t_cact], writes=[t_cactb], out=cact_b, in_=cact)
    cact3 = cact_b.rearrange("p (c b) -> p c b", b=2)
    modT, t_mod = sbt("modT", [128, DEPTH, 48, 2])
    mods, t_mods = sbt("mods", [128, DEPTH, 2, 6, 8])
    bmod_t, t_bmod = sbt("bmod_t", [128, 64])
    for l in range(LAYERS):
        k.dma("sp", bmod_t, smallp_d[:, l * SP_LAYER:l * SP_LAYER + 64], writes=[t_bmod])
        for g in range(24):
            w32, t_w32 = load_w([(wrows(w_mod_d[l])[:, :, g * 256:(g + 1) * 256], KC)], KC, 256)
            pb, t_pb = psA.next()
            fns = []
            for j in range(2):
                for kc in range(KC):
                    fns.append(mm(pb[:, j * 2:j * 2 + 2], w32[:, kc, j * 128:(j + 1) * 128], cact3[:, kc, :],
                                  start=(kc == 0), stop=(kc == KC - 1)))
            k.group("pe", fns, reads=[t_w32, t_cactb], writes=[t_pb])
            op("dve", "tensor_copy", reads=[t_pb], writes=[t_mod], out=modT[:, l, g * 2:(g + 1) * 2, :],
               in_=pb[:, 0:4].rearrange("p (j b) -> p j b", b=2))
        for b in range(2):
            op("dve", "tensor_tensor", reads=[t_bmod], writes=[t_mod], out=modT[:, l, :, b], in0=modT[:, l, :, b],
               in1=bmod_t[:, 16:64], op=ALU.add)
        for b in range(2):
            for part in range(6):
                src = modT[:, l, part * 8:(part + 1) * 8, b]
                dst = mods[:, l, b, part, :]
                if part in (1, 4):
                    go = 0 if part == 1 else 8
                    op("dve", "scalar_tensor_tensor", reads=[t_mod, t_bmod], writes=[t_mods], out=dst, in0=src, scalar=1.0,
                       in1=bmod_t[:, go:go + 8], op0=ALU.add, op1=ALU.mult)
                else:
                    op("dve", "tensor_copy", reads=[t_mod], writes=[t_mods], out=dst, in_=src)
    tap("mods", mods, t_mods)
    SHIFT1, G1S, GATE1, SHIFT2, G2S, GATE2 = 0, 1, 2, 3, 4, 5

    wsT_b, t_wst = sbt("wsT_b", [128, 4, 128], BF16)
    lay, t_lay = sbt("lay", [128, 16])
    subgs, t_subgs = sbt("subgs", [128, 128])
    halo, t_halo = sbt("halo", [128, 12, 3])
    Sst, t_S = sbt("Sst", [128, 4, 128])
    lamtmp = sbt("lamtmp", [128, 2, 64])

    rem = int(nc.sbuf_bytes_remaining)
    AW = (rem - 1024) // 4
    arena = sb("arena", [128, AW])
    ar = {"off": 0}
    tokcache = {}
    tokgen = {}

    def aa(name, shape, dt=F32):
        n = 1
        for s_ in shape[1:]:
            n *= s_
        words = (n * (4 if dt in (F32, I32) else 2) + 3) // 4
        words = (words + 7) // 8 * 8
        o = ar["off"]
        assert o + words <= AW, ("arena overflow", name, o, words, AW)
        ar["off"] = o + words
        v = arena[0:shape[0], o:o + words]
        if dt != F32:
            v = v.bitcast(dt)
        v = v[:, 0:n]
        if len(shape) == 3:
            v = v.rearrange("p (a b) -> p a b", b=shape[2])
        elif len(shape) == 4:
            v = v.rearrange("p (a b c) -> p a b c", b=shape[2], c=shape[3])
        key = (name, o)
        if key not in tokcache:
            tokcache[key] = Tok(name)
        tk = tokcache[key]
        if LAZY and tokgen.get(key) != fence["gen"]:
            tokgen[key] = fence["gen"]
            tk.r = k._compact(list(tk.r) + fence["evs"])
        return v, tk

    def layer_setup(l):
        k.dma("sp", spl_t, smallp_d[:, l * SP_LAYER:(l + 1) * SP_LAYER], writes=[t_spl])
        k.dma("pool", wsT_b, wst_d[:, l * 512:(l + 1) * 512].rearrange("p (g i) -> p g i", i=128), writes=[t_wst])
        op("dve", "memset", writes=[t_wst], ap=wsT_b[64:128, :, 0:64], constant=0.0)
        lam_v = spl("lam").rearrange("p (a d) -> p a d", d=64)
        pr_, t_pr = lamtmp
        op("dve", "tensor_tensor", reads=[t_spl], writes=[t_pr], out=pr_[:, 0, :], in0=lam_v[:, 0, :], in1=lam_v[:, 1, :], op=ALU.mult)
        op("dve", "tensor_tensor", reads=[t_spl], writes=[t_pr], out=pr_[:, 1, :], in0=lam_v[:, 2, :], in1=lam_v[:, 3, :], op=ALU.mult)
        op("dve", "reduce_sum", reads=[t_pr], writes=[t_lay], out=lay[:, 8:10], in_=pr_, axis=AX.X)
        op("act", "activation", reads=[], writes=[t_lay], out=lay[:, 8:10], in_=lay[:, 8:10], func=AF.Exp)
        op("dve", "scalar_tensor_tensor", reads=[], writes=[t_lay], out=lay[:, 0:1], in0=lay[:, 9:10], scalar=-lambda_init_fn(l),
           in1=lay[:, 8:9], op0=ALU.add, op1=ALU.subtract)
        op("dve", "tensor_scalar", reads=[t_spl], writes=[t_subgs], out=subgs, in0=spl("subg"), scalar1=1.0 - lambda_init_fn(l),
           scalar2=None, op0=ALU.mult)
        op("act", "activation", reads=[t_spl], writes=[t_lay], out=lay[:, 4:8], in_=spl("alog"), func=AF.Exp)
        op("dve", "tensor_scalar", reads=[], writes=[t_lay], out=lay[:, 4:8], in0=lay[:, 4:8], scalar1=-1.0, scalar2=None, op0=ALU.mult)

    def modnorm(xsrc, t_xsrc, c0, l, b, gidx, sidx, hdst, t_hd, sq_b, t_sq, rb, t_rb, tmps, h32=None):
        for c in range(KC):
            op("act", "activation", reads=[t_xsrc], writes=[t_sq], out=sq_b[:, c, :], in_=xsrc[:, c, c0:c0 + TT], func=AF.Square)
        pb, t_pb = psA.next()
        k.group("pe", [mm(pb, ones_b, sq_b[:, c, :], start=(c == 0), stop=(c == KC - 1)) for c in range(KC)],
                reads=[t_sq, t_onesb], writes=[t_pb])
        op("act", "activation", reads=[t_pb], writes=[t_rb], out=rb, in_=pb, func=AF.Sqrt, bias=eps_c, scale=1.0 / D)
        op("dve", "reciprocal", writes=[t_rb], out=rb, in_=rb)
        for c in range(KC):
            tm, t_tm = tmps.next()
            op("dve", "scalar_tensor_tensor", reads=[t_xsrc, t_mods, t_rb], writes=[t_tm], out=tm, in0=xsrc[:, c, c0:c0 + TT],
               scalar=mods[:, l, b, gidx, c:c + 1], in1=rb, op0=ALU.mult, op1=ALU.mult)
            if h32 is not None:
                op("act", "activation", reads=[t_tm, t_mods], writes=[h32[1]], out=h32[0][:, c, :], in_=tm, func=AF.Identity,
                   bias=mods[:, l, b, sidx, c:c + 1], scale=1.0)
                op("dve", "tensor_copy", reads=[h32[1]], writes=[t_hd], out=hdst[:, c, :], in_=h32[0][:, c, :])
            else:
                op("act", "activation", reads=[t_tm, t_mods], writes=[t_hd], out=hdst[:, c, :], in_=tm, func=AF.Identity,
                   bias=mods[:, l, b, sidx, c:c + 1], scale=1.0)

    epsc, t_epsc = sbt("epsc", [128, 4])
    op("dve", "memset", writes=[t_epsc], ap=epsc[:, 0:1], constant=EPS)
    op("dve", "memset", writes=[t_epsc], ap=epsc[:, 1:2], constant=1.0)
    op("dve", "memset", writes=[t_epsc], ap=epsc[:, 2:3], constant=-PI)
    eps_c = epsc[:, 0:1]
    one_c = epsc[:, 1:2]
    npi_c = epsc[:, 2:3]

    def mixer(l, b):
        barrier()
        ar["off"] = 0
        hT, t_h = aa("hT", [128, KC, TT], BF16)
        kT, t_kT = aa("kT", [128, 4, S], BF16)
        Vt, t_V = aa("Vt", [128, 16, 512], BF16)
        y_gm, t_ygm = aa("y_gm", [128, 4, TT], BF16)
        y_da, t_yda = aa("y_da", [128, 4, TT], BF16)
        y_dn, t_ydn = aa("y_dn", [128, 4, TT], BF16)
        tmps = Pool_([aa("tmpA%d" % i, [128, TT]) for i in range(3)])
        tmpb = Pool_([aa("tmpB%d" % i, [128, TT], BF16) for i in range(2)])
        st, t_st = aa("stat", [128, 32])
        P1_END = ar["off"]
        op("dve", "memset", writes=[t_halo], ap=halo, constant=0.0)
        op("dve", "memset", writes=[t_S], ap=Sst, constant=0.0)

        def wg(col0, ncols):
            return load_w([(wrows(w_in_d[l])[:, :, col0:col0 + ncols], KC)], KC, ncols)

        def proj_fm(wb, t_wb, nchunk, consume):
            for j in range(nchunk):
                pb, t_pb = psA.next()
                k.group("pe", [mm(pb, wb[:, kc, j * 128:(j + 1) * 128], hT[:, kc, :], start=(kc == 0), stop=(kc == KC - 1))
                               for kc in range(KC)], reads=[t_wb, t_h], writes=[t_pb])
                consume(j, pb, t_pb)

        def proj_tm(wb, t_wb, ncols, consume):
            for blk in range(4):
                pb, t_pb = psA.next()
                k.group("pe", [mm(pb[:, 0:ncols], hT[:, kc, blk * 128:(blk + 1) * 128], wb[:, kc, 0:ncols], start=(kc == 0),
                                  stop=(kc == KC - 1)) for kc in range(KC)], reads=[t_wb, t_h], writes=[t_pb])
                consume(blk, pb, t_pb)

        for t in range(NTILE):
            c0 = t * TT
            ar["off"] = P1_END
            xt, t_xt = aa("xt", [128, KC, TT])
            if l == 0:
                for c in range(KC):
                    k.dma("sp", xt[:, c, :], xT_d[b, c * 128:(c + 1) * 128, c0:c0 + TT], writes=[t_xt], group=(c > 0))
            else:
                for c in range(KC):
                    k.dma("sp", xt[:, c, :], xs_d[:, c, c0:c0 + TT], reads=[t_xs[t][c]], writes=[t_xt], group=(c > 0))
            rb, t_rb = aa("rb", [128, TT])
            sq_b, t_sq = aa("sq_b", [128, KC, TT], BF16)
            modnorm(xt, t_xt, 0, l, b, G1S, SHIFT1, hT, t_h, sq_b, t_sq, rb, t_rb, tmps)
            if l == 0 and t == 0:
                tap("h0", hT, t_h)
            if STAGE <= 1:
                continue
            barrier()
            ar["off"] = P1_END
            SUB0 = P1_END

            u_g, t_ug = aa("u_g", [128, 4, TT], BF16)
            vg, t_vg = aa("vg", [128, 4, 512])
            vln, t_vln = aa("vln", [128, 4, 512], BF16)
            for g2 in range(2):
                wb, t_wb = wg(COL_U + g2 * 256, 256)
                proj_fm(wb, t_wb, 2, lambda j, pb, t_pb, g2=g2: op(
                    "act", "activation", reads=[t_pb], writes=[t_ug], out=u_g[:, g2 * 2 + j, :], in_=pb, func=AF.Gelu))
            for g2 in range(2):
                wb, t_wb = wg(COL_V + g2 * 256, 256)
                proj_tm(wb, t_wb, 256, lambda blk, pb, t_pb, g2=g2: op(
                    "act", "activation", reads=[t_pb], writes=[t_vg], out=vg[:, blk, g2 * 256:(g2 + 1) * 256], in_=pb[:, 0:256], func=AF.Gelu))
            for blk in range(4):
                op("dve", "bn_stats", reads=[t_vg], writes=[t_st], out=st[:, 0:6], in_=vg[:, blk, :])
                op("dve", "bn_aggr", writes=[t_st], out=st[:, 8:10], in_=st[:, 0:6])
                op("act", "activation", writes=[t_st], out=st[:, 10:11], in_=st[:, 9:10], func=AF.Sqrt, bias=eps_c, scale=1.0)
                op("dve", "reciprocal", writes=[t_st], out=st[:, 10:11], in_=st[:, 10:11])
                tm, t_tm = tmps.next()
                op("dve", "tensor_scalar", reads=[t_vg, t_st], writes=[t_tm], out=tm, in0=vg[:, blk, :], scalar1=st[:, 8:9],
                   scalar2=st[:, 10:11], op0=ALU.subtract, op1=ALU.mult)
                op("dve", "tensor_tensor", reads=[t_spl], writes=[t_tm], out=tm, in0=tm, in1=spl("lng"), op=ALU.mult)
                op("dve", "tensor_tensor", reads=[t_spl, t_tm], writes=[t_vln], out=vln[:, blk, :], in0=tm, in1=spl("lnb"), op=ALU.add)
            for g in range(4):
                pb, t_pb = psA.next()
                k.group("pe", [mm(pb[:, blk * 128:(blk + 1) * 128], vln[:, blk, g * 128:(g + 1) * 128], wsT_b[:, g, :])
                               for blk in range(4)], reads=[t_vln, t_wst], writes=[t_pb])
                tm, t_tm = tmps.next()
                bsb = spl("bs")[:, g * 128:(g + 1) * 128].unsqueeze(1).to_broadcast([128, 4, 128])
                op("dve", "tensor_tensor", reads=[t_pb, t_spl], writes=[t_tm], out=tm.rearrange("p (a b) -> p a b", b=128),
                   in0=pb.rearrange("p (a b) -> p a b", b=128), in1=bsb, op=ALU.add)
                op("dve", "tensor_tensor", reads=[t_tm, t_ug], writes=[t_ygm], out=y_gm[:, g, :], in0=tm, in1=u_g[:, g, :], op=ALU.mult)
            if l == 0 and t == 0:
                tap("ygm", y_gm, t_ygm)
            if STAGE <= 2:
                continue
            barrier()
            ar["off"] = SUB0

            cosT, t_cos = aa("cosT", [128, TT])
            sinT, t_sin = aa("sinT", [128, TT])
            qT, t_qT = aa("qT", [128, 4, TT], BF16)
            PT, t_PT = aa("PT", [128, 16, 128], BF16)
            yv, t_yv = aa("yv", [128, 128])
            junk, t_junk = aa("junk", [128, 128])
            Ssb = [aa("Ssb%d" % i, [128, S]) for i in range(2)]
            Pms = [aa("Pm%d" % i, [128, S], BF16) for i in range(2)]
            asts = [aa("ast%d" % i, [128, 8]) for i in range(2)]
            rss = [aa("rs%d" % i, [128, 8]) for i in range(2)]
            ytms = [aa("ytm%d" % i, [128, 512], BF16) for i in range(2)]
            pOs = {}
            qn, t_qn = aa("qn", [128, 4, TT], BF16)
            kn, t_kn = aa("kn", [128, 4, TT], BF16)
            vn, t_vn = aa("vn", [128, 4, TT], BF16)
            Sb, t_Sb = aa("Sb", [128, 4, 128], BF16)
            zs, t_zs = aa("zs", [128, 4, TT], BF16)
            oT, t_oT = aa("oT", [128, 4, TT])
            ab, t_ab = aa("ab", [64, 8, 8])
            gb, t_gb = aa("gb", [64, 4, 8, 4])
            dst_ = {0: (qn, t_qn), 1: (kn, t_kn), 2: (vn, t_vn)}
            op("act", "copy", reads=[t_S], writes=[t_Sb], out=Sb, in_=Sst)
            cwv = spl("convw").rearrange("p (c j) -> p c j", j=4)
            OV0 = ar["off"]
            posi, t_posi = aa("posi", [128, TT], I32)
            k.dma("sp", posi, posb_d[b, :, c0:c0 + TT], writes=[t_posi])
            ang, t_ang = aa("ang", [128, TT])
            kq, t_kq = aa("kq", [128, TT])
            ki, t_ki = aa("ki", [128, TT], I32)
            op("dve", "tensor_copy", reads=[t_posi], writes=[t_ang], out=ang, in_=posi)
            op("dve", "tensor_scalar", reads=[t_consts], writes=[t_ang], out=ang, in0=ang, scalar1=cst("invf"), scalar2=None, op0=ALU.mult)
            op("dve", "tensor_scalar", reads=[t_ang], writes=[t_kq], out=kq, in0=ang, scalar1=1.0 / (2 * PI), scalar2=None, op0=ALU.mult)
            op("dve", "tensor_copy", reads=[t_kq], writes=[t_ki], out=ki, in_=kq)
            op("dve", "tensor_copy", reads=[t_ki], writes=[t_kq], out=kq, in_=ki)
            op("dve", "scalar_tensor_tensor", reads=[t_kq], writes=[t_ang], out=ang, in0=kq, scalar=-C1, in1=ang, op0=ALU.mult, op1=ALU.add)
            op("dve", "scalar_tensor_tensor", reads=[t_kq], writes=[t_ang], out=ang, in0=kq, scalar=-C2, in1=ang, op0=ALU.mult, op1=ALU.add)

            op("dve", "tensor_scalar", reads=[t_ang], writes=[t_kq], out=kq, in0=ang, scalar1=PI, scalar2=None, op0=ALU.is_gt)
            op("dve", "scalar_tensor_tensor", reads=[t_kq, t_ang], writes=[t_sin], out=sinT, in0=kq, scalar=-2 * PI, in1=ang, op0=ALU.mult, op1=ALU.add)
            op("dve", "tensor_scalar", reads=[t_sin], writes=[t_kq], out=kq, in0=sinT, scalar1=PI / 2, scalar2=None, op0=ALU.is_gt)
            op("dve", "scalar_tensor_tensor", reads=[t_kq, t_sin], writes=[t_cos], out=cosT, in0=kq, scalar=-2 * PI, in1=sinT, op0=ALU.mult, op1=ALU.add)
            op("dve", "tensor_scalar", writes=[t_cos], out=cosT, in0=cosT, scalar1=PI / 2, scalar2=None, op0=ALU.add)
            op("act", "activation", writes=[t_sin], out=sinT, in_=sinT, func=AF.Sin)
            op("act", "activation", writes=[t_cos], out=cosT, in_=cosT, func=AF.Sin)
            if l == 0 and t == 0:
                tap("cos", cosT, t_cos)
                tap("sin", sinT, t_sin)
            cws = Pool_([aa("cw%d" % i, [128, TT + 3]) for i in range(2)])

            def rope_consume(dst_fn, t_dst):
                def f(j, pb, t_pb, hbase):
                    h = hbase + j
                    rw, t_rw = tmpb.next()
                    op("act", "copy", reads=[t_pb], writes=[t_rw], out=rw, in_=pb)
                    pr, t_pr = psA.next()
                    k.group("pe", [mm(pr, rot_b, rw)], reads=[t_rw, t_rotb], writes=[t_pr])
                    t1, t_t1 = tmps.next()
                    t2, t_t2 = tmps.next()
                    op("dve", "tensor_tensor", reads=[t_pb, t_cos], writes=[t_t1], out=t1, in0=pb, in1=cosT, op=ALU.mult)
                    op("dve", "tensor_tensor", reads=[t_pr, t_sin], writes=[t_t2], out=t2, in0=pr, in1=sinT, op=ALU.mult)
                    op("dve", "tensor_tensor", reads=[t_t1, t_t2], writes=[t_dst], out=dst_fn(h), in0=t1, in1=t2, op=ALU.add)
                return f
            qcons = rope_consume(lambda h: qT[:, h, :], t_qT)
            kcons = rope_consume(lambda h: kT[:, h, c0:c0 + TT], t_kT)
            for g2 in range(2 if "q" in cfg.get("parts", "qkv") else 0):
                wb, t_wb = wg(COL_Q + g2 * 256, 256)
                proj_fm(wb, t_wb, 2, lambda j, pb, t_pb, g2=g2: qcons(j, pb, t_pb, g2 * 2))
            for g2 in range(2 if "k" in cfg.get("parts", "qkv") else 0):
                wb, t_wb = wg(COL_KK + g2 * 256, 256)
                proj_fm(wb, t_wb, 2, lambda j, pb, t_pb, g2=g2: kcons(j, pb, t_pb, g2 * 2))
            for g2 in range(2 if "v" in cfg.get("parts", "qkv") else 0):
                wb, t_wb = wg(COL_VA + g2 * 256, 256)
                proj_tm(wb, t_wb, 256, lambda blk, pb, t_pb, g2=g2: op(
                    "act", "copy", reads=[t_pb], writes=[t_V], out=Vt[:, t * 4 + blk, g2 * 256:(g2 + 1) * 256], in_=pb[:, 0:256]))
            if l == 0 and t == 0:
                tap("qT", qT, t_qT)
                tap("kT", kT[:, :, 0:TT], t_kT)

            def dn_consume(j, pb, t_pb, ci0):
                ci = ci0 + j
                which, h = ci // 4, ci % 4
                dst, t_dst = dst_[which]
                cw, t_cw = cws.next()
                op("dve", "tensor_copy", reads=[t_halo], writes=[t_cw], out=cw[:, 0:3], in_=halo[:, ci, :])
                op("act", "copy", reads=[t_pb], writes=[t_cw], out=cw[:, 3:TT + 3], in_=pb)
                op("dve", "tensor_copy", reads=[t_cw], writes=[t_halo], out=halo[:, ci, :], in_=cw[:, TT:TT + 3])
                acc, t_acc = tmps.next()
                op("dve", "tensor_scalar", reads=[t_cw, t_spl], writes=[t_acc], out=acc, in0=cw[:, 3:TT + 3], scalar1=cwv[:, ci, 3:4],
                   scalar2=None, op0=ALU.mult)
                for jj in range(3):
                    op("dve", "scalar_tensor_tensor", reads=[t_cw, t_spl], writes=[t_acc], out=acc, in0=cw[:, jj:jj + TT],
                       scalar=cwv[:, ci, jj:jj + 1], in1=acc, op0=ALU.mult, op1=ALU.add)
                if which == 2:
                    op("act", "activation", reads=[t_acc], writes=[t_dst], out=dst[:, h, :], in_=acc, func=AF.Silu)
                else:
                    op("act", "activation", reads=[], writes=[t_acc], out=acc, in_=acc, func=AF.Silu)
                    sqb, t_sqb = tmpb.next()
                    op("act", "activation", reads=[t_acc], writes=[t_sqb], out=sqb, in_=acc, func=AF.Square)
                    pn, t_pn = ps8.next()
                    k.group("pe", [mm(pn, ones_b, sqb)], reads=[t_sqb, t_onesb], writes=[t_pn])
                    rn, t_rn = tmps.next()
                    op("act", "activation", reads=[t_pn], writes=[t_rn], out=rn, in_=pn, func=AF.Sqrt, bias=eps_c, scale=1.0)
                    op("dve", "reciprocal", writes=[t_rn], out=rn, in_=rn)
                    op("dve", "scalar_tensor_tensor", reads=[t_rn, t_acc], writes=[t_dst], out=dst[:, h, :], in0=acc,
                       scalar=(128.0 ** -0.5 if which == 0 else 1.0), in1=rn, op0=ALU.mult, op1=ALU.mult)
            for gi in range(6):
                wb, t_wb = wg(COL_DQ + gi * 256, 256)
                proj_fm(wb, t_wb, 2, lambda j, pb, t_pb, gi=gi: dn_consume(j, pb, t_pb, gi * 2))
            for g2 in range(2):
                wb, t_wb = wg(COL_DZ + g2 * 256, 256)
                proj_fm(wb, t_wb, 2, lambda j, pb, t_pb, g2=g2: op(
                    "act", "activation", reads=[t_pb], writes=[t_zs], out=zs[:, g2 * 2 + j, :], in_=pb, func=AF.Silu))
            wab, t_wab = wg(COL_AB, 8)
            pb, t_pb = ps8.next()
            for c in range(8):
                k.group("pe", [mm(pb[0:64, c * 8:(c + 1) * 8], hT[:, kc, c * 64:(c + 1) * 64], wab[:, kc, 0:8], start=(kc == 0),
                                  stop=(kc == KC - 1)) for kc in range(KC)], reads=[t_wab, t_h], writes=[t_pb])
            op("dve", "tensor_copy", reads=[t_pb], writes=[t_ab], out=ab, in_=pb[0:64, 0:64].rearrange("p (c e) -> p c e", e=8))
            dtb_b = spl("dtb")[0:64, :].unsqueeze(1).to_broadcast([64, 8, 4])
            nA_b = lay[0:64, 4:8].unsqueeze(1).to_broadcast([64, 8, 4])
            op("dve", "tensor_tensor", reads=[t_ab, t_spl], writes=[t_gb], out=gb[:, 3], in0=ab[:, :, 0:4], in1=dtb_b, op=ALU.add)
            op("act", "activation", writes=[t_gb], out=gb[:, 3], in_=gb[:, 3], func=AF.Exp)
            op("act", "activation", writes=[t_gb], out=gb[:, 3], in_=gb[:, 3], func=AF.Ln, bias=one_c[0:64, :], scale=1.0)
            op("dve", "tensor_tensor", reads=[t_lay], writes=[t_gb], out=gb[:, 0], in0=gb[:, 3], in1=nA_b, op=ALU.mult)
            op("act", "activation", reads=[t_ab], writes=[t_gb], out=gb[:, 1], in_=ab[:, :, 4:8], func=AF.Sigmoid)
            op("dve", "tensor_scalar", writes=[t_gb], out=gb[:, 2], in0=gb[:, 1], scalar1=-1.0, scalar2=None, op0=ALU.mult)
            if l == 0 and t == 0:
                tap("qn", qn, t_qn); tap("kn", kn, t_kn); tap("vn", vn, t_vn); tap("gb", gb, t_gb)

            barrier()
            ar["off"] = OV0
            def a64(name, cols=256, rows=64):
                return aa(name, [rows, cols])

            def mkset(s_):
                def b64(name, cols=256):
                    return aa(name, [64, cols], BF16)
                return {"ktok": b64("ktok" + s_, 512), "vtok": b64("vtok" + s_, 512), "R1": a64("R1" + s_), "R2": a64("R2" + s_),
                        "Dm": a64("Dm" + s_), "DTm": a64("DTm" + s_), "egs": a64("egs" + s_, 16), "glb": aa("glb" + s_, [128, 4]),
                        "egrow": aa("egrow" + s_, [128, 256]), "Pb": [b64("Pa" + s_), b64("Pb" + s_)], "Bb": [b64("Ba" + s_), b64("Bb" + s_)],
                        "Nb": [b64("Na" + s_), b64("Nb" + s_)], "rhs_w": b64("rhs_w" + s_, 512), "u_sb": a64("u_sb" + s_, 512),
                        "wT": aa("wT" + s_, [128, 256], BF16), "qgT": aa("qgT" + s_, [128, 256], BF16), "vnew": b64("vnew" + s_, 512),
                        "qkT": b64("qkT" + s_)}
            NW = cfg.get("nw", 2)
            BSs = [mkset("_%d" % i) for i in range(NW)]
            tt_c = cst("tt", 64); su_c = cst("su", 64); negu = cst("negu", 64); negl = cst("negl", 64)
            mstr = cst("mstrict", 64); i4 = cst("i4", 64); id64 = consts[0:64, CO["ident"][0]:CO["ident"][0] + 64]
            units = [(qb, h, p) for qb in range(4 if ACUT >= 2 else 0) for h in range(4) for p in range(2)]

            def stageA(ui):
                qb, h, p = units[ui]
                j = ui % 2
                gq = t * 4 + qb
                kend = (gq + 1) * 128
                Sb_, t_Sb_ = Ssb[j]
                Pj, t_Pj = Pms[j]
                aj, t_aj = asts[j]
                rs_, t_rs = rss[h % 2]
                ps_ = slice(p * 64, (p + 1) * 64)
                for pc in range((kend + 511) // 512):
                    n = min(512, kend - pc * 512)
                    sbk, t_sbk = psS.next()
                    k.group("pe", [mm(sbk[:, 0:n], qT[ps_, h, qb * 128:(qb + 1) * 128], kT[ps_, h, pc * 512:pc * 512 + n])],
                            reads=[t_qT, t_kT], writes=[t_sbk])
                    op("act", "copy", reads=[t_sbk], writes=[t_Sb_], out=Sb_[:, pc * 512:pc * 512 + n], in_=sbk[:, 0:n])
                op("dve", "tensor_scalar", writes=[t_Sb_], out=Sb_[0:64, kend - 64:kend], in0=Sb_[0:64, kend - 64:kend],
                   scalar1=NEG, scalar2=None, op0=ALU.add)
                op("dve", "reduce_max", reads=[t_Sb_], writes=[t_aj], out=aj[:, 0:1], in_=Sb_[:, 0:kend], axis=AX.X)
                op("dve", "tensor_scalar", writes=[t_aj], out=aj[:, 1:2], in0=aj[:, 0:1], scalar1=-0.125, scalar2=None, op0=ALU.mult)
                op("act", "activation", reads=[t_Sb_, t_aj], writes=[t_Pj, t_rs], out=Pj[:, 0:kend], in_=Sb_[:, 0:kend],
                   func=AF.Exp, bias=aj[:, 1:2], scale=0.125, accum_out=rs_[:, p:p + 1])

            def stageB(ui):
                qb, h, p = units[ui]
                j = ui % 2
                gq = t * 4 + qb
                nkb = gq + 1
                Pj, t_Pj = Pms[j]
                rs_, t_rs = rss[h % 2]
                ytm, t_ytm = ytms[qb % 2]
                if p == 0:
                    pOs[(qb, h)] = psO.next()
                pO, t_pO = pOs[(qb, h)]
                for kb0 in range(0, nkb, 8):
                    nb = min(8, nkb - kb0)
                    ptb, t_ptb = psT.next()
                    ptv = ptb.bitcast(BF16)
                    k.group("pe", [tr(ptv[:, i * 128:(i + 1) * 128], Pj[:, (kb0 + i) * 128:(kb0 + i + 1) * 128], ident_b)
                                   for i in range(nb)], reads=[t_Pj, t_identb], writes=[t_ptb])
                    op("dve", "tensor_copy", reads=[t_ptb], writes=[t_PT], out=PT[:, kb0:kb0 + nb, :],
                       in_=ptv[:, 0:nb * 128].rearrange("p (a b) -> p a b", b=128))
                k.group("pe", [mm(pO[:, p * 128:(p + 1) * 128], PT[:, kb, :], Vt[:, kb, h * 128:(h + 1) * 128],
                                  start=(kb == 0), stop=(kb == nkb - 1)) for kb in range(nkb)], reads=[t_PT, t_V], writes=[t_pO])
                if p == 0:
                    return
                op("dve", "reciprocal", writes=[t_rs], out=rs_[:, 2:4], in_=rs_[:, 0:2])
                op("dve", "tensor_tensor", reads=[t_lay], writes=[t_rs], out=rs_[:, 3:4], in0=rs_[:, 3:4], in1=lay[:, 0:1], op=ALU.mult)
                op("dve", "tensor_scalar", reads=[t_pO, t_rs], writes=[t_yv], out=yv, in0=pO[:, 0:128], scalar1=rs_[:, 2:3], scalar2=None, op0=ALU.mult)
                op("dve", "scalar_tensor_tensor", reads=[t_pO, t_rs], writes=[t_yv], out=yv, in0=pO[:, 128:256], scalar=rs_[:, 3:4], in1=yv,
                   op0=ALU.mult, op1=ALU.add)
                op("dve", "tensor_tensor", reads=[t_yv], writes=[t_junk], out=junk, in0=yv, in1=yv, op=ALU.mult)
                op("dve", "reduce_sum", reads=[t_junk], writes=[t_rs], out=rs_[:, 4:5], in_=junk, axis=AX.X)
                op("act", "activation", writes=[t_rs], out=rs_[:, 5:6], in_=rs_[:, 4:5], func=AF.Ln, bias=eps_c, scale=1.0 / 128)
                op("act", "activation", writes=[t_rs], out=rs_[:, 5:6], in_=rs_[:, 5:6], func=AF.Exp, scale=-0.5)
                op("dve", "scalar_tensor_tensor", reads=[t_yv, t_rs, t_subgs], writes=[t_ytm], out=ytm[:, h * 128:(h + 1) * 128], in0=yv,
                   scalar=rs_[:, 5:6], in1=subgs, op0=ALU.mult, op1=ALU.mult)
                if h == 3:
                    ptb, t_ptb = psT.next()
                    ptv = ptb.bitcast(BF16)
                    k.group("pe", [tr(ptv[:, hh * 128:(hh + 1) * 128], ytm[:, hh * 128:(hh + 1) * 128], ident_b) for hh in range(4)],
                            reads=[t_ytm, t_identb], writes=[t_ptb])
                    op("dve", "tensor_copy", reads=[t_ptb], writes=[t_yda], out=y_da[:, :, qb * 128:(qb + 1) * 128],
                       in_=ptv[:, 0:512].rearrange("p (a b) -> p a b", b=128))


            def v3(ap, inner):
                return ap.rearrange("p (h j) -> p h j", j=inner)


            def chunk_prep(c, BS):
                ktok, t_ktok = BS["ktok"]; vtok, t_vtok = BS["vtok"]
                R1, t_R1 = BS["R1"]; R2, t_R2 = BS["R2"]
                Dm, t_D = BS["Dm"]; DTm, t_DT = BS["DTm"]; Ds, t_Ds = Dm, t_D
                egs, t_egs = BS["egs"]; glb, t_glb = BS["glb"]; egrow, t_egrow = BS["egrow"]
                Pb = BS["Pb"]; Bb = BS["Bb"]; Nb = BS["Nb"]
                rhs_u, t_ru = vtok, t_vtok; rhs_w, t_rw_ = BS["rhs_w"]; kd, t_kd = ktok, t_ktok
                u_sb, t_u = BS["u_sb"]; wT, t_wT = BS["wT"]; qkT, t_qk = BS["qkT"]
                qgT, t_qg = BS["qgT"]; vnew, t_vnew = BS["vnew"]
                cs = slice(c * 64, (c + 1) * 64)
                gr = gb[:, 0, c, :]
                be = gb[:, 1, c, :]
                nbe = gb[:, 2, c, :]
                pk, t_pk = psDN.next()
                pkb = pk.bitcast(BF16)
                k.group("pe", [tr(pkb[0:64, h * 128:(h + 1) * 128], kn[:, h, cs], ident_b) for h in range(4)], reads=[t_kn, t_identb], writes=[t_pk])
                op("act", "copy", reads=[t_pk], writes=[t_ktok], out=ktok, in_=pkb[0:64, 0:512])
                yield
                pv, t_pv = psDN.next()
                pvb = pv.bitcast(BF16)
                k.group("pe", [tr(pvb[0:64, h * 128:(h + 1) * 128], vn[:, h, cs], ident_b) for h in range(4)], reads=[t_vn, t_identb], writes=[t_pv])
                op("dve", "tensor_copy", reads=[t_pv], writes=[t_vtok], out=vtok, in_=pvb[0:64, 0:512])
                yield
                op("dve", "tensor_tensor", reads=[t_gb, t_consts], writes=[t_R1], out=v3(R1, 64), in0=gr.unsqueeze(2).to_broadcast([64, 4, 64]),
                   in1=tt_c.unsqueeze(1).to_broadcast([64, 4, 64]), op=ALU.mult)
                yield
                op("dve", "tensor_copy", reads=[t_gb], writes=[t_R2], out=v3(R2, 64), in_=gr.unsqueeze(2).to_broadcast([64, 4, 64]))
                yield
                pg, t_pg = psDN.next()
                k.group("pe", [mm(pg[0:64, 0:256], tt_c, R2, start=True, stop=False),
                               mm(pg[0:64, 0:256], nones_f[0:64, 0:64], R1, start=False, stop=False),
                               mm(pg[0:64, 0:256], id64, negu, start=False, stop=True)],
                        reads=[t_R1, t_R2, t_consts, t_nones], writes=[t_pg])
                op("act", "activation", reads=[t_pg], writes=[t_D], out=Dm, in_=pg[0:64, 0:256], func=AF.Exp)
                yield
                pgt, t_pgt = psDN.next()
                k.group("pe", [mm(pgt[0:64, 0:256], ones_f[0:64, 0:64], R1, start=True, stop=False),
                               mm(pgt[0:64, 0:256], ntt_f, R2, start=False, stop=False),
                               mm(pgt[0:64, 0:256], id64, negl, start=False, stop=True)],
                        reads=[t_R1, t_R2, t_consts, t_ones, t_ntt], writes=[t_pgt])
                op("act", "activation", reads=[t_pgt], writes=[t_DT], out=DTm, in_=pgt[0:64, 0:256], func=AF.Exp)
                yield
                op("dve", "tensor_tensor", reads=[t_consts], writes=[t_Ds], out=Ds, in0=Dm, in1=mstr, op=ALU.mult)
                yield
                px, t_px = psDN.next()
                k.group("pe", [mm(px[:, 0:256], ones_f[0:64, :], R1), mm(px[:, 256:260], ones_f[0:64, :], gr),
                               mm(px[0:64, 264:268], tt_c, gr), mm(px[0:64, 268:272], su_c, gr)],
                        reads=[t_R1, t_gb, t_ones, t_consts], writes=[t_px])
                op("act", "activation", reads=[t_px], writes=[t_egrow], out=egrow, in_=px[:, 0:256], func=AF.Exp)
                op("act", "activation", reads=[t_px], writes=[t_glb], out=glb, in_=px[:, 256:260], func=AF.Exp)
                op("act", "activation", reads=[t_px], writes=[t_egs], out=egs[:, 0:8], in_=px[0:64, 264:272], func=AF.Exp)
                yield
                op("dve", "tensor_tensor", reads=[t_gb], writes=[t_egs], out=egs[:, 8:12], in0=egs[:, 0:4], in1=be, op=ALU.mult)
                yield
                pkk, t_pkk = psDN.next()
                k.group("pe", [mm(pkk[0:64, h * 64:(h + 1) * 64], kn[:, h, cs], kn[:, h, cs]) for h in range(4)], reads=[t_kn], writes=[t_pkk])
                P_, t_P_ = Pb[0]
                for h in range(4):
                    op("dve", "scalar_tensor_tensor", reads=[t_pkk, t_gb, t_Ds], writes=[t_P_], out=P_[:, h * 64:(h + 1) * 64],
                       in0=pkk[0:64, h * 64:(h + 1) * 64], scalar=nbe[:, h:h + 1], in1=Ds[:, h * 64:(h + 1) * 64], op0=ALU.mult, op1=ALU.mult)
                pt_, t_pt_ = psDN.next()
                pt_ = pt_.bitcast(BF16)
                k.group("pe", [tr(pt_[0:64, h * 64:(h + 1) * 64], P_[:, h * 64:(h + 1) * 64], ident_b[0:64, 0:64]) for h in range(4)],
                        reads=[t_P_, t_identb], writes=[t_pt_])
                B_, t_B_ = Bb[0]
                N_, t_N_ = Nb[0]
                op("act", "copy", reads=[t_pt_], writes=[t_B_], out=B_, in_=pt_[0:64, 0:256])
                op("dve", "tensor_tensor", reads=[t_pt_, t_consts], writes=[t_N_], out=N_, in0=pt_[0:64, 0:256], in1=i4, op=ALU.add)
                yield
                for j in range(1, 6):
                    Pn, t_Pn = Pb[j % 2]
                    Bn, t_Bn = Bb[j % 2]
                    Nn, t_Nn = Nb[j % 2]
                    pp, t_pp = psDN.next()
                    k.group("pe", [mm(pp[0:64, h * 64:(h + 1) * 64], B_[:, h * 64:(h + 1) * 64], P_[:, h * 64:(h + 1) * 64]) for h in range(4)],
                            reads=[t_B_, t_P_], writes=[t_pp])
                    op("act", "copy", reads=[t_pp], writes=[t_Pn], out=Pn, in_=pp[0:64, 0:256])
                    if j < 5:
                        pbb, t_pbb = psDN.next()
                        k.group("pe", [mm(pbb[0:64, h * 64:(h + 1) * 64], P_[:, h * 64:(h + 1) * 64], B_[:, h * 64:(h + 1) * 64]) for h in range(4)],
                                reads=[t_B_, t_P_], writes=[t_pbb])
                        op("dve", "tensor_copy", reads=[t_pbb], writes=[t_Bn], out=Bn, in_=pbb[0:64, 0:256])
                    pn2, t_pn2 = psDN.next()
                    k.group("pe", [mm(pn2[0:64, h * 64:(h + 1) * 64], Pn[:, h * 64:(h + 1) * 64], N_[:, h * 64:(h + 1) * 64]) for h in range(4)],
                            reads=[t_Pn, t_N_], writes=[t_pn2])
                    op("dve", "tensor_tensor", reads=[t_pn2, t_N_], writes=[t_Nn], out=Nn, in0=pn2[0:64, 0:256], in1=N_, op=ALU.add)
                    P_, t_P_, B_, t_B_, N_, t_N_ = Pn, t_Pn, Bn, t_Bn, Nn, t_Nn
                op("dve", "tensor_tensor", reads=[t_vtok, t_gb], writes=[t_ru], out=v3(rhs_u, 128), in0=v3(vtok, 128),
                   in1=be.unsqueeze(2).to_broadcast([64, 4, 128]), op=ALU.mult)
                yield
                op("dve", "tensor_tensor", reads=[t_ktok, t_egs], writes=[t_rw_], out=v3(rhs_w, 128), in0=v3(ktok, 128),
                   in1=egs[:, 8:12].unsqueeze(2).to_broadcast([64, 4, 128]), op=ALU.mult)
                yield
                op("dve", "tensor_tensor", reads=[t_ktok, t_egs], writes=[t_kd], out=v3(kd, 128), in0=v3(ktok, 128),
                   in1=egs[:, 4:8].unsqueeze(2).to_broadcast([64, 4, 128]), op=ALU.mult)
                yield
                pu, t_pu = psDN.next()
                k.group("pe", [mm(pu[0:64, h * 128:(h + 1) * 128], N_[:, h * 64:(h + 1) * 64], rhs_u[:, h * 128:(h + 1) * 128]) for h in range(4)],
                        reads=[t_N_, t_ru], writes=[t_pu])
                op("act", "copy", reads=[t_pu], writes=[t_u], out=u_sb, in_=pu[0:64, :])
                yield
                pw, t_pw = psDN.next()
                k.group("pe", [mm(pw[:, h * 64:(h + 1) * 64], rhs_w[:, h * 128:(h + 1) * 128], N_[:, h * 64:(h + 1) * 64]) for h in range(4)],
                        reads=[t_N_, t_rw_], writes=[t_pw])
                op("dve", "tensor_copy", reads=[t_pw], writes=[t_wT], out=wT, in_=pw[:, 0:256])
                yield
                pq, t_pq = psDN.next()
                k.group("pe", [mm(pq[0:64, h * 64:(h + 1) * 64], kn[:, h, cs], qn[:, h, cs]) for h in range(4)], reads=[t_kn, t_qn], writes=[t_pq])
                op("dve", "tensor_tensor", reads=[t_pq, t_DT], writes=[t_qk], out=qkT, in0=pq[0:64, 0:256], in1=DTm, op=ALU.mult)
                yield
                op("dve", "tensor_tensor", reads=[t_qn, t_egrow], writes=[t_qg], out=v3(qgT, 64), in0=qn[:, :, cs], in1=v3(egrow, 64), op=ALU.mult)
                yield

            def chunk_recur(c, BS):
                ktok, t_ktok = BS["ktok"]; vtok, t_vtok = BS["vtok"]
                R1, t_R1 = BS["R1"]; R2, t_R2 = BS["R2"]
                Dm, t_D = BS["Dm"]; DTm, t_DT = BS["DTm"]; Ds, t_Ds = Dm, t_D
                egs, t_egs = BS["egs"]; glb, t_glb = BS["glb"]; egrow, t_egrow = BS["egrow"]
                Pb = BS["Pb"]; Bb = BS["Bb"]; Nb = BS["Nb"]
                rhs_u, t_ru = vtok, t_vtok; rhs_w, t_rw_ = BS["rhs_w"]; kd, t_kd = ktok, t_ktok
                u_sb, t_u = BS["u_sb"]; wT, t_wT = BS["wT"]; qkT, t_qk = BS["qkT"]
                qgT, t_qg = BS["qgT"]; vnew, t_vnew = BS["vnew"]
                cs = slice(c * 64, (c + 1) * 64)
                gr = gb[:, 0, c, :]
                be = gb[:, 1, c, :]
                nbe = gb[:, 2, c, :]
                pws, t_pws = psDN.next()
                k.group("pe", [mm(pws[0:64, h * 128:(h + 1) * 128], wT[:, h * 64:(h + 1) * 64], Sb[:, h, :]) for h in range(4)],
                        reads=[t_wT, t_Sb], writes=[t_pws])
                op("dve", "tensor_tensor", reads=[t_u, t_pws], writes=[t_vnew], out=vnew, in0=u_sb, in1=pws[0:64, :], op=ALU.subtract)
                po, t_po = psDN.next()
                fns = []
                for h in range(4):
                    fns.append(mm(po[:, h * 64:(h + 1) * 64], Sb[:, h, :], qgT[:, h * 64:(h + 1) * 64], start=True, stop=False))
                    fns.append(mm(po[:, h * 64:(h + 1) * 64], vnew[:, h * 128:(h + 1) * 128], qkT[:, h * 64:(h + 1) * 64], start=False, stop=True))
                k.group("pe", fns, reads=[t_Sb, t_qg, t_vnew, t_qk], writes=[t_po])
                op("act", "copy", reads=[t_po], writes=[t_oT], out=oT[:, :, cs], in_=v3(po[:, 0:256], 64))
                pds, t_pds = psDN.next()
                k.group("pe", [mm(pds[:, h * 128:(h + 1) * 128], kd[:, h * 128:(h + 1) * 128], vnew[:, h * 128:(h + 1) * 128]) for h in range(4)],
                        reads=[t_kd, t_vnew], writes=[t_pds])
                for h in range(4):
                    op("dve", "scalar_tensor_tensor", reads=[t_pds, t_glb], writes=[t_S], out=Sst[:, h, :], in0=Sst[:, h, :], scalar=glb[:, h:h + 1],
                       in1=pds[:, h * 128:(h + 1) * 128], op0=ALU.mult, op1=ALU.add)
                op("act", "copy", reads=[t_S], writes=[t_Sb], out=Sb, in_=Sst)


            def att_gen():
                for ui in range(len(units) + 1):
                    if ui < len(units):
                        stageA(ui)
                        yield
                    if ui >= 1:
                        stageB(ui - 1)
                        yield

            def dn_gen():
                for c0_ in range(0, 8, NW):
                    gens = [chunk_prep(c0_ + i, BSs[i]) for i in range(NW)]
                    alive = list(gens)
                    while alive:
                        for g_ in list(alive):
                            try:
                                next(g_)
                            except StopIteration:
                                alive.remove(g_)
                            yield
                    for i in range(NW):
                        chunk_recur(c0_ + i, BSs[i])
                        yield

            RATIO = cfg.get("dn_ratio", 6)
            streams = [[att_gen(), 1], [dn_gen(), RATIO]]
            while streams:
                for st_ in list(streams):
                    for _ in range(st_[1]):
                        try:
                            next(st_[0])
                        except StopIteration:
                            streams.remove(st_)
                            break

            if l == 0 and t == 0 and ACUT >= 8:
                tap("yda", y_da, t_yda)
            if l == 0 and t == 0:
                tap("oT", oT, t_oT)
            for h in range(4):
                sqb, t_sqb = tmpb.next()
                op("act", "activation", reads=[t_oT], writes=[t_sqb], out=sqb, in_=oT[:, h, :], func=AF.Square)
                pn, t_pn = ps8.next()
                k.group("pe", [mm(pn, ones_b, sqb)], reads=[t_sqb, t_onesb], writes=[t_pn])
                rn, t_rn = tmps.next()
                op("act", "activation", reads=[t_pn], writes=[t_rn], out=rn, in_=pn, func=AF.Sqrt, bias=eps_c, scale=1.0 / 128)
                op("dve", "reciprocal", writes=[t_rn], out=rn, in_=rn)
                op("dve", "scalar_tensor_tensor", reads=[t_oT, t_spl], writes=[t_rn], out=rn, in0=oT[:, h, :], scalar=spl("dng"), in1=rn,
                   op0=ALU.mult, op1=ALU.mult)
                op("dve", "tensor_tensor", reads=[t_rn, t_zs], writes=[t_ydn], out=y_dn[:, h, :], in0=rn, in1=zs[:, h, :], op=ALU.mult)
            if l == 0 and t == 0:
                tap("ydn", y_dn, t_ydn)
            if STAGE <= 4:
                continue
            barrier()
            ar["off"] = SUB0
            merged, t_mg = aa("merged", [128, KC, TT], BF16)
            sgs = Pool_([aa("sg%d" % i, [128, TT]) for i in range(3)])
            macc, t_macc = aa("macc", [128, TT])
            xins = Pool_([aa("xin%d" % i, [128, TT]) for i in range(3)])
            ybr = [(y_gm, t_ygm), (y_da, t_yda), (y_dn, t_ydn)]
            for d in range(KC):
                for br in range(3):
                    wgt, t_wgt = load_w([(wrows(w_in_d[l])[:, :, COL_G + br * 1024 + d * 128:COL_G + br * 1024 + (d + 1) * 128], KC)], KC, 128)
                    pg, t_pg = ps8.next()
                    k.group("pe", [mm(pg, wgt[:, kc, :], hT[:, kc, :], start=(kc == 0), stop=(kc == KC - 1)) for kc in range(KC)],
                            reads=[t_wgt, t_h], writes=[t_pg])
                    sg, t_sg = sgs.next()
                    op("act", "activation", reads=[t_pg], writes=[t_sg], out=sg, in_=pg, func=AF.Sigmoid)
                    pp, t_pp = ps8.next()
                    yb, t_yb = ybr[br]
                    wbr, t_wbr = load_w([(wrows(w_br_d[br][l])[:, :, d * 128:(d + 1) * 128], 4)], 4, 128)
                    k.group("pe", [mm(pp, wbr[:, kc, :], yb[:, kc, :], start=(kc == 0), stop=(kc == 3)) for kc in range(4)],
                            reads=[t_wbr, t_yb], writes=[t_pp])
                    if br == 0:
                        op("dve", "tensor_tensor", reads=[t_pp, t_sg], writes=[t_macc], out=macc, in0=pp, in1=sg, op=ALU.mult)
                    else:
                        op("dve", "tensor_tensor", reads=[t_pp], writes=[t_sg], out=sg, in0=pp, in1=sg, op=ALU.mult)
                        if br == 1:
                            op("dve", "tensor_tensor", reads=[t_sg], writes=[t_macc], out=macc, in0=macc, in1=sg, op=ALU.add)
                        else:
                            op("dve", "tensor_tensor", reads=[t_sg, t_macc], writes=[t_mg], out=merged[:, d, :], in0=macc, in1=sg, op=ALU.add)
            if l == 0 and t == 0:
                tap("merged", merged, t_mg)
            for g4 in range(4):
                wo, t_wo = load_w([(wrows(w_out_d[l])[:, :, g4 * 256:(g4 + 1) * 256], KC)], KC, 256)
                for j in range(2):
                    e_ = g4 * 2 + j
                    po, t_po = ps8.next()
                    k.group("pe", [mm(po, wo[:, dc, j * 128:(j + 1) * 128], merged[:, dc, :], start=(dc == 0), stop=(dc == KC - 1))
                                   for dc in range(KC)], reads=[t_wo, t_mg], writes=[t_po])
                    xin, t_xin = xins.next()
                    if l == 0:
                        k.dma("sp", xin, xT_d[b, e_ * 128:(e_ + 1) * 128, c0:c0 + TT], writes=[t_xin])
                    else:
                        k.dma("sp", xin, xs_d[:, e_, c0:c0 + TT], reads=[t_xs[t][e_]], writes=[t_xin])
                    op("dve", "scalar_tensor_tensor", reads=[t_po, t_mods], writes=[t_xin], out=xin, in0=po,
                       scalar=mods[:, l, b, GATE1, e_:e_ + 1], in1=xin, op0=ALU.mult, op1=ALU.add)
                    k.dma("sp", xs_d[:, e_, c0:c0 + TT], xin, reads=[t_xin], writes=[t_xs[t][e_]])
            barrier()


    def ffn(l, b, last):
        barrier()
        ar["off"] = 0
        xT, t_x = aa("xT", [128, KC, S])
        rb, t_rb = aa("rb", [128, TT])
        sq_b, t_sq = aa("sq_b", [128, KC, TT], BF16)
        tmps = Pool_([aa("tmpA%d" % i, [128, TT]) for i in range(3)])
        FMARK = ar["off"]
        h2, t_h2 = aa("h2", [128, KC, S], BF16)
        moe = (l % 2 == 1)
        for t in range(NTILE):
            k.dma("sp", xT[:, :, t * TT:(t + 1) * TT], xs_d[:, :, t * TT:(t + 1) * TT], reads=t_xs[t], writes=[t_x], group=(t > 0))
        if moe:
            h32, t_h32 = aa("h32", [128, KC, TT])
            WT, t_WT = aa("WT", [8, S])
            rhe, t_rhe = aa("rhe", [8, TT])
            Wb0, t_Wb0 = aa("Wbc0", [128, S], BF16)
            Wb1, t_Wb1 = aa("Wbc1", [128, S], BF16)
            Wbcs = [Wb0, Wb1]; t_Wbcs = [t_Wb0, t_Wb1]
            lg, t_lg = aa("lg", [128, 64])
            wr3 = spg[:, 8:72].rearrange("p (c e) -> p c e", e=8)
        for t in range(NTILE):
            modnorm(xT, t_x, t * TT, l, b, G2S, SHIFT2, h2[:, :, t * TT:(t + 1) * TT], t_h2, sq_b, t_sq, rb, t_rb, tmps,
                    h32=((h32, t_h32) if moe else None))
            if moe:
                for blk in range(4):
                    gblk = t * 4 + blk
                    pl, t_pl = ps8.next()
                    k.group("pe", [mm(pl[:, 0:8], h32[:, kc, blk * 128:(blk + 1) * 128], wr3[:, kc, :], start=(kc == 0), stop=(kc == KC - 1))
                                   for kc in range(KC)], reads=[t_h32, t_spg], writes=[t_pl])
                    L = lg[:, 0:8]; E1 = lg[:, 8:16]; M2 = lg[:, 16:24]; E2 = lg[:, 24:32]; Wt = lg[:, 32:40]
                    m1 = lg[:, 40:41]; m2 = lg[:, 41:42]; dd = lg[:, 42:43]; w1 = lg[:, 43:44]; w2 = lg[:, 44:45]
                    op("dve", "tensor_copy", reads=[t_pl], writes=[t_lg], out=L, in_=pl[:, 0:8])
                    op("dve", "reduce_max", writes=[t_lg], out=m1, in_=L, axis=AX.X)
                    op("dve", "tensor_scalar", writes=[t_lg], out=E1, in0=L, scalar1=m1, scalar2=None, op0=ALU.is_equal)
                    op("dve", "scalar_tensor_tensor", writes=[t_lg], out=M2, in0=E1, scalar=-1e30, in1=L, op0=ALU.mult, op1=ALU.add)
                    op("dve", "reduce_max", writes=[t_lg], out=m2, in_=M2, axis=AX.X)
                    op("dve", "tensor_scalar", writes=[t_lg], out=E2, in0=M2, scalar1=m2, scalar2=None, op0=ALU.is_equal)
                    op("dve", "tensor_tensor", writes=[t_lg], out=dd, in0=m2, in1=m1, op=ALU.subtract)
                    op("act", "activation", writes=[t_lg], out=dd, in_=dd, func=AF.Exp)
                    op("dve", "tensor_scalar", writes=[t_lg], out=w1, in0=dd, scalar1=1.0, scalar2=None, op0=ALU.add)
                    op("dve", "reciprocal", writes=[t_lg], out=w1, in_=w1)
                    op("dve", "tensor_tensor", writes=[t_lg], out=w2, in0=dd, in1=w1, op=ALU.mult)
                    op("dve", "tensor_scalar", writes=[t_lg], out=Wt, in0=E1, scalar1=w1, scalar2=None, op0=ALU.mult)
                    op("dve", "scalar_tensor_tensor", writes=[t_lg], out=Wt, in0=E2, scalar=w2, in1=Wt, op0=ALU.mult, op1=ALU.add)
                    pt_, t_pt_ = ps8.next()
                    k.group("pe", [tr(pt_[0:8, 0:128], Wt, ident)], reads=[t_lg, t_consts], writes=[t_pt_])
                    op("act", "copy", reads=[t_pt_], writes=[t_WT], out=WT[:, gblk * 128:(gblk + 1) * 128], in_=pt_[0:8, 0:128])
        if moe and b == 0:
            tap("WT", WT, t_WT)
        if b == 0:
            tap("h2_%d" % l, h2, t_h2)

        hids = Pool_([aa("hid%d" % i, [128, 2, TT], BF16) for i in range(2)])
        pend = {"f": None}

        def expert_pass(wg_d, wu_d, wd_d, F, scale_by=None):
            for fg in range(F // 256):
                wgb, t_wgb = load_w([(wrows(wg_d)[:, :, fg * 256:(fg + 1) * 256], KC)], KC, 256)
                wub, t_wub = load_w([(wrows(wu_d)[:, :, fg * 256:(fg + 1) * 256], KC)], KC, 256)
                wdb, t_wdb = load_w([(wrows(wd_d[fg * 256:(fg + 1) * 256, :]), 2)], 2, D)
                for t in range(NTILE):
                    tc_ = slice(t * TT, (t + 1) * TT)
                    hid, t_hid = hids.next()
                    pg_ = pend["f"]

                    def adv():
                        if pend["f"] is not None:
                            try:
                                next(pend["f"])
                            except StopIteration:
                                pend["f"] = None
                    for j in range(2):
                        pa, t_pa = psF.next()
                        k.group("pe", [mm(pa, wgb[:, kc, j * 128:(j + 1) * 128], h2[:, kc, tc_], start=(kc == 0), stop=(kc == KC - 1))
                                       for kc in range(KC)], reads=[t_wgb, t_h2], writes=[t_pa])
                        adv()
                        pu_, t_pu_ = psF.next()
                        k.group("pe", [mm(pu_, wub[:, kc, j * 128:(j + 1) * 128], h2[:, kc, tc_], start=(kc == 0), stop=(kc == KC - 1))
                                       for kc in range(KC)], reads=[t_wub, t_h2], writes=[t_pu_])
                        adv()
                        sa, t_sa = tmps.next()
                        op("act", "activation", reads=[t_pa], writes=[t_sa], out=sa, in_=pa, func=AF.Silu)
                        if scale_by is not None:
                            op("dve", "tensor_tensor", reads=[scale_by[1]], writes=[t_sa], out=sa, in0=sa, in1=scale_by[0][:, tc_], op=ALU.mult)
                        op("dve", "tensor_tensor", reads=[t_pu_, t_sa], writes=[t_hid], out=hid[:, j, :], in0=pu_, in1=sa, op=ALU.mult)
                    while pend["f"] is not None:
                        adv()

                    def down(wdb=wdb, t_wdb=t_wdb, hid=hid, t_hid=t_hid, tc_=tc_):
                        for d in range(KC):
                            pd, t_pd = psD.next()
                            k.group("pe", [mm(pd, wdb[:, j, d * 128:(d + 1) * 128], hid[:, j, :], start=(j == 0), stop=(j == 1)) for j in range(2)],
                                    reads=[t_wdb, t_hid], writes=[t_pd])
                            op("dve", "scalar_tensor_tensor", reads=[t_pd, t_mods], writes=[t_x], out=xT[:, d, tc_], in0=pd,
                               scalar=mods[:, l, b, GATE2, d:d + 1], in1=xT[:, d, tc_], op0=ALU.mult, op1=ALU.add)
                            if d % 2 == 1:
                                yield
                    pend["f"] = down()

        def flush():
            while pend["f"] is not None:
                try:
                    next(pend["f"])
                except StopIteration:
                    pend["f"] = None

        if not moe:
            expert_pass(ffn_g_d[0], ffn_u_d[0], ffn_d_d[0], FF_DENSE)
            flush()
        else:
            for e_ in range(cfg.get("nexp", NEXP)):
                for t in range(NTILE):
                    op("dve", "tensor_scalar", reads=[t_WT, t_consts], writes=[t_rhe], out=rhe, in0=WT[:, t * TT:(t + 1) * TT],
                       scalar1=consts[0:8, CO["ident"][0] + e_:CO["ident"][0] + e_ + 1], scalar2=None, op0=ALU.mult)
                    pw, t_pw = ps8.next()
                    k.group("pe", [mm(pw, ones_f[0:8, :], rhe)], reads=[t_rhe, t_ones], writes=[t_pw])
                    op("act", "copy", reads=[t_pw], writes=[t_Wbcs[e_ % 2]], out=Wbcs[e_ % 2][:, t * TT:(t + 1) * TT], in_=pw)
                expert_pass(moe_g_d[0, e_], moe_u_d[0, e_], moe_d_d[0, e_], FF_EXPERT, scale_by=(Wbcs[e_ % 2], t_Wbcs[e_ % 2]))
            flush()
        if b == 0:
            tap("xffn_%d" % l, xT, t_x)
        if not last:
            for t in range(NTILE):
                for c in range(KC):
                    k.dma("sp", xs_d[:, c, t * TT:(t + 1) * TT], xT[:, c, t * TT:(t + 1) * TT], reads=[t_x], writes=[t_xs[t][c]])
            return
        barrier()
        ar["off"] = FMARK
        xn, t_xn = aa("xn", [128, KC, TT])
        otm = Pool_([aa("otm%d" % i, [128, D]) for i in range(2)])
        for t in range(NTILE):
            for c in range(KC):
                op("act", "activation", reads=[t_x], writes=[t_sq], out=sq_b[:, c, :], in_=xT[:, c, t * TT:(t + 1) * TT], func=AF.Square)
            pb, t_pb = ps8.next()
            k.group("pe", [mm(pb, ones_b, sq_b[:, c, :], start=(c == 0), stop=(c == KC - 1)) for c in range(KC)],
                    reads=[t_sq, t_onesb], writes=[t_pb])
            op("act", "activation", reads=[t_pb], writes=[t_rb], out=rb, in_=pb, func=AF.Sqrt, bias=eps_c, scale=1.0 / D)
            op("dve", "reciprocal", writes=[t_rb], out=rb, in_=rb)
            for c in range(KC):
                op("dve", "scalar_tensor_tensor", reads=[t_x, t_spg, t_rb], writes=[t_xn], out=xn[:, c, :], in0=xT[:, c, t * TT:(t + 1) * TT],
                   scalar=spg[:, c:c + 1], in1=rb, op0=ALU.mult, op1=ALU.mult)
            for blk in range(4):
                ot, t_ot = otm.next()
                for half in range(2):
                    pt_, t_pt_ = ps8.next()
                    k.group("pe", [tr(pt_[:, i * 128:(i + 1) * 128], xn[:, half * 4 + i, blk * 128:(blk + 1) * 128], ident) for i in range(4)],
                            reads=[t_xn, t_consts], writes=[t_pt_])
                    if half == 0:
                        op("act", "copy", reads=[t_pt_], writes=[t_ot], out=ot[:, 0:512], in_=pt_)
                    else:
                        op("dve", "tensor_copy", reads=[t_pt_], writes=[t_ot], out=ot[:, 512:1024], in_=pt_)
                r0 = t * TT + blk * 128
                k.dma("sp", out_d[b, r0:r0 + 128, :], ot, reads=[t_ot], writes=[out_tok])

    for b in range(NSEQ):
        for l in range(LAYERS):
            layer_setup(l)
            mixer(l, b)
            if STAGE >= 6:
                ffn(l, b, last=(l == LAYERS - 1))

    fin = [out_tok] + list(tapd.values())
    if STAGE < 99:
        z, t_z = sbt("zz", [128, 8])
        op("dve", "memset", writes=[t_z], ap=z, constant=0.0)
        k.dma("sp", out_d[0, 0:128, 0:8], z, reads=[t_z], writes=[out_tok])
    k.finish(fin)
    nc._k = k
    return nc


def prep_core_inputs(inp, core):
    x = np.asarray(inp["x"], np.float32)
    b0 = 2 * core
    xT = np.ascontiguousarray(x[b0:b0 + 2].transpose(0, 2, 1))
    pos = np.asarray(inp["positions"], np.int32)[b0:b0 + 2]
    posb = np.ascontiguousarray(np.broadcast_to(pos[:, None, :], (2, 128, S)))
    c = np.asarray(inp["c"], np.float32)[b0:b0 + 2]
    cT = np.ascontiguousarray(c.reshape(2, KC, 128).transpose(2, 1, 0)).reshape(128, KC * 2)
    return {"xT": xT, "posb": posb, "cT": cT}


def shared_inputs(inp):
    ws = np.asarray(inp["gm_w_s"], np.float32)
    wst = np.ascontiguousarray(ws.transpose(3, 0, 1, 2)).reshape(128, DEPTH * 4 * 128)
    sh = {"smallp": make_smallp(inp), "consts": make_consts(), "wst": wst}
    for n in ["w_mod", "w_in", "w_br_gm", "w_br_da", "w_br_dn", "w_out", "ffn_w_gate", "ffn_w_up",
              "ffn_w_down", "moe_w_gate", "moe_w_up", "moe_w_down"]:
        sh[n] = np.ascontiguousarray(np.asarray(inp[n], np.float32))
    return sh


def kernel(**inputs):
    nc = build()
    sh = shared_inputs(inputs)
    in_maps = []
    for core in range(8):
        m = dict(sh)
        m.update(prep_core_inputs(inputs, core))
        in_maps.append(m)
    res = run_bass_kernel_spmd(nc, in_maps, core_ids=list(range(8)))
    return np.concatenate([np.asarray(r["out"], np.float32) for r in res.results], axis=0)
```

```python
import math
import numpy as np
import concourse.bass as bass
import concourse.mybir as mybir
from concourse.bass_utils import run_bass_kernel_spmd

F32 = mybir.dt.float32
BF16 = mybir.dt.bfloat16
I32 = mybir.dt.int32
AF = mybir.ActivationFunctionType
ALU = mybir.AluOpType
AX = mybir.AxisListType

SEM_LIMIT = 20000
ENGS = ("pe", "dve", "act", "pool", "sp")


class Tok:
    __slots__ = ("name", "w", "r", "dsem", "pr", "excl")

    def __init__(self, name, excl=False):
        self.name = name
        self.excl = excl
        self.w = None
        self.r = []
        self.dsem = None
        self.pr = []


class K:
    def __init__(self, nc, same=("dve", "act", "pool"), eager=True):
        self.nc = nc
        self.eager = eager
        self.embed = True
        self._collect = None
        self.e = {"pe": nc.tensor, "dve": nc.vector, "act": nc.scalar,
                  "pool": nc.gpsimd, "sp": nc.sync}
        self.nsem = 0
        self.sem = {}
        self.cnt = {}
        for n in ENGS:
            self._new_epoch(n)
        self.seq = {n: 0 for n in ENGS}
        self.last = {n: None for n in ENGS}
        self.sigmap = {n: [] for n in ENGS}
        self.waited = {n: {} for n in ENGS}
        self.same = set(same)
        self.rsem = {}
        self.store_recs = []
        self.ninstr = 0
        self.nwait = 0

    def _alloc(self, name):
        self.nsem += 1
        return self.nc.alloc_semaphore("%s_%d" % (name, self.nsem))

    def _new_epoch(self, n):
        self.sem[n] = self._alloc("e_" + n)
        self.cnt[n] = 0

    def _resolve(self, ev):
        if ev[0] == "c":
            _, eng, seq = ev
            lst = self.sigmap[eng]
            lo, hi = 0, len(lst)
            while lo < hi:
                mid = (lo + hi) // 2
                if lst[mid][0] >= seq:
                    hi = mid
                else:
                    lo = mid + 1
            if lo < len(lst):
                f = lst[lo]
                return f[1], f[2]
            assert self.seq[eng] >= seq and self.last[eng] is not None
            if self.cnt[eng] >= SEM_LIMIT:
                self._new_epoch(eng)
            self.cnt[eng] += 1
            self.last[eng].then_inc(self.sem[eng], 1)
            lst.append((self.seq[eng], self.sem[eng], self.cnt[eng]))
            return self.sem[eng], self.cnt[eng]
        rec = ev[1]
        return rec[0], rec[1]

    def _wait(self, eng, ev):
        if ev is None:
            return
        if ev[0] == "c" and ev[1] == eng and eng not in self.same:
            return
        sem, val = self._resolve(ev)
        w = self.waited[eng]
        if w.get(id(sem), 0) >= val:
            return
        w[id(sem)] = val
        if self._collect is not None:
            self._collect.append((sem, val))
            return
        self.e[eng].wait_ge(sem, val)
        self.nwait += 1

    def _pre(self, eng, reads, writes):
        for t in reads:
            self._wait(eng, t.w)
        for t in writes:
            self._wait(eng, t.w)
            for ev in t.r:
                self._wait(eng, ev)

    def _signal_last(self, eng):
        if self.cnt[eng] >= SEM_LIMIT:
            self._new_epoch(eng)
        self.cnt[eng] += 1
        self.last[eng].then_inc(self.sem[eng], 1)
        self.sigmap[eng].append((self.seq[eng], self.sem[eng], self.cnt[eng]))

    def _post(self, eng, reads, writes):
        if self.eager and (reads or writes):
            self._signal_last(eng)
        ev = ("c", eng, self.seq[eng])
        for t in reads:
            t.r.append(ev)
            if len(t.r) > 16:
                t.r = self._compact(t.r)
        for t in writes:
            t.w = ev
            t.r = []

    @staticmethod
    def _split(reads, writes):
        xr = [t for t in reads if t.excl]
        if xr:
            writes = list(writes) + [t for t in xr if t not in writes]
            reads = [t for t in reads if not t.excl]
        return reads, writes

    def _pre_embed(self, eng, reads, writes):
        if not self.embed:
            self._pre(eng, reads, writes)
            return None
        self._collect = []
        self._pre(eng, reads, writes)
        waits, self._collect = self._collect, None
        for sem, val in waits[:-1]:
            self.e[eng].wait_ge(sem, val)
            self.nwait += 1
        return waits[-1] if waits else None

    def op(self, eng, fn, reads=(), writes=()):
        reads, writes = self._split(reads, writes)
        w_ = self._pre_embed(eng, reads, writes)
        ins = fn(self.e[eng])
        if w_ is not None:
            ins._wait_ge(w_[0], w_[1])
        self.seq[eng] += 1
        self.last[eng] = ins
        self.ninstr += 1
        self._post(eng, reads, writes)
        return ins

    def group(self, eng, fns, reads=(), writes=()):
        reads, writes = self._split(reads, writes)
        w_ = self._pre_embed(eng, reads, writes)
        ins = None
        for i_, fn in enumerate(fns):
            ins = fn(self.e[eng])
            if i_ == 0 and w_ is not None:
                ins._wait_ge(w_[0], w_[1])
            self.seq[eng] += 1
            self.ninstr += 1
        self.last[eng] = ins
        self._post(eng, reads, writes)
        return ins

    def _compact(self, evs):
        best = {}
        other = []
        for ev in evs:
            if ev[0] == "c":
                k = ev[1]
                if k not in best or ev[2] >= best[k][2]:
                    best[k] = ev
            else:
                if not any(o[1] is ev[1] for o in other):
                    other.append(ev)
        return list(best.values()) + other

    def dma(self, q, out, in_, reads=(), writes=(), group=False, **kw):
        for t in reads:
            self._wait(q, t.w)
        for t in writes:
            if group:
                for ev in t.pr:
                    self._wait(q, ev)
            else:
                self._wait(q, t.w)
                for ev in t.r:
                    self._wait(q, ev)
                t.pr = ([t.w] if t.w is not None else []) + list(t.r)
        ins = self.e[q].dma_start(out=out, in_=in_, **kw)
        assert len(writes) <= 1
        if writes:
            t = writes[0]
            if t.dsem is None or t.dsem[1] >= SEM_LIMIT:
                t.dsem = [self._alloc("d_" + t.name), 0]
            rec = t.dsem
        else:
            if q not in self.rsem or self.rsem[q][1] >= SEM_LIMIT:
                self.rsem[q] = [self._alloc("r_" + q), 0]
            rec = self.rsem[q]
        rec[1] += 16
        ins.then_inc(rec[0], 16)
        ev = ("d", rec)
        if reads and not any(r is rec for r in self.store_recs):
            self.store_recs.append(rec)
        if writes:
            writes[0].w = ev
            writes[0].r = []
        for s in reads:
            s.r.append(ev)
            if len(s.r) > 16:
                s.r = self._compact(s.r)
        self.ninstr += 1
        return ins

    def finish(self, toks, eng="sp"):
        for t in toks:
            self._wait(eng, t.w)
            for ev in t.r:
                self._wait(eng, ev)


D = 1024
KC = 8
S = 2048
TT = 512
NT = S // TT
INW = 7688
DEPTH = 2
EPS = 1e-6
FF_DENSE = 2816
FF_EXPERT = 3584
NEXP = 8
PI = math.pi
C1 = 6.28125
C2 = 2.0 * math.pi - 6.28125
NEG = -30000.0

COL_U, COL_V, COL_Q, COL_KK, COL_VA = 0, 512, 1024, 1536, 2048
COL_DQ, COL_DK, COL_DV, COL_DZ, COL_AB, COL_G = 2560, 3072, 3584, 4096, 4608, 4616

SP_LAYOUT = [("n1g", 8), ("n2g", 8), ("bmod", 48), ("lng", 512), ("lnb", 512), ("bs", 512),
             ("lam", 256), ("subg", 128), ("convw", 48), ("alog", 4), ("dtb", 4), ("dng", 1)]
SP_OFF = {}
_o = 0
for _n, _w in SP_LAYOUT:
    SP_OFF[_n] = (_o, _w)
    _o += _w
SP_LAYER = _o
SP_FING = 2 * SP_LAYER
SP_WR = SP_FING + 8
SP_TOTAL = SP_WR + 64

CO = {}
_o = 0
for _n, _w in [("ident", 128), ("rot", 128), ("tt", 64), ("su", 64), ("negu", 256), ("negl", 256),
               ("mstrict", 256), ("i4", 256), ("invf", 1)]:
    CO[_n] = (_o, _w)
    _o += _w
CO_TOTAL = _o


def make_consts():
    c = np.zeros((128, CO_TOTAL), np.float32)
    o = CO["ident"][0]
    c[:, o:o + 128] = np.eye(128, dtype=np.float32)
    o = CO["rot"][0]
    for m in range(128):
        if (m % 64) < 32:
            c[m + 32, o + m] = -1.0
        else:
            c[m - 32, o + m] = 1.0
    p = np.arange(64)[:, None]
    j = np.arange(64)[None, :]
    o = CO["tt"][0]
    c[:64, o:o + 64] = (p <= j)
    o = CO["su"][0]
    c[:64, o:o + 64] = (p > j)
    for h in range(4):
        o = CO["negu"][0] + h * 64
        c[:64, o:o + 64] = np.where(j > p, NEG, 0.0)
        o = CO["negl"][0] + h * 64
        c[:64, o:o + 64] = np.where(j < p, NEG, 0.0)
        o = CO["mstrict"][0] + h * 64
        c[:64, o:o + 64] = (j < p)
        o = CO["i4"][0] + h * 64
        c[:64, o:o + 64] = (j == p)
    inv_freq = (10000.0 ** (-np.arange(0, 64, 2, dtype=np.float32) / np.float32(64))).astype(np.float32)
    c[:, CO["invf"][0]] = inv_freq[np.arange(128) % 32]
    return c


def fm(v):
    v = np.asarray(v, np.float32)
    return np.ascontiguousarray(v.reshape(-1, 128).T)


def bc(v):
    v = np.asarray(v, np.float32).reshape(1, -1)
    return np.broadcast_to(v, (128, v.shape[1]))


def make_smallp(inp):
    sp = np.zeros((128, SP_TOTAL), np.float32)
    for l in range(DEPTH):
        base = l * SP_LAYER

        def put(name, arr):
            o, w = SP_OFF[name]
            assert arr.shape == (128, w), (name, arr.shape, w)
            sp[:, base + o:base + o + w] = arr
        put("n1g", fm(inp["norm1_g"][l]))
        put("n2g", fm(inp["norm2_g"][l]))
        put("bmod", fm(inp["b_mod"][l]))
        put("lng", bc(inp["gm_ln_g"][l]))
        put("lnb", bc(inp["gm_ln_b"][l]))
        put("bs", bc(inp["gm_b_s"][l].reshape(-1)))
        put("lam", bc(inp["da_lambda"][l].reshape(-1)))
        put("subg", bc(inp["da_subln_g"][l]))
        cw = np.asarray(inp["dn_conv_w"][l], np.float32)
        put("convw", np.ascontiguousarray(cw.reshape(4, 12, 128).transpose(2, 1, 0)).reshape(128, 48))
        put("alog", bc(inp["dn_a_log"][l]))
        put("dtb", bc(inp["dn_dt_bias"][l]))
        put("dng", np.asarray(inp["dn_norm_g"][l], np.float32).reshape(128, 1))
    sp[:, SP_FING:SP_FING + 8] = fm(inp["final_g"])
    wr = np.asarray(inp["moe_w_router"][0], np.float32)
    sp[:, SP_WR:SP_WR + 64] = wr.reshape(8, 128, 8).transpose(1, 0, 2).reshape(128, 64)
    return sp


def lambda_init_fn(layer):
    return 0.8 - 0.6 * math.exp(-0.3 * layer)


class Pool_:
    def __init__(self, items):
        self.items = items
        self.i = 0

    def next(self):
        it = self.items[self.i % len(self.items)]
        self.i += 1
        return it


def build(cfg=None):
    cfg = dict(cfg or {})
    NSEQ = cfg.get("nseq", 2)
    NTILE = cfg.get("ntile", NT)
    LAYERS = cfg.get("layers", DEPTH)
    STAGE = cfg.get("stage", 99)
    taps = cfg.get("taps", ())
    ACUT = cfg.get("acut", 99)

    nc = bass.Bass("TRN2", target_bir_lowering=False)
    k = K(nc, same=tuple(cfg.get("same", ("dve", "act", "pool"))), eager=cfg.get("eager", True))
    k.embed = cfg.get("embed", True)
    k.trans = cfg.get("trans", True)

    def din(name, shape, dt=F32):
        return nc.dram_tensor(name, list(shape), dt, kind="ExternalInput").ap()

    xT_d = din("xT", [2, D, S])
    posb_d = din("posb", [2, 128, S], I32)
    cT_d = din("cT", [128, KC * 2])
    smallp_d = din("smallp", [128, SP_TOTAL])
    consts_d = din("consts", [128, CO_TOTAL])
    wst_d = din("wst", [128, DEPTH * 4 * 128])
    w_mod_d = din("w_mod", [DEPTH, D, 6 * D])
    w_in_d = din("w_in", [DEPTH, D, INW])
    w_br_d = [din("w_br_gm", [DEPTH, 512, D]), din("w_br_da", [DEPTH, 512, D]), din("w_br_dn", [DEPTH, 512, D])]
    w_out_d = din("w_out", [DEPTH, D, D])
    ffn_g_d = din("ffn_w_gate", [1, D, FF_DENSE])
    ffn_u_d = din("ffn_w_up", [1, D, FF_DENSE])
    ffn_d_d = din("ffn_w_down", [1, FF_DENSE, D])
    moe_g_d = din("moe_w_gate", [1, NEXP, D, FF_EXPERT])
    moe_u_d = din("moe_w_up", [1, NEXP, D, FF_EXPERT])
    moe_d_d = din("moe_w_down", [1, NEXP, FF_EXPERT, D])
    out_d = nc.dram_tensor("out", [2, S, D], F32, kind="ExternalOutput").ap()
    out_tok = Tok("out")
    xs_d = nc.dram_tensor("xs_scratch", [128, KC, S], F32, kind="Internal").ap()
    t_xs = [[Tok("xs%d_%d" % (i, e)) for e in range(KC)] for i in range(NT)]

    tapd = {}

    def sb(name, shape, dt=F32):
        return nc.alloc_sbuf_tensor("s_" + name, list(shape), dt).ap()

    def sbt(name, shape, dt=F32):
        return sb(name, shape, dt), Tok(name)

    def tap(name, ap, tok):
        if name not in taps or name in tapd:
            return
        d = nc.dram_tensor("tap_" + name, list(ap.shape), ap.dtype, kind="ExternalOutput").ap()
        t = Tok("tap_" + name)
        k.dma("sp", d, ap, reads=[tok], writes=[t])
        tapd[name] = t

    def op(eng, fn, reads=(), writes=(), **kw):
        return k.op(eng, lambda e: getattr(e, fn)(**kw), reads=reads, writes=writes)

    def mm(out, lhsT, rhs, start=True, stop=True):
        return lambda e: e.matmul(out, lhsT=lhsT, rhs=rhs, start=start, stop=stop)

    def tr(out, in_, idn):
        return lambda e: e.transpose(out, in_, idn)

    def barrier():
        engs = ("pe", "dve", "act", "pool")
        evs = {n: ("c", n, k.seq[n]) for n in engs if k.last[n] is not None}
        for a in engs + ("sp",):
            for b_, ev in evs.items():
                if a != b_:
                    k._wait(a, ev)
            for rec in k.store_recs:
                k._wait(a, ("d", rec))

    consts, t_consts = sbt("consts", [128, CO_TOTAL])
    spl_t, t_spl = sbt("spl", [128, SP_LAYER])
    spg, t_spg = sbt("spg", [128, 72])
    k.dma("sp", consts, consts_d, writes=[t_consts])
    k.dma("sp", spg, smallp_d[:, SP_FING:SP_FING + 72], writes=[t_spg])

    def cst(name, rows=128):
        o, w = CO[name]
        return consts[0:rows, o:o + w]

    def spl(name):
        o, w = SP_OFF[name]
        return spl_t[:, o:o + w]

    ident = cst("ident")
    ones_f, t_ones = sbt("ones_f", [128, 128])
    ones_b, t_onesb = sbt("ones_b", [128, 128], BF16)
    nones_f, t_nones = sbt("nones_f", [128, 128])
    ident_b, t_identb = sbt("ident_b", [128, 128], BF16)
    rot_b, t_rotb = sbt("rot_b", [128, 128], BF16)
    ntt_f, t_ntt = sbt("ntt_f", [64, 64])
    op("dve", "memset", writes=[t_ones], ap=ones_f, constant=1.0)
    op("dve", "memset", writes=[t_onesb], ap=ones_b, constant=1.0)
    op("dve", "memset", writes=[t_nones], ap=nones_f, constant=-1.0)
    op("dve", "tensor_copy", reads=[t_consts], writes=[t_identb], out=ident_b, in_=ident)
    op("dve", "tensor_copy", reads=[t_consts], writes=[t_rotb], out=rot_b, in_=cst("rot"))
    op("dve", "tensor_scalar", reads=[t_consts], writes=[t_ntt], out=ntt_f, in0=cst("tt", 64), scalar1=-1.0,
       scalar2=None, op0=ALU.mult)
    TC = [t_consts, t_ones, t_onesb, t_nones, t_identb, t_rotb, t_ntt]

    psall = nc.alloc_psum_tensor("psall", [128, 8 * 512], F32).ap()
    banks = [(psall[:, i * 512:(i + 1) * 512], Tok("bank%d" % i, excl=True)) for i in range(8)]
    psA = Pool_(banks[0:4])
    psF = Pool_(banks[0:4])
    psD = Pool_(banks[4:8])
    psS = Pool_(banks[4:6])
    psO = Pool_(banks[0:2])
    psT = Pool_(banks[2:4])
    ps8 = Pool_(banks)
    psDN = Pool_(banks[6:8]) if not cfg.get("dn8") else ps8
    S_big = psall[:, 4 * 512:8 * 512]
    t_Sbig = [b_[1] for b_ in banks[4:8]]

    NSLOT = cfg.get("nslot", 7)
    wbf = Pool_([sbt("wbf%d" % i, [128, 2048], BF16) for i in range(NSLOT)])

    def load_w(srcs, a, b_):
        wb, t_wb = wbf.next()
        wbv = wb[:, 0:a * b_].rearrange("p (a b) -> p a b", b=b_)
        o = 0
        for i, (src, ai) in enumerate(srcs):
            k.dma("pool", wbv[:, o:o + ai, :], src, writes=[t_wb], group=(i > 0))
            o += ai
        assert o == a
        return wbv, t_wb

    def wrows(ap2d):
        return ap2d.rearrange("(kc p) n -> p kc n", p=128)

    cact, t_cact = sbt("cact", [128, KC * 2])
    k.dma("sp", cact, cT_d, writes=[t_cact])
    op("act", "activation", writes=[t_cact], out=cact, in_=cact, func=AF.Silu)
    cact_b, t_cactb = sbt("cact_b", [128, KC * 2], BF16)
    op("dve", "tensor_copy", reads=[t_cact], writes=[t_cactb], out=cact_b, in_=cact)
    cact3 = cact_b.rearrange("p (c b) -> p c b", b=2)
    modT, t_mod = sbt("modT", [128, DEPTH, 48, 2])
    mods, t_mods = sbt("mods", [128, DEPTH, 2, 6, 8])
    bmod_t, t_bmod = sbt("bmod_t", [128, 64])
    for l in range(LAYERS):
        k.dma("sp", bmod_t, smallp_d[:, l * SP_LAYER:l * SP_LAYER + 64], writes=[t_bmod])
        for g in range(24):
            w32, t_w32 = load_w([(wrows(w_mod_d[l])[:, :, g * 256:(g + 1) * 256], KC)], KC, 256)
            pb, t_pb = psA.next()
            fns = []
            for j in range(2):
                for kc in range(KC):
                    fns.append(mm(pb[:, j * 2:j * 2 + 2], w32[:, kc, j * 128:(j + 1) * 128], cact3[:, kc, :],
                                  start=(kc == 0), stop=(kc == KC - 1)))
            k.group("pe", fns, reads=[t_w32, t_cactb], writes=[t_pb])
            op("dve", "tensor_copy", reads=[t_pb], writes=[t_mod], out=modT[:, l, g * 2:(g + 1) * 2, :],
               in_=pb[:, 0:4].rearrange("p (j b) -> p j b", b=2))
        for b in range(2):
            op("dve", "tensor_tensor", reads=[t_bmod], writes=[t_mod], out=modT[:, l, :, b], in0=modT[:, l, :, b],
               in1=bmod_t[:, 16:64], op=ALU.add)
        for b in range(2):
            for part in range(6):
                src = modT[:, l, part * 8:(part + 1) * 8, b]
                dst = mods[:, l, b, part, :]
                if part in (1, 4):
                    go = 0 if part == 1 else 8
                    op("dve", "scalar_tensor_tensor", reads=[t_mod, t_bmod], writes=[t_mods], out=dst, in0=src, scalar=1.0,
                       in1=bmod_t[:, go:go + 8], op0=ALU.add, op1=ALU.mult)
                else:
                    op("dve", "tensor_copy", reads=[t_mod], writes=[t_mods], out=dst, in_=src)
    tap("mods", mods, t_mods)
    SHIFT1, G1S, GATE1, SHIFT2, G2S, GATE2 = 0, 1, 2, 3, 4, 5

    wsT_b, t_wst = sbt("wsT_b", [128, 4, 128], BF16)
    lay, t_lay = sbt("lay", [128, 16])
    subgs, t_subgs = sbt("subgs", [128, 128])
    halo, t_halo = sbt("halo", [128, 12, 3])
    Sst, t_S = sbt("Sst", [128, 4, 128])
    lamtmp = sbt("lamtmp", [128, 2, 64])

    rem = int(nc.sbuf_bytes_remaining)
    AW = (rem - 1024) // 4
    arena = sb("arena", [128, AW])
    ar = {"off": 0}
    tokcache = {}

    def aa(name, shape, dt=F32):
        n = 1
        for s_ in shape[1:]:
            n *= s_
        words = (n * (4 if dt in (F32, I32) else 2) + 3) // 4
        words = (words + 7) // 8 * 8
        o = ar["off"]
        assert o + words <= AW, ("arena overflow", name, o, words, AW)
        ar["off"] = o + words
        v = arena[0:shape[0], o:o + words]
        if dt != F32:
            v = v.bitcast(dt)
        v = v[:, 0:n]
        if len(shape) == 3:
            v = v.rearrange("p (a b) -> p a b", b=shape[2])
        elif len(shape) == 4:
            v = v.rearrange("p (a b c) -> p a b c", b=shape[2], c=shape[3])
        key = (name, o)
        if key not in tokcache:
            tokcache[key] = Tok(name)
        return v, tokcache[key]

    def layer_setup(l):
        k.dma("sp", spl_t, smallp_d[:, l * SP_LAYER:(l + 1) * SP_LAYER], writes=[t_spl])
        k.dma("pool", wsT_b, wst_d[:, l * 512:(l + 1) * 512].rearrange("p (g i) -> p g i", i=128), writes=[t_wst])
        op("dve", "memset", writes=[t_wst], ap=wsT_b[64:128, :, 0:64], constant=0.0)
        lam_v = spl("lam").rearrange("p (a d) -> p a d", d=64)
        pr_, t_pr = lamtmp
        op("dve", "tensor_tensor", reads=[t_spl], writes=[t_pr], out=pr_[:, 0, :], in0=lam_v[:, 0, :], in1=lam_v[:, 1, :], op=ALU.mult)
        op("dve", "tensor_tensor", reads=[t_spl], writes=[t_pr], out=pr_[:, 1, :], in0=lam_v[:, 2, :], in1=lam_v[:, 3, :], op=ALU.mult)
        op("dve", "reduce_sum", reads=[t_pr], writes=[t_lay], out=lay[:, 8:10], in_=pr_, axis=AX.X)
        op("act", "activation", reads=[], writes=[t_lay], out=lay[:, 8:10], in_=lay[:, 8:10], func=AF.Exp)
        op("dve", "scalar_tensor_tensor", reads=[], writes=[t_lay], out=lay[:, 0:1], in0=lay[:, 9:10], scalar=-lambda_init_fn(l),
           in1=lay[:, 8:9], op0=ALU.add, op1=ALU.subtract)
        op("dve", "tensor_scalar", reads=[t_spl], writes=[t_subgs], out=subgs, in0=spl("subg"), scalar1=1.0 - lambda_init_fn(l),
           scalar2=None, op0=ALU.mult)
        op("act", "activation", reads=[t_spl], writes=[t_lay], out=lay[:, 4:8], in_=spl("alog"), func=AF.Exp)
        op("dve", "tensor_scalar", reads=[], writes=[t_lay], out=lay[:, 4:8], in0=lay[:, 4:8], scalar1=-1.0, scalar2=None, op0=ALU.mult)

    def modnorm(xsrc, t_xsrc, c0, l, b, gidx, sidx, hdst, t_hd, sq_b, t_sq, rb, t_rb, tmps, h32=None):
        for c in range(KC):
            op("act", "activation", reads=[t_xsrc], writes=[t_sq], out=sq_b[:, c, :], in_=xsrc[:, c, c0:c0 + TT], func=AF.Square)
        pb, t_pb = psA.next()
        k.group("pe", [mm(pb, ones_b, sq_b[:, c, :], start=(c == 0), stop=(c == KC - 1)) for c in range(KC)],
                reads=[t_sq, t_onesb], writes=[t_pb])
        op("act", "activation", reads=[t_pb], writes=[t_rb], out=rb, in_=pb, func=AF.Sqrt, bias=eps_c, scale=1.0 / D)
        op("dve", "reciprocal", writes=[t_rb], out=rb, in_=rb)
        for c in range(KC):
            tm, t_tm = tmps.next()
            op("dve", "scalar_tensor_tensor", reads=[t_xsrc, t_mods, t_rb], writes=[t_tm], out=tm, in0=xsrc[:, c, c0:c0 + TT],
               scalar=mods[:, l, b, gidx, c:c + 1], in1=rb, op0=ALU.mult, op1=ALU.mult)
            if h32 is not None:
                op("act", "activation", reads=[t_tm, t_mods], writes=[h32[1]], out=h32[0][:, c, :], in_=tm, func=AF.Identity,
                   bias=mods[:, l, b, sidx, c:c + 1], scale=1.0)
                op("dve", "tensor_copy", reads=[h32[1]], writes=[t_hd], out=hdst[:, c, :], in_=h32[0][:, c, :])
            else:
                op("act", "activation", reads=[t_tm, t_mods], writes=[t_hd], out=hdst[:, c, :], in_=tm, func=AF.Identity,
                   bias=mods[:, l, b, sidx, c:c + 1], scale=1.0)

    epsc, t_epsc = sbt("epsc", [128, 4])
    op("dve", "memset", writes=[t_epsc], ap=epsc[:, 0:1], constant=EPS)
    op("dve", "memset", writes=[t_epsc], ap=epsc[:, 1:2], constant=1.0)
    op("dve", "memset", writes=[t_epsc], ap=epsc[:, 2:3], constant=-PI)
    eps_c = epsc[:, 0:1]
    one_c = epsc[:, 1:2]
    npi_c = epsc[:, 2:3]

    def mixer(l, b):
        barrier()
        ar["off"] = 0
        hT, t_h = aa("hT", [128, KC, TT], BF16)
        kT, t_kT = aa("kT", [128, 4, S], BF16)
        Vt, t_V = aa("Vt", [128, 16, 512], BF16)
        y_gm, t_ygm = aa("y_gm", [128, 4, TT], BF16)
        y_da, t_yda = aa("y_da", [128, 4, TT], BF16)
        y_dn, t_ydn = aa("y_dn", [128, 4, TT], BF16)
        tmps = Pool_([aa("tmpA%d" % i, [128, TT]) for i in range(3)])
        tmpb = Pool_([aa("tmpB%d" % i, [128, TT], BF16) for i in range(2)])
        st, t_st = aa("stat", [128, 32])
        P1_END = ar["off"]
        op("dve", "memset", writes=[t_halo], ap=halo, constant=0.0)
        op("dve", "memset", writes=[t_S], ap=Sst, constant=0.0)

        def wg(col0, ncols):
            return load_w([(wrows(w_in_d[l])[:, :, col0:col0 + ncols], KC)], KC, ncols)

        def proj_fm(wb, t_wb, nchunk, consume):
            for j in range(nchunk):
                pb, t_pb = psA.next()
                k.group("pe", [mm(pb, wb[:, kc, j * 128:(j + 1) * 128], hT[:, kc, :], start=(kc == 0), stop=(kc == KC - 1))
                               for kc in range(KC)], reads=[t_wb, t_h], writes=[t_pb])
                consume(j, pb, t_pb)

        def proj_tm(wb, t_wb, ncols, consume):
            for blk in range(4):
                pb, t_pb = psA.next()
                k.group("pe", [mm(pb[:, 0:ncols], hT[:, kc, blk * 128:(blk + 1) * 128], wb[:, kc, 0:ncols], start=(kc == 0),
                                  stop=(kc == KC - 1)) for kc in range(KC)], reads=[t_wb, t_h], writes=[t_pb])
                consume(blk, pb, t_pb)

        for t in range(NTILE):
            c0 = t * TT
            ar["off"] = P1_END
            xt, t_xt = aa("xt", [128, KC, TT])
            if l == 0:
                for c in range(KC):
                    k.dma("sp", xt[:, c, :], xT_d[b, c * 128:(c + 1) * 128, c0:c0 + TT], writes=[t_xt], group=(c > 0))
            else:
                for c in range(KC):
                    k.dma("sp", xt[:, c, :], xs_d[:, c, c0:c0 + TT], reads=[t_xs[t][c]], writes=[t_xt], group=(c > 0))
            rb, t_rb = aa("rb", [128, TT])
            sq_b, t_sq = aa("sq_b", [128, KC, TT], BF16)
            modnorm(xt, t_xt, 0, l, b, G1S, SHIFT1, hT, t_h, sq_b, t_sq, rb, t_rb, tmps)
            if l == 0 and t == 0:
                tap("h0", hT, t_h)
            if STAGE <= 1:
                continue
            barrier()
            ar["off"] = P1_END
            SUB0 = P1_END

            u_g, t_ug = aa("u_g", [128, 4, TT], BF16)
            vg, t_vg = aa("vg", [128, 4, 512])
            vln, t_vln = aa("vln", [128, 4, 512], BF16)
            for g2 in range(2):
                wb, t_wb = wg(COL_U + g2 * 256, 256)
                proj_fm(wb, t_wb, 2, lambda j, pb, t_pb, g2=g2: op(
                    "act", "activation", reads=[t_pb], writes=[t_ug], out=u_g[:, g2 * 2 + j, :], in_=pb, func=AF.Gelu))
            for g2 in range(2):
                wb, t_wb = wg(COL_V + g2 * 256, 256)
                proj_tm(wb, t_wb, 256, lambda blk, pb, t_pb, g2=g2: op(
                    "act", "activation", reads=[t_pb], writes=[t_vg], out=vg[:, blk, g2 * 256:(g2 + 1) * 256], in_=pb[:, 0:256], func=AF.Gelu))
            for blk in range(4):
                op("dve", "bn_stats", reads=[t_vg], writes=[t_st], out=st[:, 0:6], in_=vg[:, blk, :])
                op("dve", "bn_aggr", writes=[t_st], out=st[:, 8:10], in_=st[:, 0:6])
                op("act", "activation", writes=[t_st], out=st[:, 10:11], in_=st[:, 9:10], func=AF.Sqrt, bias=eps_c, scale=1.0)
                op("dve", "reciprocal", writes=[t_st], out=st[:, 10:11], in_=st[:, 10:11])
                tm, t_tm = tmps.next()
                op("dve", "tensor_scalar", reads=[t_vg, t_st], writes=[t_tm], out=tm, in0=vg[:, blk, :], scalar1=st[:, 8:9],
                   scalar2=st[:, 10:11], op0=ALU.subtract, op1=ALU.mult)
                op("dve", "tensor_tensor", reads=[t_spl], writes=[t_tm], out=tm, in0=tm, in1=spl("lng"), op=ALU.mult)
                op("dve", "tensor_tensor", reads=[t_spl, t_tm], writes=[t_vln], out=vln[:, blk, :], in0=tm, in1=spl("lnb"), op=ALU.add)
            for g in range(4):
                pb, t_pb = psA.next()
                k.group("pe", [mm(pb[:, blk * 128:(blk + 1) * 128], vln[:, blk, g * 128:(g + 1) * 128], wsT_b[:, g, :])
                               for blk in range(4)], reads=[t_vln, t_wst], writes=[t_pb])
                tm, t_tm = tmps.next()
                bsb = spl("bs")[:, g * 128:(g + 1) * 128].unsqueeze(1).to_broadcast([128, 4, 128])
                op("dve", "tensor_tensor", reads=[t_pb, t_spl], writes=[t_tm], out=tm.rearrange("p (a b) -> p a b", b=128),
                   in0=pb.rearrange("p (a b) -> p a b", b=128), in1=bsb, op=ALU.add)
                op("dve", "tensor_tensor", reads=[t_tm, t_ug], writes=[t_ygm], out=y_gm[:, g, :], in0=tm, in1=u_g[:, g, :], op=ALU.mult)
            if l == 0 and t == 0:
                tap("ygm", y_gm, t_ygm)
            if STAGE <= 2:
                continue
            barrier()
            ar["off"] = SUB0

            cosT, t_cos = aa("cosT", [128, TT])
            sinT, t_sin = aa("sinT", [128, TT])
            qT, t_qT = aa("qT", [128, 4, TT], BF16)
            PT, t_PT = aa("PT", [128, 16, 128], BF16)
            yv, t_yv = aa("yv", [128, 128])
            junk, t_junk = aa("junk", [128, 128])
            Ssb = [aa("Ssb%d" % i, [128, S]) for i in range(2)]
            Pms = [aa("Pm%d" % i, [128, S], BF16) for i in range(2)]
            asts = [aa("ast%d" % i, [128, 8]) for i in range(2)]
            rss = [aa("rs%d" % i, [128, 8]) for i in range(2)]
            ytms = [aa("ytm%d" % i, [128, 512], BF16) for i in range(2)]
            pOs = {}
            qn, t_qn = aa("qn", [128, 4, TT], BF16)
            kn, t_kn = aa("kn", [128, 4, TT], BF16)
            vn, t_vn = aa("vn", [128, 4, TT], BF16)
            Sb, t_Sb = aa("Sb", [128, 4, 128], BF16)
            zs, t_zs = aa("zs", [128, 4, TT], BF16)
            oT, t_oT = aa("oT", [128, 4, TT])
            ab, t_ab = aa("ab", [64, 8, 8])
            gb, t_gb = aa("gb", [64, 4, 8, 4])
            dst_ = {0: (qn, t_qn), 1: (kn, t_kn), 2: (vn, t_vn)}
            op("act", "copy", reads=[t_S], writes=[t_Sb], out=Sb, in_=Sst)
            cwv = spl("convw").rearrange("p (c j) -> p c j", j=4)
            OV0 = ar["off"]
            posi, t_posi = aa("posi", [128, TT], I32)
            k.dma("sp", posi, posb_d[b, :, c0:c0 + TT], writes=[t_posi])
            ang, t_ang = aa("ang", [128, TT])
            kq, t_kq = aa("kq", [128, TT])
            ki, t_ki = aa("ki", [128, TT], I32)
            op("dve", "tensor_copy", reads=[t_posi], writes=[t_ang], out=ang, in_=posi)
            op("dve", "tensor_scalar", reads=[t_consts], writes=[t_ang], out=ang, in0=ang, scalar1=cst("invf"), scalar2=None, op0=ALU.mult)
            op("dve", "tensor_scalar", reads=[t_ang], writes=[t_kq], out=kq, in0=ang, scalar1=1.0 / (2 * PI), scalar2=None, op0=ALU.mult)
            op("dve", "tensor_copy", reads=[t_kq], writes=[t_ki], out=ki, in_=kq)
            op("dve", "tensor_copy", reads=[t_ki], writes=[t_kq], out=kq, in_=ki)
            op("dve", "scalar_tensor_tensor", reads=[t_kq], writes=[t_ang], out=ang, in0=kq, scalar=-C1, in1=ang, op0=ALU.mult, op1=ALU.add)
            op("dve", "scalar_tensor_tensor", reads=[t_kq], writes=[t_ang], out=ang, in0=kq, scalar=-C2, in1=ang, op0=ALU.mult, op1=ALU.add)

            op("dve", "tensor_scalar", reads=[t_ang], writes=[t_kq], out=kq, in0=ang, scalar1=PI, scalar2=None, op0=ALU.is_gt)
            op("dve", "scalar_tensor_tensor", reads=[t_kq, t_ang], writes=[t_sin], out=sinT, in0=kq, scalar=-2 * PI, in1=ang, op0=ALU.mult, op1=ALU.add)
            op("dve", "tensor_scalar", reads=[t_sin], writes=[t_kq], out=kq, in0=sinT, scalar1=PI / 2, scalar2=None, op0=ALU.is_gt)
            op("dve", "scalar_tensor_tensor", reads=[t_kq, t_sin], writes=[t_cos], out=cosT, in0=kq, scalar=-2 * PI, in1=sinT, op0=ALU.mult, op1=ALU.add)
            op("dve", "tensor_scalar", writes=[t_cos], out=cosT, in0=cosT, scalar1=PI / 2, scalar2=None, op0=ALU.add)
            op("act", "activation", writes=[t_sin], out=sinT, in_=sinT, func=AF.Sin)
            op("act", "activation", writes=[t_cos], out=cosT, in_=cosT, func=AF.Sin)
            if l == 0 and t == 0:
                tap("cos", cosT, t_cos)
                tap("sin", sinT, t_sin)
            cws = Pool_([aa("cw%d" % i, [128, TT + 3]) for i in range(2)])

            def rope_consume(dst_fn, t_dst):
                def f(j, pb, t_pb, hbase):
                    h = hbase + j
                    rw, t_rw = tmpb.next()
                    op("act", "copy", reads=[t_pb], writes=[t_rw], out=rw, in_=pb)
                    pr, t_pr = psA.next()
                    k.group("pe", [mm(pr, rot_b, rw)], reads=[t_rw, t_rotb], writes=[t_pr])
                    t1, t_t1 = tmps.next()
                    t2, t_t2 = tmps.next()
                    op("dve", "tensor_tensor", reads=[t_pb, t_cos], writes=[t_t1], out=t1, in0=pb, in1=cosT, op=ALU.mult)
                    op("dve", "tensor_tensor", reads=[t_pr, t_sin], writes=[t_t2], out=t2, in0=pr, in1=sinT, op=ALU.mult)
                    op("dve", "tensor_tensor", reads=[t_t1, t_t2], writes=[t_dst], out=dst_fn(h), in0=t1, in1=t2, op=ALU.add)
                return f
            qcons = rope_consume(lambda h: qT[:, h, :], t_qT)
            kcons = rope_consume(lambda h: kT[:, h, c0:c0 + TT], t_kT)
            for g2 in range(2 if "q" in cfg.get("parts", "qkv") else 0):
                wb, t_wb = wg(COL_Q + g2 * 256, 256)
                proj_fm(wb, t_wb, 2, lambda j, pb, t_pb, g2=g2: qcons(j, pb, t_pb, g2 * 2))
            for g2 in range(2 if "k" in cfg.get("parts", "qkv") else 0):
                wb, t_wb = wg(COL_KK + g2 * 256, 256)
                proj_fm(wb, t_wb, 2, lambda j, pb, t_pb, g2=g2: kcons(j, pb, t_pb, g2 * 2))
            for g2 in range(2 if "v" in cfg.get("parts", "qkv") else 0):
                wb, t_wb = wg(COL_VA + g2 * 256, 256)
                proj_tm(wb, t_wb, 256, lambda blk, pb, t_pb, g2=g2: op(
                    "act", "copy", reads=[t_pb], writes=[t_V], out=Vt[:, t * 4 + blk, g2 * 256:(g2 + 1) * 256], in_=pb[:, 0:256]))
            if l == 0 and t == 0:
                tap("qT", qT, t_qT)
                tap("kT", kT[:, :, 0:TT], t_kT)

            def dn_consume(j, pb, t_pb, ci0):
                ci = ci0 + j
                which, h = ci // 4, ci % 4
                dst, t_dst = dst_[which]
                cw, t_cw = cws.next()
                op("dve", "tensor_copy", reads=[t_halo], writes=[t_cw], out=cw[:, 0:3], in_=halo[:, ci, :])
                op("act", "copy", reads=[t_pb], writes=[t_cw], out=cw[:, 3:TT + 3], in_=pb)
                op("dve", "tensor_copy", reads=[t_cw], writes=[t_halo], out=halo[:, ci, :], in_=cw[:, TT:TT + 3])
                acc, t_acc = tmps.next()
                op("dve", "tensor_scalar", reads=[t_cw, t_spl], writes=[t_acc], out=acc, in0=cw[:, 3:TT + 3], scalar1=cwv[:, ci, 3:4],
                   scalar2=None, op0=ALU.mult)
                for jj in range(3):
                    op("dve", "scalar_tensor_tensor", reads=[t_cw, t_spl], writes=[t_acc], out=acc, in0=cw[:, jj:jj + TT],
                       scalar=cwv[:, ci, jj:jj + 1], in1=acc, op0=ALU.mult, op1=ALU.add)
                if which == 2:
                    op("act", "activation", reads=[t_acc], writes=[t_dst], out=dst[:, h, :], in_=acc, func=AF.Silu)
                else:
                    op("act", "activation", reads=[], writes=[t_acc], out=acc, in_=acc, func=AF.Silu)
                    sqb, t_sqb = tmpb.next()
                    op("act", "activation", reads=[t_acc], writes=[t_sqb], out=sqb, in_=acc, func=AF.Square)
                    pn, t_pn = ps8.next()
                    k.group("pe", [mm(pn, ones_b, sqb)], reads=[t_sqb, t_onesb], writes=[t_pn])
                    rn, t_rn = tmps.next()
                    op("act", "activation", reads=[t_pn], writes=[t_rn], out=rn, in_=pn, func=AF.Sqrt, bias=eps_c, scale=1.0)
                    op("dve", "reciprocal", writes=[t_rn], out=rn, in_=rn)
                    op("dve", "scalar_tensor_tensor", reads=[t_rn, t_acc], writes=[t_dst], out=dst[:, h, :], in0=acc,
                       scalar=(128.0 ** -0.5 if which == 0 else 1.0), in1=rn, op0=ALU.mult, op1=ALU.mult)
            for gi in range(6):
                wb, t_wb = wg(COL_DQ + gi * 256, 256)
                proj_fm(wb, t_wb, 2, lambda j, pb, t_pb, gi=gi: dn_consume(j, pb, t_pb, gi * 2))
            for g2 in range(2):
                wb, t_wb = wg(COL_DZ + g2 * 256, 256)
                proj_fm(wb, t_wb, 2, lambda j, pb, t_pb, g2=g2: op(
                    "act", "activation", reads=[t_pb], writes=[t_zs], out=zs[:, g2 * 2 + j, :], in_=pb, func=AF.Silu))
            wab, t_wab = wg(COL_AB, 8)
            pb, t_pb = ps8.next()
            for c in range(8):
                k.group("pe", [mm(pb[0:64, c * 8:(c + 1) * 8], hT[:, kc, c * 64:(c + 1) * 64], wab[:, kc, 0:8], start=(kc == 0),
                                  stop=(kc == KC - 1)) for kc in range(KC)], reads=[t_wab, t_h], writes=[t_pb])
            op("dve", "tensor_copy", reads=[t_pb], writes=[t_ab], out=ab, in_=pb[0:64, 0:64].rearrange("p (c e) -> p c e", e=8))
            dtb_b = spl("dtb")[0:64, :].unsqueeze(1).to_broadcast([64, 8, 4])
            nA_b = lay[0:64, 4:8].unsqueeze(1).to_broadcast([64, 8, 4])
            op("dve", "tensor_tensor", reads=[t_ab, t_spl], writes=[t_gb], out=gb[:, 3], in0=ab[:, :, 0:4], in1=dtb_b, op=ALU.add)
            op("act", "activation", writes=[t_gb], out=gb[:, 3], in_=gb[:, 3], func=AF.Exp)
            op("act", "activation", writes=[t_gb], out=gb[:, 3], in_=gb[:, 3], func=AF.Ln, bias=one_c[0:64, :], scale=1.0)
            op("dve", "tensor_tensor", reads=[t_lay], writes=[t_gb], out=gb[:, 0], in0=gb[:, 3], in1=nA_b, op=ALU.mult)
            op("act", "activation", reads=[t_ab], writes=[t_gb], out=gb[:, 1], in_=ab[:, :, 4:8], func=AF.Sigmoid)
            op("dve", "tensor_scalar", writes=[t_gb], out=gb[:, 2], in0=gb[:, 1], scalar1=-1.0, scalar2=None, op0=ALU.mult)
            if l == 0 and t == 0:
                tap("qn", qn, t_qn); tap("kn", kn, t_kn); tap("vn", vn, t_vn); tap("gb", gb, t_gb)

            barrier()
            ar["off"] = OV0
            def a64(name, cols=256, rows=64):
                return aa(name, [rows, cols])

            def mkset(s_):
                def b64(name, cols=256):
                    return aa(name, [64, cols], BF16)
                return {"ktok": b64("ktok" + s_, 512), "vtok": b64("vtok" + s_, 512), "R1": a64("R1" + s_), "R2": a64("R2" + s_),
                        "Dm": a64("Dm" + s_), "DTm": a64("DTm" + s_), "egs": a64("egs" + s_, 16), "glb": aa("glb" + s_, [128, 4]),
                        "egrow": aa("egrow" + s_, [128, 256]), "Pb": [b64("Pa" + s_), b64("Pb" + s_)], "Bb": [b64("Ba" + s_), b64("Bb" + s_)],
                        "Nb": [b64("Na" + s_), b64("Nb" + s_)], "rhs_w": b64("rhs_w" + s_, 512), "u_sb": a64("u_sb" + s_, 512),
                        "wT": aa("wT" + s_, [128, 256], BF16), "qgT": aa("qgT" + s_, [128, 256], BF16), "vnew": b64("vnew" + s_, 512),
                        "qkT": b64("qkT" + s_)}
            NW = cfg.get("nw", 2)
            BSs = [mkset("_%d" % i) for i in range(NW)]
            tt_c = cst("tt", 64); su_c = cst("su", 64); negu = cst("negu", 64); negl = cst("negl", 64)
            mstr = cst("mstrict", 64); i4 = cst("i4", 64); id64 = consts[0:64, CO["ident"][0]:CO["ident"][0] + 64]
            units = [(qb, h, p) for qb in range(4 if ACUT >= 2 else 0) for h in range(4) for p in range(2)]

            def stageA(ui):
                qb, h, p = units[ui]
                j = ui % 2
                gq = t * 4 + qb
                kend = (gq + 1) * 128
                Sb_, t_Sb_ = Ssb[j]
                Pj, t_Pj = Pms[j]
                aj, t_aj = asts[j]
                rs_, t_rs = rss[h % 2]
                ps_ = slice(p * 64, (p + 1) * 64)
                for pc in range((kend + 511) // 512):
                    n = min(512, kend - pc * 512)
                    sbk, t_sbk = psS.next()
                    k.group("pe", [mm(sbk[:, 0:n], qT[ps_, h, qb * 128:(qb + 1) * 128], kT[ps_, h, pc * 512:pc * 512 + n])],
                            reads=[t_qT, t_kT], writes=[t_sbk])
                    op("act", "copy", reads=[t_sbk], writes=[t_Sb_], out=Sb_[:, pc * 512:pc * 512 + n], in_=sbk[:, 0:n])
                op("dve", "tensor_scalar", writes=[t_Sb_], out=Sb_[0:64, kend - 64:kend], in0=Sb_[0:64, kend - 64:kend],
                   scalar1=NEG, scalar2=None, op0=ALU.add)
                op("dve", "reduce_max", reads=[t_Sb_], writes=[t_aj], out=aj[:, 0:1], in_=Sb_[:, 0:kend], axis=AX.X)
                op("dve", "tensor_scalar", writes=[t_aj], out=aj[:, 1:2], in0=aj[:, 0:1], scalar1=-0.125, scalar2=None, op0=ALU.mult)
                op("act", "activation", reads=[t_Sb_, t_aj], writes=[t_Pj, t_rs], out=Pj[:, 0:kend], in_=Sb_[:, 0:kend],
                   func=AF.Exp, bias=aj[:, 1:2], scale=0.125, accum_out=rs_[:, p:p + 1])

            def stageB(ui):
                qb, h, p = units[ui]
                j = ui % 2
                gq = t * 4 + qb
                nkb = gq + 1
                Pj, t_Pj = Pms[j]
                rs_, t_rs = rss[h % 2]
                ytm, t_ytm = ytms[qb % 2]
                if p == 0:
                    pOs[(qb, h)] = psO.next()
                pO, t_pO = pOs[(qb, h)]
                for kb0 in range(0, nkb, 8):
                    nb = min(8, nkb - kb0)
                    ptb, t_ptb = psT.next()
                    ptv = ptb.bitcast(BF16)
                    k.group("pe", [tr(ptv[:, i * 128:(i + 1) * 128], Pj[:, (kb0 + i) * 128:(kb0 + i + 1) * 128], ident_b)
                                   for i in range(nb)], reads=[t_Pj, t_identb], writes=[t_ptb])
                    op("dve", "tensor_copy", reads=[t_ptb], writes=[t_PT], out=PT[:, kb0:kb0 + nb, :],
                       in_=ptv[:, 0:nb * 128].rearrange("p (a b) -> p a b", b=128))
                k.group("pe", [mm(pO[:, p * 128:(p + 1) * 128], PT[:, kb, :], Vt[:, kb, h * 128:(h + 1) * 128],
                                  start=(kb == 0), stop=(kb == nkb - 1)) for kb in range(nkb)], reads=[t_PT, t_V], writes=[t_pO])
                if p == 0:
                    return
                op("dve", "reciprocal", writes=[t_rs], out=rs_[:, 2:4], in_=rs_[:, 0:2])
                op("dve", "tensor_tensor", reads=[t_lay], writes=[t_rs], out=rs_[:, 3:4], in0=rs_[:, 3:4], in1=lay[:, 0:1], op=ALU.mult)
                op("dve", "tensor_scalar", reads=[t_pO, t_rs], writes=[t_yv], out=yv, in0=pO[:, 0:128], scalar1=rs_[:, 2:3], scalar2=None, op0=ALU.mult)
                op("dve", "scalar_tensor_tensor", reads=[t_pO, t_rs], writes=[t_yv], out=yv, in0=pO[:, 128:256], scalar=rs_[:, 3:4], in1=yv,
                   op0=ALU.mult, op1=ALU.add)
                op("dve", "tensor_tensor", reads=[t_yv], writes=[t_junk], out=junk, in0=yv, in1=yv, op=ALU.mult)
                op("dve", "reduce_sum", reads=[t_junk], writes=[t_rs], out=rs_[:, 4:5], in_=junk, axis=AX.X)
                op("act", "activation", writes=[t_rs], out=rs_[:, 5:6], in_=rs_[:, 4:5], func=AF.Ln, bias=eps_c, scale=1.0 / 128)
                op("act", "activation", writes=[t_rs], out=rs_[:, 5:6], in_=rs_[:, 5:6], func=AF.Exp, scale=-0.5)
                op("dve", "scalar_tensor_tensor", reads=[t_yv, t_rs, t_subgs], writes=[t_ytm], out=ytm[:, h * 128:(h + 1) * 128], in0=yv,
                   scalar=rs_[:, 5:6], in1=subgs, op0=ALU.mult, op1=ALU.mult)
                if h == 3:
                    ptb, t_ptb = psT.next()
                    ptv = ptb.bitcast(BF16)
                    k.group("pe", [tr(ptv[:, hh * 128:(hh + 1) * 128], ytm[:, hh * 128:(hh + 1) * 128], ident_b) for hh in range(4)],
                            reads=[t_ytm, t_identb], writes=[t_ptb])
                    op("dve", "tensor_copy", reads=[t_ptb], writes=[t_yda], out=y_da[:, :, qb * 128:(qb + 1) * 128],
                       in_=ptv[:, 0:512].rearrange("p (a b) -> p a b", b=128))


            def v3(ap, inner):
                return ap.rearrange("p (h j) -> p h j", j=inner)


            def chunk_prep(c, BS):
                ktok, t_ktok = BS["ktok"]; vtok, t_vtok = BS["vtok"]
                R1, t_R1 = BS["R1"]; R2, t_R2 = BS["R2"]
                Dm, t_D = BS["Dm"]; DTm, t_DT = BS["DTm"]; Ds, t_Ds = Dm, t_D
                egs, t_egs = BS["egs"]; glb, t_glb = BS["glb"]; egrow, t_egrow = BS["egrow"]
                Pb = BS["Pb"]; Bb = BS["Bb"]; Nb = BS["Nb"]
                rhs_u, t_ru = vtok, t_vtok; rhs_w, t_rw_ = BS["rhs_w"]; kd, t_kd = ktok, t_ktok
                u_sb, t_u = BS["u_sb"]; wT, t_wT = BS["wT"]; qkT, t_qk = BS["qkT"]
                qgT, t_qg = BS["qgT"]; vnew, t_vnew = BS["vnew"]
                cs = slice(c * 64, (c + 1) * 64)
                gr = gb[:, 0, c, :]
                be = gb[:, 1, c, :]
                nbe = gb[:, 2, c, :]
                pk, t_pk = psDN.next()
                pkb = pk.bitcast(BF16)
                k.group("pe", [tr(pkb[0:64, h * 128:(h + 1) * 128], kn[:, h, cs], ident_b) for h in range(4)], reads=[t_kn, t_identb], writes=[t_pk])
                op("act", "copy", reads=[t_pk], writes=[t_ktok], out=ktok, in_=pkb[0:64, 0:512])
                yield
                pv, t_pv = psDN.next()
                pvb = pv.bitcast(BF16)
                k.group("pe", [tr(pvb[0:64, h * 128:(h + 1) * 128], vn[:, h, cs], ident_b) for h in range(4)], reads=[t_vn, t_identb], writes=[t_pv])
                op("dve", "tensor_copy", reads=[t_pv], writes=[t_vtok], out=vtok, in_=pvb[0:64, 0:512])
                yield
                op("dve", "tensor_tensor", reads=[t_gb, t_consts], writes=[t_R1], out=v3(R1, 64), in0=gr.unsqueeze(2).to_broadcast([64, 4, 64]),
                   in1=tt_c.unsqueeze(1).to_broadcast([64, 4, 64]), op=ALU.mult)
                yield
                op("dve", "tensor_copy", reads=[t_gb], writes=[t_R2], out=v3(R2, 64), in_=gr.unsqueeze(2).to_broadcast([64, 4, 64]))
                yield
                pg, t_pg = psDN.next()
                k.group("pe", [mm(pg[0:64, 0:256], tt_c, R2, start=True, stop=False),
                               mm(pg[0:64, 0:256], nones_f[0:64, 0:64], R1, start=False, stop=False),
                               mm(pg[0:64, 0:256], id64, negu, start=False, stop=True)],
                        reads=[t_R1, t_R2, t_consts, t_nones], writes=[t_pg])
                op("act", "activation", reads=[t_pg], writes=[t_D], out=Dm, in_=pg[0:64, 0:256], func=AF.Exp)
                yield
                pgt, t_pgt = psDN.next()
                k.group("pe", [mm(pgt[0:64, 0:256], ones_f[0:64, 0:64], R1, start=True, stop=False),
                               mm(pgt[0:64, 0:256], ntt_f, R2, start=False, stop=False),
                               mm(pgt[0:64, 0:256], id64, negl, start=False, stop=True)],
                        reads=[t_R1, t_R2, t_consts, t_ones, t_ntt], writes=[t_pgt])
                op("act", "activation", reads=[t_pgt], writes=[t_DT], out=DTm, in_=pgt[0:64, 0:256], func=AF.Exp)
                yield
                op("dve", "tensor_tensor", reads=[t_consts], writes=[t_Ds], out=Ds, in0=Dm, in1=mstr, op=ALU.mult)
                yield
                px, t_px = psDN.next()
                k.group("pe", [mm(px[:, 0:256], ones_f[0:64, :], R1), mm(px[:, 256:260], ones_f[0:64, :], gr),
                               mm(px[0:64, 264:268], tt_c, gr), mm(px[0:64, 268:272], su_c, gr)],
                        reads=[t_R1, t_gb, t_ones, t_consts], writes=[t_px])
                op("act", "activation", reads=[t_px], writes=[t_egrow], out=egrow, in_=px[:, 0:256], func=AF.Exp)
                op("act", "activation", reads=[t_px], writes=[t_glb], out=glb, in_=px[:, 256:260], func=AF.Exp)
                op("act", "activation", reads=[t_px], writes=[t_egs], out=egs[:, 0:8], in_=px[0:64, 264:272], func=AF.Exp)
                yield
                op("dve", "tensor_tensor", reads=[t_gb], writes=[t_egs], out=egs[:, 8:12], in0=egs[:, 0:4], in1=be, op=ALU.mult)
                yield
                pkk, t_pkk = psDN.next()
                k.group("pe", [mm(pkk[0:64, h * 64:(h + 1) * 64], kn[:, h, cs], kn[:, h, cs]) for h in range(4)], reads=[t_kn], writes=[t_pkk])
                P_, t_P_ = Pb[0]
                for h in range(4):
                    op("dve", "scalar_tensor_tensor", reads=[t_pkk, t_gb, t_Ds], writes=[t_P_], out=P_[:, h * 64:(h + 1) * 64],
                       in0=pkk[0:64, h * 64:(h + 1) * 64], scalar=nbe[:, h:h + 1], in1=Ds[:, h * 64:(h + 1) * 64], op0=ALU.mult, op1=ALU.mult)
                pt_, t_pt_ = psDN.next()
                pt_ = pt_.bitcast(BF16)
                k.group("pe", [tr(pt_[0:64, h * 64:(h + 1) * 64], P_[:, h * 64:(h + 1) * 64], ident_b[0:64, 0:64]) for h in range(4)],
                        reads=[t_P_, t_identb], writes=[t_pt_])
                B_, t_B_ = Bb[0]
                N_, t_N_ = Nb[0]
                op("act", "copy", reads=[t_pt_], writes=[t_B_], out=B_, in_=pt_[0:64, 0:256])
                op("dve", "tensor_tensor", reads=[t_pt_, t_consts], writes=[t_N_], out=N_, in0=pt_[0:64, 0:256], in1=i4, op=ALU.add)
                yield
                for j in range(1, 6):
                    Pn, t_Pn = Pb[j % 2]
                    Bn, t_Bn = Bb[j % 2]
                    Nn, t_Nn = Nb[j % 2]
                    pp, t_pp = psDN.next()
                    k.group("pe", [mm(pp[0:64, h * 64:(h + 1) * 64], B_[:, h * 64:(h + 1) * 64], P_[:, h * 64:(h + 1) * 64]) for h in range(4)],
                            reads=[t_B_, t_P_], writes=[t_pp])
                    op("act", "copy", reads=[t_pp], writes=[t_Pn], out=Pn, in_=pp[0:64, 0:256])
                    if j < 5:
                        pbb, t_pbb = psDN.next()
                        k.group("pe", [mm(pbb[0:64, h * 64:(h + 1) * 64], P_[:, h * 64:(h + 1) * 64], B_[:, h * 64:(h + 1) * 64]) for h in range(4)],
                                reads=[t_B_, t_P_], writes=[t_pbb])
                        op("dve", "tensor_copy", reads=[t_pbb], writes=[t_Bn], out=Bn, in_=pbb[0:64, 0:256])
                    pn2, t_pn2 = psDN.next()
                    k.group("pe", [mm(pn2[0:64, h * 64:(h + 1) * 64], Pn[:, h * 64:(h + 1) * 64], N_[:, h * 64:(h + 1) * 64]) for h in range(4)],
                            reads=[t_Pn, t_N_], writes=[t_pn2])
                    op("dve", "tensor_tensor", reads=[t_pn2, t_N_], writes=[t_Nn], out=Nn, in0=pn2[0:64, 0:256], in1=N_, op=ALU.add)
                    P_, t_P_, B_, t_B_, N_, t_N_ = Pn, t_Pn, Bn, t_Bn, Nn, t_Nn
                op("dve", "tensor_tensor", reads=[t_vtok, t_gb], writes=[t_ru], out=v3(rhs_u, 128), in0=v3(vtok, 128),
                   in1=be.unsqueeze(2).to_broadcast([64, 4, 128]), op=ALU.mult)
                yield
                op("dve", "tensor_tensor", reads=[t_ktok, t_egs], writes=[t_rw_], out=v3(rhs_w, 128), in0=v3(ktok, 128),
                   in1=egs[:, 8:12].unsqueeze(2).to_broadcast([64, 4, 128]), op=ALU.mult)
                yield
                op("dve", "tensor_tensor", reads=[t_ktok, t_egs], writes=[t_kd], out=v3(kd, 128), in0=v3(ktok, 128),
                   in1=egs[:, 4:8].unsqueeze(2).to_broadcast([64, 4, 128]), op=ALU.mult)
                yield
                pu, t_pu = psDN.next()
                k.group("pe", [mm(pu[0:64, h * 128:(h + 1) * 128], N_[:, h * 64:(h + 1) * 64], rhs_u[:, h * 128:(h + 1) * 128]) for h in range(4)],
                        reads=[t_N_, t_ru], writes=[t_pu])
                op("act", "copy", reads=[t_pu], writes=[t_u], out=u_sb, in_=pu[0:64, :])
                yield
                pw, t_pw = psDN.next()
                k.group("pe", [mm(pw[:, h * 64:(h + 1) * 64], rhs_w[:, h * 128:(h + 1) * 128], N_[:, h * 64:(h + 1) * 64]) for h in range(4)],
                        reads=[t_N_, t_rw_], writes=[t_pw])
                op("dve", "tensor_copy", reads=[t_pw], writes=[t_wT], out=wT, in_=pw[:, 0:256])
                yield
                pq, t_pq = psDN.next()
                k.group("pe", [mm(pq[0:64, h * 64:(h + 1) * 64], kn[:, h, cs], qn[:, h, cs]) for h in range(4)], reads=[t_kn, t_qn], writes=[t_pq])
                op("dve", "tensor_tensor", reads=[t_pq, t_DT], writes=[t_qk], out=qkT, in0=pq[0:64, 0:256], in1=DTm, op=ALU.mult)
                yield
                op("dve", "tensor_tensor", reads=[t_qn, t_egrow], writes=[t_qg], out=v3(qgT, 64), in0=qn[:, :, cs], in1=v3(egrow, 64), op=ALU.mult)
                yield

            def chunk_recur(c, BS):
                ktok, t_ktok = BS["ktok"]; vtok, t_vtok = BS["vtok"]
                R1, t_R1 = BS["R1"]; R2, t_R2 = BS["R2"]
                Dm, t_D = BS["Dm"]; DTm, t_DT = BS["DTm"]; Ds, t_Ds = Dm, t_D
                egs, t_egs = BS["egs"]; glb, t_glb = BS["glb"]; egrow, t_egrow = BS["egrow"]
                Pb = BS["Pb"]; Bb = BS["Bb"]; Nb = BS["Nb"]
                rhs_u, t_ru = vtok, t_vtok; rhs_w, t_rw_ = BS["rhs_w"]; kd, t_kd = ktok, t_ktok
                u_sb, t_u = BS["u_sb"]; wT, t_wT = BS["wT"]; qkT, t_qk = BS["qkT"]
                qgT, t_qg = BS["qgT"]; vnew, t_vnew = BS["vnew"]
                cs = slice(c * 64, (c + 1) * 64)
                gr = gb[:, 0, c, :]
                be = gb[:, 1, c, :]
                nbe = gb[:, 2, c, :]
                pws, t_pws = psDN.next()
                k.group("pe", [mm(pws[0:64, h * 128:(h + 1) * 128], wT[:, h * 64:(h + 1) * 64], Sb[:, h, :]) for h in range(4)],
                        reads=[t_wT, t_Sb], writes=[t_pws])
                op("dve", "tensor_tensor", reads=[t_u, t_pws], writes=[t_vnew], out=vnew, in0=u_sb, in1=pws[0:64, :], op=ALU.subtract)
                po, t_po = psDN.next()
                fns = []
                for h in range(4):
                    fns.append(mm(po[:, h * 64:(h + 1) * 64], Sb[:, h, :], qgT[:, h * 64:(h + 1) * 64], start=True, stop=False))
                    fns.append(mm(po[:, h * 64:(h + 1) * 64], vnew[:, h * 128:(h + 1) * 128], qkT[:, h * 64:(h + 1) * 64], start=False, stop=True))
                k.group("pe", fns, reads=[t_Sb, t_qg, t_vnew, t_qk], writes=[t_po])
                op("act", "copy", reads=[t_po], writes=[t_oT], out=oT[:, :, cs], in_=v3(po[:, 0:256], 64))
                pds, t_pds = psDN.next()
                k.group("pe", [mm(pds[:, h * 128:(h + 1) * 128], kd[:, h * 128:(h + 1) * 128], vnew[:, h * 128:(h + 1) * 128]) for h in range(4)],
                        reads=[t_kd, t_vnew], writes=[t_pds])
                for h in range(4):
                    op("dve", "scalar_tensor_tensor", reads=[t_pds, t_glb], writes=[t_S], out=Sst[:, h, :], in0=Sst[:, h, :], scalar=glb[:, h:h + 1],
                       in1=pds[:, h * 128:(h + 1) * 128], op0=ALU.mult, op1=ALU.add)
                op("act", "copy", reads=[t_S], writes=[t_Sb], out=Sb, in_=Sst)


            def att_gen():
                for ui in range(len(units) + 1):
                    if ui < len(units):
                        stageA(ui)
                        yield
                    if ui >= 1:
                        stageB(ui - 1)
                        yield

            def dn_gen():
                for c0_ in range(0, 8, NW):
                    gens = [chunk_prep(c0_ + i, BSs[i]) for i in range(NW)]
                    alive = list(gens)
                    while alive:
                        for g_ in list(alive):
                            try:
                                next(g_)
                            except StopIteration:
                                alive.remove(g_)
                            yield
                    for i in range(NW):
                        chunk_recur(c0_ + i, BSs[i])
                        yield

            RATIO = cfg.get("dn_ratio", 6)
            streams = [[att_gen(), 1], [dn_gen(), RATIO]]
            while streams:
                for st_ in list(streams):
                    for _ in range(st_[1]):
                        try:
                            next(st_[0])
                        except StopIteration:
                            streams.remove(st_)
                            break

            if l == 0 and t == 0 and ACUT >= 8:
                tap("yda", y_da, t_yda)
            if l == 0 and t == 0:
                tap("oT", oT, t_oT)
            for h in range(4):
                sqb, t_sqb = tmpb.next()
                op("act", "activation", reads=[t_oT], writes=[t_sqb], out=sqb, in_=oT[:, h, :], func=AF.Square)
                pn, t_pn = ps8.next()
                k.group("pe", [mm(pn, ones_b, sqb)], reads=[t_sqb, t_onesb], writes=[t_pn])
                rn, t_rn = tmps.next()
                op("act", "activation", reads=[t_pn], writes=[t_rn], out=rn, in_=pn, func=AF.Sqrt, bias=eps_c, scale=1.0 / 128)
                op("dve", "reciprocal", writes=[t_rn], out=rn, in_=rn)
                op("dve", "scalar_tensor_tensor", reads=[t_oT, t_spl], writes=[t_rn], out=rn, in0=oT[:, h, :], scalar=spl("dng"), in1=rn,
                   op0=ALU.mult, op1=ALU.mult)
                op("dve", "tensor_tensor", reads=[t_rn, t_zs], writes=[t_ydn], out=y_dn[:, h, :], in0=rn, in1=zs[:, h, :], op=ALU.mult)
            if l == 0 and t == 0:
                tap("ydn", y_dn, t_ydn)
            if STAGE <= 4:
                continue
            barrier()
            ar["off"] = SUB0
            merged, t_mg = aa("merged", [128, KC, TT], BF16)
            sgs = Pool_([aa("sg%d" % i, [128, TT]) for i in range(3)])
            macc, t_macc = aa("macc", [128, TT])
            xins = Pool_([aa("xin%d" % i, [128, TT]) for i in range(3)])
            ybr = [(y_gm, t_ygm), (y_da, t_yda), (y_dn, t_ydn)]
            for d in range(KC):
                for br in range(3):
                    wgt, t_wgt = load_w([(wrows(w_in_d[l])[:, :, COL_G + br * 1024 + d * 128:COL_G + br * 1024 + (d + 1) * 128], KC)], KC, 128)
                    pg, t_pg = ps8.next()
                    k.group("pe", [mm(pg, wgt[:, kc, :], hT[:, kc, :], start=(kc == 0), stop=(kc == KC - 1)) for kc in range(KC)],
                            reads=[t_wgt, t_h], writes=[t_pg])
                    sg, t_sg = sgs.next()
                    op("act", "activation", reads=[t_pg], writes=[t_sg], out=sg, in_=pg, func=AF.Sigmoid)
                    pp, t_pp = ps8.next()
                    yb, t_yb = ybr[br]
                    wbr, t_wbr = load_w([(wrows(w_br_d[br][l])[:, :, d * 128:(d + 1) * 128], 4)], 4, 128)
                    k.group("pe", [mm(pp, wbr[:, kc, :], yb[:, kc, :], start=(kc == 0), stop=(kc == 3)) for kc in range(4)],
                            reads=[t_wbr, t_yb], writes=[t_pp])
                    if br == 0:
                        op("dve", "tensor_tensor", reads=[t_pp, t_sg], writes=[t_macc], out=macc, in0=pp, in1=sg, op=ALU.mult)
                    else:
                        op("dve", "tensor_tensor", reads=[t_pp], writes=[t_sg], out=sg, in0=pp, in1=sg, op=ALU.mult)
                        if br == 1:
                            op("dve", "tensor_tensor", reads=[t_sg], writes=[t_macc], out=macc, in0=macc, in1=sg, op=ALU.add)
                        else:
                            op("dve", "tensor_tensor", reads=[t_sg, t_macc], writes=[t_mg], out=merged[:, d, :], in0=macc, in1=sg, op=ALU.add)
            if l == 0 and t == 0:
                tap("merged", merged, t_mg)
            for g4 in range(4):
                wo, t_wo = load_w([(wrows(w_out_d[l])[:, :, g4 * 256:(g4 + 1) * 256], KC)], KC, 256)
                for j in range(2):
                    e_ = g4 * 2 + j
                    po, t_po = ps8.next()
                    k.group("pe", [mm(po, wo[:, dc, j * 128:(j + 1) * 128], merged[:, dc, :], start=(dc == 0), stop=(dc == KC - 1))
                                   for dc in range(KC)], reads=[t_wo, t_mg], writes=[t_po])
                    xin, t_xin = xins.next()
                    if l == 0:
                        k.dma("sp", xin, xT_d[b, e_ * 128:(e_ + 1) * 128, c0:c0 + TT], writes=[t_xin])
                    else:
                        k.dma("sp", xin, xs_d[:, e_, c0:c0 + TT], reads=[t_xs[t][e_]], writes=[t_xin])
                    op("dve", "scalar_tensor_tensor", reads=[t_po, t_mods], writes=[t_xin], out=xin, in0=po,
                       scalar=mods[:, l, b, GATE1, e_:e_ + 1], in1=xin, op0=ALU.mult, op1=ALU.add)
                    k.dma("sp", xs_d[:, e_, c0:c0 + TT], xin, reads=[t_xin], writes=[t_xs[t][e_]])
            barrier()


    def ffn(l, b, last):
        barrier()
        ar["off"] = 0
        xT, t_x = aa("xT", [128, KC, S])
        rb, t_rb = aa("rb", [128, TT])
        sq_b, t_sq = aa("sq_b", [128, KC, TT], BF16)
        tmps = Pool_([aa("tmpA%d" % i, [128, TT]) for i in range(3)])
        FMARK = ar["off"]
        h2, t_h2 = aa("h2", [128, KC, S], BF16)
        moe = (l % 2 == 1)
        for t in range(NTILE):
            k.dma("sp", xT[:, :, t * TT:(t + 1) * TT], xs_d[:, :, t * TT:(t + 1) * TT], reads=t_xs[t], writes=[t_x], group=(t > 0))
        if moe:
            h32, t_h32 = aa("h32", [128, KC, TT])
            WT, t_WT = aa("WT", [8, S])
            rhe, t_rhe = aa("rhe", [8, TT])
            Wb0, t_Wb0 = aa("Wbc0", [128, S], BF16)
            Wb1, t_Wb1 = aa("Wbc1", [128, S], BF16)
            Wbcs = [Wb0, Wb1]; t_Wbcs = [t_Wb0, t_Wb1]
            lg, t_lg = aa("lg", [128, 64])
            wr3 = spg[:, 8:72].rearrange("p (c e) -> p c e", e=8)
        for t in range(NTILE):
            modnorm(xT, t_x, t * TT, l, b, G2S, SHIFT2, h2[:, :, t * TT:(t + 1) * TT], t_h2, sq_b, t_sq, rb, t_rb, tmps,
                    h32=((h32, t_h32) if moe else None))
            if moe:
                for blk in range(4):
                    gblk = t * 4 + blk
                    pl, t_pl = ps8.next()
                    k.group("pe", [mm(pl[:, 0:8], h32[:, kc, blk * 128:(blk + 1) * 128], wr3[:, kc, :], start=(kc == 0), stop=(kc == KC - 1))
                                   for kc in range(KC)], reads=[t_h32, t_spg], writes=[t_pl])
                    L = lg[:, 0:8]; E1 = lg[:, 8:16]; M2 = lg[:, 16:24]; E2 = lg[:, 24:32]; Wt = lg[:, 32:40]
                    m1 = lg[:, 40:41]; m2 = lg[:, 41:42]; dd = lg[:, 42:43]; w1 = lg[:, 43:44]; w2 = lg[:, 44:45]
                    op("dve", "tensor_copy", reads=[t_pl], writes=[t_lg], out=L, in_=pl[:, 0:8])
                    op("dve", "reduce_max", writes=[t_lg], out=m1, in_=L, axis=AX.X)
                    op("dve", "tensor_scalar", writes=[t_lg], out=E1, in0=L, scalar1=m1, scalar2=None, op0=ALU.is_equal)
                    op("dve", "scalar_tensor_tensor", writes=[t_lg], out=M2, in0=E1, scalar=-1e30, in1=L, op0=ALU.mult, op1=ALU.add)
                    op("dve", "reduce_max", writes=[t_lg], out=m2, in_=M2, axis=AX.X)
                    op("dve", "tensor_scalar", writes=[t_lg], out=E2, in0=M2, scalar1=m2, scalar2=None, op0=ALU.is_equal)
                    op("dve", "tensor_tensor", writes=[t_lg], out=dd, in0=m2, in1=m1, op=ALU.subtract)
                    op("act", "activation", writes=[t_lg], out=dd, in_=dd, func=AF.Exp)
                    op("dve", "tensor_scalar", writes=[t_lg], out=w1, in0=dd, scalar1=1.0, scalar2=None, op0=ALU.add)
                    op("dve", "reciprocal", writes=[t_lg], out=w1, in_=w1)
                    op("dve", "tensor_tensor", writes=[t_lg], out=w2, in0=dd, in1=w1, op=ALU.mult)
                    op("dve", "tensor_scalar", writes=[t_lg], out=Wt, in0=E1, scalar1=w1, scalar2=None, op0=ALU.mult)
                    op("dve", "scalar_tensor_tensor", writes=[t_lg], out=Wt, in0=E2, scalar=w2, in1=Wt, op0=ALU.mult, op1=ALU.add)
                    pt_, t_pt_ = ps8.next()
                    k.group("pe", [tr(pt_[0:8, 0:128], Wt, ident)], reads=[t_lg, t_consts], writes=[t_pt_])
                    op("act", "copy", reads=[t_pt_], writes=[t_WT], out=WT[:, gblk * 128:(gblk + 1) * 128], in_=pt_[0:8, 0:128])
        if moe and b == 0:
            tap("WT", WT, t_WT)
        if b == 0:
            tap("h2_%d" % l, h2, t_h2)

        hids = Pool_([aa("hid%d" % i, [128, 2, TT], BF16) for i in range(2)])
        sabs = Pool_([aa("sab%d" % i, [128, TT], BF16) for i in range(3)])
        pend = {"f": None}

        def expert_pass(wg_d, wu_d, wd_d, F, scale_by=None):
            for fg in range(F // 256):
                wgb, t_wgb = load_w([(wrows(wg_d)[:, :, fg * 256:(fg + 1) * 256], KC)], KC, 256)
                wub, t_wub = load_w([(wrows(wu_d)[:, :, fg * 256:(fg + 1) * 256], KC)], KC, 256)
                wdb, t_wdb = load_w([(wrows(wd_d[fg * 256:(fg + 1) * 256, :]), 2)], 2, D)
                for t in range(NTILE):
                    tc_ = slice(t * TT, (t + 1) * TT)
                    hid, t_hid = hids.next()
                    pg_ = pend["f"]

                    def adv():
                        if pend["f"] is not None:
                            try:
                                next(pend["f"])
                            except StopIteration:
                                pend["f"] = None
                    for j in range(2):
                        pa, t_pa = psF.next()
                        k.group("pe", [mm(pa, wgb[:, kc, j * 128:(j + 1) * 128], h2[:, kc, tc_], start=(kc == 0), stop=(kc == KC - 1))
                                       for kc in range(KC)], reads=[t_wgb, t_h2], writes=[t_pa])
                        adv()
                        pu_, t_pu_ = psF.next()
                        k.group("pe", [mm(pu_, wub[:, kc, j * 128:(j + 1) * 128], h2[:, kc, tc_], start=(kc == 0), stop=(kc == KC - 1))
                                       for kc in range(KC)], reads=[t_wub, t_h2], writes=[t_pu_])
                        adv()
                        sa, t_sa = sabs.next()
                        op("act", "activation", reads=[t_pa], writes=[t_sa], out=sa, in_=pa, func=AF.Silu)
                        if scale_by is not None:
                            op("dve", "tensor_tensor", reads=[scale_by[1]], writes=[t_sa], out=sa, in0=sa, in1=scale_by[0][:, tc_], op=ALU.mult)
                        op("dve", "tensor_tensor", reads=[t_pu_, t_sa], writes=[t_hid], out=hid[:, j, :], in0=pu_, in1=sa, op=ALU.mult)
                    while pend["f"] is not None:
                        adv()

                    def down(wdb=wdb, t_wdb=t_wdb, hid=hid, t_hid=t_hid, tc_=tc_):
                        for d in range(KC):
                            pd, t_pd = psD.next()
                            k.group("pe", [mm(pd, wdb[:, j, d * 128:(d + 1) * 128], hid[:, j, :], start=(j == 0), stop=(j == 1)) for j in range(2)],
                                    reads=[t_wdb, t_hid], writes=[t_pd])
                            op("dve", "scalar_tensor_tensor", reads=[t_pd, t_mods], writes=[t_x], out=xT[:, d, tc_], in0=pd,
                               scalar=mods[:, l, b, GATE2, d:d + 1], in1=xT[:, d, tc_], op0=ALU.mult, op1=ALU.add)
                            if d % 2 == 1:
                                yield
                    pend["f"] = down()

        def flush():
            while pend["f"] is not None:
                try:
                    next(pend["f"])
                except StopIteration:
                    pend["f"] = None

        if not moe:
            expert_pass(ffn_g_d[0], ffn_u_d[0], ffn_d_d[0], FF_DENSE)
            flush()
        else:
            for e_ in range(cfg.get("nexp", NEXP)):
                for t in range(NTILE):
                    op("dve", "tensor_scalar", reads=[t_WT, t_consts], writes=[t_rhe], out=rhe, in0=WT[:, t * TT:(t + 1) * TT],
                       scalar1=consts[0:8, CO["ident"][0] + e_:CO["ident"][0] + e_ + 1], scalar2=None, op0=ALU.mult)
                    pw, t_pw = ps8.next()
                    k.group("pe", [mm(pw, ones_f[0:8, :], rhe)], reads=[t_rhe, t_ones], writes=[t_pw])
                    op("act", "copy", reads=[t_pw], writes=[t_Wbcs[e_ % 2]], out=Wbcs[e_ % 2][:, t * TT:(t + 1) * TT], in_=pw)
                expert_pass(moe_g_d[0, e_], moe_u_d[0, e_], moe_d_d[0, e_], FF_EXPERT, scale_by=(Wbcs[e_ % 2], t_Wbcs[e_ % 2]))
            flush()
        if b == 0:
            tap("xffn_%d" % l, xT, t_x)
        if not last:
            for t in range(NTILE):
                for c in range(KC):
                    k.dma("sp", xs_d[:, c, t * TT:(t + 1) * TT], xT[:, c, t * TT:(t + 1) * TT], reads=[t_x], writes=[t_xs[t][c]])
            return
        barrier()
        ar["off"] = FMARK
        xn, t_xn = aa("xn", [128, KC, TT])
        otm = Pool_([aa("otm%d" % i, [128, D]) for i in range(2)])
        for t in range(NTILE):
            for c in range(KC):
                op("act", "activation", reads=[t_x], writes=[t_sq], out=sq_b[:, c, :], in_=xT[:, c, t * TT:(t + 1) * TT], func=AF.Square)
            pb, t_pb = ps8.next()
            k.group("pe", [mm(pb, ones_b, sq_b[:, c, :], start=(c == 0), stop=(c == KC - 1)) for c in range(KC)],
                    reads=[t_sq, t_onesb], writes=[t_pb])
            op("act", "activation", reads=[t_pb], writes=[t_rb], out=rb, in_=pb, func=AF.Sqrt, bias=eps_c, scale=1.0 / D)
            op("dve", "reciprocal", writes=[t_rb], out=rb, in_=rb)
            for c in range(KC):
                op("dve", "scalar_tensor_tensor", reads=[t_x, t_spg, t_rb], writes=[t_xn], out=xn[:, c, :], in0=xT[:, c, t * TT:(t + 1) * TT],
                   scalar=spg[:, c:c + 1], in1=rb, op0=ALU.mult, op1=ALU.mult)
            for blk in range(4):
                ot, t_ot = otm.next()
                for half in range(2):
                    pt_, t_pt_ = ps8.next()
                    k.group("pe", [tr(pt_[:, i * 128:(i + 1) * 128], xn[:, half * 4 + i, blk * 128:(blk + 1) * 128], ident) for i in range(4)],
                            reads=[t_xn, t_consts], writes=[t_pt_])
                    if half == 0:
                        op("act", "copy", reads=[t_pt_], writes=[t_ot], out=ot[:, 0:512], in_=pt_)
                    else:
                        op("dve", "tensor_copy", reads=[t_pt_], writes=[t_ot], out=ot[:, 512:1024], in_=pt_)
                r0 = t * TT + blk * 128
                k.dma("sp", out_d[b, r0:r0 + 128, :], ot, reads=[t_ot], writes=[out_tok])

    for b in range(NSEQ):
        for l in range(LAYERS):
            layer_setup(l)
            mixer(l, b)
            if STAGE >= 6:
                ffn(l, b, last=(l == LAYERS - 1))

    fin = [out_tok] + list(tapd.values())
    if STAGE < 99:
        z, t_z = sbt("zz", [128, 8])
        op("dve", "memset", writes=[t_z], ap=z, constant=0.0)
        k.dma("sp", out_d[0, 0:128, 0:8], z, reads=[t_z], writes=[out_tok])
    k.finish(fin)
    nc._k = k
    return nc


def prep_core_inputs(inp, core):
    x = np.asarray(inp["x"], np.float32)
    b0 = 2 * core
    xT = np.ascontiguousarray(x[b0:b0 + 2].transpose(0, 2, 1))
    pos = np.asarray(inp["positions"], np.int32)[b0:b0 + 2]
    posb = np.ascontiguousarray(np.broadcast_to(pos[:, None, :], (2, 128, S)))
    c = np.asarray(inp["c"], np.float32)[b0:b0 + 2]
    cT = np.ascontiguousarray(c.reshape(2, KC, 128).transpose(2, 1, 0)).reshape(128, KC * 2)
    return {"xT": xT, "posb": posb, "cT": cT}


def shared_inputs(inp):
    ws = np.asarray(inp["gm_w_s"], np.float32)
    wst = np.ascontiguousarray(ws.transpose(3, 0, 1, 2)).reshape(128, DEPTH * 4 * 128)
    sh = {"smallp": make_smallp(inp), "consts": make_consts(), "wst": wst}
    for n in ["w_mod", "w_in", "w_br_gm", "w_br_da", "w_br_dn", "w_out", "ffn_w_gate", "ffn_w_up",
              "ffn_w_down", "moe_w_gate", "moe_w_up", "moe_w_down"]:
        sh[n] = np.ascontiguousarray(np.asarray(inp[n], np.float32))
    return sh


def kernel(**inputs):
    nc = build()
    sh = shared_inputs(inputs)
    in_maps = []
    for core in range(8):
        m = dict(sh)
        m.update(prep_core_inputs(inputs, core))
        in_maps.append(m)
    res = run_bass_kernel_spmd(nc, in_maps, core_ids=list(range(8)))
    return np.concatenate([np.asarray(r["out"], np.float32) for r in res.results], axis=0)
```

```python
import math
import numpy as np
import concourse.bass as bass
import concourse.mybir as mybir
from concourse.bass_utils import run_bass_kernel_spmd

F32 = mybir.dt.float32
BF16 = mybir.dt.bfloat16
I32 = mybir.dt.int32
AF = mybir.ActivationFunctionType
ALU = mybir.AluOpType
AX = mybir.AxisListType

SEM_LIMIT = 20000
ENGS = ("pe", "dve", "act", "pool", "sp")


class Tok:
    __slots__ = ("name", "w", "r", "dsem", "pr", "excl")

    def __init__(self, name, excl=False):
        self.name = name
        self.excl = excl
        self.w = None
        self.r = []
        self.dsem = None
        self.pr = []


class K:
    def __init__(self, nc, same=("dve", "act", "pool"), eager=True):
        self.nc = nc
        self.eager = eager
        self.embed = True
        self._collect = None
        self.e = {"pe": nc.tensor, "dve": nc.vector, "act": nc.scalar,
                  "pool": nc.gpsimd, "sp": nc.sync}
        self.nsem = 0
        self.sem = {}
        self.cnt = {}
        for n in ENGS:
            self._new_epoch(n)
        self.seq = {n: 0 for n in ENGS}
        self.last = {n: None for n in ENGS}
        self.sigmap = {n: [] for n in ENGS}
        self.waited = {n: {} for n in ENGS}
        self.same = set(same)
        self.rsem = {}
        self.store_recs = []
        self.ninstr = 0
        self.nwait = 0

    def _alloc(self, name):
        self.nsem += 1
        return self.nc.alloc_semaphore("%s_%d" % (name, self.nsem))

    def _new_epoch(self, n):
        self.sem[n] = self._alloc("e_" + n)
        self.cnt[n] = 0

    def _resolve(self, ev):
        if ev[0] == "c":
            _, eng, seq = ev
            lst = self.sigmap[eng]
            lo, hi = 0, len(lst)
            while lo < hi:
                mid = (lo + hi) // 2
                if lst[mid][0] >= seq:
                    hi = mid
                else:
                    lo = mid + 1
            if lo < len(lst):
                f = lst[lo]
                return f[1], f[2]
            assert self.seq[eng] >= seq and self.last[eng] is not None
            if self.cnt[eng] >= SEM_LIMIT:
                self._new_epoch(eng)
            self.cnt[eng] += 1
            self.last[eng].then_inc(self.sem[eng], 1)
            lst.append((self.seq[eng], self.sem[eng], self.cnt[eng]))
            return self.sem[eng], self.cnt[eng]
        rec = ev[1]
        return rec[0], rec[1]

    def _wait(self, eng, ev):
        if ev is None:
            return
        if ev[0] == "c" and ev[1] == eng and eng not in self.same:
            return
        sem, val = self._resolve(ev)
        w = self.waited[eng]
        if w.get(id(sem), 0) >= val:
            return
        w[id(sem)] = val
        if self._collect is not None:
            self._collect.append((sem, val))
            return
        self.e[eng].wait_ge(sem, val)
        self.nwait += 1

    def _pre(self, eng, reads, writes):
        for t in reads:
            self._wait(eng, t.w)
        for t in writes:
            self._wait(eng, t.w)
            for ev in t.r:
                self._wait(eng, ev)

    def _signal_last(self, eng):
        if self.cnt[eng] >= SEM_LIMIT:
            self._new_epoch(eng)
        self.cnt[eng] += 1
        self.last[eng].then_inc(self.sem[eng], 1)
        self.sigmap[eng].append((self.seq[eng], self.sem[eng], self.cnt[eng]))

    def _post(self, eng, reads, writes):
        if self.eager and (reads or writes):
            self._signal_last(eng)
        ev = ("c", eng, self.seq[eng])
        for t in reads:
            t.r.append(ev)
            if len(t.r) > 16:
                t.r = self._compact(t.r)
        for t in writes:
            t.w = ev
            t.r = []

    @staticmethod
    def _split(reads, writes):
        xr = [t for t in reads if t.excl]
        if xr:
            writes = list(writes) + [t for t in xr if t not in writes]
            reads = [t for t in reads if not t.excl]
        return reads, writes

    def _pre_embed(self, eng, reads, writes):
        if not self.embed:
            self._pre(eng, reads, writes)
            return None
        self._collect = []
        self._pre(eng, reads, writes)
        waits, self._collect = self._collect, None
        for sem, val in waits[:-1]:
            self.e[eng].wait_ge(sem, val)
            self.nwait += 1
        return waits[-1] if waits else None

    def op(self, eng, fn, reads=(), writes=()):
        reads, writes = self._split(reads, writes)
        w_ = self._pre_embed(eng, reads, writes)
        ins = fn(self.e[eng])
        if w_ is not None:
            ins._wait_ge(w_[0], w_[1])
        self.seq[eng] += 1
        self.last[eng] = ins
        self.ninstr += 1
        self._post(eng, reads, writes)
        return ins

    def group(self, eng, fns, reads=(), writes=()):
        reads, writes = self._split(reads, writes)
        w_ = self._pre_embed(eng, reads, writes)
        ins = None
        for i_, fn in enumerate(fns):
            ins = fn(self.e[eng])
            if i_ == 0 and w_ is not None:
                ins._wait_ge(w_[0], w_[1])
            self.seq[eng] += 1
            self.ninstr += 1
        self.last[eng] = ins
        self._post(eng, reads, writes)
        return ins

    def _compact(self, evs):
        best = {}
        other = []
        for ev in evs:
            if ev[0] == "c":
                k = ev[1]
                if k not in best or ev[2] >= best[k][2]:
                    best[k] = ev
            else:
                if not any(o[1] is ev[1] for o in other):
                    other.append(ev)
        return list(best.values()) + other

    def dma(self, q, out, in_, reads=(), writes=(), group=False, **kw):
        for t in reads:
            self._wait(q, t.w)
        for t in writes:
            if group:
                for ev in t.pr:
                    self._wait(q, ev)
            else:
                self._wait(q, t.w)
                for ev in t.r:
                    self._wait(q, ev)
                t.pr = ([t.w] if t.w is not None else []) + list(t.r)
        ins = self.e[q].dma_start(out=out, in_=in_, **kw)
        assert len(writes) <= 1
        if writes:
            t = writes[0]
            if t.dsem is None or t.dsem[1] >= SEM_LIMIT:
                t.dsem = [self._alloc("d_" + t.name), 0]
            rec = t.dsem
        else:
            if q not in self.rsem or self.rsem[q][1] >= SEM_LIMIT:
                self.rsem[q] = [self._alloc("r_" + q), 0]
            rec = self.rsem[q]
        rec[1] += 16
        ins.then_inc(rec[0], 16)
        ev = ("d", rec)
        if reads and not any(r is rec for r in self.store_recs):
            self.store_recs.append(rec)
        if writes:
            writes[0].w = ev
            writes[0].r = []
        for s in reads:
            s.r.append(ev)
            if len(s.r) > 16:
                s.r = self._compact(s.r)
        self.ninstr += 1
        return ins

    def finish(self, toks, eng="sp"):
        for t in toks:
            self._wait(eng, t.w)
            for ev in t.r:
                self._wait(eng, ev)


D = 1024
KC = 8
S = 2048
TT = 512
NT = S // TT
INW = 7688
DEPTH = 2
EPS = 1e-6
FF_DENSE = 2816
FF_EXPERT = 3584
NEXP = 8
PI = math.pi
C1 = 6.28125
C2 = 2.0 * math.pi - 6.28125
NEG = -30000.0

COL_U, COL_V, COL_Q, COL_KK, COL_VA = 0, 512, 1024, 1536, 2048
COL_DQ, COL_DK, COL_DV, COL_DZ, COL_AB, COL_G = 2560, 3072, 3584, 4096, 4608, 4616

SP_LAYOUT = [("n1g", 8), ("n2g", 8), ("bmod", 48), ("lng", 512), ("lnb", 512), ("bs", 512),
             ("lam", 256), ("subg", 128), ("convw", 48), ("alog", 4), ("dtb", 4), ("dng", 1)]
SP_OFF = {}
_o = 0
for _n, _w in SP_LAYOUT:
    SP_OFF[_n] = (_o, _w)
    _o += _w
SP_LAYER = _o
SP_FING = 2 * SP_LAYER
SP_WR = SP_FING + 8
SP_TOTAL = SP_WR + 64

CO = {}
_o = 0
for _n, _w in [("ident", 128), ("rot", 128), ("tt", 64), ("su", 64), ("negu", 256), ("negl", 256),
               ("mstrict", 256), ("i4", 256), ("invf", 1)]:
    CO[_n] = (_o, _w)
    _o += _w
CO_TOTAL = _o


def make_consts():
    c = np.zeros((128, CO_TOTAL), np.float32)
    o = CO["ident"][0]
    c[:, o:o + 128] = np.eye(128, dtype=np.float32)
    o = CO["rot"][0]
    for m in range(128):
        if (m % 64) < 32:
            c[m + 32, o + m] = -1.0
        else:
            c[m - 32, o + m] = 1.0
    p = np.arange(64)[:, None]
    j = np.arange(64)[None, :]
    o = CO["tt"][0]
    c[:64, o:o + 64] = (p <= j)
    o = CO["su"][0]
    c[:64, o:o + 64] = (p > j)
    for h in range(4):
        o = CO["negu"][0] + h * 64
        c[:64, o:o + 64] = np.where(j > p, NEG, 0.0)
        o = CO["negl"][0] + h * 64
        c[:64, o:o + 64] = np.where(j < p, NEG, 0.0)
        o = CO["mstrict"][0] + h * 64
        c[:64, o:o + 64] = (j < p)
        o = CO["i4"][0] + h * 64
        c[:64, o:o + 64] = (j == p)
    inv_freq = (10000.0 ** (-np.arange(0, 64, 2, dtype=np.float32) / np.float32(64))).astype(np.float32)
    c[:, CO["invf"][0]] = inv_freq[np.arange(128) % 32]
    return c


def fm(v):
    v = np.asarray(v, np.float32)
    return np.ascontiguousarray(v.reshape(-1, 128).T)


def bc(v):
    v = np.asarray(v, np.float32).reshape(1, -1)
    return np.broadcast_to(v, (128, v.shape[1]))


def make_smallp(inp):
    sp = np.zeros((128, SP_TOTAL), np.float32)
    for l in range(DEPTH):
        base = l * SP_LAYER

        def put(name, arr):
            o, w = SP_OFF[name]
            assert arr.shape == (128, w), (name, arr.shape, w)
            sp[:, base + o:base + o + w] = arr
        put("n1g", fm(inp["norm1_g"][l]))
        put("n2g", fm(inp["norm2_g"][l]))
        put("bmod", fm(inp["b_mod"][l]))
        put("lng", bc(inp["gm_ln_g"][l]))
        put("lnb", bc(inp["gm_ln_b"][l]))
        put("bs", bc(inp["gm_b_s"][l].reshape(-1)))
        put("lam", bc(inp["da_lambda"][l].reshape(-1)))
        put("subg", bc(inp["da_subln_g"][l]))
        cw = np.asarray(inp["dn_conv_w"][l], np.float32)
        put("convw", np.ascontiguousarray(cw.reshape(4, 12, 128).transpose(2, 1, 0)).reshape(128, 48))
        put("alog", bc(inp["dn_a_log"][l]))
        put("dtb", bc(inp["dn_dt_bias"][l]))
        put("dng", np.asarray(inp["dn_norm_g"][l], np.float32).reshape(128, 1))
    sp[:, SP_FING:SP_FING + 8] = fm(inp["final_g"])
    wr = np.asarray(inp["moe_w_router"][0], np.float32)
    sp[:, SP_WR:SP_WR + 64] = wr.reshape(8, 128, 8).transpose(1, 0, 2).reshape(128, 64)
    return sp


def lambda_init_fn(layer):
    return 0.8 - 0.6 * math.exp(-0.3 * layer)


class Pool_:
    def __init__(self, items):
        self.items = items
        self.i = 0

    def next(self):
        it = self.items[self.i % len(self.items)]
        self.i += 1
        return it


def build(cfg=None):
    cfg = dict(cfg or {})
    NSEQ = cfg.get("nseq", 2)
    NTILE = cfg.get("ntile", NT)
    LAYERS = cfg.get("layers", DEPTH)
    STAGE = cfg.get("stage", 99)
    taps = cfg.get("taps", ())
    ACUT = cfg.get("acut", 99)

    nc = bass.Bass("TRN2", target_bir_lowering=False)
    k = K(nc, same=tuple(cfg.get("same", ("dve", "act", "pool"))), eager=cfg.get("eager", True))
    k.embed = cfg.get("embed", True)
    k.trans = cfg.get("trans", True)

    def din(name, shape, dt=F32):
        return nc.dram_tensor(name, list(shape), dt, kind="ExternalInput").ap()

    xT_d = din("xT", [2, D, S])
    posb_d = din("posb", [2, 128, S], I32)
    cT_d = din("cT", [128, KC * 2])
    smallp_d = din("smallp", [128, SP_TOTAL])
    consts_d = din("consts", [128, CO_TOTAL])
    wst_d = din("wst", [128, DEPTH * 4 * 128])
    w_mod_d = din("w_mod", [DEPTH, D, 6 * D])
    w_in_d = din("w_in", [DEPTH, D, INW])
    w_br_d = [din("w_br_gm", [DEPTH, 512, D]), din("w_br_da", [DEPTH, 512, D]), din("w_br_dn", [DEPTH, 512, D])]
    w_out_d = din("w_out", [DEPTH, D, D])
    ffn_g_d = din("ffn_w_gate", [1, D, FF_DENSE])
    ffn_u_d = din("ffn_w_up", [1, D, FF_DENSE])
    ffn_d_d = din("ffn_w_down", [1, FF_DENSE, D])
    moe_g_d = din("moe_w_gate", [1, NEXP, D, FF_EXPERT])
    moe_u_d = din("moe_w_up", [1, NEXP, D, FF_EXPERT])
    moe_d_d = din("moe_w_down", [1, NEXP, FF_EXPERT, D])
    out_d = nc.dram_tensor("out", [2, S, D], F32, kind="ExternalOutput").ap()
    out_tok = Tok("out")
    xs_d = nc.dram_tensor("xs_scratch", [128, KC, S], F32, kind="Internal").ap()
    t_xs = [[Tok("xs%d_%d" % (i, e)) for e in range(KC)] for i in range(NT)]

    tapd = {}

    def sb(name, shape, dt=F32):
        return nc.alloc_sbuf_tensor("s_" + name, list(shape), dt).ap()

    def sbt(name, shape, dt=F32):
        return sb(name, shape, dt), Tok(name)

    def tap(name, ap, tok):
        if name not in taps or name in tapd:
            return
        d = nc.dram_tensor("tap_" + name, list(ap.shape), ap.dtype, kind="ExternalOutput").ap()
        t = Tok("tap_" + name)
        k.dma("sp", d, ap, reads=[tok], writes=[t])
        tapd[name] = t

    def op(eng, fn, reads=(), writes=(), **kw):
        return k.op(eng, lambda e: getattr(e, fn)(**kw), reads=reads, writes=writes)

    def mm(out, lhsT, rhs, start=True, stop=True):
        return lambda e: e.matmul(out, lhsT=lhsT, rhs=rhs, start=start, stop=stop)

    def tr(out, in_, idn):
        return lambda e: e.transpose(out, in_, idn)

    fence = {"gen": 0, "evs": []}
    LAZY = cfg.get("lazy_fence", True)

    def barrier():
        engs = ("pe", "dve", "act", "pool")
        evs = {n: ("c", n, k.seq[n]) for n in engs if k.last[n] is not None}
        if LAZY:
            fence["evs"] = list(evs.values()) + [("d", rec) for rec in k.store_recs]
            fence["gen"] += 1
            return
        for a in engs + ("sp",):
            for b_, ev in evs.items():
                if a != b_:
                    k._wait(a, ev)
            for rec in k.store_recs:
                k._wait(a, ("d", rec))

    consts, t_consts = sbt("consts", [128, CO_TOTAL])
    spl_t, t_spl = sbt("spl", [128, SP_LAYER])
    spg, t_spg = sbt("spg", [128, 72])
    k.dma("sp", consts, consts_d, writes=[t_consts])
    k.dma("sp", spg, smallp_d[:, SP_FING:SP_FING + 72], writes=[t_spg])

    def cst(name, rows=128):
        o, w = CO[name]
        return consts[0:rows, o:o + w]

    def spl(name):
        o, w = SP_OFF[name]
        return spl_t[:, o:o + w]

    ident = cst("ident")
    ones_f, t_ones = sbt("ones_f", [128, 128])
    ones_b, t_onesb = sbt("ones_b", [128, 128], BF16)
    nones_f, t_nones = sbt("nones_f", [128, 128])
    ident_b, t_identb = sbt("ident_b", [128, 128], BF16)
    rot_b, t_rotb = sbt("rot_b", [128, 128], BF16)
    ntt_f, t_ntt = sbt("ntt_f", [64, 64])
    op("dve", "memset", writes=[t_ones], ap=ones_f, constant=1.0)
    op("dve", "memset", writes=[t_onesb], ap=ones_b, constant=1.0)
    op("dve", "memset", writes=[t_nones], ap=nones_f, constant=-1.0)
    op("dve", "tensor_copy", reads=[t_consts], writes=[t_identb], out=ident_b, in_=ident)
    op("dve", "tensor_copy", reads=[t_consts], writes=[t_rotb], out=rot_b, in_=cst("rot"))
    op("dve", "tensor_scalar", reads=[t_consts], writes=[t_ntt], out=ntt_f, in0=cst("tt", 64), scalar1=-1.0,
       scalar2=None, op0=ALU.mult)
    TC = [t_consts, t_ones, t_onesb, t_nones, t_identb, t_rotb, t_ntt]

    psall = nc.alloc_psum_tensor("psall", [128, 8 * 512], F32).ap()
    banks = [(psall[:, i * 512:(i + 1) * 512], Tok("bank%d" % i, excl=True)) for i in range(8)]
    psA = Pool_(banks[0:4])
    psF = Pool_(banks[0:4])
    psD = Pool_(banks[4:8])
    psS = Pool_(banks[4:6])
    psO = Pool_(banks[0:2])
    psT = Pool_(banks[2:4])
    ps8 = Pool_(banks)
    psDN = Pool_(banks[6:8]) if not cfg.get("dn8") else ps8
    S_big = psall[:, 4 * 512:8 * 512]
    t_Sbig = [b_[1] for b_ in banks[4:8]]

    NSLOT = cfg.get("nslot", 7)
    wbf = Pool_([sbt("wbf%d" % i, [128, 2048], BF16) for i in range(NSLOT)])

    def load_w(srcs, a, b_):
        wb, t_wb = wbf.next()
        wbv = wb[:, 0:a * b_].rearrange("p (a b) -> p a b", b=b_)
        o = 0
        for i, (src, ai) in enumerate(srcs):
            k.dma("pool", wbv[:, o:o + ai, :], src, writes=[t_wb], group=(i > 0))
            o += ai
        assert o == a
        return wbv, t_wb

    def wrows(ap2d):
        return ap2d.rearrange("(kc p) n -> p kc n", p=128)

    cact, t_cact = sbt("cact", [128, KC * 2])
    k.dma("sp", cact, cT_d, writes=[t_cact])
    op("act", "activation", writes=[t_cact], out=cact, in_=cact, func=AF.Silu)
    cact_b, t_cactb = sbt("cact_b", [128, KC * 2], BF16)
    op("dve", "tensor_copy", reads=[t_cact], writes=[t_cactb], out=cact_b, in_=cact)
    cact3 = cact_b.rearrange("p (c b) -> p c b", b=2)
    modT, t_mod = sbt("modT", [128, DEPTH, 48, 2])
    mods, t_mods = sbt("mods", [128, DEPTH, 2, 6, 8])
    bmod_t, t_bmod = sbt("bmod_t", [128, 64])
    for l in range(LAYERS):
        k.dma("sp", bmod_t, smallp_d[:, l * SP_LAYER:l * SP_LAYER + 64], writes=[t_bmod])
        for g in range(24):
            w32, t_w32 = load_w([(wrows(w_mod_d[l])[:, :, g * 256:(g + 1) * 256], KC)], KC, 256)
            pb, t_pb = psA.next()
            fns = []
            for j in range(2):
                for kc in range(KC):
                    fns.append(mm(pb[:, j * 2:j * 2 + 2], w32[:, kc, j * 128:(j + 1) * 128], cact3[:, kc, :],
                                  start=(kc == 0), stop=(kc == KC - 1)))
            k.group("pe", fns, reads=[t_w32, t_cactb], writes=[t_pb])
            op("dve", "tensor_copy", reads=[t_pb], writes=[t_mod], out=modT[:, l, g * 2:(g + 1) * 2, :],
               in_=pb[:, 0:4].rearrange("p (j b) -> p j b", b=2))
        for b in range(2):
            op("dve", "tensor_tensor", reads=[t_bmod], writes=[t_mod], out=modT[:, l, :, b], in0=modT[:, l, :, b],
               in1=bmod_t[:, 16:64], op=ALU.add)
        for b in range(2):
            for part in range(6):
                src = modT[:, l, part * 8:(part + 1) * 8, b]
                dst = mods[:, l, b, part, :]
                if part in (1, 4):
                    go = 0 if part == 1 else 8
                    op("dve", "scalar_tensor_tensor", reads=[t_mod, t_bmod], writes=[t_mods], out=dst, in0=src, scalar=1.0,
                       in1=bmod_t[:, go:go + 8], op0=ALU.add, op1=ALU.mult)
                else:
                    op("dve", "tensor_copy", reads=[t_mod], writes=[t_mods], out=dst, in_=src)
    tap("mods", mods, t_mods)
    SHIFT1, G1S, GATE1, SHIFT2, G2S, GATE2 = 0, 1, 2, 3, 4, 5

    wsT_b, t_wst = sbt("wsT_b", [128, 4, 128], BF16)
    lay, t_lay = sbt("lay", [128, 16])
    subgs, t_subgs = sbt("subgs", [128, 128])
    halo, t_halo = sbt("halo", [128, 12, 3])
    Sst, t_S = sbt("Sst", [128, 4, 128])
    lamtmp = sbt("lamtmp", [128, 2, 64])

    rem = int(nc.sbuf_bytes_remaining)
    AW = (rem - 1024) // 4
    arena = sb("arena", [128, AW])
    ar = {"off": 0}
    tokcache = {}
    tokgen = {}

    def aa(name, shape, dt=F32):
        n = 1
        for s_ in shape[1:]:
            n *= s_
        words = (n * (4 if dt in (F32, I32) else 2) + 3) // 4
        words = (words + 7) // 8 * 8
        o = ar["off"]
        assert o + words <= AW, ("arena overflow", name, o, words, AW)
        ar["off"] = o + words
        v = arena[0:shape[0], o:o + words]
        if dt != F32:
            v = v.bitcast(dt)
        v = v[:, 0:n]
        if len(shape) == 3:
            v = v.rearrange("p (a b) -> p a b", b=shape[2])
        elif len(shape) == 4:
            v = v.rearrange("p (a b c) -> p a b c", b=shape[2], c=shape[3])
        key = (name, o)
        if key not in tokcache:
            tokcache[key] = Tok(name)
        tk = tokcache[key]
        if LAZY and tokgen.get(key) != fence["gen"]:
            tokgen[key] = fence["gen"]
            tk.r = k._compact(list(tk.r) + fence["evs"])
        return v, tk

    def layer_setup(l):
        k.dma("sp", spl_t, smallp_d[:, l * SP_LAYER:(l + 1) * SP_LAYER], writes=[t_spl])
        k.dma("pool", wsT_b, wst_d[:, l * 512:(l + 1) * 512].rearrange("p (g i) -> p g i", i=128), writes=[t_wst])
        op("dve", "memset", writes=[t_wst], ap=wsT_b[64:128, :, 0:64], constant=0.0)
        lam_v = spl("lam").rearrange("p (a d) -> p a d", d=64)
        pr_, t_pr = lamtmp
        op("dve", "tensor_tensor", reads=[t_spl], writes=[t_pr], out=pr_[:, 0, :], in0=lam_v[:, 0, :], in1=lam_v[:, 1, :], op=ALU.mult)
        op("dve", "tensor_tensor", reads=[t_spl], writes=[t_pr], out=pr_[:, 1, :], in0=lam_v[:, 2, :], in1=lam_v[:, 3, :], op=ALU.mult)
        op("dve", "reduce_sum", reads=[t_pr], writes=[t_lay], out=lay[:, 8:10], in_=pr_, axis=AX.X)
        op("act", "activation", reads=[], writes=[t_lay], out=lay[:, 8:10], in_=lay[:, 8:10], func=AF.Exp)
        op("dve", "scalar_tensor_tensor", reads=[], writes=[t_lay], out=lay[:, 0:1], in0=lay[:, 9:10], scalar=-lambda_init_fn(l),
           in1=lay[:, 8:9], op0=ALU.add, op1=ALU.subtract)
        op("dve", "tensor_scalar", reads=[t_spl], writes=[t_subgs], out=subgs, in0=spl("subg"), scalar1=1.0 - lambda_init_fn(l),
           scalar2=None, op0=ALU.mult)
        op("act", "activation", reads=[t_spl], writes=[t_lay], out=lay[:, 4:8], in_=spl("alog"), func=AF.Exp)
        op("dve", "tensor_scalar", reads=[], writes=[t_lay], out=lay[:, 4:8], in0=lay[:, 4:8], scalar1=-1.0, scalar2=None, op0=ALU.mult)

    def modnorm(xsrc, t_xsrc, c0, l, b, gidx, sidx, hdst, t_hd, sq_b, t_sq, rb, t_rb, tmps, h32=None):
        for c in range(KC):
            op("act", "activation", reads=[t_xsrc], writes=[t_sq], out=sq_b[:, c, :], in_=xsrc[:, c, c0:c0 + TT], func=AF.Square)
        pb, t_pb = psA.next()
        k.group("pe", [mm(pb, ones_b, sq_b[:, c, :], start=(c == 0), stop=(c == KC - 1)) for c in range(KC)],
                reads=[t_sq, t_onesb], writes=[t_pb])
        op("act", "activation", reads=[t_pb], writes=[t_rb], out=rb, in_=pb, func=AF.Sqrt, bias=eps_c, scale=1.0 / D)
        op("dve", "reciprocal", writes=[t_rb], out=rb, in_=rb)
        for c in range(KC):
            tm, t_tm = tmps.next()
            op("dve", "scalar_tensor_tensor", reads=[t_xsrc, t_mods, t_rb], writes=[t_tm], out=tm, in0=xsrc[:, c, c0:c0 + TT],
               scalar=mods[:, l, b, gidx, c:c + 1], in1=rb, op0=ALU.mult, op1=ALU.mult)
            if h32 is not None:
                op("act", "activation", reads=[t_tm, t_mods], writes=[h32[1]], out=h32[0][:, c, :], in_=tm, func=AF.Identity,
                   bias=mods[:, l, b, sidx, c:c + 1], scale=1.0)
                op("dve", "tensor_copy", reads=[h32[1]], writes=[t_hd], out=hdst[:, c, :], in_=h32[0][:, c, :])
            else:
                op("act", "activation", reads=[t_tm, t_mods], writes=[t_hd], out=hdst[:, c, :], in_=tm, func=AF.Identity,
                   bias=mods[:, l, b, sidx, c:c + 1], scale=1.0)

    epsc, t_epsc = sbt("epsc", [128, 4])
    op("dve", "memset", writes=[t_epsc], ap=epsc[:, 0:1], constant=EPS)
    op("dve", "memset", writes=[t_epsc], ap=epsc[:, 1:2], constant=1.0)
    op("dve", "memset", writes=[t_epsc], ap=epsc[:, 2:3], constant=-PI)
    eps_c = epsc[:, 0:1]
    one_c = epsc[:, 1:2]
    npi_c = epsc[:, 2:3]

    def mixer(l, b):
        barrier()
        ar["off"] = 0
        hT, t_h = aa("hT", [128, KC, TT], BF16)
        kT, t_kT = aa("kT", [128, 4, S], BF16)
        Vt, t_V = aa("Vt", [128, 16, 512], BF16)
        y_gm, t_ygm = aa("y_gm", [128, 4, TT], BF16)
        y_da, t_yda = aa("y_da", [128, 4, TT], BF16)
        y_dn, t_ydn = aa("y_dn", [128, 4, TT], BF16)
        tmps = Pool_([aa("tmpA%d" % i, [128, TT]) for i in range(3)])
        tmpb = Pool_([aa("tmpB%d" % i, [128, TT], BF16) for i in range(2)])
        st, t_st = aa("stat", [128, 32])
        P1_END = ar["off"]
        op("dve", "memset", writes=[t_halo], ap=halo, constant=0.0)
        op("dve", "memset", writes=[t_S], ap=Sst, constant=0.0)

        def wg(col0, ncols):
            return load_w([(wrows(w_in_d[l])[:, :, col0:col0 + ncols], KC)], KC, ncols)

        def proj_fm(wb, t_wb, nchunk, consume):
            for j in range(nchunk):
                pb, t_pb = psA.next()
                k.group("pe", [mm(pb, wb[:, kc, j * 128:(j + 1) * 128], hT[:, kc, :], start=(kc == 0), stop=(kc == KC - 1))
                               for kc in range(KC)], reads=[t_wb, t_h], writes=[t_pb])
                consume(j, pb, t_pb)

        def proj_tm(wb, t_wb, ncols, consume):
            for blk in range(4):
                pb, t_pb = psA.next()
                k.group("pe", [mm(pb[:, 0:ncols], hT[:, kc, blk * 128:(blk + 1) * 128], wb[:, kc, 0:ncols], start=(kc == 0),
                                  stop=(kc == KC - 1)) for kc in range(KC)], reads=[t_wb, t_h], writes=[t_pb])
                consume(blk, pb, t_pb)

        for t in range(NTILE):
            c0 = t * TT
            ar["off"] = P1_END
            xt, t_xt = aa("xt", [128, KC, TT])
            if l == 0:
                for c in range(KC):
                    k.dma("sp", xt[:, c, :], xT_d[b, c * 128:(c + 1) * 128, c0:c0 + TT], writes=[t_xt], group=(c > 0))
            else:
                for c in range(KC):
                    k.dma("sp", xt[:, c, :], xs_d[:, c, c0:c0 + TT], reads=[t_xs[t][c]], writes=[t_xt], group=(c > 0))
            rb, t_rb = aa("rb", [128, TT])
            sq_b, t_sq = aa("sq_b", [128, KC, TT], BF16)
            modnorm(xt, t_xt, 0, l, b, G1S, SHIFT1, hT, t_h, sq_b, t_sq, rb, t_rb, tmps)
            if l == 0 and t == 0:
                tap("h0", hT, t_h)
            if STAGE <= 1:
                continue
            barrier()
            ar["off"] = P1_END
            SUB0 = P1_END

            u_g, t_ug = aa("u_g", [128, 4, TT], BF16)
            vg, t_vg = aa("vg", [128, 4, 512])
            vln, t_vln = aa("vln", [128, 4, 512], BF16)
            for g2 in range(2):
                wb, t_wb = wg(COL_U + g2 * 256, 256)
                proj_fm(wb, t_wb, 2, lambda j, pb, t_pb, g2=g2: op(
                    "act", "activation", reads=[t_pb], writes=[t_ug], out=u_g[:, g2 * 2 + j, :], in_=pb, func=AF.Gelu))
            for g2 in range(2):
                wb, t_wb = wg(COL_V + g2 * 256, 256)
                proj_tm(wb, t_wb, 256, lambda blk, pb, t_pb, g2=g2: op(
                    "act", "activation", reads=[t_pb], writes=[t_vg], out=vg[:, blk, g2 * 256:(g2 + 1) * 256], in_=pb[:, 0:256], func=AF.Gelu))
            for blk in range(4):
                op("dve", "bn_stats", reads=[t_vg], writes=[t_st], out=st[:, 0:6], in_=vg[:, blk, :])
                op("dve", "bn_aggr", writes=[t_st], out=st[:, 8:10], in_=st[:, 0:6])
                op("act", "activation", writes=[t_st], out=st[:, 10:11], in_=st[:, 9:10], func=AF.Sqrt, bias=eps_c, scale=1.0)
                op("dve", "reciprocal", writes=[t_st], out=st[:, 10:11], in_=st[:, 10:11])
                tm, t_tm = tmps.next()
                op("dve", "tensor_scalar", reads=[t_vg, t_st], writes=[t_tm], out=tm, in0=vg[:, blk, :], scalar1=st[:, 8:9],
                   scalar2=st[:, 10:11], op0=ALU.subtract, op1=ALU.mult)
                op("dve", "tensor_tensor", reads=[t_spl], writes=[t_tm], out=tm, in0=tm, in1=spl("lng"), op=ALU.mult)
                op("dve", "tensor_tensor", reads=[t_spl, t_tm], writes=[t_vln], out=vln[:, blk, :], in0=tm, in1=spl("lnb"), op=ALU.add)
            for g in range(4):
                pb, t_pb = psA.next()
                k.group("pe", [mm(pb[:, blk * 128:(blk + 1) * 128], vln[:, blk, g * 128:(g + 1) * 128], wsT_b[:, g, :])
                               for blk in range(4)], reads=[t_vln, t_wst], writes=[t_pb])
                tm, t_tm = tmps.next()
                bsb = spl("bs")[:, g * 128:(g + 1) * 128].unsqueeze(1).to_broadcast([128, 4, 128])
                op("dve", "tensor_tensor", reads=[t_pb, t_spl], writes=[t_tm], out=tm.rearrange("p (a b) -> p a b", b=128),
                   in0=pb.rearrange("p (a b) -> p a b", b=128), in1=bsb, op=ALU.add)
                op("dve", "tensor_tensor", reads=[t_tm, t_ug], writes=[t_ygm], out=y_gm[:, g, :], in0=tm, in1=u_g[:, g, :], op=ALU.mult)
            if l == 0 and t == 0:
                tap("ygm", y_gm, t_ygm)
            if STAGE <= 2:
                continue
            barrier()
            ar["off"] = SUB0

            cosT, t_cos = aa("cosT", [128, TT])
            sinT, t_sin = aa("sinT", [128, TT])
            qT, t_qT = aa("qT", [128, 4, TT], BF16)
            PT, t_PT = aa("PT", [128, 16, 128], BF16)
            yv, t_yv = aa("yv", [128, 128])
            junk, t_junk = aa("junk", [128, 128])
            Ssb = [aa("Ssb%d" % i, [128, S]) for i in range(2)]
            Pms = [aa("Pm%d" % i, [128, S], BF16) for i in range(2)]
            asts = [aa("ast%d" % i, [128, 8]) for i in range(2)]
            rss = [aa("rs%d" % i, [128, 8]) for i in range(2)]
            ytms = [aa("ytm%d" % i, [128, 512], BF16) for i in range(2)]
            pOs = {}
            qn, t_qn = aa("qn", [128, 4, TT], BF16)
            kn, t_kn = aa("kn", [128, 4, TT], BF16)
            vn, t_vn = aa("vn", [128, 4, TT], BF16)
            Sb, t_Sb = aa("Sb", [128, 4, 128], BF16)
            zs, t_zs = aa("zs", [128, 4, TT], BF16)
            oT, t_oT = aa("oT", [128, 4, TT])
            ab, t_ab = aa("ab", [64, 8, 8])
            gb, t_gb = aa("gb", [64, 4, 8, 4])
            dst_ = {0: (qn, t_qn), 1: (kn, t_kn), 2: (vn, t_vn)}
            op("act", "copy", reads=[t_S], writes=[t_Sb], out=Sb, in_=Sst)
            cwv = spl("convw").rearrange("p (c j) -> p c j", j=4)
            OV0 = ar["off"]
            posi, t_posi = aa("posi", [128, TT], I32)
            k.dma("sp", posi, posb_d[b, :, c0:c0 + TT], writes=[t_posi])
            ang, t_ang = aa("ang", [128, TT])
            kq, t_kq = aa("kq", [128, TT])
            ki, t_ki = aa("ki", [128, TT], I32)
            op("dve", "tensor_copy", reads=[t_posi], writes=[t_ang], out=ang, in_=posi)
            op("dve", "tensor_scalar", reads=[t_consts], writes=[t_ang], out=ang, in0=ang, scalar1=cst("invf"), scalar2=None, op0=ALU.mult)
            op("dve", "tensor_scalar", reads=[t_ang], writes=[t_kq], out=kq, in0=ang, scalar1=1.0 / (2 * PI), scalar2=None, op0=ALU.mult)
            op("dve", "tensor_copy", reads=[t_kq], writes=[t_ki], out=ki, in_=kq)
            op("dve", "tensor_copy", reads=[t_ki], writes=[t_kq], out=kq, in_=ki)
            op("dve", "scalar_tensor_tensor", reads=[t_kq], writes=[t_ang], out=ang, in0=kq, scalar=-C1, in1=ang, op0=ALU.mult, op1=ALU.add)
            op("dve", "scalar_tensor_tensor", reads=[t_kq], writes=[t_ang], out=ang, in0=kq, scalar=-C2, in1=ang, op0=ALU.mult, op1=ALU.add)

            op("dve", "tensor_scalar", reads=[t_ang], writes=[t_kq], out=kq, in0=ang, scalar1=PI, scalar2=None, op0=ALU.is_gt)
            op("dve", "scalar_tensor_tensor", reads=[t_kq, t_ang], writes=[t_sin], out=sinT, in0=kq, scalar=-2 * PI, in1=ang, op0=ALU.mult, op1=ALU.add)
            op("dve", "tensor_scalar", reads=[t_sin], writes=[t_kq], out=kq, in0=sinT, scalar1=PI / 2, scalar2=None, op0=ALU.is_gt)
            op("dve", "scalar_tensor_tensor", reads=[t_kq, t_sin], writes=[t_cos], out=cosT, in0=kq, scalar=-2 * PI, in1=sinT, op0=ALU.mult, op1=ALU.add)
            op("dve", "tensor_scalar", writes=[t_cos], out=cosT, in0=cosT, scalar1=PI / 2, scalar2=None, op0=ALU.add)
            op("act", "activation", writes=[t_sin], out=sinT, in_=sinT, func=AF.Sin)
            op("act", "activation", writes=[t_cos], out=cosT, in_=cosT, func=AF.Sin)
            if l == 0 and t == 0:
                tap("cos", cosT, t_cos)
                tap("sin", sinT, t_sin)
            cws = Pool_([aa("cw%d" % i, [128, TT + 3]) for i in range(2)])

            def rope_consume(dst_fn, t_dst):
                def f(j, pb, t_pb, hbase):
                    h = hbase + j
                    rw, t_rw = tmpb.next()
                    op("act", "copy", reads=[t_pb], writes=[t_rw], out=rw, in_=pb)
                    pr, t_pr = psA.next()
                    k.group("pe", [mm(pr, rot_b, rw)], reads=[t_rw, t_rotb], writes=[t_pr])
                    t1, t_t1 = tmps.next()
                    t2, t_t2 = tmps.next()
                    op("dve", "tensor_tensor", reads=[t_pb, t_cos], writes=[t_t1], out=t1, in0=pb, in1=cosT, op=ALU.mult)
                    op("dve", "tensor_tensor", reads=[t_pr, t_sin], writes=[t_t2], out=t2, in0=pr, in1=sinT, op=ALU.mult)
                    op("dve", "tensor_tensor", reads=[t_t1, t_t2], writes=[t_dst], out=dst_fn(h), in0=t1, in1=t2, op=ALU.add)
                return f
            qcons = rope_consume(lambda h: qT[:, h, :], t_qT)
            kcons = rope_consume(lambda h: kT[:, h, c0:c0 + TT], t_kT)
            for g2 in range(2 if "q" in cfg.get("parts", "qkv") else 0):
                wb, t_wb = wg(COL_Q + g2 * 256, 256)
                proj_fm(wb, t_wb, 2, lambda j, pb, t_pb, g2=g2: qcons(j, pb, t_pb, g2 * 2))
            for g2 in range(2 if "k" in cfg.get("parts", "qkv") else 0):
                wb, t_wb = wg(COL_KK + g2 * 256, 256)
                proj_fm(wb, t_wb, 2, lambda j, pb, t_pb, g2=g2: kcons(j, pb, t_pb, g2 * 2))
            for g2 in range(2 if "v" in cfg.get("parts", "qkv") else 0):
                wb, t_wb = wg(COL_VA + g2 * 256, 256)
                proj_tm(wb, t_wb, 256, lambda blk, pb, t_pb, g2=g2: op(
                    "act", "copy", reads=[t_pb], writes=[t_V], out=Vt[:, t * 4 + blk, g2 * 256:(g2 + 1) * 256], in_=pb[:, 0:256]))
            if l == 0 and t == 0:
                tap("qT", qT, t_qT)
                tap("kT", kT[:, :, 0:TT], t_kT)

            def dn_consume(j, pb, t_pb, ci0):
                ci = ci0 + j
                which, h = ci // 4, ci % 4
                dst, t_dst = dst_[which]
                cw, t_cw = cws.next()
                op("dve", "tensor_copy", reads=[t_halo], writes=[t_cw], out=cw[:, 0:3], in_=halo[:, ci, :])
                op("act", "copy", reads=[t_pb], writes=[t_cw], out=cw[:, 3:TT + 3], in_=pb)
                op("dve", "tensor_copy", reads=[t_cw], writes=[t_halo], out=halo[:, ci, :], in_=cw[:, TT:TT + 3])
                acc, t_acc = tmps.next()
                op("dve", "tensor_scalar", reads=[t_cw, t_spl], writes=[t_acc], out=acc, in0=cw[:, 3:TT + 3], scalar1=cwv[:, ci, 3:4],
                   scalar2=None, op0=ALU.mult)
                for jj in range(3):
                    op("dve", "scalar_tensor_tensor", reads=[t_cw, t_spl], writes=[t_acc], out=acc, in0=cw[:, jj:jj + TT],
                       scalar=cwv[:, ci, jj:jj + 1], in1=acc, op0=ALU.mult, op1=ALU.add)
                if which == 2:
                    op("act", "activation", reads=[t_acc], writes=[t_dst], out=dst[:, h, :], in_=acc, func=AF.Silu)
                else:
                    op("act", "activation", reads=[], writes=[t_acc], out=acc, in_=acc, func=AF.Silu)
                    sqb, t_sqb = tmpb.next()
                    op("act", "activation", reads=[t_acc], writes=[t_sqb], out=sqb, in_=acc, func=AF.Square)
                    pn, t_pn = ps8.next()
                    k.group("pe", [mm(pn, ones_b, sqb)], reads=[t_sqb, t_onesb], writes=[t_pn])
                    rn, t_rn = tmps.next()
                    op("act", "activation", reads=[t_pn], writes=[t_rn], out=rn, in_=pn, func=AF.Sqrt, bias=eps_c, scale=1.0)
                    op("dve", "reciprocal", writes=[t_rn], out=rn, in_=rn)
                    op("dve", "scalar_tensor_tensor", reads=[t_rn, t_acc], writes=[t_dst], out=dst[:, h, :], in0=acc,
                       scalar=(128.0 ** -0.5 if which == 0 else 1.0), in1=rn, op0=ALU.mult, op1=ALU.mult)
            for gi in range(6):
                wb, t_wb = wg(COL_DQ + gi * 256, 256)
                proj_fm(wb, t_wb, 2, lambda j, pb, t_pb, gi=gi: dn_consume(j, pb, t_pb, gi * 2))
            for g2 in range(2):
                wb, t_wb = wg(COL_DZ + g2 * 256, 256)
                proj_fm(wb, t_wb, 2, lambda j, pb, t_pb, g2=g2: op(
                    "act", "activation", reads=[t_pb], writes=[t_zs], out=zs[:, g2 * 2 + j, :], in_=pb, func=AF.Silu))
            wab, t_wab = wg(COL_AB, 8)
            pb, t_pb = ps8.next()
            for c in range(8):
                k.group("pe", [mm(pb[0:64, c * 8:(c + 1) * 8], hT[:, kc, c * 64:(c + 1) * 64], wab[:, kc, 0:8], start=(kc == 0),
                                  stop=(kc == KC - 1)) for kc in range(KC)], reads=[t_wab, t_h], writes=[t_pb])
            op("dve", "tensor_copy", reads=[t_pb], writes=[t_ab], out=ab, in_=pb[0:64, 0:64].rearrange("p (c e) -> p c e", e=8))
            dtb_b = spl("dtb")[0:64, :].unsqueeze(1).to_broadcast([64, 8, 4])
            nA_b = lay[0:64, 4:8].unsqueeze(1).to_broadcast([64, 8, 4])
            op("dve", "tensor_tensor", reads=[t_ab, t_spl], writes=[t_gb], out=gb[:, 3], in0=ab[:, :, 0:4], in1=dtb_b, op=ALU.add)
            op("act", "activation", writes=[t_gb], out=gb[:, 3], in_=gb[:, 3], func=AF.Exp)
            op("act", "activation", writes=[t_gb], out=gb[:, 3], in_=gb[:, 3], func=AF.Ln, bias=one_c[0:64, :], scale=1.0)
            op("dve", "tensor_tensor", reads=[t_lay], writes=[t_gb], out=gb[:, 0], in0=gb[:, 3], in1=nA_b, op=ALU.mult)
            op("act", "activation", reads=[t_ab], writes=[t_gb], out=gb[:, 1], in_=ab[:, :, 4:8], func=AF.Sigmoid)
            op("dve", "tensor_scalar", writes=[t_gb], out=gb[:, 2], in0=gb[:, 1], scalar1=-1.0, scalar2=None, op0=ALU.mult)
            if l == 0 and t == 0:
                tap("qn", qn, t_qn); tap("kn", kn, t_kn); tap("vn", vn, t_vn); tap("gb", gb, t_gb)

            barrier()
            ar["off"] = OV0
            def a64(name, cols=256, rows=64):
                return aa(name, [rows, cols])

            def mkset(s_):
                def b64(name, cols=256):
                    return aa(name, [64, cols], BF16)
                return {"ktok": b64("ktok" + s_, 512), "vtok": b64("vtok" + s_, 512), "R1": a64("R1" + s_), "R2": a64("R2" + s_),
                        "Dm": a64("Dm" + s_), "DTm": a64("DTm" + s_), "egs": a64("egs" + s_, 16), "glb": aa("glb" + s_, [128, 4]),
                        "egrow": aa("egrow" + s_, [128, 256]), "Pb": [b64("Pa" + s_), b64("Pb" + s_)], "Bb": [b64("Ba" + s_), b64("Bb" + s_)],
                        "Nb": [b64("Na" + s_), b64("Nb" + s_)], "rhs_w": b64("rhs_w" + s_, 512), "u_sb": a64("u_sb" + s_, 512),
                        "wT": aa("wT" + s_, [128, 256], BF16), "qgT": aa("qgT" + s_, [128, 256], BF16), "vnew": b64("vnew" + s_, 512),
                        "qkT": b64("qkT" + s_)}
            NW = cfg.get("nw", 2)
            BSs = [mkset("_%d" % i) for i in range(NW)]
            tt_c = cst("tt", 64); su_c = cst("su", 64); negu = cst("negu", 64); negl = cst("negl", 64)
            mstr = cst("mstrict", 64); i4 = cst("i4", 64); id64 = consts[0:64, CO["ident"][0]:CO["ident"][0] + 64]
            units = [(qb, h, p) for qb in range(4 if ACUT >= 2 else 0) for h in range(4) for p in range(2)]

            def stageA(ui):
                qb, h, p = units[ui]
                j = ui % 2
                gq = t * 4 + qb
                kend = (gq + 1) * 128
                Sb_, t_Sb_ = Ssb[j]
                Pj, t_Pj = Pms[j]
                aj, t_aj = asts[j]
                rs_, t_rs = rss[h % 2]
                ps_ = slice(p * 64, (p + 1) * 64)
                for pc in range((kend + 511) // 512):
                    n = min(512, kend - pc * 512)
                    sbk, t_sbk = psS.next()
                    k.group("pe", [mm(sbk[:, 0:n], qT[ps_, h, qb * 128:(qb + 1) * 128], kT[ps_, h, pc * 512:pc * 512 + n])],
                            reads=[t_qT, t_kT], writes=[t_sbk])
                    op("act", "copy", reads=[t_sbk], writes=[t_Sb_], out=Sb_[:, pc * 512:pc * 512 + n], in_=sbk[:, 0:n])
                op("dve", "tensor_scalar", writes=[t_Sb_], out=Sb_[0:64, kend - 64:kend], in0=Sb_[0:64, kend - 64:kend],
                   scalar1=NEG, scalar2=None, op0=ALU.add)
                op("dve", "reduce_max", reads=[t_Sb_], writes=[t_aj], out=aj[:, 0:1], in_=Sb_[:, 0:kend], axis=AX.X)
                op("dve", "tensor_scalar", writes=[t_aj], out=aj[:, 1:2], in0=aj[:, 0:1], scalar1=-0.125, scalar2=None, op0=ALU.mult)
                op("act", "activation", reads=[t_Sb_, t_aj], writes=[t_Pj, t_rs], out=Pj[:, 0:kend], in_=Sb_[:, 0:kend],
                   func=AF.Exp, bias=aj[:, 1:2], scale=0.125, accum_out=rs_[:, p:p + 1])

            def stageB(ui):
                qb, h, p = units[ui]
                j = ui % 2
                gq = t * 4 + qb
                nkb = gq + 1
                Pj, t_Pj = Pms[j]
                rs_, t_rs = rss[h % 2]
                ytm, t_ytm = ytms[qb % 2]
                if p == 0:
                    pOs[(qb, h)] = psO.next()
                pO, t_pO = pOs[(qb, h)]
                for kb0 in range(0, nkb, 8):
                    nb = min(8, nkb - kb0)
                    ptb, t_ptb = psT.next()
                    ptv = ptb.bitcast(BF16)
                    k.group("pe", [tr(ptv[:, i * 128:(i + 1) * 128], Pj[:, (kb0 + i) * 128:(kb0 + i + 1) * 128], ident_b)
                                   for i in range(nb)], reads=[t_Pj, t_identb], writes=[t_ptb])
                    op("dve", "tensor_copy", reads=[t_ptb], writes=[t_PT], out=PT[:, kb0:kb0 + nb, :],
                       in_=ptv[:, 0:nb * 128].rearrange("p (a b) -> p a b", b=128))
                k.group("pe", [mm(pO[:, p * 128:(p + 1) * 128], PT[:, kb, :], Vt[:, kb, h * 128:(h + 1) * 128],
                                  start=(kb == 0), stop=(kb == nkb - 1)) for kb in range(nkb)], reads=[t_PT, t_V], writes=[t_pO])
                if p == 0:
                    return
                op("dve", "reciprocal", writes=[t_rs], out=rs_[:, 2:4], in_=rs_[:, 0:2])
                op("dve", "tensor_tensor", reads=[t_lay], writes=[t_rs], out=rs_[:, 3:4], in0=rs_[:, 3:4], in1=lay[:, 0:1], op=ALU.mult)
                op("dve", "tensor_scalar", reads=[t_pO, t_rs], writes=[t_yv], out=yv, in0=pO[:, 0:128], scalar1=rs_[:, 2:3], scalar2=None, op0=ALU.mult)
                op("dve", "scalar_tensor_tensor", reads=[t_pO, t_rs], writes=[t_yv], out=yv, in0=pO[:, 128:256], scalar=rs_[:, 3:4], in1=yv,
                   op0=ALU.mult, op1=ALU.add)
                op("dve", "tensor_tensor", reads=[t_yv], writes=[t_junk], out=junk, in0=yv, in1=yv, op=ALU.mult)
                op("dve", "reduce_sum", reads=[t_junk], writes=[t_rs], out=rs_[:, 4:5], in_=junk, axis=AX.X)
                op("act", "activation", writes=[t_rs], out=rs_[:, 5:6], in_=rs_[:, 4:5], func=AF.Ln, bias=eps_c, scale=1.0 / 128)
                op("act", "activation", writes=[t_rs], out=rs_[:, 5:6], in_=rs_[:, 5:6], func=AF.Exp, scale=-0.5)
                op("dve", "scalar_tensor_tensor", reads=[t_yv, t_rs, t_subgs], writes=[t_ytm], out=ytm[:, h * 128:(h + 1) * 128], in0=yv,
                   scalar=rs_[:, 5:6], in1=subgs, op0=ALU.mult, op1=ALU.mult)
                if h == 3:
                    ptb, t_ptb = psT.next()
                    ptv = ptb.bitcast(BF16)
                    k.group("pe", [tr(ptv[:, hh * 128:(hh + 1) * 128], ytm[:, hh * 128:(hh + 1) * 128], ident_b) for hh in range(4)],
                            reads=[t_ytm, t_identb], writes=[t_ptb])
                    op("dve", "tensor_copy", reads=[t_ptb], writes=[t_yda], out=y_da[:, :, qb * 128:(qb + 1) * 128],
                       in_=ptv[:, 0:512].rearrange("p (a b) -> p a b", b=128))


            def v3(ap, inner):
                return ap.rearrange("p (h j) -> p h j", j=inner)


            def chunk_prep(c, BS):
                ktok, t_ktok = BS["ktok"]; vtok, t_vtok = BS["vtok"]
                R1, t_R1 = BS["R1"]; R2, t_R2 = BS["R2"]
                Dm, t_D = BS["Dm"]; DTm, t_DT = BS["DTm"]; Ds, t_Ds = Dm, t_D
                egs, t_egs = BS["egs"]; glb, t_glb = BS["glb"]; egrow, t_egrow = BS["egrow"]
                Pb = BS["Pb"]; Bb = BS["Bb"]; Nb = BS["Nb"]
                rhs_u, t_ru = vtok, t_vtok; rhs_w, t_rw_ = BS["rhs_w"]; kd, t_kd = ktok, t_ktok
                u_sb, t_u = BS["u_sb"]; wT, t_wT = BS["wT"]; qkT, t_qk = BS["qkT"]
                qgT, t_qg = BS["qgT"]; vnew, t_vnew = BS["vnew"]
                cs = slice(c * 64, (c + 1) * 64)
                gr = gb[:, 0, c, :]
                be = gb[:, 1, c, :]
                nbe = gb[:, 2, c, :]
                pk, t_pk = psDN.next()
                pkb = pk.bitcast(BF16)
                k.group("pe", [tr(pkb[0:64, h * 128:(h + 1) * 128], kn[:, h, cs], ident_b) for h in range(4)], reads=[t_kn, t_identb], writes=[t_pk])
                op("act", "copy", reads=[t_pk], writes=[t_ktok], out=ktok, in_=pkb[0:64, 0:512])
                yield
                pv, t_pv = psDN.next()
                pvb = pv.bitcast(BF16)
                k.group("pe", [tr(pvb[0:64, h * 128:(h + 1) * 128], vn[:, h, cs], ident_b) for h in range(4)], reads=[t_vn, t_identb], writes=[t_pv])
                op("dve", "tensor_copy", reads=[t_pv], writes=[t_vtok], out=vtok, in_=pvb[0:64, 0:512])
                yield
                op("dve", "tensor_tensor", reads=[t_gb, t_consts], writes=[t_R1], out=v3(R1, 64), in0=gr.unsqueeze(2).to_broadcast([64, 4, 64]),
                   in1=tt_c.unsqueeze(1).to_broadcast([64, 4, 64]), op=ALU.mult)
                yield
                op("dve", "tensor_copy", reads=[t_gb], writes=[t_R2], out=v3(R2, 64), in_=gr.unsqueeze(2).to_broadcast([64, 4, 64]))
                yield
                pg, t_pg = psDN.next()
                k.group("pe", [mm(pg[0:64, 0:256], tt_c, R2, start=True, stop=False),
                               mm(pg[0:64, 0:256], nones_f[0:64, 0:64], R1, start=False, stop=False),
                               mm(pg[0:64, 0:256], id64, negu, start=False, stop=True)],
                        reads=[t_R1, t_R2, t_consts, t_nones], writes=[t_pg])
                op("act", "activation", reads=[t_pg], writes=[t_D], out=Dm, in_=pg[0:64, 0:256], func=AF.Exp)
                yield
                pgt, t_pgt = psDN.next()
                k.group("pe", [mm(pgt[0:64, 0:256], ones_f[0:64, 0:64], R1, start=True, stop=False),
                               mm(pgt[0:64, 0:256], ntt_f, R2, start=False, stop=False),
                               mm(pgt[0:64, 0:256], id64, negl, start=False, stop=True)],
                        reads=[t_R1, t_R2, t_consts, t_ones, t_ntt], writes=[t_pgt])
                op("act", "activation", reads=[t_pgt], writes=[t_DT], out=DTm, in_=pgt[0:64, 0:256], func=AF.Exp)
                yield
                op("dve", "tensor_tensor", reads=[t_consts], writes=[t_Ds], out=Ds, in0=Dm, in1=mstr, op=ALU.mult)
                yield
                px, t_px = psDN.next()
                k.group("pe", [mm(px[:, 0:256], ones_f[0:64, :], R1), mm(px[:, 256:260], ones_f[0:64, :], gr),
                               mm(px[0:64, 264:268], tt_c, gr), mm(px[0:64, 268:272], su_c, gr)],
                        reads=[t_R1, t_gb, t_ones, t_consts], writes=[t_px])
                op("act", "activation", reads=[t_px], writes=[t_egrow], out=egrow, in_=px[:, 0:256], func=AF.Exp)
                op("act", "activation", reads=[t_px], writes=[t_glb], out=glb, in_=px[:, 256:260], func=AF.Exp)
                op("act", "activation", reads=[t_px], writes=[t_egs], out=egs[:, 0:8], in_=px[0:64, 264:272], func=AF.Exp)
                yield
                op("dve", "tensor_tensor", reads=[t_gb], writes=[t_egs], out=egs[:, 8:12], in0=egs[:, 0:4], in1=be, op=ALU.mult)
                yield
                pkk, t_pkk = psDN.next()
                k.group("pe", [mm(pkk[0:64, h * 64:(h + 1) * 64], kn[:, h, cs], kn[:, h, cs]) for h in range(4)], reads=[t_kn], writes=[t_pkk])
                P_, t_P_ = Pb[0]
                for h in range(4):
                    op("dve", "scalar_tensor_tensor", reads=[t_pkk, t_gb, t_Ds], writes=[t_P_], out=P_[:, h * 64:(h + 1) * 64],
                       in0=pkk[0:64, h * 64:(h + 1) * 64], scalar=nbe[:, h:h + 1], in1=Ds[:, h * 64:(h + 1) * 64], op0=ALU.mult, op1=ALU.mult)
                pt_, t_pt_ = psDN.next()
                pt_ = pt_.bitcast(BF16)
                k.group("pe", [tr(pt_[0:64, h * 64:(h + 1) * 64], P_[:, h * 64:(h + 1) * 64], ident_b[0:64, 0:64]) for h in range(4)],
                        reads=[t_P_, t_identb], writes=[t_pt_])
                B_, t_B_ = Bb[0]
                N_, t_N_ = Nb[0]
                op("act", "copy", reads=[t_pt_], writes=[t_B_], out=B_, in_=pt_[0:64, 0:256])
                op("dve", "tensor_tensor", reads=[t_pt_, t_consts], writes=[t_N_], out=N_, in0=pt_[0:64, 0:256], in1=i4, op=ALU.add)
                yield
                for j in range(1, 6):
                    Pn, t_Pn = Pb[j % 2]
                    Bn, t_Bn = Bb[j % 2]
                    Nn, t_Nn = Nb[j % 2]
                    pp, t_pp = psDN.next()
                    k.group("pe", [mm(pp[0:64, h * 64:(h + 1) * 64], B_[:, h * 64:(h + 1) * 64], P_[:, h * 64:(h + 1) * 64]) for h in range(4)],
                            reads=[t_B_, t_P_], writes=[t_pp])
                    op("act", "copy", reads=[t_pp], writes=[t_Pn], out=Pn, in_=pp[0:64, 0:256])
                    if j < 5:
                        pbb, t_pbb = psDN.next()
                        k.group("pe", [mm(pbb[0:64, h * 64:(h + 1) * 64], P_[:, h * 64:(h + 1) * 64], B_[:, h * 64:(h + 1) * 64]) for h in range(4)],
                                reads=[t_B_, t_P_], writes=[t_pbb])
                        op("dve", "tensor_copy", reads=[t_pbb], writes=[t_Bn], out=Bn, in_=pbb[0:64, 0:256])
                    pn2, t_pn2 = psDN.next()
                    k.group("pe", [mm(pn2[0:64, h * 64:(h + 1) * 64], Pn[:, h * 64:(h + 1) * 64], N_[:, h * 64:(h + 1) * 64]) for h in range(4)],
                            reads=[t_Pn, t_N_], writes=[t_pn2])
                    op("dve", "tensor_tensor", reads=[t_pn2, t_N_], writes=[t_Nn], out=Nn, in0=pn2[0:64, 0:256], in1=N_, op=ALU.add)
                    P_, t_P_, B_, t_B_, N_, t_N_ = Pn, t_Pn, Bn, t_Bn, Nn, t_Nn
                op("dve", "tensor_tensor", reads=[t_vtok, t_gb], writes=[t_ru], out=v3(rhs_u, 128), in0=v3(vtok, 128),
                   in1=be.unsqueeze(2).to_broadcast([64, 4, 128]), op=ALU.mult)
                yield
                op("dve", "tensor_tensor", reads=[t_ktok, t_egs], writes=[t_rw_], out=v3(rhs_w, 128), in0=v3(ktok, 128),
                   in1=egs[:, 8:12].unsqueeze(2).to_broadcast([64, 4, 128]), op=ALU.mult)
                yield
                op("dve", "tensor_tensor", reads=[t_ktok, t_egs], writes=[t_kd], out=v3(kd, 128), in0=v3(ktok, 128),
                   in1=egs[:, 4:8].unsqueeze(2).to_broadcast([64, 4, 128]), op=ALU.mult)
                yield
                pu, t_pu = psDN.next()
                k.group("pe", [mm(pu[0:64, h * 128:(h + 1) * 128], N_[:, h * 64:(h + 1) * 64], rhs_u[:, h * 128:(h + 1) * 128]) for h in range(4)],
                        reads=[t_N_, t_ru], writes=[t_pu])
                op("act", "copy", reads=[t_pu], writes=[t_u], out=u_sb, in_=pu[0:64, :])
                yield
                pw, t_pw = psDN.next()
                k.group("pe", [mm(pw[:, h * 64:(h + 1) * 64], rhs_w[:, h * 128:(h + 1) * 128], N_[:, h * 64:(h + 1) * 64]) for h in range(4)],
                        reads=[t_N_, t_rw_], writes=[t_pw])
                op("dve", "tensor_copy", reads=[t_pw], writes=[t_wT], out=wT, in_=pw[:, 0:256])
                yield
                pq, t_pq = psDN.next()
                k.group("pe", [mm(pq[0:64, h * 64:(h + 1) * 64], kn[:, h, cs], qn[:, h, cs]) for h in range(4)], reads=[t_kn, t_qn], writes=[t_pq])
                op("dve", "tensor_tensor", reads=[t_pq, t_DT], writes=[t_qk], out=qkT, in0=pq[0:64, 0:256], in1=DTm, op=ALU.mult)
                yield
                op("dve", "tensor_tensor", reads=[t_qn, t_egrow], writes=[t_qg], out=v3(qgT, 64), in0=qn[:, :, cs], in1=v3(egrow, 64), op=ALU.mult)
                yield

            def chunk_recur(c, BS):
                ktok, t_ktok = BS["ktok"]; vtok, t_vtok = BS["vtok"]
                R1, t_R1 = BS["R1"]; R2, t_R2 = BS["R2"]
                Dm, t_D = BS["Dm"]; DTm, t_DT = BS["DTm"]; Ds, t_Ds = Dm, t_D
                egs, t_egs = BS["egs"]; glb, t_glb = BS["glb"]; egrow, t_egrow = BS["egrow"]
                Pb = BS["Pb"]; Bb = BS["Bb"]; Nb = BS["Nb"]
                rhs_u, t_ru = vtok, t_vtok; rhs_w, t_rw_ = BS["rhs_w"]; kd, t_kd = ktok, t_ktok
                u_sb, t_u = BS["u_sb"]; wT, t_wT = BS["wT"]; qkT, t_qk = BS["qkT"]
                qgT, t_qg = BS["qgT"]; vnew, t_vnew = BS["vnew"]
                cs = slice(c * 64, (c + 1) * 64)
                gr = gb[:, 0, c, :]
                be = gb[:, 1, c, :]
                nbe = gb[:, 2, c, :]
                pws, t_pws = psDN.next()
                k.group("pe", [mm(pws[0:64, h * 128:(h + 1) * 128], wT[:, h * 64:(h + 1) * 64], Sb[:, h, :]) for h in range(4)],
                        reads=[t_wT, t_Sb], writes=[t_pws])
                op("dve", "tensor_tensor", reads=[t_u, t_pws], writes=[t_vnew], out=vnew, in0=u_sb, in1=pws[0:64, :], op=ALU.subtract)
                po, t_po = psDN.next()
                fns = []
                for h in range(4):
                    fns.append(mm(po[:, h * 64:(h + 1) * 64], Sb[:, h, :], qgT[:, h * 64:(h + 1) * 64], start=True, stop=False))
                    fns.append(mm(po[:, h * 64:(h + 1) * 64], vnew[:, h * 128:(h + 1) * 128], qkT[:, h * 64:(h + 1) * 64], start=False, stop=True))
                k.group("pe", fns, reads=[t_Sb, t_qg, t_vnew, t_qk], writes=[t_po])
                op("act", "copy", reads=[t_po], writes=[t_oT], out=oT[:, :, cs], in_=v3(po[:, 0:256], 64))
                pds, t_pds = psDN.next()
                k.group("pe", [mm(pds[:, h * 128:(h + 1) * 128], kd[:, h * 128:(h + 1) * 128], vnew[:, h * 128:(h + 1) * 128]) for h in range(4)],
                        reads=[t_kd, t_vnew], writes=[t_pds])
                for h in range(4):
                    op("dve", "scalar_tensor_tensor", reads=[t_pds, t_glb], writes=[t_S], out=Sst[:, h, :], in0=Sst[:, h, :], scalar=glb[:, h:h + 1],
                       in1=pds[:, h * 128:(h + 1) * 128], op0=ALU.mult, op1=ALU.add)
                op("act", "copy", reads=[t_S], writes=[t_Sb], out=Sb, in_=Sst)


            def att_gen():
                for ui in range(len(units) + 1):
                    if ui < len(units):
                        stageA(ui)
                        yield
                    if ui >= 1:
                        stageB(ui - 1)
                        yield

            def dn_gen():
                for c0_ in range(0, 8, NW):
                    gens = [chunk_prep(c0_ + i, BSs[i]) for i in range(NW)]
                    alive = list(gens)
                    while alive:
                        for g_ in list(alive):
                            try:
                                next(g_)
                            except StopIteration:
                                alive.remove(g_)
                            yield
                    for i in range(NW):
                        chunk_recur(c0_ + i, BSs[i])
                        yield

            RATIO = cfg.get("dn_ratio", 6)
            streams = [[att_gen(), 1], [dn_gen(), RATIO]]
            while streams:
                for st_ in list(streams):
                    for _ in range(st_[1]):
                        try:
                            next(st_[0])
                        except StopIteration:
                            streams.remove(st_)
                            break

            if l == 0 and t == 0 and ACUT >= 8:
                tap("yda", y_da, t_yda)
            if l == 0 and t == 0:
                tap("oT", oT, t_oT)
            for h in range(4):
                sqb, t_sqb = tmpb.next()
                op("act", "activation", reads=[t_oT], writes=[t_sqb], out=sqb, in_=oT[:, h, :], func=AF.Square)
                pn, t_pn = ps8.next()
                k.group("pe", [mm(pn, ones_b, sqb)], reads=[t_sqb, t_onesb], writes=[t_pn])
                rn, t_rn = tmps.next()
                op("act", "activation", reads=[t_pn], writes=[t_rn], out=rn, in_=pn, func=AF.Sqrt, bias=eps_c, scale=1.0 / 128)
                op("dve", "reciprocal", writes=[t_rn], out=rn, in_=rn)
                op("dve", "scalar_tensor_tensor", reads=[t_oT, t_spl], writes=[t_rn], out=rn, in0=oT[:, h, :], scalar=spl("dng"), in1=rn,
                   op0=ALU.mult, op1=ALU.mult)
                op("dve", "tensor_tensor", reads=[t_rn, t_zs], writes=[t_ydn], out=y_dn[:, h, :], in0=rn, in1=zs[:, h, :], op=ALU.mult)
            if l == 0 and t == 0:
                tap("ydn", y_dn, t_ydn)
            if STAGE <= 4:
                continue
            barrier()
            ar["off"] = SUB0
            merged, t_mg = aa("merged", [128, KC, TT], BF16)
            sgs = Pool_([aa("sg%d" % i, [128, TT], BF16) for i in range(3)])
            macc, t_macc = aa("macc", [128, TT], BF16)
            xins = Pool_([aa("xin%d" % i, [128, TT]) for i in range(3)])
            ybr = [(y_gm, t_ygm), (y_da, t_yda), (y_dn, t_ydn)]
            for d in range(KC):
                for br in range(3):
                    wgt, t_wgt = load_w([(wrows(w_in_d[l])[:, :, COL_G + br * 1024 + d * 128:COL_G + br * 1024 + (d + 1) * 128], KC)], KC, 128)
                    pg, t_pg = ps8.next()
                    k.group("pe", [mm(pg, wgt[:, kc, :], hT[:, kc, :], start=(kc == 0), stop=(kc == KC - 1)) for kc in range(KC)],
                            reads=[t_wgt, t_h], writes=[t_pg])
                    sg, t_sg = sgs.next()
                    op("act", "activation", reads=[t_pg], writes=[t_sg], out=sg, in_=pg, func=AF.Sigmoid)
                    pp, t_pp = ps8.next()
                    yb, t_yb = ybr[br]
                    wbr, t_wbr = load_w([(wrows(w_br_d[br][l])[:, :, d * 128:(d + 1) * 128], 4)], 4, 128)
                    k.group("pe", [mm(pp, wbr[:, kc, :], yb[:, kc, :], start=(kc == 0), stop=(kc == 3)) for kc in range(4)],
                            reads=[t_wbr, t_yb], writes=[t_pp])
                    if br == 0:
                        op("dve", "tensor_tensor", reads=[t_pp, t_sg], writes=[t_macc], out=macc, in0=pp, in1=sg, op=ALU.mult)
                    else:
                        op("dve", "tensor_tensor", reads=[t_pp], writes=[t_sg], out=sg, in0=pp, in1=sg, op=ALU.mult)
                        if br == 1:
                            op("dve", "tensor_tensor", reads=[t_sg], writes=[t_macc], out=macc, in0=macc, in1=sg, op=ALU.add)
                        else:
                            op("dve", "tensor_tensor", reads=[t_sg, t_macc], writes=[t_mg], out=merged[:, d, :], in0=macc, in1=sg, op=ALU.add)
            if l == 0 and t == 0:
                tap("merged", merged, t_mg)
            for g4 in range(4):
                wo, t_wo = load_w([(wrows(w_out_d[l])[:, :, g4 * 256:(g4 + 1) * 256], KC)], KC, 256)
                for j in range(2):
                    e_ = g4 * 2 + j
                    po, t_po = ps8.next()
                    k.group("pe", [mm(po, wo[:, dc, j * 128:(j + 1) * 128], merged[:, dc, :], start=(dc == 0), stop=(dc == KC - 1))
                                   for dc in range(KC)], reads=[t_wo, t_mg], writes=[t_po])
                    xin, t_xin = xins.next()
                    if l == 0:
                        k.dma("sp", xin, xT_d[b, e_ * 128:(e_ + 1) * 128, c0:c0 + TT], writes=[t_xin])
                    else:
                        k.dma("sp", xin, xs_d[:, e_, c0:c0 + TT], reads=[t_xs[t][e_]], writes=[t_xin])
                    op("dve", "scalar_tensor_tensor", reads=[t_po, t_mods], writes=[t_xin], out=xin, in0=po,
                       scalar=mods[:, l, b, GATE1, e_:e_ + 1], in1=xin, op0=ALU.mult, op1=ALU.add)
                    k.dma("sp", xs_d[:, e_, c0:c0 + TT], xin, reads=[t_xin], writes=[t_xs[t][e_]])
            barrier()


    def ffn(l, b, last):
        barrier()
        ar["off"] = 0
        xT, t_x = aa("xT", [128, KC, S])
        rb, t_rb = aa("rb", [128, TT])
        sq_b, t_sq = aa("sq_b", [128, KC, TT], BF16)
        tmps = Pool_([aa("tmpA%d" % i, [128, TT]) for i in range(3)])
        FMARK = ar["off"]
        h2, t_h2 = aa("h2", [128, KC, S], BF16)
        moe = (l % 2 == 1)
        for t in range(NTILE):
            k.dma("sp", xT[:, :, t * TT:(t + 1) * TT], xs_d[:, :, t * TT:(t + 1) * TT], reads=t_xs[t], writes=[t_x], group=(t > 0))
        if moe:
            h32, t_h32 = aa("h32", [128, KC, TT])
            WT, t_WT = aa("WT", [8, S])
            rhe, t_rhe = aa("rhe", [8, TT])
            Wb0, t_Wb0 = aa("Wbc0", [128, S], BF16)
            Wb1, t_Wb1 = aa("Wbc1", [128, S], BF16)
            Wbcs = [Wb0, Wb1]; t_Wbcs = [t_Wb0, t_Wb1]
            lg, t_lg = aa("lg", [128, 64])
            wr3 = spg[:, 8:72].rearrange("p (c e) -> p c e", e=8)
        for t in range(NTILE):
            modnorm(xT, t_x, t * TT, l, b, G2S, SHIFT2, h2[:, :, t * TT:(t + 1) * TT], t_h2, sq_b, t_sq, rb, t_rb, tmps,
                    h32=((h32, t_h32) if moe else None))
            if moe:
                for blk in range(4):
                    gblk = t * 4 + blk
                    pl, t_pl = ps8.next()
                    k.group("pe", [mm(pl[:, 0:8], h32[:, kc, blk * 128:(blk + 1) * 128], wr3[:, kc, :], start=(kc == 0), stop=(kc == KC - 1))
                                   for kc in range(KC)], reads=[t_h32, t_spg], writes=[t_pl])
                    L = lg[:, 0:8]; E1 = lg[:, 8:16]; M2 = lg[:, 16:24]; E2 = lg[:, 24:32]; Wt = lg[:, 32:40]
                    m1 = lg[:, 40:41]; m2 = lg[:, 41:42]; dd = lg[:, 42:43]; w1 = lg[:, 43:44]; w2 = lg[:, 44:45]
                    op("dve", "tensor_copy", reads=[t_pl], writes=[t_lg], out=L, in_=pl[:, 0:8])
                    op("dve", "reduce_max", writes=[t_lg], out=m1, in_=L, axis=AX.X)
                    op("dve", "tensor_scalar", writes=[t_lg], out=E1, in0=L, scalar1=m1, scalar2=None, op0=ALU.is_equal)
                    op("dve", "scalar_tensor_tensor", writes=[t_lg], out=M2, in0=E1, scalar=-1e30, in1=L, op0=ALU.mult, op1=ALU.add)
                    op("dve", "reduce_max", writes=[t_lg], out=m2, in_=M2, axis=AX.X)
                    op("dve", "tensor_scalar", writes=[t_lg], out=E2, in0=M2, scalar1=m2, scalar2=None, op0=ALU.is_equal)
                    op("dve", "tensor_tensor", writes=[t_lg], out=dd, in0=m2, in1=m1, op=ALU.subtract)
                    op("act", "activation", writes=[t_lg], out=dd, in_=dd, func=AF.Exp)
                    op("dve", "tensor_scalar", writes=[t_lg], out=w1, in0=dd, scalar1=1.0, scalar2=None, op0=ALU.add)
                    op("dve", "reciprocal", writes=[t_lg], out=w1, in_=w1)
                    op("dve", "tensor_tensor", writes=[t_lg], out=w2, in0=dd, in1=w1, op=ALU.mult)
                    op("dve", "tensor_scalar", writes=[t_lg], out=Wt, in0=E1, scalar1=w1, scalar2=None, op0=ALU.mult)
                    op("dve", "scalar_tensor_tensor", writes=[t_lg], out=Wt, in0=E2, scalar=w2, in1=Wt, op0=ALU.mult, op1=ALU.add)
                    pt_, t_pt_ = ps8.next()
                    k.group("pe", [tr(pt_[0:8, 0:128], Wt, ident)], reads=[t_lg, t_consts], writes=[t_pt_])
                    op("act", "copy", reads=[t_pt_], writes=[t_WT], out=WT[:, gblk * 128:(gblk + 1) * 128], in_=pt_[0:8, 0:128])
        if moe and b == 0:
            tap("WT", WT, t_WT)
        if b == 0:
            tap("h2_%d" % l, h2, t_h2)

        hids = Pool_([aa("hid%d" % i, [128, 2, TT], BF16) for i in range(2)])
        sabs = Pool_([aa("sab%d" % i, [128, TT], BF16) for i in range(3)])
        pend = {"f": None}

        def expert_pass(wg_d, wu_d, wd_d, F, scale_by=None):
            for fg in range(F // 256):
                wgb, t_wgb = load_w([(wrows(wg_d)[:, :, fg * 256:(fg + 1) * 256], KC)], KC, 256)
                wub, t_wub = load_w([(wrows(wu_d)[:, :, fg * 256:(fg + 1) * 256], KC)], KC, 256)
                wdb, t_wdb = load_w([(wrows(wd_d[fg * 256:(fg + 1) * 256, :]), 2)], 2, D)
                for t in range(NTILE):
                    tc_ = slice(t * TT, (t + 1) * TT)
                    hid, t_hid = hids.next()
                    pg_ = pend["f"]

                    def adv():
                        if pend["f"] is not None:
                            try:
                                next(pend["f"])
                            except StopIteration:
                                pend["f"] = None
                    for j in range(2):
                        pa, t_pa = psF.next()
                        k.group("pe", [mm(pa, wgb[:, kc, j * 128:(j + 1) * 128], h2[:, kc, tc_], start=(kc == 0), stop=(kc == KC - 1))
                                       for kc in range(KC)], reads=[t_wgb, t_h2], writes=[t_pa])
                        adv()
                        pu_, t_pu_ = psF.next()
                        k.group("pe", [mm(pu_, wub[:, kc, j * 128:(j + 1) * 128], h2[:, kc, tc_], start=(kc == 0), stop=(kc == KC - 1))
                                       for kc in range(KC)], reads=[t_wub, t_h2], writes=[t_pu_])
                        adv()
                        sa, t_sa = sabs.next()
                        op("act", "activation", reads=[t_pa], writes=[t_sa], out=sa, in_=pa, func=AF.Silu)
                        if scale_by is not None:
                            op("dve", "tensor_tensor", reads=[scale_by[1]], writes=[t_sa], out=sa, in0=sa, in1=scale_by[0][:, tc_], op=ALU.mult)
                        op("dve", "tensor_tensor", reads=[t_pu_, t_sa], writes=[t_hid], out=hid[:, j, :], in0=pu_, in1=sa, op=ALU.mult)
                    while pend["f"] is not None:
                        adv()

                    def down(wdb=wdb, t_wdb=t_wdb, hid=hid, t_hid=t_hid, tc_=tc_):
                        for d in range(KC):
                            pd, t_pd = psD.next()
                            k.group("pe", [mm(pd, wdb[:, j, d * 128:(d + 1) * 128], hid[:, j, :], start=(j == 0), stop=(j == 1)) for j in range(2)],
                                    reads=[t_wdb, t_hid], writes=[t_pd])
                            op("dve", "scalar_tensor_tensor", reads=[t_pd, t_mods], writes=[t_x], out=xT[:, d, tc_], in0=pd,
                               scalar=mods[:, l, b, GATE2, d:d + 1], in1=xT[:, d, tc_], op0=ALU.mult, op1=ALU.add)
                            if d % 2 == 1:
                                yield
                    pend["f"] = down()

        def flush():
            while pend["f"] is not None:
                try:
                    next(pend["f"])
                except StopIteration:
                    pend["f"] = None

        if not moe:
            expert_pass(ffn_g_d[0], ffn_u_d[0], ffn_d_d[0], FF_DENSE)
            flush()
        else:
            for e_ in range(cfg.get("nexp", NEXP)):
                for t in range(NTILE):
                    op("dve", "tensor_scalar", reads=[t_WT, t_consts], writes=[t_rhe], out=rhe, in0=WT[:, t * TT:(t + 1) * TT],
                       scalar1=consts[0:8, CO["ident"][0] + e_:CO["ident"][0] + e_ + 1], scalar2=None, op0=ALU.mult)
                    pw, t_pw = ps8.next()
                    k.group("pe", [mm(pw, ones_f[0:8, :], rhe)], reads=[t_rhe, t_ones], writes=[t_pw])
                    op("act", "copy", reads=[t_pw], writes=[t_Wbcs[e_ % 2]], out=Wbcs[e_ % 2][:, t * TT:(t + 1) * TT], in_=pw)
                expert_pass(moe_g_d[0, e_], moe_u_d[0, e_], moe_d_d[0, e_], FF_EXPERT, scale_by=(Wbcs[e_ % 2], t_Wbcs[e_ % 2]))
            flush()
        if b == 0:
            tap("xffn_%d" % l, xT, t_x)
        if not last:
            for t in range(NTILE):
                for c in range(KC):
                    k.dma("sp", xs_d[:, c, t * TT:(t + 1) * TT], xT[:, c, t * TT:(t + 1) * TT], reads=[t_x], writes=[t_xs[t][c]])
            return
        barrier()
        ar["off"] = FMARK
        xn, t_xn = aa("xn", [128, KC, TT])
        otm = Pool_([aa("otm%d" % i, [128, D]) for i in range(2)])
        for t in range(NTILE):
            for c in range(KC):
                op("act", "activation", reads=[t_x], writes=[t_sq], out=sq_b[:, c, :], in_=xT[:, c, t * TT:(t + 1) * TT], func=AF.Square)
            pb, t_pb = ps8.next()
            k.group("pe", [mm(pb, ones_b, sq_b[:, c, :], start=(c == 0), stop=(c == KC - 1)) for c in range(KC)],
                    reads=[t_sq, t_onesb], writes=[t_pb])
            op("act", "activation", reads=[t_pb], writes=[t_rb], out=rb, in_=pb, func=AF.Sqrt, bias=eps_c, scale=1.0 / D)
            op("dve", "reciprocal", writes=[t_rb], out=rb, in_=rb)
            for c in range(KC):
                op("dve", "scalar_tensor_tensor", reads=[t_x, t_spg, t_rb], writes=[t_xn], out=xn[:, c, :], in0=xT[:, c, t * TT:(t + 1) * TT],
                   scalar=spg[:, c:c + 1], in1=rb, op0=ALU.mult, op1=ALU.mult)
            for blk in range(4):
                ot, t_ot = otm.next()
                for half in range(2):
                    pt_, t_pt_ = ps8.next()
                    k.group("pe", [tr(pt_[:, i * 128:(i + 1) * 128], xn[:, half * 4 + i, blk * 128:(blk + 1) * 128], ident) for i in range(4)],
                            reads=[t_xn, t_consts], writes=[t_pt_])
                    if half == 0:
                        op("act", "copy", reads=[t_pt_], writes=[t_ot], out=ot[:, 0:512], in_=pt_)
                    else:
                        op("dve", "tensor_copy", reads=[t_pt_], writes=[t_ot], out=ot[:, 512:1024], in_=pt_)
                r0 = t * TT + blk * 128
                k.dma("sp", out_d[b, r0:r0 + 128, :], ot, reads=[t_ot], writes=[out_tok])

    for b in range(NSEQ):
        for l in range(LAYERS):
            layer_setup(l)
            mixer(l, b)
            if STAGE >= 6:
                ffn(l, b, last=(l == LAYERS - 1))

    fin = [out_tok] + list(tapd.values())
    if STAGE < 99:
        z, t_z = sbt("zz", [128, 8])
        op("dve", "memset", writes=[t_z], ap=z, constant=0.0)
        k.dma("sp", out_d[0, 0:128, 0:8], z, reads=[t_z], writes=[out_tok])
    k.finish(fin)
    nc._k = k
    return nc


def prep_core_inputs(inp, core):
    x = np.asarray(inp["x"], np.float32)
    b0 = 2 * core
    xT = np.ascontiguousarray(x[b0:b0 + 2].transpose(0, 2, 1))
    pos = np.asarray(inp["positions"], np.int32)[b0:b0 + 2]
    posb = np.ascontiguousarray(np.broadcast_to(pos[:, None, :], (2, 128, S)))
    c = np.asarray(inp["c"], np.float32)[b0:b0 + 2]
    cT = np.ascontiguousarray(c.reshape(2, KC, 128).transpose(2, 1, 0)).reshape(128, KC * 2)
    return {"xT": xT, "posb": posb, "cT": cT}


def shared_inputs(inp):
    ws = np.asarray(inp["gm_w_s"], np.float32)
    wst = np.ascontiguousarray(ws.transpose(3, 0, 1, 2)).reshape(128, DEPTH * 4 * 128)
    sh = {"smallp": make_smallp(inp), "consts": make_consts(), "wst": wst}
    for n in ["w_mod", "w_in", "w_br_gm", "w_br_da", "w_br_dn", "w_out", "ffn_w_gate", "ffn_w_up",
              "ffn_w_down", "moe_w_gate", "moe_w_up", "moe_w_down"]:
        sh[n] = np.ascontiguousarray(np.asarray(inp[n], np.float32))
    return sh


def kernel(**inputs):
    nc = build()
    sh = shared_inputs(inputs)
    in_maps = []
    for core in range(8):
        m = dict(sh)
        m.update(prep_core_inputs(inputs, core))
        in_maps.append(m)
    res = run_bass_kernel_spmd(nc, in_maps, core_ids=list(range(8)))
    return np.concatenate([np.asarray(r["out"], np.float32) for r in res.results], axis=0)
```
